# Optimizing a Trainium2 kernel written in Bass

```python
import jax, jax.numpy as jnp
from jax import lax
import numpy as np

D_MODEL = 1024
BATCH = 16
SEQ = 2048
DEPTH = 1

CTX_LEN = 256
GRID_W = 64

RW_WIDTH = 512
RW_HEAD = 64
RW_HEADS = RW_WIDTH // RW_HEAD
RW_DECAY_LORA = 32
RW_ICLR_LORA = 32
RW_GATE_LORA = 96
RW_GN_EPS = 64e-5
GLA_HEADS = 4
GLA_DK = 64
GLA_DV = 128
GLA_KEY = GLA_HEADS * GLA_DK
GLA_VAL = GLA_HEADS * GLA_DV
GLA_GATE_LORA = 16
GLA_GATE_NORM = 16.0
GLA_CHUNK = 64
GLA_NORM_EPS = 1e-5
MIX_WIDTH = RW_WIDTH + GLA_VAL

RW_SIZES = (RW_WIDTH, RW_WIDTH, RW_WIDTH, RW_DECAY_LORA, RW_DECAY_LORA,
            RW_ICLR_LORA, RW_ICLR_LORA, RW_GATE_LORA)
RW_IN = 3 * RW_WIDTH + 2 * RW_DECAY_LORA + 2 * RW_ICLR_LORA + RW_GATE_LORA
GLA_SIZES = (GLA_KEY, GLA_KEY, GLA_VAL, GLA_VAL, GLA_GATE_LORA, GLA_GATE_LORA)
GLA_IN = 2 * GLA_KEY + 2 * GLA_VAL + 2 * GLA_GATE_LORA
IN_WIDTH = RW_IN + GLA_IN

N_EXPERTS = 16
EXPERT_FF = 2816
CAPACITY_FACTOR = 2

DN_ALPHA = (2 * DEPTH) ** 0.25
DN_BETA = (8 * DEPTH) ** -0.25
LN_EPS = 1e-5

kernel_name = 'hymba_rwkv7_gla_ecmoe_diffusion_block'


def split_cols(z, sizes):
    offsets = []
    acc = 0
    for s in sizes[:-1]:
        acc += s
        offsets.append(acc)
    return jnp.split(z, offsets, axis=-1)


def ln_plain(x):
    xf = x.astype(jnp.float32)
    mu = jnp.mean(xf, -1, keepdims=True)
    var = jnp.mean(jnp.square(xf - mu), -1, keepdims=True)
    return ((xf - mu) * lax.rsqrt(var + LN_EPS)).astype(x.dtype)


def ln_affine(x, g, b):
    return ln_plain(x) * g + b


def modulate(h, shift, scale):
    return h * (1.0 + scale) + shift


def seq_shift_delta(z):
    zp = jnp.pad(z, ((0, 0), (1, 1), (0, 0)))
    return 0.5 * (zp[:, :-2] + zp[:, 2:]) - z


def grid_shift_delta(z):
    B, T, F = z.shape
    rows = T // GRID_W
    g = z.reshape(B, rows, GRID_W, F)
    gp = jnp.pad(g, ((0, 0), (1, 1), (1, 1), (0, 0)))
    nb = 0.25 * (gp[:, :-2, 1:-1] + gp[:, 2:, 1:-1] + gp[:, 1:-1, :-2] + gp[:, 1:-1, 2:])
    return nb.reshape(B, T, F) - z


def wkv7_scan(r, decay, kk, kka, k, v, s0, reverse):
    def step(s, inp):
        r_t, w_t, kk_t, kka_t, k_t, v_t = inp
        s_kk = jnp.einsum('bhvk,bhk->bhv', s, kk_t)
        s = s * w_t[:, :, None, :] - s_kk[..., None] * kka_t[:, :, None, :] + v_t[..., None] * k_t[:, :, None, :]
        y = jnp.einsum('bhvk,bhk->bhv', s, r_t)
        return s, y
    xs = tuple(jnp.moveaxis(t.astype(jnp.float32), 1, 0) for t in (r, decay, kk, kka, k, v))
    s, ys = lax.scan(step, s0, xs, reverse=reverse)
    return jnp.moveaxis(ys, 0, 1), s


def rwkv_direction(r_h, k, v_h, kk, wd, ad, w0, w2, a0, a2, k_a, r_k, s0, reverse):
    B, T = k.shape[:2]
    heads = lambda t: t.reshape(B, T, RW_HEADS, RW_HEAD)
    w_log = -jax.nn.softplus(-(w0 + jnp.tanh(wd) @ w2)) - 0.5
    decay = jnp.exp(-jnp.exp(w_log.astype(jnp.float32)))
    a = jax.nn.sigmoid(a0 + ad @ a2)
    k_h = heads(k * (1.0 + (a - 1.0) * k_a))
    y, s = wkv7_scan(r_h, heads(decay), kk, kk * heads(a), k_h, v_h, s0, reverse)
    bonus = jnp.sum(r_h * k_h * r_k, -1, keepdims=True) * v_h
    return y, bonus, s


def gla_chunked(q, k, v, log_a, s0):
    B, T, H, K = q.shape
    V = v.shape[-1]
    C = GLA_CHUNK
    n = T // C
    f32 = jnp.float32
    q = q.astype(f32).reshape(B, n, C, H, K)
    k = k.astype(f32).reshape(B, n, C, H, K)
    v = v.astype(f32).reshape(B, n, C, H, V)
    b = jnp.cumsum(log_a.astype(f32).reshape(B, n, C, H, K), axis=2)
    b_last = b[:, :, -1]
    q_in = q * jnp.exp(b)
    k_in = k * jnp.exp(-b)
    k_out = k * jnp.exp(b_last[:, :, None] - b)
    scores = jnp.einsum('bnihk,bnjhk->bnhij', q_in, k_in)
    mask = jnp.tril(jnp.ones((C, C), dtype=bool))
    scores = jnp.where(mask, scores, 0.0)
    o_intra = jnp.einsum('bnhij,bnjhv->bnihv', scores, v)
    d_state = jnp.einsum('bnjhk,bnjhv->bnhkv', k_out, v)

    def step(s, inp):
        ds_c, bl = inp
        return s * jnp.exp(bl)[..., None] + ds_c, s

    s_final, s_starts = lax.scan(step, s0, (jnp.moveaxis(d_state, 1, 0), jnp.moveaxis(b_last, 1, 0)))
    s_starts = jnp.moveaxis(s_starts, 0, 1)
    o_inter = jnp.einsum('bnihk,bnhkv->bnihv', q_in, s_starts)
    return (o_intra + o_inter).reshape(B, T, H, V), s_final


def mix_tokens(h, delta_fn, p, states0):
    B, T, _ = h.shape
    proj = jnp.einsum('btd,df->btf', h, p['w_in'])
    z_rw, z_gla = proj[..., :RW_IN], proj[..., RW_IN:]

    z_rw = z_rw + delta_fn(z_rw) * p['rw_mu']
    r, k, v, wd_f, wd_b, ad_f, ad_b, gd = split_cols(z_rw, RW_SIZES)
    heads = lambda t: t.reshape(B, T, RW_HEADS, RW_HEAD)
    r_h, v_h = heads(r), heads(v)
    kk = heads(k * p['rw_k_k']).astype(jnp.float32)
    kk = kk * lax.rsqrt(jnp.sum(kk * kk, -1, keepdims=True) + 1e-12)
    g_out = jax.nn.sigmoid(gd) @ p['rw_g2']
    y_f, bo_f, s_rw_f = rwkv_direction(r_h, k, v_h, kk, wd_f, ad_f, p['rw_w0'][0], p['rw_w2'][0],
                                       p['rw_a0'][0], p['rw_a2'][0], p['rw_k_a'], p['rw_r_k'],
                                       states0[0], False)
    y_b, bo_b, s_rw_b = rwkv_direction(r_h, k, v_h, kk, wd_b, ad_b, p['rw_w0'][1], p['rw_w2'][1],
                                       p['rw_a0'][1], p['rw_a2'][1], p['rw_k_a'], p['rw_r_k'],
                                       states0[1], True)
    y = y_f + y_b
    mu = jnp.mean(y, -1, keepdims=True)
    var = jnp.mean(jnp.square(y - mu), -1, keepdims=True)
    y_n = ((y - mu) * lax.rsqrt(var + RW_GN_EPS)).reshape(B, T, RW_WIDTH) * p['rw_gn_w'] + p['rw_gn_b']
    rw_out = (y_n + (bo_f + bo_b).reshape(B, T, RW_WIDTH)) * g_out

    q, kg, vg, gg, gad_f, gad_b = split_cols(z_gla, GLA_SIZES)
    q_h = q.reshape(B, T, GLA_HEADS, GLA_DK) * (GLA_DK ** -0.5)
    k_g = kg.reshape(B, T, GLA_HEADS, GLA_DK)
    v_g = vg.reshape(B, T, GLA_HEADS, GLA_DV)
    la_f = (jax.nn.log_sigmoid((gad_f @ p['gla_a2'][0] + p['gla_a_b'][0]).astype(jnp.float32))
            / GLA_GATE_NORM).reshape(B, T, GLA_HEADS, GLA_DK)
    la_b = (jax.nn.log_sigmoid((gad_b @ p['gla_a2'][1] + p['gla_a_b'][1]).astype(jnp.float32))
            / GLA_GATE_NORM).reshape(B, T, GLA_HEADS, GLA_DK)
    o_f, s_gla_f = gla_chunked(q_h, k_g, v_g, la_f, states0[2])
    flip = lambda t: jnp.flip(t, axis=1)
    o_b, s_gla_b = gla_chunked(flip(q_h), flip(k_g), flip(v_g), flip(la_b), states0[3])
    o = o_f + flip(o_b)
    o = o * lax.rsqrt(jnp.mean(jnp.square(o), -1, keepdims=True) + GLA_NORM_EPS)
    gla_out = o.reshape(B, T, GLA_VAL) * p['gla_norm_w'] * jax.nn.silu(gg)

    return jnp.concatenate([rw_out, gla_out], axis=-1), (s_rw_f, s_rw_b, s_gla_f, s_gla_b)


def expert_choice_ffn(h, router_w, w_gate, w_up, w_down):
    B, T, D = h.shape
    cap = CAPACITY_FACTOR * T // N_EXPERTS
    logits = jnp.einsum('btd,de->bte', h, router_w).astype(jnp.float32)
    aff = jax.nn.softmax(logits, axis=-1)
    gates, idx = lax.top_k(jnp.swapaxes(aff, 1, 2), cap)
    xin = jax.vmap(lambda hb, ib: hb[ib])(h, idx)

    def expert(args):
        xe, wg, wu, wd = args
        return (jax.nn.silu(xe @ wg) * (xe @ wu)) @ wd

    ye = lax.map(expert, (jnp.swapaxes(xin, 0, 1), w_gate, w_up, w_down))
    ye = jnp.swapaxes(ye, 0, 1) * gates[..., None]
    out = jnp.zeros(h.shape, ye.dtype)
    return out.at[jnp.arange(B)[:, None, None], idx].add(ye)


def setup_inputs(seed: int = 0) -> dict:
    key = jax.random.key(seed)
    ks = jax.random.split(key, 30)
    f32 = jnp.float32
    nrm = lambda i, shape, s: s * jax.random.normal(ks[i], shape, f32)
    L, D, E, F = DEPTH, D_MODEL, N_EXPERTS, EXPERT_FF
    return {
        'x': nrm(0, (BATCH, SEQ, D), 1.0),
        'c': nrm(1, (BATCH, D), 1.0),
        'ctx': nrm(2, (BATCH, CTX_LEN, D), 1.0),
        'c_ctx': nrm(3, (D,), 1.0),
        'ada_w': nrm(4, (L, D, 6 * D), D ** -0.5),
        'ada_b': nrm(5, (L, 6 * D), 0.02),
        'w_in': nrm(6, (L, D, IN_WIDTH), D ** -0.5),
        'rw_mu': jax.random.uniform(ks[7], (L, RW_IN), f32),
        'rw_w0': jax.random.uniform(ks[8], (L, 2, RW_WIDTH), f32, -6.5, -1.5),
        'rw_w2': nrm(9, (L, 2, RW_DECAY_LORA, RW_WIDTH), 0.5 * RW_DECAY_LORA ** -0.5),
        'rw_a0': nrm(10, (L, 2, RW_WIDTH), 0.3),
        'rw_a2': nrm(11, (L, 2, RW_ICLR_LORA, RW_WIDTH), 0.5 * RW_ICLR_LORA ** -0.5),
        'rw_g2': nrm(12, (L, RW_GATE_LORA, RW_WIDTH), RW_GATE_LORA ** -0.5),
        'rw_k_k': 0.85 + nrm(13, (L, RW_WIDTH), 0.05),
        'rw_k_a': 1.0 + nrm(14, (L, RW_WIDTH), 0.05),
        'rw_r_k': nrm(15, (L, RW_HEADS, RW_HEAD), 0.1),
        'rw_gn_w': 1.0 + nrm(16, (L, RW_WIDTH), 0.05),
        'rw_gn_b': nrm(17, (L, RW_WIDTH), 0.02),
        'gla_a2': nrm(18, (L, 2, GLA_GATE_LORA, GLA_KEY), GLA_GATE_LORA ** -0.5),
        'gla_a_b': 1.0 + nrm(19, (L, 2, GLA_KEY), 0.5),
        'gla_norm_w': 1.0 + nrm(20, (L, GLA_VAL), 0.05),
        'w_out': nrm(21, (L, MIX_WIDTH, D), DN_BETA * MIX_WIDTH ** -0.5),
        'ln1_g': 1.0 + nrm(22, (L, D), 0.05),
        'ln1_b': nrm(23, (L, D), 0.02),
        'router_w': nrm(24, (L, D, E), D ** -0.5),
        'ex_gate': nrm(25, (L, E, D, F), D ** -0.5),
        'ex_up': nrm(26, (L, E, D, F), D ** -0.5),
        'ex_down': nrm(27, (L, E, F, D), DN_BETA * F ** -0.5),
        'ln2_g': 1.0 + nrm(28, (L, D), 0.05),
        'ln2_b': nrm(29, (L, D), 0.02),
    }


def reference(x, c, ctx, c_ctx, ada_w, ada_b, w_in, rw_mu, rw_w0, rw_w2, rw_a0, rw_a2, rw_g2,
              rw_k_k, rw_k_a, rw_r_k, rw_gn_w, rw_gn_b, gla_a2, gla_a_b, gla_norm_w, w_out,
              ln1_g, ln1_b, router_w, ex_gate, ex_up, ex_down, ln2_g, ln2_b):
    B = x.shape[0]
    f32 = jnp.float32
    zero_states = (jnp.zeros((B, RW_HEADS, RW_HEAD, RW_HEAD), f32),
                   jnp.zeros((B, RW_HEADS, RW_HEAD, RW_HEAD), f32),
                   jnp.zeros((B, GLA_HEADS, GLA_DK, GLA_DV), f32),
                   jnp.zeros((B, GLA_HEADS, GLA_DK, GLA_DV), f32))
    silu_c = jax.nn.silu(c)
    silu_cc = jax.nn.silu(c_ctx)
    for layer in range(DEPTH):
        last = layer == DEPTH - 1
        p = dict(w_in=w_in[layer], rw_mu=rw_mu[layer], rw_w0=rw_w0[layer], rw_w2=rw_w2[layer],
                 rw_a0=rw_a0[layer], rw_a2=rw_a2[layer], rw_g2=rw_g2[layer], rw_k_k=rw_k_k[layer],
                 rw_k_a=rw_k_a[layer], rw_r_k=rw_r_k[layer], rw_gn_w=rw_gn_w[layer],
                 rw_gn_b=rw_gn_b[layer], gla_a2=gla_a2[layer], gla_a_b=gla_a_b[layer],
                 gla_norm_w=gla_norm_w[layer])
        mod = (silu_c @ ada_w[layer] + ada_b[layer])[:, None, :]
        mod_c = silu_cc @ ada_w[layer] + ada_b[layer]
        sh1, sc1, g1, sh2, sc2, g2 = jnp.split(mod, 6, axis=-1)
        sh1c, sc1c, g1c, sh2c, sc2c, g2c = jnp.split(mod_c, 6, axis=-1)

        h_ctx = modulate(ln_plain(ctx), sh1c, sc1c)
        h_lat = modulate(ln_plain(x), sh1, sc1)
        o_ctx, ctx_states = mix_tokens(h_ctx, seq_shift_delta, p, zero_states)
        o_lat, _ = mix_tokens(h_lat, grid_shift_delta, p, ctx_states)
        x = ln_affine(DN_ALPHA * x + g1 * (o_lat @ w_out[layer]), ln1_g[layer], ln1_b[layer])

        h2 = modulate(ln_plain(x), sh2, sc2)
        moe = expert_choice_ffn(h2, router_w[layer], ex_gate[layer], ex_up[layer], ex_down[layer])
        x = ln_affine(DN_ALPHA * x + g2 * moe, ln2_g[layer], ln2_b[layer])

        if not last:
            ctx = ln_affine(DN_ALPHA * ctx + g1c * (o_ctx @ w_out[layer]), ln1_g[layer], ln1_b[layer])
            h2c = modulate(ln_plain(ctx), sh2c, sc2c)
            moe_c = expert_choice_ffn(h2c, router_w[layer], ex_gate[layer], ex_up[layer], ex_down[layer])
            ctx = ln_affine(DN_ALPHA * ctx + g2c * moe_c, ln2_g[layer], ln2_b[layer])
    return x
```

```python
from contextlib import ExitStack
import numpy as np
import concourse.bass as bass
import concourse.mybir as mybir
from concourse.bass_utils import run_bass_kernel_spmd

F32 = mybir.dt.float32
BF16 = mybir.dt.bfloat16
AF = mybir.ActivationFunctionType
ALU = mybir.AluOpType
AX = mybir.AxisListType


class Buf:
    __slots__ = ("name", "lw", "rd")

    def __init__(self, name):
        self.name = name
        self.lw = None
        self.rd = {}


class V:
    __slots__ = ("buf", "ap")

    def __init__(self, buf, ap):
        self.buf = buf
        self.ap = ap

    def __getitem__(self, k):
        return V(self.buf, self.ap[k])

    def bc(self, shape):
        return V(self.buf, self.ap.to_broadcast(list(shape)))

    def us(self, axis):
        return V(self.buf, self.ap.unsqueeze(axis))

    def re(self, pat, **kw):
        return V(self.buf, self.ap.rearrange(pat, **kw))

    def pb(self, n=128):
        return V(self.buf, self.ap.partition_broadcast(n))


class Sched:
    ENGS = ("pe", "act", "dve", "pool", "sp")

    def __init__(self, nc, ndma=12):
        self.nc = nc
        self.stack = ExitStack()
        self.scopes = [self.stack]
        self.sem = {}
        self.cnt = {}
        self.prog = {}
        self.seen = {}
        for e in self.ENGS:
            self.sem[e] = self.stack.enter_context(nc.semaphore("s_" + e))
            self.cnt[e] = 0
            self.prog[e] = []
            self.seen[e] = {}
        self.ep = 0
        self.dq = {}
        for q in ("sp", "pool", "act"):
            sems = [self.stack.enter_context(nc.semaphore("d_%s%d" % (q, i))) for i in range(ndma)]
            self.dq[q] = dict(sems=sems, vals=[0] * ndma, nxt=0)
        self.n_ops = 0

    def sb(self, name, shape, dtype):
        self.n_alloc = getattr(self, "n_alloc", 0) + 1
        t = self.scopes[-1].enter_context(self.nc.sbuf_tensor("t%d_%s" % (self.n_alloc, name), list(shape), dtype))
        return V(Buf(name), t[:])

    def ps(self, name, shape, dtype):
        t = self.scopes[-1].enter_context(self.nc.psum_tensor("p_" + name, list(shape), dtype))
        return V(Buf(name), t[:])

    def dram(self, name, shape, dtype, kind="Internal"):
        if DEBUG["dump"]:
            kind = "ExternalOutput"
        t = self.nc.dram_tensor(name, list(shape), dtype, kind=kind)
        return V(Buf(name), t.ap())

    def epoch(self):
        self.barrier()
        self.ep += 1
        for e in self.ENGS:
            self.sem[e] = self.stack.enter_context(self.nc.semaphore("s_%s_%d" % (e, self.ep)))
            self.cnt[e] = 0
            for k in [k for k in self.seen[e] if isinstance(k, str)]:
                del self.seen[e][k]

    def push(self):
        self.scopes.append(ExitStack())

    def pop(self):
        self.barrier()
        self.flush()
        self.scopes.pop().close()

    def _semof(self, key):
        if isinstance(key, str):
            return self.sem[key]
        q, slot = key
        return self.dq[q]["sems"][slot]

    def _collect(self, eng, reads, writes, is_dma):
        waits = {}

        def need(ev, war=False):
            if ev is None:
                return
            key, val, dep_ep = ev
            if dep_ep < self.ep:
                return
            if not is_dma and isinstance(key, str) and key == eng:
                if eng == "pe" or war:
                    return
            if self.seen[eng].get(key, 0) >= val:
                return
            if waits.get(key, 0) < val:
                waits[key] = val

        for b in reads:
            need(b.lw)
        for b in writes:
            need(b.lw)
            for ev in b.rd.values():
                need(ev, war=True)
        return waits

    def _commit(self, eng, waits):
        for k, v in waits.items():
            self.seen[eng][k] = v
        return [(self._semof(k), v) for k, v in waits.items()]

    def _record(self, ev, reads, writes):
        for b in reads:
            b.rd[ev[0]] = ev
        for b in writes:
            b.lw = ev
            b.rd = {}

    def op(self, eng, fn, reads=(), writes=()):
        waits = self._collect(eng, reads, writes, False)
        wl = self._commit(eng, waits)
        self.cnt[eng] += 1
        ev = (eng, self.cnt[eng], self.ep)
        self.prog[eng].append((wl, fn, (self.sem[eng], 1)))
        self._record(ev, reads, writes)
        self.n_ops += 1

    def dma(self, q, out, in_, **kw):
        oap, iap = out.ap, in_.ap
        d = self.dq[q]
        slot = d["nxt"]
        d["nxt"] = (slot + 1) % len(d["sems"])
        waits = self._collect(q, [in_.buf], [out.buf], True)
        key = (q, slot)
        prev = d["vals"][slot]
        if prev and self.seen[q].get(key, 0) < prev:
            waits[key] = max(waits.get(key, 0), prev)
        wl = self._commit(q, waits)
        d["vals"][slot] = prev + 16
        ev = (key, prev + 16, self.ep)
        sem = d["sems"][slot]
        self.prog[q].append((wl, lambda e: e.dma_start(out=oap, in_=iap, **kw), (sem, 16)))
        self._record(ev, [in_.buf], [out.buf])
        self.n_ops += 1

    @staticmethod
    def _bufs(*xs):
        return [x.buf for x in xs if isinstance(x, V)]

    @staticmethod
    def _a(x):
        return x.ap if isinstance(x, V) else x

    def mm(self, out, lhsT, rhs, start=True, stop=True):
        o, l, r = out.ap, lhsT.ap, rhs.ap
        self.op("pe", lambda e: e.matmul(o, l, r, start=start, stop=stop),
                reads=self._bufs(lhsT, rhs), writes=[out.buf])

    def tr(self, out, in_, ident):
        o, i, d = out.ap, in_.ap, ident.ap
        self.op("pe", lambda e: e.transpose(o, i, d), reads=self._bufs(in_, ident), writes=[out.buf])

    def act(self, out, in_, func, bias=None, scale=None):
        kw = {}
        if bias is not None:
            kw["bias"] = self._a(bias)
        if scale is not None:
            kw["scale"] = self._a(scale)
        o, i = out.ap, in_.ap
        self.op("act", lambda e: e.activation(o, i, func, **kw),
                reads=self._bufs(in_, bias, scale), writes=[out.buf])

    def tt(self, eng, out, a, b, op):
        o, x, y = out.ap, a.ap, b.ap
        self.op(eng, lambda e: e.tensor_tensor(o, x, y, op), reads=self._bufs(a, b), writes=[out.buf])

    def ts(self, eng, out, a, s1, op0, s2=None, op1=None):
        o, x, p1, p2 = out.ap, a.ap, self._a(s1), self._a(s2)
        if op1 is None:
            self.op(eng, lambda e: e.tensor_scalar(o, x, p1, None, op0),
                    reads=self._bufs(a, s1), writes=[out.buf])
        else:
            self.op(eng, lambda e: e.tensor_scalar(o, x, p1, p2, op0, op1),
                    reads=self._bufs(a, s1, s2), writes=[out.buf])

    def stt(self, eng, out, a, sc, b, op0, op1):
        o, x, p, y = out.ap, a.ap, self._a(sc), b.ap
        eng = "dve"
        self.op(eng, lambda e: e.scalar_tensor_tensor(o, x, p, y, op0, op1),
                reads=self._bufs(a, sc, b), writes=[out.buf])

    def cp(self, eng, out, a):
        o, x = out.ap, a.ap
        if eng == "act":
            self.op("act", lambda e: e.activation(o, x, AF.Copy), reads=[a.buf], writes=[out.buf])
        else:
            self.op(eng, lambda e: e.tensor_copy(o, x), reads=[a.buf], writes=[out.buf])

    def red(self, eng, out, a, op=None):
        o, x = out.ap, a.ap
        op = ALU.add if op is None else op
        self.op(eng, lambda e: e.tensor_reduce(o, x, AX.X, op), reads=[a.buf], writes=[out.buf])

    def memset(self, eng, out, val):
        o = out.ap
        self.op(eng, lambda e: e.memset(o, val), writes=[out.buf])

    def barrier(self):
        for e in self.ENGS:
            waits = {}
            for e2 in self.ENGS:
                if e2 != e and self.cnt[e2] > self.seen[e].get(e2, 0):
                    waits[e2] = self.cnt[e2]
            if e not in ("pe",) and self.cnt[e] > self.seen[e].get(e, 0):
                waits[e] = self.cnt[e]
            for q, d in self.dq.items():
                for slot, v in enumerate(d["vals"]):
                    if v and self.seen[e].get((q, slot), 0) < v:
                        waits[(q, slot)] = v
            wl = self._commit(e, waits)
            if wl:
                self.prog[e].append((wl, None, None))

    def flush(self):
        nc = self.nc
        prog = self.prog
        self.prog = {e: [] for e in self.ENGS}

        def replay(name, e):
            for wl, fn, inc in prog[name]:
                for sem, val in wl:
                    e.wait_ge(sem, val)
                if fn is not None:
                    ins = fn(e)
                    ins.then_inc(inc[0], inc[1])

        with nc.Block() as block:
            @block.tensor
            def _(e):
                replay("pe", e)

            @block.scalar
            def _(e):
                replay("act", e)

            @block.vector
            def _(e):
                replay("dve", e)

            @block.gpsimd
            def _(e):
                replay("pool", e)

            @block.sync
            def _(e):
                replay("sp", e)

    def finish(self):
        self.barrier()
        self.flush()
        self.stack.close()


NB = 2
ALPHA = float(2.0 ** 0.25)
CRW = -float(np.exp(-0.5))
GN_EPS = 64e-5
DEBUG = {"stop_after": None, "cut": 99, "dump": False, "res": None, "dbg": False}


def host_consts():
    p = np.arange(128)[:, None]
    q = np.arange(128)[None, :]
    Us = (p < q).astype(np.float32)
    Ui = (p <= q).astype(np.float32)
    Ls = (p > q).astype(np.float32)
    Li = (p >= q).astype(np.float32)
    ident = (p == q).astype(np.float32)
    sm = []
    for l in range(7):
        sz = 2 ** l
        same = (p // (2 * sz)) == (q // (2 * sz))
        lo = same & ((p % (2 * sz)) >= sz) & ((q % (2 * sz)) < sz)
        sm.append((lo | lo.T).astype(np.float32))
    cbf = np.concatenate([ident, Us, Ui, Ls, Li, np.ones((128, 128), np.float32)] + sm, axis=1)
    cf = np.concatenate([Ui * CRW, Us * CRW, Ls * CRW, Li * CRW,
                         Ui / 16.0, Us / 16.0, Ls / 16.0, Li / 16.0,
                         np.full((128, 1), CRW, np.float32), np.full((128, 1), 1.0 / 16.0, np.float32),
                         np.arange(128, dtype=np.float32)[:, None], np.arange(128, dtype=np.float32)[:, None] + 128.0,
                         np.tile(np.arange(256, dtype=np.float32)[None, :], (128, 1))], axis=1)
    return np.ascontiguousarray(cbf), np.ascontiguousarray(cf.astype(np.float32))


def build():
    nc = bass.Bass("TRN2", target_bir_lowering=False)
    S = Sched(nc)

    def din(name, shape):
        return V(Buf(name), nc.dram_tensor(name, list(shape), F32, kind="ExternalInput").ap())

    x_d = din("x", [NB, 2048, 1024])
    ctx_d = din("ctx", [NB, 256, 1024])
    cT_d = din("cT", [128, 8, 3])
    adaw_d = din("ada_w", [1024, 6144])
    adab_d = din("ada_b", [1, 6144])
    adabT_d = din("ada_bT", [128, 48])
    win_d = din("w_in", [1024, 3328])
    mu_d = din("mu_ext", [1, 3328])
    w0_d = din("rw_w0", [2, 512])
    w2_d = din("rw_w2", [2, 32, 512])
    a0_d = din("rw_a0", [2, 512])
    a2_d = din("rw_a2", [2, 32, 512])
    g2_d = din("rw_g2", [96, 512])
    kk_d = din("rw_k_k", [1, 512])
    ka_d = din("rw_k_a", [1, 512])
    rk_d = din("rw_r_k", [1, 512])
    gnw_d = din("rw_gn_w", [1, 512])
    gnb_d = din("rw_gn_b", [1, 512])
    ga2_d = din("gla_a2", [2, 16, 256])
    gab_d = din("gla_a_b", [2, 256])
    gnorm_d = din("gla_norm_w", [1, 512])
    wout_d = din("w_out", [1024, 1024])
    ln1g_d = din("ln1_g", [1, 1024])
    ln1b_d = din("ln1_b", [1, 1024])
    rtr_d = din("router_w", [1024, 16])
    exg_d = din("ex_gate", [16, 1024, 2816])
    exu_d = din("ex_up", [16, 1024, 2816])
    exd_d = din("ex_down", [16, 2816, 1024])
    ln2g_d = din("ln2_g", [1, 1024])
    ln2b_d = din("ln2_b", [1, 1024])
    cbf_d = din("cbf", [128, 1664])
    cf_d = din("cf", [128, 8 * 128 + 4 + 256])
    eoh_d = din("eoh", [16, 16 * 128])
    out_d = V(Buf("out"), nc.dram_tensor("out", [NB, 2048, 1024], F32, kind="ExternalOutput").ap())
    mix_d = S.dram("mixs", [NB, 2048, 1024], BF16)
    yf_d = S.dram("yfs", [16, 128, 1536], F32)
    modrow_d = S.dram("modrows", [NB, 4, 1024], F32)
    dbgf_d = S.dram("dbgf", [24, 128, 512], F32)
    dbgb_d = S.dram("dbgb", [24, 128, 512], BF16)
    dbgn = {"f": 0, "b": 0, "names": []}

    def dump(name, v, bf):
        k = "b" if bf else "f"
        i = dbgn[k]
        dbgn[k] += 1
        dbgn["names"].append((name, k, i, tuple(v.ap.shape)))
        dst = (dbgb_d if bf else dbgf_d)
        p = v.ap.shape[0]
        n = 1
        for d_ in v.ap.shape[1:]:
            n *= d_
        dv = dst[i, 0:p, 0:n]
        if len(v.ap.shape) == 3:
            dv = dv.re("p (a c) -> p a c", a=v.ap.shape[1])
        S.dma("sp", dv, v)
    DEBUG["names"] = dbgn["names"]

    cb = S.sb("cb", [128, 1664], BF16)
    S.dma("pool", cb, cbf_d)
    ident, mUs, mUi, mLs, mLi = (cb[:, i * 128:(i + 1) * 128] for i in range(5))
    ones_bf = cb[:, 640:768]
    smask = [cb[:, 768 + l * 128:896 + l * 128] for l in range(7)]
    cf = S.sb("cf", [128, 8 * 128 + 4 + 256], F32)
    S.dma("sp", cf, cf_d)
    fm = [cf[:, i * 128:(i + 1) * 128] for i in range(8)]
    negcol = cf[:, 1024:1025]
    g16col = cf[:, 1025:1026]
    iota_p0 = cf[:, 1026:1027]
    iota_p1 = cf[:, 1027:1028]
    iota_f = cf[:, 1028:1284]
    modT = S.sb("modT", [128, 16, 3], F32)

    pbanks = [S.ps("pb%d" % i, [128, 512], F32) for i in range(6)]
    ptbanks = [S.ps("ptb%d" % i, [128, 1024], BF16) for i in range(2)]
    rot = {"p": 0, "t": 0}

    def P():
        rot["p"] = (rot["p"] + 1) % 6
        return pbanks[rot["p"]]

    def PT():
        rot["t"] = (rot["t"] + 1) % 2
        return ptbanks[rot["t"]]

    def layer_norm_stats(xt, st, mv, rstd, eps=1e-5):
        for c in range(2):
            o, i = st.ap[:, c, :], xt.ap[:, c * 512:(c + 1) * 512]
            S.op("dve", lambda e, o=o, i=i: e.bn_stats(o, i), reads=[xt.buf], writes=[st.buf])
        S.op("dve", lambda e: e.bn_aggr(mv.ap, st.ap), reads=[st.buf], writes=[mv.buf])
        S.act(rstd, mv[:, 1:2], AF.Sqrt, bias=eps)
        S.op("dve", lambda e: e.reciprocal(rstd.ap, rstd.ap), reads=[rstd.buf], writes=[rstd.buf])

    S.push()
    cT = S.sb("cT", [128, 8, 3], F32)
    S.dma("sp", cT, cT_d)
    scb = S.sb("scb", [128, 8, 3], BF16)
    S.act(scb, cT, AF.Silu)
    rep = []
    for b in range(NB):
        r_ = S.sb("rep%d" % b, [128, 8, 128], BF16)
        S.cp("dve", r_, scb[:, :, b:b + 1].bc([128, 8, 128]))
        rep.append(r_)
    adabT = S.sb("adabT", [128, 48], F32)
    S.dma("sp", adabT, adabT_d)
    blk = [S.sb("adablk%d" % i, [128, 8, 512], BF16) for i in range(2)]
    brow = [S.sb("adabrow%d" % i, [128, 512], F32) for i in range(2)]
    mrow = [S.sb("mrow%d" % i, [128, 512], F32) for i in range(2)]
    psT = P()
    psTv = psT[:, 0:48].re("p (j c) -> p j c", c=3)
    adaw_v = adaw_d.re("(kc p) c -> p kc c", p=128)
    for jb in range(12):
        bk = blk[jb % 2]
        S.dma("pool", bk, adaw_v[:, :, jb * 512:(jb + 1) * 512])
        if jb < 4:
            for jj in range(4):
                j = jb * 4 + jj
                for kc in range(8):
                    S.mm(psTv[:, j, :], bk[:, kc, jj * 128:(jj + 1) * 128], scb[:, kc, :],
                         start=(kc == 0), stop=(kc == 7))
            if jb == 3:
                for c in range(3):
                    S.tt("dve", modT[:, 0:8, c], psTv[:, 0:8, c], adabT[:, 0:8], ALU.add)
                    S.stt("dve", modT[:, 8:16, c], psTv[:, 8:16, c], 1.0, adabT[:, 8:16], ALU.add, ALU.add)
        else:
            which = (jb - 4) // 2
            half = (jb - 4) % 2
            br = brow[jb % 2]
            S.dma("sp", br, adab_d[:, jb * 512:(jb + 1) * 512].pb(128))
            for b in range(NB):
                pr = P()
                for kc in range(8):
                    S.mm(pr, rep[b][:, kc, :], bk[:, kc, :], start=(kc == 0), stop=(kc == 7))
                mr = mrow[b]
                if which == 2:
                    S.stt("dve", mr, pr, 1.0, br, ALU.add, ALU.add)
                else:
                    S.tt("dve", mr, pr, br, ALU.add)
                S.dma("sp", modrow_d[b, which:which + 1, half * 512:(half + 1) * 512], mr[0:1, :])
    S.pop()
    if DEBUG["stop_after"] == "A":
        S.finish()
        return nc

    S.push()
    Wa = S.sb("Wa", [128, 8, 3072], BF16)
    Wb = S.sb("Wb", [128, 8, 1536], BF16)
    Wl = [S.sb("Wl%d" % d, [128, 8, 96], BF16) for d in range(2)]
    Wlb = [S.sb("Wlb%d" % d, [128, 8, 96], BF16) for d in range(2)]
    Wgd = S.sb("Wgd", [128, 8, 96], BF16)
    Wgdb = S.sb("Wgdb", [128, 8, 96], BF16)
    sw = [S.sb("sw%d" % d, [96, 512], BF16) for d in range(2)]
    g2w = S.sb("g2w", [96, 512], BF16)
    br3 = S.sb("br3", [96, 2, 512], F32)
    onesf = S.sb("onesf", [96, 128], F32)
    S.memset("dve", onesf, 1.0)
    for d in range(2):
        S.dma("sp", br3[0:1, d, :], w0_d[d:d + 1, :])
        S.dma("sp", br3[32:33, d, :], a0_d[d:d + 1, :])
        S.dma("sp", br3[64:65, d, 0:256], gab_d[d:d + 1, :])
    rows = S.sb("rows", [128, 6, 512], F32)
    kkrow, karow, rkrow, gnwrow, gnbrow, gnormrow = (rows[:, i, :] for i in range(6))
    for i, dsrc in enumerate((kk_d, ka_d, rk_d, gnw_d, gnb_d, gnorm_d)):
        S.dma("sp", rows[:, i, :], dsrc.pb(128))
    for d in range(2):
        S.dma("pool", sw[d][0:32, :], w2_d[d])
        S.dma("pool", sw[d][32:64, :], a2_d[d])
        S.dma("pool", sw[d][64:80, 0:256], ga2_d[d])
    S.dma("pool", g2w, g2_d)

    S.push()
    mub = S.sb("mub", [128, 3328], F32)
    omm = S.sb("omm", [128, 3328], F32)
    S.dma("sp", mub, mu_d.pb(128))
    S.ts("dve", omm, mub, -1.0, ALU.mult, 1.0, ALU.add)
    stg = [S.sb("stg%d" % i, [128, 3328], F32) for i in range(2)]
    for kc in range(8):
        st_ = stg[kc % 2]
        S.dma("sp", st_, win_d[kc * 128:(kc + 1) * 128, :])
        S.tt("dve", Wa[:, kc, 0:1536], st_[:, 0:1536], omm[:, 0:1536], ALU.mult)
        S.tt("dve", Wa[:, kc, 1536:3072], st_[:, 1760:3296], omm[:, 1760:3296], ALU.mult)
        S.tt("pool", Wb[:, kc, :], st_[:, 0:1536], mub[:, 0:1536], ALU.mult)
        for d in range(2):
            for (o0, c0, n) in ((0, 1536 + 32 * d, 32), (32, 1600 + 32 * d, 32), (64, 3296 + 16 * d, 16)):
                S.tt("pool", Wl[d][:, kc, o0:o0 + n], st_[:, c0:c0 + n], omm[:, c0:c0 + n], ALU.mult)
            for (o0, c0, n) in ((0, 1536 + 32 * d, 32), (32, 1600 + 32 * d, 32)):
                S.tt("pool", Wlb[d][:, kc, o0:o0 + n], st_[:, c0:c0 + n], mub[:, c0:c0 + n], ALU.mult)
        S.tt("pool", Wgd[:, kc, :], st_[:, 1664:1760], omm[:, 1664:1760], ALU.mult)
        S.tt("pool", Wgdb[:, kc, :], st_[:, 1664:1760], mub[:, 1664:1760], ALU.mult)
    S.pop()
    for d in range(2):
        S.memset("pool", Wl[d][:, :, 80:96], 0.0)
        S.memset("pool", Wlb[d][:, :, 64:96], 0.0)

    def f32t(name, n=512, p=128):
        return S.sb(name, [p, n], F32)

    def bft(name, n=512, p=128):
        return S.sb(name, [p, n], BF16)

    xring = [f32t("xr%d" % i, 1024) for i in range(1)]
    hring = [S.sb("hT%d" % i, [128, 8, 128], BF16) for i in range(3)]
    hnb = S.sb("hnb", [128, 8, 128], BF16)
    st6 = S.sb("st6", [128, 2, 6], F32)
    mv2 = S.sb("mv2", [128, 2], F32)
    rstd1 = S.sb("rstd1", [128, 1], F32)
    r_sb, k_sb, v_sb, qk_sb = f32t("r_sb"), f32t("k_sb"), f32t("v_sb"), f32t("qk_sb")
    v_bf, vg_bf = bft("v_bf"), bft("vg_bf")
    lo = S.sb("lo", [96, 128], BF16)
    lo_gd = S.sb("lo_gd", [96, 128], BF16)
    sg, a_sb, lg = f32t("sg"), f32t("a_sb"), f32t("lg", 256)
    E1, E2, E3, E4 = f32t("E1"), f32t("E2"), f32t("E3"), f32t("E4")
    t0, t1, kk, kka, kh = f32t("t0"), f32t("t1"), f32t("kk"), f32t("kka"), f32t("kh")
    xn_bf = V(t0.buf, t0.ap.bitcast(BF16))
    nbacc = V(t1.buf, t1.ap.bitcast(BF16)).re("p (k t) -> p k t", k=8)
    ss = S.sb("ss", [128, 8], F32)
    bs = S.sb("bs", [128, 8], F32)
    pc_sb = S.sb("pc_sb", [64, 8], F32)
    pcg_sb = S.sb("pcg_sb", [64, 4], F32)
    At_bf, Bt_bf, Kt_bf, Rt_bf, Bh_bf, Kh_bf = (bft(n) for n in ("At", "Bt", "Kt", "Rt", "Bh", "Kh"))
    qt_bf, kt_bf, khg_bf = bft("qt", 256), bft("kt", 256), bft("khg", 256)

    def ht(name, p=128):
        return S.sb(name, [p, 4, 128], BF16)

    AtT, BtT, KtT, RtT = ht("AtT", 64), ht("BtT", 64), ht("KtT", 64), ht("RtT", 64)
    LakT, ArbT, ArkT, AqkT = ht("LakT"), ht("ArbT"), ht("ArkT"), ht("AqkT")
    Xr = [ht("Xr0"), ht("Xr1")]
    XTr = [ht("XTr0"), ht("XTr1")]
    Zr = [ht("Zr0"), ht("Zr1")]
    def halves(t):
        tb = V(t.buf, t.ap.bitcast(BF16))
        return [tb[:, i * 512:(i + 1) * 512].re("p (h t) -> p h t", h=4) for i in range(2)]

    Dr = halves(E1)
    DTr = halves(E2)
    Y1, Y1t = halves(E3)
    QeffT, qtT, ktT = ht("QeffT", 64), ht("qtT", 64), ht("ktT", 64)
    HT = S.sb("HT", [64, 4, 64], BF16)
    M_f = [S.sb("M_f%d" % d, [64, 8, 64], F32) for d in range(2)]
    M_bf = [S.sb("M_bf%d" % d, [64, 8, 64], BF16) for d in range(2)]
    Mg_f = [S.sb("Mg_f%d" % d, [64, 4, 128], F32) for d in range(2)]
    Mg_bf = [S.sb("Mg_bf%d" % d, [64, 4, 128], BF16) for d in range(2)]
    y_sb, bo_sb, o_sb = f32t("y_sb"), f32t("bo_sb"), f32t("o_sb")
    yfl = f32t("yfl", 1536)
    gout_sb, gsil = sg, a_sb
    mix_bf = bft("mix_bf", 1024)
    s8a, s8b, s8c, s8d = (S.sb("s8%s" % n, [128, 8], F32) for n in "abcd")

    def v4(pbank, p=128, w=128):
        return pbank[0:p, 0:4 * w].re("p (h t) -> p h t", h=4)

    def proj_block(dst, hT, hn, c0, ncols, rw):
        n = 16 if rw else 8
        i = 0
        for kc in range(8):
            S.mm(dst[:, 0:ncols], hT[:, kc, :], Wa[:, kc, c0:c0 + ncols], start=(i == 0), stop=(i == n - 1))
            i += 1
        if rw:
            for kc in range(8):
                S.mm(dst[:, 0:ncols], hn[:, kc, :], Wb[:, kc, c0:c0 + ncols], start=False, stop=(i == n - 1))
                i += 1

    def produce(b, kind, idx):
        xt = xring[0]
        src = ctx_d[b, idx * 128:(idx + 1) * 128, :] if kind == "c" else x_d[b, idx * 128:(idx + 1) * 128, :]
        S.dma("sp", xt, src)
        layer_norm_stats(xt, st6, mv2, rstd1)
        S.ts("dve", xn_bf, xt, mv2[:, 0:1], ALU.subtract, rstd1, ALU.mult)
        pt = PT()
        ptv = pt.re("p (k t) -> p k t", k=8)
        col = 2 if kind == "c" else b
        hs = hring[idx % 3]
        for kc in range(8):
            S.tr(ptv[:, kc, :], xn_bf[:, kc * 128:(kc + 1) * 128], ident)
        for kc in range(8):
            S.act(hs[:, kc, :], ptv[:, kc, :], AF.Identity, bias=modT[:, kc, col:col + 1],
                  scale=modT[:, 8 + kc, col:col + 1])

    def neighbor(kind, idx, n_tiles):
        C = hring[idx % 3]
        Pv = hring[(idx - 1) % 3] if idx > 0 else None
        Nx = hring[(idx + 1) % 3] if idx < n_tiles - 1 else None
        if kind == "l":
            if Pv is not None:
                S.tt("dve", nbacc[:, :, 0:64], C[:, :, 64:128], Pv[:, :, 64:128], ALU.add)
            else:
                S.cp("dve", nbacc[:, :, 0:64], C[:, :, 64:128])
            if Nx is not None:
                S.tt("dve", nbacc[:, :, 64:128], C[:, :, 0:64], Nx[:, :, 0:64], ALU.add)
            else:
                S.cp("dve", nbacc[:, :, 64:128], C[:, :, 0:64])
            a4 = nbacc.re("p k (r c) -> p k r c", r=2)
            c4 = C.re("p k (r c) -> p k r c", r=2)
            S.tt("dve", a4[:, :, :, 1:64], a4[:, :, :, 1:64], c4[:, :, :, 0:63], ALU.add)
            S.tt("dve", a4[:, :, :, 0:63], a4[:, :, :, 0:63], c4[:, :, :, 1:64], ALU.add)
            S.ts("dve", hnb, nbacc, 0.25, ALU.mult)
        else:
            S.cp("dve", nbacc[:, :, 1:128], C[:, :, 0:127])
            if Pv is not None:
                S.cp("dve", nbacc[:, :, 0:1], Pv[:, :, 127:128])
            else:
                S.memset("dve", nbacc[:, :, 0:1], 0.0)
            S.tt("dve", nbacc[:, :, 0:127], nbacc[:, :, 0:127], C[:, :, 1:128], ALU.add)
            if Nx is not None:
                S.tt("dve", nbacc[:, :, 127:128], nbacc[:, :, 127:128], Nx[:, :, 0:1], ALU.add)
            S.ts("dve", hnb, nbacc, 0.5, ALU.mult)

    def mix_tile(b, kind, idx, d, want_out):
        hT = hring[idx % 3]
        final = want_out and d == 1
        p = P(); proj_block(p, hT, hnb, 0, 512, True); S.cp("act", r_sb, p)
        p = P(); proj_block(p, hT, hnb, 512, 512, True); S.cp("act", k_sb, p)
        p = P(); proj_block(p, hT, hnb, 1024, 512, True); S.cp("act", v_sb, p); S.cp("pool", v_bf, v_sb)
        p = P(); proj_block(p, hT, hnb, 1536, 512, False); S.cp("act", qk_sb, p)
        p = P(); proj_block(p, hT, hnb, 2048, 512, False); S.cp("act", vg_bf, p)
        if DEBUG["cut"] <= 0:
            return
        pl = P()
        for kc in range(8):
            S.mm(pl[0:96, 0:128], Wl[d][:, kc, :], hT[:, kc, :], start=(kc == 0), stop=False)
        for kc in range(8):
            S.mm(pl[0:96, 0:128], Wlb[d][:, kc, :], hnb[:, kc, :], start=False, stop=(kc == 7))
        S.act(lo[0:32, :], pl[0:32, 0:128], AF.Tanh)
        S.act(lo[32:64, :], pl[32:64, 0:128], AF.Copy)
        S.act(lo[64:80, :], pl[64:80, 0:128], AF.Copy)
        pw = P()
        S.mm(pw, lo[0:32, :], sw[d][0:32, :], start=True, stop=False)
        S.mm(pw, onesf[0:1, :], br3[0:1, d, :], start=False, stop=True)
        S.act(sg, pw, AF.Sigmoid)
        pa = P()
        S.mm(pa, lo[32:64, :], sw[d][32:64, :], start=True, stop=False)
        S.mm(pa, onesf[32:33, :], br3[32:33, d, :], start=False, stop=True)
        S.act(a_sb, pa, AF.Sigmoid)
        pg = P()
        S.mm(pg[:, 0:256], lo[64:80, :], sw[d][64:80, 0:256], start=True, stop=False)
        S.mm(pg[:, 0:256], onesf[64:65, :], br3[64:65, d, 0:256], start=False, stop=True)
        S.act(lg, pg[:, 0:256], AF.Sigmoid)
        S.act(lg, lg, AF.Ln)
        if DEBUG["cut"] <= 1:
            return
        if d == 0:
            mi, me, mr = fm[0], fm[1], fm[2]
            gmi, gmr = fm[4], fm[6]
            m_ij_s, m_ji_s, m_ji_i = mLs, mUs, mUi
        else:
            mi, me, mr = fm[3], fm[2], fm[1]
            gmi, gmr = fm[7], fm[5]
            m_ij_s, m_ji_s, m_ji_i = mUs, mLs, mLi
        pc1 = P(); S.mm(pc1, mi, sg)
        S.act(E1, pc1, AF.Exp); S.act(E2, pc1, AF.Exp, scale=-1.0)
        pc2 = P(); S.mm(pc2, me, sg)
        S.act(E3, pc2, AF.Exp)
        pc3 = P(); S.mm(pc3, mr, sg)
        S.act(E4, pc3, AF.Exp)
        pp = P()
        for h in range(8):
            S.mm(pp[0:64, h:h + 1], sg[:, h * 64:(h + 1) * 64], negcol)
        for h in range(4):
            S.mm(pp[0:64, 8 + h:9 + h], lg[:, h * 64:(h + 1) * 64], g16col)
        S.act(pc_sb, pp[0:64, 0:8], AF.Exp)
        S.act(pcg_sb, pp[0:64, 8:12], AF.Exp)
        if DEBUG["cut"] <= 2:
            return
        S.tt("dve", t0, k_sb, kkrow, ALU.mult)
        S.tt("pool", t1, t0, t0, ALU.mult)
        S.red("dve", ss, t1.re("p (h c) -> p h c", h=8))
        S.act(ss, ss, AF.Sqrt, bias=1e-12)
        S.op("dve", lambda e: e.reciprocal(ss.ap, ss.ap), reads=[ss.buf], writes=[ss.buf])
        h8 = lambda t: t.re("p (h c) -> p h c", h=8)
        S.tt("dve", h8(kk), h8(t0), ss.us(2).bc([128, 8, 64]), ALU.mult)
        S.tt("pool", kka, kk, a_sb, ALU.mult)
        S.stt("pool", t1, a_sb, -1.0, karow, ALU.add, ALU.mult)
        S.stt("pool", kh, t1, 1.0, k_sb, ALU.add, ALU.mult)
        S.stt("dve", At_bf, kk, -1.0, E3, ALU.mult, ALU.mult)
        S.tt("dve", Bt_bf, kka, E2, ALU.mult)
        S.tt("dve", Kt_bf, kh, E2, ALU.mult)
        S.tt("dve", Rt_bf, r_sb, E1, ALU.mult)
        S.tt("pool", Bh_bf, kka, E4, ALU.mult)
        S.tt("pool", Kh_bf, kh, E4, ALU.mult)
        if want_out:
            S.tt("pool", t0, r_sb, kh, ALU.mult)
            S.tt("pool", t0, t0, rkrow, ALU.mult)
            S.red("dve", bs, h8(t0))
            S.tt("pool", h8(bo_sb), h8(v_sb), bs.us(2).bc([128, 8, 64]), ALU.mult)
        if DEBUG["cut"] <= 3:
            return
        dbg = DEBUG["dbg"] and b == 0 and kind == "c" and idx == 0 and d == 0
        if dbg:
            for nm, t_ in (("E1", E1), ("E2", E2), ("E3", E3), ("E4", E4), ("kk", kk), ("a", a_sb), ("sg", sg), ("kh", kh),
                           ("r", r_sb), ("v", v_sb)):
                dump(nm, t_, False)
            for nm, t_ in (("At", At_bf), ("Bt", Bt_bf), ("Kt", Kt_bf), ("Rt", Rt_bf), ("Bh", Bh_bf), ("Kh", Kh_bf), ("vb", v_bf)):
                dump(nm, t_, True)
            dump("pc", pc_sb, False)
        G1, G2, G4 = E1, E2, E3
        pg1 = P(); S.mm(pg1[:, 0:256], gmi, lg)
        pg3 = P(); S.mm(pg3[:, 0:256], gmr, lg)
        S.act(G1[:, 0:256], pg1[:, 0:256], AF.Exp)
        S.act(G2[:, 0:256], pg1[:, 0:256], AF.Exp, scale=-1.0)
        S.act(G4[:, 0:256], pg3[:, 0:256], AF.Exp)
        S.stt("dve", qt_bf, qk_sb[:, 0:256], 0.125, G1[:, 0:256], ALU.mult, ALU.mult)
        S.tt("dve", kt_bf, qk_sb[:, 256:512], G2[:, 0:256], ALU.mult)
        S.tt("pool", khg_bf, qk_sb[:, 256:512], G4[:, 0:256], ALU.mult)
        if DEBUG["cut"] <= 4:
            return
        for g in range(2):
            for src, dstT, eng in ((At_bf, AtT, "act"), (Bt_bf, BtT, "dve"), (Kt_bf, KtT, "act"), (Rt_bf, RtT, "dve")):
                pt = PT()
                ptv = v4(pt, 64)
                for hh in range(4):
                    h = 4 * g + hh
                    S.tr(ptv[:, hh, :], src[:, h * 64:(h + 1) * 64], ident)
                S.cp(eng, dstT, ptv)

            def score(dst, L, R, mask):
                p_ = P()
                pv = v4(p_)
                for hh in range(4):
                    S.mm(pv[:, hh, :], L[0:64, hh, :], R[0:64, hh, :])
                S.tt("dve", dst, pv, mask.us(1).bc([128, 4, 128]), ALU.mult)

            score(Xr[0], AtT, BtT, m_ij_s)
            score(XTr[0], BtT, AtT, m_ji_s)
            score(LakT, KtT, AtT, m_ji_s)
            score(ArbT, BtT, RtT, m_ji_i)
            score(ArkT, KtT, RtT, m_ji_i)
            if dbg and g == 0:
                for nm, t_ in (("AtT", AtT), ("X0", Xr[0]), ("XT0", XTr[0]), ("LakT", LakT), ("ArbT", ArbT), ("ArkT", ArkT)):
                    dump(nm, t_, True)
            p = P()
            pv = p[:, 0:256].re("p (h v) -> p h v", h=4)
            for hh in range(4):
                h = 4 * g + hh
                S.mm(pv[:, hh, :], LakT[:, hh, :], v_bf[:, h * 64:(h + 1) * 64])
            S.cp("act", Zr[0][:, :, 0:64], pv)
            S.cp("pool", Zr[0][:, :, 64:128], At_bf[:, g * 256:(g + 1) * 256].re("p (h c) -> p h c", h=4))
            if dbg and g == 0:
                dump("Z0", Zr[0], True)
            X0, XT0 = Xr[0], XTr[0]
            Lm, LTm = Xr[1], XTr[1]
            identb = ident.us(1).bc([128, 4, 128])
            m0 = smask[0].us(1).bc([128, 4, 128])
            S.tt("pool", Lm, X0, m0, ALU.mult)
            S.tt("pool", LTm, XT0, m0, ALU.mult)
            Dc, DTc = Dr[0], DTr[0]
            S.tt("dve", Dc, Lm, identb, ALU.add)
            S.tt("dve", DTc, LTm, identb, ALU.add)
            for l in range(1, 7):
                ml = smask[l].us(1).bc([128, 4, 128])
                S.tt("pool", Lm, X0, ml, ALU.mult)
                S.tt("pool", LTm, XT0, ml, ALU.mult)
                p = P(); pv = v4(p)
                for hh in range(4):
                    S.mm(pv[:, hh, :], LTm[:, hh, :], Dc[:, hh, :])
                S.cp("act", Y1, pv)
                p = P(); pv = v4(p)
                for hh in range(4):
                    S.mm(pv[:, hh, :], Lm[:, hh, :], DTc[:, hh, :])
                S.cp("dve", Y1t, pv)
                Dn, DTn = Dr[l % 2], DTr[l % 2]
                p = P(); pv = v4(p)
                for hh in range(4):
                    S.mm(pv[:, hh, :], ident, Dc[:, hh, :], start=True, stop=False)
                    S.mm(pv[:, hh, :], DTc[:, hh, :], Y1[:, hh, :], start=False, stop=True)
                S.cp("act", Dn, pv)
                p = P(); pv = v4(p)
                for hh in range(4):
                    S.mm(pv[:, hh, :], ident, DTc[:, hh, :], start=True, stop=False)
                    S.mm(pv[:, hh, :], Dc[:, hh, :], Y1t[:, hh, :], start=False, stop=True)
                S.cp("dve", DTn, pv)
                Dc, DTc = Dn, DTn
            p = P(); pv = v4(p)
            for hh in range(4):
                S.mm(pv[:, hh, :], DTc[:, hh, :], Zr[0][:, hh, :])
            S.cp("act", Zr[1], pv)
            Zf = Zr[1]
            if dbg and g == 0:
                dump("Zf", Zf, True)
            p = P()
            pv = v4(p, 64)
            for hh in range(4):
                h = 4 * g + hh
                S.mm(pv[:, hh, :], Rt_bf[:, h * 64:(h + 1) * 64], ident, start=True, stop=False)
                S.mm(pv[:, hh, :], Zf[:, hh, 64:128], ArbT[:, hh, :], start=False, stop=True)
            S.cp("act", QeffT, pv)
            p = P()
            pvh = p[0:64, 0:256].re("p (h k) -> p h k", h=4)
            for hh in range(4):
                h = 4 * g + hh
                S.mm(pvh[:, hh, :], Zf[:, hh, 64:128], Bh_bf[:, h * 64:(h + 1) * 64])
            S.cp("dve", HT, pvh)
            if dbg and g == 0:
                dump("QeffT", QeffT, True)
                dump("HT", HT, True)
            if want_out:
                p = P()
                pv = p[:, 0:256].re("p (h v) -> p h v", h=4)
                for hh in range(4):
                    h = 4 * g + hh
                    S.mm(pv[:, hh, :], QeffT[:, hh, :], M_bf[d][:, h, :], start=True, stop=False)
                    S.mm(pv[:, hh, :], ArbT[:, hh, :], Zf[:, hh, 0:64], start=False, stop=False)
                    S.mm(pv[:, hh, :], ArkT[:, hh, :], v_bf[:, h * 64:(h + 1) * 64], start=False, stop=True)
                S.cp("act", y_sb[:, g * 256:(g + 1) * 256], p[:, 0:256])
            p = P()
            pvm = p[0:64, 0:256].re("p (h v) -> p h v", h=4)
            for hh in range(4):
                h = 4 * g + hh
                S.mm(pvm[:, hh, :], HT[:, hh, :], M_bf[d][:, h, :], start=True, stop=False)
                S.mm(pvm[:, hh, :], Bh_bf[:, h * 64:(h + 1) * 64], Zf[:, hh, 0:64], start=False, stop=False)
                S.mm(pvm[:, hh, :], Kh_bf[:, h * 64:(h + 1) * 64], v_bf[:, h * 64:(h + 1) * 64], start=False, stop=True)
            Mv = M_f[d][:, 4 * g:4 * g + 4, :]
            S.tt("dve", Mv, Mv, pc_sb[:, 4 * g:4 * g + 4].us(2).bc([64, 4, 64]), ALU.mult)
            S.tt("dve", Mv, Mv, pvm, ALU.add)
            S.cp("dve", M_bf[d][:, 4 * g:4 * g + 4, :], Mv)
            if dbg and g == 0:
                dump("Mf", M_f[d], False)
        if DEBUG["cut"] <= 5:
            return
        for src, dstT, eng in ((qt_bf, qtT, "act"), (kt_bf, ktT, "dve")):
            pt = PT()
            ptv = v4(pt, 64)
            for hh in range(4):
                S.tr(ptv[:, hh, :], src[:, hh * 64:(hh + 1) * 64], ident)
            S.cp(eng, dstT, ptv)
        p = P()
        pv = v4(p)
        for hh in range(4):
            S.mm(pv[:, hh, :], ktT[:, hh, :], qtT[:, hh, :])
        S.tt("dve", AqkT, pv, m_ji_i.us(1).bc([128, 4, 128]), ALU.mult)
        if want_out:
            p = P()
            pv = v4(p)
            for hh in range(4):
                S.mm(pv[:, hh, :], AqkT[:, hh, :], vg_bf[:, hh * 128:(hh + 1) * 128], start=True, stop=False)
                S.mm(pv[:, hh, :], qtT[:, hh, :], Mg_bf[d][:, hh, :], start=False, stop=True)
            S.cp("act", o_sb, p)
        p = P()
        pvg = v4(p, 64)
        for hh in range(4):
            S.mm(pvg[:, hh, :], khg_bf[:, hh * 64:(hh + 1) * 64], vg_bf[:, hh * 128:(hh + 1) * 128])
        S.tt("dve", Mg_f[d], Mg_f[d], pcg_sb.us(2).bc([64, 4, 128]), ALU.mult)
        S.tt("dve", Mg_f[d], Mg_f[d], pvg, ALU.add)
        S.cp("dve", Mg_bf[d], Mg_f[d])
        if not want_out:
            return
        if d == 0:
            S.dma("sp", yf_d[idx, :, 0:512], y_sb)
            S.dma("sp", yf_d[idx, :, 512:1024], bo_sb)
            S.dma("sp", yf_d[idx, :, 1024:1536], o_sb)
            return
        pl2 = P()
        for kc in range(8):
            S.mm(pl2[0:96, 0:128], Wgd[:, kc, :], hT[:, kc, :], start=(kc == 0), stop=False)
        for kc in range(8):
            S.mm(pl2[0:96, 0:128], Wgdb[:, kc, :], hnb[:, kc, :], start=False, stop=(kc == 7))
        S.act(lo_gd, pl2[0:96, 0:128], AF.Sigmoid)
        pgo = P()
        S.mm(pgo, lo_gd, g2w)
        S.cp("act", gout_sb, pgo)
        p = P(); proj_block(p, hT, hnb, 2560, 512, False)
        S.act(gsil, p, AF.Silu)
        S.dma("sp", yfl, yf_d[idx])
        S.tt("dve", y_sb, y_sb, yfl[:, 0:512], ALU.add)
        S.red("dve", s8a, h8(y_sb))
        S.tt("pool", t0, y_sb, y_sb, ALU.mult)
        S.red("dve", s8b, h8(t0))
        S.ts("dve", s8a, s8a, 1.0 / 64.0, ALU.mult)
        S.tt("dve", s8c, s8a, s8a, ALU.mult)
        S.stt("dve", s8b, s8b, 1.0 / 64.0, s8c, ALU.mult, ALU.subtract)
        S.act(s8b, s8b, AF.Sqrt, bias=GN_EPS)
        S.op("dve", lambda e: e.reciprocal(s8b.ap, s8b.ap), reads=[s8b.buf], writes=[s8b.buf])
        S.tt("dve", h8(y_sb), h8(y_sb), s8a.us(2).bc([128, 8, 64]), ALU.subtract)
        S.tt("dve", h8(y_sb), h8(y_sb), s8b.us(2).bc([128, 8, 64]), ALU.mult)
        S.tt("dve", y_sb, y_sb, gnwrow, ALU.mult)
        S.tt("dve", y_sb, y_sb, gnbrow, ALU.add)
        S.tt("dve", y_sb, y_sb, bo_sb, ALU.add)
        S.tt("dve", y_sb, y_sb, yfl[:, 512:1024], ALU.add)
        S.tt("dve", mix_bf[:, 0:512], y_sb, gout_sb, ALU.mult)
        S.tt("dve", o_sb, o_sb, yfl[:, 1024:1536], ALU.add)
        S.tt("pool", t0, o_sb, o_sb, ALU.mult)
        S.red("dve", s8d[:, 0:4], t0.re("p (h c) -> p h c", h=4))
        S.act(s8d[:, 0:4], s8d[:, 0:4], AF.Sqrt, bias=1e-5, scale=1.0 / 128.0)
        S.op("dve", lambda e: e.reciprocal(s8d.ap[:, 0:4], s8d.ap[:, 0:4]), reads=[s8d.buf], writes=[s8d.buf])
        o4 = o_sb.re("p (h c) -> p h c", h=4)
        S.tt("dve", o4, o4, s8d[:, 0:4].us(2).bc([128, 4, 128]), ALU.mult)
        S.tt("dve", o_sb, o_sb, gnormrow, ALU.mult)
        S.tt("dve", mix_bf[:, 512:1024], o_sb, gsil, ALU.mult)
        S.dma("sp", mix_d[b, idx * 128:(idx + 1) * 128, :], mix_bf)

    if DEBUG["stop_after"] == "Bs":
        S.pop()
        S.finish()
        return nc
    for b in range(NB):
        for d in range(2):
            S.memset("dve", M_f[d], 0.0)
            S.memset("dve", M_bf[d], 0.0)
            S.memset("pool", Mg_f[d], 0.0)
            S.memset("pool", Mg_bf[d], 0.0)
        for d in range(2):
            for kind, n_t in (("c", 2), ("l", 16)):
                order = list(range(n_t)) if d == 0 else list(range(n_t - 1, -1, -1))
                step = 1 if d == 0 else -1
                produce(b, kind, order[0])
                for i, idx in enumerate(order):
                    nxt = idx + step
                    if 0 <= nxt < n_t:
                        produce(b, kind, nxt)
                    neighbor(kind, idx, n_t)
                    mix_tile(b, kind, idx, d, kind == "l")
                    if DEBUG["stop_after"] == "B1":
                        break
                if DEBUG["stop_after"] == "B1":
                    break
    S.pop()
    if DEBUG["stop_after"] in ("B", "B1"):
        S.finish()
        return nc
    return moe_phase(nc, S, locals())


def moe_phase(nc, S, L):
    out_d, modrow_d = L["out_d"], L["modrow_d"]
    rtr_d, exg_d, exu_d, exd_d = L["rtr_d"], L["exg_d"], L["exu_d"], L["exd_d"]
    ln2g_d, ln2b_d, eoh_d = L["ln2g_d"], L["ln2b_d"], L["eoh_d"]
    ident, mUi, ones_bf = L["ident"], L["mUi"], L["ones_bf"]
    iota_f = L["iota_f"]
    pbanks, ptbanks = L["pbanks"], L["ptbanks"]
    layer_norm_stats = L["layer_norm_stats"]
    rot = {"p": 0, "t": 0}

    def P():
        rot["p"] = (rot["p"] + 1) % 2
        return pbanks[4 + rot["p"]]

    def PT():
        rot["t"] = (rot["t"] + 1) % 2
        return ptbanks[rot["t"]]

    Y = pbanks[0:4]
    exg_v = [exg_d[e].re("(kc p) f -> p kc f", p=128) for e in range(16)]
    exu_v = [exu_d[e].re("(kc p) f -> p kc f", p=128) for e in range(16)]
    exd_v = [exd_d[e].re("(fc p) d -> p fc d", p=128) for e in range(16)]

    x_d, mix_d, wout_d, ln1g_d, ln1b_d = L["x_d"], L["mix_d"], L["wout_d"], L["ln1g_d"], L["ln1b_d"]
    for b in range(NB):
        S.epoch()
        S.push()
        h2 = S.sb("h2", [128, 16, 1024], BF16)
        acc = S.sb("acc", [128, 16, 1024], F32)
        mrows = S.sb("mrows", [128, 3, 1024], F32)
        S.dma("sp", mrows[:, 0, :], modrow_d[b, 1:2, :].pb(128))
        S.dma("sp", mrows[:, 1, :], modrow_d[b, 2:3, :].pb(128))
        S.dma("sp", mrows[:, 2, :], modrow_d[b, 3:4, :].pb(128))
        identf = S.sb("identf", [128, 128], F32)
        S.cp("dve", identf, ident)
        aff_tok = S.sb("aff_tok", [128, 16, 16], F32)
        mask_tok = S.sb("mask_tok", [128, 16, 16], BF16)
        gate_tok = S.sb("gate_tok", [128, 16, 16], BF16)
        slot_tok = S.sb("slot_tok", [128, 16, 16], F32)
        m8 = S.sb("m8", [16, 8], F32)
        st6 = S.sb("mst6", [128, 2, 6], F32)
        mv2 = S.sb("mmv2", [128, 2], F32)
        rstd1 = S.sb("mrstd1", [128, 1], F32)
        xt = [S.sb("mx%d" % i, [128, 1024], F32) for i in range(2)]

        S.push()
        affT = S.sb("affT", [16, 2048], F32)
        wk = S.sb("wk", [16, 2048], F32)
        S.push()
        woutg = S.sb("woutg", [128, 8, 1024], BF16)
        g1row = S.sb("g1row", [128, 1024], F32)
        wstg = [S.sb("wstg%d" % i, [128, 1024], F32) for i in range(2)]
        lnrows = S.sb("lnrows", [128, 2, 1024], F32)
        S.dma("sp", lnrows[:, 0, :], ln1g_d.pb(128))
        S.dma("sp", lnrows[:, 1, :], ln1b_d.pb(128))
        S.dma("sp", g1row, modrow_d[b, 0:1, :].pb(128))
        for kc in range(8):
            ws = wstg[kc % 2]
            S.dma("sp", ws, wout_d[kc * 128:(kc + 1) * 128, :])
            S.tt("pool", woutg[:, kc, :], ws, g1row, ALU.mult)
        rtr = S.sb("rtr", [128, 8, 16], BF16)
        S.dma("pool", rtr, rtr_d.re("(kc p) e -> p kc e", p=128))
        mixt = [S.sb("mixt%d" % i, [128, 1024], BF16) for i in range(2)]
        mixT = S.sb("mixT", [128, 8, 128], BF16)
        x1t = S.sb("x1t", [128, 1024], F32)
        xnf = S.sb("xnf", [128, 1024], F32)
        h2T = S.sb("h2T", [128, 8, 128], BF16)
        sm1 = S.sb("sm1", [128, 1], F32)
        sm2 = S.sb("sm2", [128, 1], F32)
        lgt = S.sb("lgt", [128, 16], F32)
        for t in range(16):
            x = xt[t % 2]
            mt = mixt[t % 2]
            S.dma("sp", x, x_d[b, t * 128:(t + 1) * 128, :])
            S.dma("sp", mt, mix_d[b, t * 128:(t + 1) * 128, :])
            pt = PT()
            ptv = pt.re("p (k t) -> p k t", k=8)
            for kc in range(8):
                S.tr(ptv[:, kc, :], mt[:, kc * 128:(kc + 1) * 128], ident)
            S.cp("act", mixT, ptv)
            for half in range(2):
                pp_ = P()
                for kc in range(8):
                    S.mm(pp_, mixT[:, kc, :], woutg[:, kc, half * 512:(half + 1) * 512], start=(kc == 0), stop=(kc == 7))
                S.stt("dve", x1t[:, half * 512:(half + 1) * 512], x[:, half * 512:(half + 1) * 512], ALPHA, pp_,
                      ALU.mult, ALU.add)
            layer_norm_stats(x1t, st6, mv2, rstd1)
            S.ts("dve", x1t, x1t, mv2[:, 0:1], ALU.subtract, rstd1, ALU.mult)
            S.tt("dve", x1t, x1t, lnrows[:, 0, :], ALU.mult)
            S.tt("dve", x1t, x1t, lnrows[:, 1, :], ALU.add)
            S.ts("pool", acc[:, t, :], x1t, ALPHA, ALU.mult)
            layer_norm_stats(x1t, st6, mv2, rstd1)
            S.ts("dve", xnf, x1t, mv2[:, 0:1], ALU.subtract, rstd1, ALU.mult)
            S.tt("dve", xnf, xnf, mrows[:, 1, :], ALU.mult)
            S.tt("dve", h2[:, t, :], xnf, mrows[:, 0, :], ALU.add)
            pt = PT()
            ptv = pt.re("p (k t) -> p k t", k=8)
            for kc in range(8):
                S.tr(ptv[:, kc, :], h2[:, t, kc * 128:(kc + 1) * 128], ident)
            S.cp("act", h2T, ptv)
            p = P()
            for kc in range(8):
                S.mm(p[:, 0:16], h2T[:, kc, :], rtr[:, kc, :], start=(kc == 0), stop=(kc == 7))
            S.op("dve", lambda e, p=p: e.reduce_max(sm1.ap, p.ap[:, 0:16], AX.X), reads=[p.buf], writes=[sm1.buf])
            S.ts("dve", sm1, sm1, -1.0, ALU.mult)
            S.act(lgt, p[:, 0:16], AF.Exp, bias=sm1)
            S.red("dve", sm2, lgt)
            S.op("dve", lambda e: e.reciprocal(sm2.ap, sm2.ap), reads=[sm2.buf], writes=[sm2.buf])
            S.ts("dve", aff_tok[:, t, :], lgt, sm2, ALU.mult)
            p2 = P()
            S.mm(p2[0:16, 0:128], aff_tok[:, t, :], identf)
            S.cp("act", affT[:, t * 128:(t + 1) * 128], p2[0:16, 0:128])
        S.pop()
        if DEBUG["stop_after"] == "C0":
            S.pop()
            S.pop()
            S.finish()
            return nc
        S.cp("dve", wk, affT)
        for r in range(32):
            S.op("dve", lambda e: e.max(m8.ap, wk.ap), reads=[wk.buf], writes=[m8.buf])
            if r < 31:
                S.op("dve", lambda e: e.match_replace(wk.ap, m8.ap, wk.ap, -1.0), reads=[wk.buf, m8.buf], writes=[wk.buf])
        S.ts("dve", wk, affT, m8[:, 7:8], ALU.is_ge)
        for t in range(16):
            p = P()
            S.mm(p[:, 0:16], wk[:, t * 128:(t + 1) * 128], identf[0:16, 0:16])
            S.cp("act", mask_tok[:, t, :], p[:, 0:16])
        for t in range(16):
            S.tt("dve", gate_tok[:, t, :], aff_tok[:, t, :], mask_tok[:, t, :], ALU.mult)
            p = P()
            for t2 in range(t + 1):
                S.mm(p[:, 0:16], mUi if t2 == t else ones_bf, mask_tok[:, t2, :], start=(t2 == 0), stop=(t2 == t))
            S.tt("dve", slot_tok[:, t, :], p[:, 0:16], mask_tok[:, t, :], ALU.mult)
            S.ts("dve", slot_tok[:, t, :], slot_tok[:, t, :], -1.0, ALU.add)
        S.pop()

        S.push()
        sel = S.sb("sel", [128, 16, 256], BF16)
        selT = [S.sb("selT%d" % i, [128, 2, 2048], BF16) for i in range(2)]
        xeT = [S.sb("xeT%d" % i, [128, 8, 256], BF16) for i in range(2)]
        actT = S.sb("actT", [128, 2, 256], BF16)
        sl = S.sb("sl", [128, 256], F32)
        y_bf = S.sb("y_bf", [128, 2, 1024], BF16)
        gslot = [S.sb("gslot%d" % i, [128, 2], F32) for i in range(2)]
        NW = 3
        wg_t = [S.sb("wg%d" % i, [128, 8, 256], BF16) for i in range(NW)]
        wu_t = [S.sb("wu%d" % i, [128, 8, 256], BF16) for i in range(NW)]
        wd_t = [S.sb("wd%d" % i, [128, 2, 1024], BF16) for i in range(NW)]
        paux = V(ptbanks[0].buf, ptbanks[0].ap.bitcast(F32))
        paux2 = V(ptbanks[1].buf, ptbanks[1].ap.bitcast(F32))
        ptr = ptbanks[1]

        def prep_pieces(e):
            k = e % 2
            pieces = []

            def mk_sel(t0_):
                def f():
                    for t in range(t0_, t0_ + 4):
                        S.ts("dve", sel[:, t, :], iota_f, slot_tok[:, t, e:e + 1], ALU.is_equal)
                return f
            for t0_ in range(0, 16, 4):
                pieces.append(mk_sel(t0_))

            def f_gslot():
                for jc in range(2):
                    for t in range(16):
                        S.mm(paux[:, jc:jc + 1], sel[:, t, jc * 128:(jc + 1) * 128], gate_tok[:, t, e:e + 1],
                             start=(t == 0), stop=(t == 15))
                S.cp("act", gslot[k], paux[:, 0:2])
            pieces.append(f_gslot)

            def mk_tr(jc, tq):
                def f():
                    ptv = ptr.re("p (k t) -> p k t", k=8)
                    for tt_ in range(8):
                        t = tq * 8 + tt_
                        S.tr(ptv[:, tt_, :], sel[:, t, jc * 128:(jc + 1) * 128], ident)
                    S.cp("act", selT[k][:, jc, tq * 1024:(tq + 1) * 1024], ptr)
                return f
            tail = []
            for jc in range(2):
                for tq in range(2):
                    tail.append(mk_tr(jc, tq))

            def mk_gather(dp):
                def f():
                    pv = paux.re("p (c j) -> p c j", c=2)
                    for c2 in range(2):
                        dc = dp * 2 + c2
                        for t in range(16):
                            S.mm(pv[:, c2, :], h2[:, t, dc * 128:(dc + 1) * 128], sel[:, t, :], start=(t == 0), stop=(t == 15))
                    S.cp("act", xeT[k][:, dp * 2:dp * 2 + 2, :], pv)
                return f
            for dp in range(4):
                pieces.append(mk_gather(dp))
            return pieces, tail

        def scatter_pieces(e):
            k = e % 2
            pieces = []

            def mk(t, dh):
                def f():
                    pa = paux if dh == 0 else paux2
                    for jc in range(2):
                        S.mm(pa, selT[k][:, jc, t * 128:(t + 1) * 128], y_bf[:, jc, dh * 512:(dh + 1) * 512],
                             start=(jc == 0), stop=(jc == 1))
                    a_ = acc[:, t, dh * 512:(dh + 1) * 512]
                    S.tt("dve", a_, a_, pa, ALU.add)
                return f
            for t in range(16):
                for dh in range(2):
                    pieces.append(mk(t, dh))
            return pieces

        pm_, pt_ = prep_pieces(0)
        for f in pm_ + pt_:
            f()
        gcount = 0
        for e in range(16):
            k = e % 2
            pend_sc = scatter_pieces(e - 1) if e > 0 else []
            pend_pr, pend_tr = prep_pieces(e + 1) if e < 15 else ([], [])
            chunk_i = 0
            wsel = {}

            def issue_w(fg):
                nonlocal gcount
                wi = gcount % NW
                gcount += 1
                f0 = fg * 256
                S.dma("pool", wg_t[wi], exg_v[e][:, :, f0:f0 + 256])
                S.dma("pool", wu_t[wi], exu_v[e][:, :, f0:f0 + 256])
                S.dma("pool", wd_t[wi], exd_v[e][:, fg * 2:fg * 2 + 2, :])
                wsel[fg] = wi

            def GU(n):
                fg, fc = n // 2, n % 2
                if fg not in wsel:
                    issue_w(fg)
                wi = wsel[fg]
                p = P()
                for kc in range(8):
                    S.mm(p[:, 0:256], wg_t[wi][:, kc, fc * 128:(fc + 1) * 128], xeT[k][:, kc, :], start=(kc == 0), stop=(kc == 7))
                for kc in range(8):
                    S.mm(p[:, 256:512], wu_t[wi][:, kc, fc * 128:(fc + 1) * 128], xeT[k][:, kc, :], start=(kc == 0), stop=(kc == 7))
                return p

            pcur = GU(0)
            for n in range(22):
                fg, fc = n // 2, n % 2
                wi = wsel[fg]
                pnext = GU(n + 1) if n < 21 else None
                S.act(sl, pcur[:, 0:256], AF.Silu)
                S.tt("dve", actT[:, fc, :], sl, pcur[:, 256:512], ALU.mult)
                for jc in range(2):
                    for dh in range(2):
                        S.mm(Y[jc * 2 + dh], actT[:, fc, jc * 128:(jc + 1) * 128], wd_t[wi][:, fc, dh * 512:(dh + 1) * 512],
                             start=(n == 0), stop=(n == 21))
                pcur = pnext
                chunk_i += 1
                if chunk_i <= 16:
                    for _ in range(min(2, len(pend_sc))):
                        pend_sc.pop(0)()
                if chunk_i <= 18 and chunk_i % 2 == 0 and pend_pr:
                    pend_pr.pop(0)()
                if chunk_i > 18:
                    while pend_sc:
                        pend_sc.pop(0)()
                    while pend_pr:
                        pend_pr.pop(0)()
                    if pend_tr:
                        pend_tr.pop(0)()
            while pend_sc:
                pend_sc.pop(0)()
            while pend_pr:
                pend_pr.pop(0)()
            while pend_tr:
                pend_tr.pop(0)()
            for jc in range(2):
                for dh in range(2):
                    S.stt("dve", y_bf[:, jc, dh * 512:(dh + 1) * 512], Y[jc * 2 + dh], gslot[k][:, jc:jc + 1],
                          mrows[:, 2, dh * 512:(dh + 1) * 512], ALU.mult, ALU.mult)
        for f in scatter_pieces(15):
            f()
        S.pop()
        S.push()
        ln2rows = S.sb("ln2rows", [128, 2, 1024], F32)
        S.dma("sp", ln2rows[:, 0, :], ln2g_d.pb(128))
        S.dma("sp", ln2rows[:, 1, :], ln2b_d.pb(128))
        for t in range(16):
            a_ = acc[:, t, :]
            layer_norm_stats(a_, st6, mv2, rstd1)
            o_ = xt[t % 2]
            S.ts("dve", o_, a_, mv2[:, 0:1], ALU.subtract, rstd1, ALU.mult)
            S.tt("dve", o_, o_, ln2rows[:, 0, :], ALU.mult)
            S.tt("dve", o_, o_, ln2rows[:, 1, :], ALU.add)
            S.dma("sp", out_d[b, t * 128:(t + 1) * 128, :], o_)
        S.pop()
        S.pop()
    S.finish()
    return nc


def kernel(x, c, ctx, c_ctx, ada_w, ada_b, w_in, rw_mu, rw_w0, rw_w2, rw_a0, rw_a2, rw_g2,
           rw_k_k, rw_k_a, rw_r_k, rw_gn_w, rw_gn_b, gla_a2, gla_a_b, gla_norm_w, w_out,
           ln1_g, ln1_b, router_w, ex_gate, ex_up, ex_down, ln2_g, ln2_b):
    f = lambda a: np.ascontiguousarray(np.asarray(a, dtype=np.float32))
    x, c, ctx, c_ctx = f(x), f(c), f(ctx), f(c_ctx)
    cbf, cfc = host_consts()
    eoh = np.zeros((16, 16 * 128), np.float32)
    for e in range(16):
        eoh[e, e * 128:(e + 1) * 128] = 1.0
    mu_ext = np.zeros((1, 3328), np.float32)
    mu_ext[0, :1760] = f(rw_mu)[0]
    shared = {
        "ada_w": f(ada_w)[0], "ada_b": f(ada_b)[0][None, :],
        "ada_bT": np.ascontiguousarray(f(ada_b)[0].reshape(48, 128).T),
        "w_in": f(w_in)[0], "mu_ext": mu_ext,
        "rw_w0": f(rw_w0)[0], "rw_w2": f(rw_w2)[0], "rw_a0": f(rw_a0)[0], "rw_a2": f(rw_a2)[0],
        "rw_g2": f(rw_g2)[0], "rw_k_k": f(rw_k_k)[0][None, :], "rw_k_a": f(rw_k_a)[0][None, :],
        "rw_r_k": f(rw_r_k)[0].reshape(1, 512), "rw_gn_w": f(rw_gn_w)[0][None, :], "rw_gn_b": f(rw_gn_b)[0][None, :],
        "gla_a2": f(gla_a2)[0], "gla_a_b": f(gla_a_b)[0], "gla_norm_w": f(gla_norm_w)[0][None, :],
        "w_out": f(w_out)[0], "ln1_g": f(ln1_g)[0][None, :], "ln1_b": f(ln1_b)[0][None, :],
        "router_w": f(router_w)[0], "ex_gate": f(ex_gate)[0], "ex_up": f(ex_up)[0], "ex_down": f(ex_down)[0],
        "ln2_g": f(ln2_g)[0][None, :], "ln2_b": f(ln2_b)[0][None, :],
        "cbf": cbf, "cf": cfc, "eoh": eoh,
    }
    in_maps = []
    for core in range(8):
        bs = slice(core * NB, (core + 1) * NB)
        cc = np.stack([c[core * NB], c[core * NB + 1], c_ctx], axis=1)
        cT = np.ascontiguousarray(cc.reshape(8, 128, 3).transpose(1, 0, 2))
        m = dict(shared)
        m["x"] = np.ascontiguousarray(x[bs])
        m["ctx"] = np.ascontiguousarray(ctx[bs])
        m["cT"] = cT
        in_maps.append(m)
    nc = build()
    res = run_bass_kernel_spmd(nc, in_maps, core_ids=list(range(8)))
    if DEBUG["dump"]:
        DEBUG["res"] = res.results
    return np.concatenate([np.asarray(r["out"], dtype=np.float32) for r in res.results], axis=0)
```

```python
from contextlib import ExitStack
import numpy as np
import concourse.bass as bass
import concourse.mybir as mybir
from concourse.bass_utils import run_bass_kernel_spmd

F32 = mybir.dt.float32
BF16 = mybir.dt.bfloat16
AF = mybir.ActivationFunctionType
ALU = mybir.AluOpType
AX = mybir.AxisListType


class Buf:
    __slots__ = ("name", "lw", "rd")

    def __init__(self, name):
        self.name = name
        self.lw = None
        self.rd = {}


class V:
    __slots__ = ("buf", "ap")

    def __init__(self, buf, ap):
        self.buf = buf
        self.ap = ap

    def __getitem__(self, k):
        return V(self.buf, self.ap[k])

    def bc(self, shape):
        return V(self.buf, self.ap.to_broadcast(list(shape)))

    def us(self, axis):
        return V(self.buf, self.ap.unsqueeze(axis))

    def re(self, pat, **kw):
        return V(self.buf, self.ap.rearrange(pat, **kw))

    def pb(self, n=128):
        return V(self.buf, self.ap.partition_broadcast(n))


class Sched:
    ENGS = ("pe", "act", "dve", "pool", "sp")

    def __init__(self, nc, ndma=12):
        self.nc = nc
        self.stack = ExitStack()
        self.scopes = [self.stack]
        self.sem = {}
        self.cnt = {}
        self.prog = {}
        self.seen = {}
        for e in self.ENGS:
            self.sem[e] = self.stack.enter_context(nc.semaphore("s_" + e))
            self.cnt[e] = 0
            self.prog[e] = []
            self.seen[e] = {}
        self.ep = 0
        self.dq = {}
        for q in ("sp", "pool", "act"):
            sems = [self.stack.enter_context(nc.semaphore("d_%s%d" % (q, i))) for i in range(ndma)]
            self.dq[q] = dict(sems=sems, vals=[0] * ndma, nxt=0)
        self.n_ops = 0

    def sb(self, name, shape, dtype):
        self.n_alloc = getattr(self, "n_alloc", 0) + 1
        t = self.scopes[-1].enter_context(self.nc.sbuf_tensor("t%d_%s" % (self.n_alloc, name), list(shape), dtype))
        return V(Buf(name), t[:])

    def ps(self, name, shape, dtype):
        t = self.scopes[-1].enter_context(self.nc.psum_tensor("p_" + name, list(shape), dtype))
        return V(Buf(name), t[:])

    def dram(self, name, shape, dtype, kind="Internal"):
        if DEBUG["dump"]:
            kind = "ExternalOutput"
        t = self.nc.dram_tensor(name, list(shape), dtype, kind=kind)
        return V(Buf(name), t.ap())

    def epoch(self):
        self.barrier()
        self.ep += 1
        for e in self.ENGS:
            self.sem[e] = self.stack.enter_context(self.nc.semaphore("s_%s_%d" % (e, self.ep)))
            self.cnt[e] = 0
            for k in [k for k in self.seen[e] if isinstance(k, str)]:
                del self.seen[e][k]

    def alias_acquire(self, parent, aps, name="al"):
        out = []
        for i, ap in enumerate(aps):
            b = Buf("%s_%s%d" % (parent.buf.name, name, i))
            b.lw = parent.buf.lw
            b.rd = dict(parent.buf.rd)
            out.append(V(b, ap))
        return out

    def alias_release(self, parent, children):
        for c in children:
            evs = list(c.buf.rd.values())
            if c.buf.lw is not None:
                evs.append(c.buf.lw)
            for ev in evs:
                old = parent.buf.rd.get(ev[0])
                if old is None or (old[2], old[1]) < (ev[2], ev[1]):
                    parent.buf.rd[ev[0]] = ev

    def push(self):
        self.scopes.append(ExitStack())

    def pop(self):
        self.barrier()
        self.flush()
        self.scopes.pop().close()

    def _semof(self, key):
        if isinstance(key, str):
            return self.sem[key]
        q, slot = key
        return self.dq[q]["sems"][slot]

    def _collect(self, eng, reads, writes, is_dma):
        waits = {}

        def need(ev, war=False):
            if ev is None:
                return
            key, val, dep_ep = ev
            if dep_ep < self.ep:
                return
            if not is_dma and isinstance(key, str) and key == eng:
                if eng == "pe" or war:
                    return
            if self.seen[eng].get(key, 0) >= val:
                return
            if waits.get(key, 0) < val:
                waits[key] = val

        for b in reads:
            need(b.lw)
        for b in writes:
            need(b.lw)
            for ev in b.rd.values():
                need(ev, war=True)
        return waits

    def _commit(self, eng, waits):
        for k, v in waits.items():
            self.seen[eng][k] = v
        return [(self._semof(k), v) for k, v in waits.items()]

    def _record(self, ev, reads, writes):
        for b in reads:
            b.rd[ev[0]] = ev
        for b in writes:
            b.lw = ev
            b.rd = {}

    def op(self, eng, fn, reads=(), writes=()):
        waits = self._collect(eng, reads, writes, False)
        wl = self._commit(eng, waits)
        self.cnt[eng] += 1
        ev = (eng, self.cnt[eng], self.ep)
        self.prog[eng].append((wl, fn, (self.sem[eng], 1)))
        self._record(ev, reads, writes)
        self.n_ops += 1

    def dma(self, q, out, in_, **kw):
        oap, iap = out.ap, in_.ap
        d = self.dq[q]
        slot = d["nxt"]
        d["nxt"] = (slot + 1) % len(d["sems"])
        waits = self._collect(q, [in_.buf], [out.buf], True)
        key = (q, slot)
        prev = d["vals"][slot]
        if prev and self.seen[q].get(key, 0) < prev:
            waits[key] = max(waits.get(key, 0), prev)
        wl = self._commit(q, waits)
        d["vals"][slot] = prev + 16
        ev = (key, prev + 16, self.ep)
        sem = d["sems"][slot]
        self.prog[q].append((wl, lambda e: e.dma_start(out=oap, in_=iap, **kw), (sem, 16)))
        self._record(ev, [in_.buf], [out.buf])
        self.n_ops += 1

    @staticmethod
    def _bufs(*xs):
        return [x.buf for x in xs if isinstance(x, V)]

    @staticmethod
    def _a(x):
        return x.ap if isinstance(x, V) else x

    def mm(self, out, lhsT, rhs, start=True, stop=True):
        o, l, r = out.ap, lhsT.ap, rhs.ap
        self.op("pe", lambda e: e.matmul(o, l, r, start=start, stop=stop),
                reads=self._bufs(lhsT, rhs), writes=[out.buf])

    def tr(self, out, in_, ident):
        o, i, d = out.ap, in_.ap, ident.ap
        self.op("pe", lambda e: e.transpose(o, i, d), reads=self._bufs(in_, ident), writes=[out.buf])

    def act(self, out, in_, func, bias=None, scale=None):
        kw = {}
        if bias is not None:
            kw["bias"] = self._a(bias)
        if scale is not None:
            kw["scale"] = self._a(scale)
        o, i = out.ap, in_.ap
        self.op("act", lambda e: e.activation(o, i, func, **kw),
                reads=self._bufs(in_, bias, scale), writes=[out.buf])

    def tt(self, eng, out, a, b, op):
        o, x, y = out.ap, a.ap, b.ap
        self.op(eng, lambda e: e.tensor_tensor(o, x, y, op), reads=self._bufs(a, b), writes=[out.buf])

    def ts(self, eng, out, a, s1, op0, s2=None, op1=None):
        o, x, p1, p2 = out.ap, a.ap, self._a(s1), self._a(s2)
        if op1 is None:
            self.op(eng, lambda e: e.tensor_scalar(o, x, p1, None, op0),
                    reads=self._bufs(a, s1), writes=[out.buf])
        else:
            self.op(eng, lambda e: e.tensor_scalar(o, x, p1, p2, op0, op1),
                    reads=self._bufs(a, s1, s2), writes=[out.buf])

    def stt(self, eng, out, a, sc, b, op0, op1):
        o, x, p, y = out.ap, a.ap, self._a(sc), b.ap
        eng = "dve"
        self.op(eng, lambda e: e.scalar_tensor_tensor(o, x, p, y, op0, op1),
                reads=self._bufs(a, sc, b), writes=[out.buf])

    def cp(self, eng, out, a):
        o, x = out.ap, a.ap
        if eng == "act":
            self.op("act", lambda e: e.activation(o, x, AF.Copy), reads=[a.buf], writes=[out.buf])
        else:
            self.op(eng, lambda e: e.tensor_copy(o, x), reads=[a.buf], writes=[out.buf])

    def red(self, eng, out, a, op=None):
        o, x = out.ap, a.ap
        op = ALU.add if op is None else op
        self.op(eng, lambda e: e.tensor_reduce(o, x, AX.X, op), reads=[a.buf], writes=[out.buf])

    def memset(self, eng, out, val):
        o = out.ap
        self.op(eng, lambda e: e.memset(o, val), writes=[out.buf])

    def barrier(self):
        for e in self.ENGS:
            waits = {}
            for e2 in self.ENGS:
                if e2 != e and self.cnt[e2] > self.seen[e].get(e2, 0):
                    waits[e2] = self.cnt[e2]
            if e not in ("pe",) and self.cnt[e] > self.seen[e].get(e, 0):
                waits[e] = self.cnt[e]
            for q, d in self.dq.items():
                for slot, v in enumerate(d["vals"]):
                    if v and self.seen[e].get((q, slot), 0) < v:
                        waits[(q, slot)] = v
            wl = self._commit(e, waits)
            if wl:
                self.prog[e].append((wl, None, None))

    def flush(self):
        nc = self.nc
        prog = self.prog
        self.prog = {e: [] for e in self.ENGS}

        def replay(name, e):
            for wl, fn, inc in prog[name]:
                for sem, val in wl:
                    e.wait_ge(sem, val)
                if fn is not None:
                    ins = fn(e)
                    ins.then_inc(inc[0], inc[1])

        with nc.Block() as block:
            @block.tensor
            def _(e):
                replay("pe", e)

            @block.scalar
            def _(e):
                replay("act", e)

            @block.vector
            def _(e):
                replay("dve", e)

            @block.gpsimd
            def _(e):
                replay("pool", e)

            @block.sync
            def _(e):
                replay("sp", e)

    def finish(self):
        self.barrier()
        self.flush()
        self.stack.close()


NB = 2
ALPHA = float(2.0 ** 0.25)
CRW = -float(np.exp(-0.5))
GN_EPS = 64e-5
DEBUG = {"stop_after": None, "cut": 99, "dump": False, "res": None, "dbg": False}


def host_consts():
    p = np.arange(128)[:, None]
    q = np.arange(128)[None, :]
    Us = (p < q).astype(np.float32)
    Ui = (p <= q).astype(np.float32)
    Ls = (p > q).astype(np.float32)
    Li = (p >= q).astype(np.float32)
    ident = (p == q).astype(np.float32)
    sm = []
    for l in range(7):
        sz = 2 ** l
        same = (p // (2 * sz)) == (q // (2 * sz))
        lo = same & ((p % (2 * sz)) >= sz) & ((q % (2 * sz)) < sz)
        sm.append((lo | lo.T).astype(np.float32))
    cbf = np.concatenate([ident, Us, Ui, Ls, Li, np.ones((128, 128), np.float32)] + sm, axis=1)
    cf = np.concatenate([Ui * CRW, Us * CRW, Ls * CRW, Li * CRW,
                         Ui / 16.0, Us / 16.0, Ls / 16.0, Li / 16.0,
                         np.full((128, 1), CRW, np.float32), np.full((128, 1), 1.0 / 16.0, np.float32),
                         np.arange(128, dtype=np.float32)[:, None], np.arange(128, dtype=np.float32)[:, None] + 128.0,
                         np.tile(np.arange(256, dtype=np.float32)[None, :], (128, 1))], axis=1)
    return np.ascontiguousarray(cbf), np.ascontiguousarray(cf.astype(np.float32))


def build():
    nc = bass.Bass("TRN2", target_bir_lowering=False)
    S = Sched(nc)

    def din(name, shape):
        return V(Buf(name), nc.dram_tensor(name, list(shape), F32, kind="ExternalInput").ap())

    x_d = din("x", [NB, 2048, 1024])
    ctx_d = din("ctx", [NB, 256, 1024])
    cT_d = din("cT", [128, 8, 3])
    adaw_d = din("ada_w", [1024, 6144])
    adab_d = din("ada_b", [1, 6144])
    adabT_d = din("ada_bT", [128, 48])
    win_d = din("w_in", [1024, 3328])
    mu_d = din("mu_ext", [1, 3328])
    w0_d = din("rw_w0", [2, 512])
    w2_d = din("rw_w2", [2, 32, 512])
    a0_d = din("rw_a0", [2, 512])
    a2_d = din("rw_a2", [2, 32, 512])
    g2_d = din("rw_g2", [96, 512])
    kk_d = din("rw_k_k", [1, 512])
    ka_d = din("rw_k_a", [1, 512])
    rk_d = din("rw_r_k", [1, 512])
    gnw_d = din("rw_gn_w", [1, 512])
    gnb_d = din("rw_gn_b", [1, 512])
    ga2_d = din("gla_a2", [2, 16, 256])
    gab_d = din("gla_a_b", [2, 256])
    gnorm_d = din("gla_norm_w", [1, 512])
    wout_d = din("w_out", [1024, 1024])
    ln1g_d = din("ln1_g", [1, 1024])
    ln1b_d = din("ln1_b", [1, 1024])
    rtr_d = din("router_w", [1024, 16])
    exg_d = din("ex_gate", [16, 1024, 2816])
    exu_d = din("ex_up", [16, 1024, 2816])
    exd_d = din("ex_down", [16, 2816, 1024])
    ln2g_d = din("ln2_g", [1, 1024])
    ln2b_d = din("ln2_b", [1, 1024])
    cbf_d = din("cbf", [128, 1664])
    cf_d = din("cf", [128, 8 * 128 + 4 + 256])
    eoh_d = din("eoh", [16, 16 * 128])
    out_d = V(Buf("out"), nc.dram_tensor("out", [NB, 2048, 1024], F32, kind="ExternalOutput").ap())
    mix_d = S.dram("mixs", [NB, 2048, 1024], BF16)
    yf_d = S.dram("yfs", [16, 128, 1536], F32)
    modrow_d = S.dram("modrows", [NB, 4, 1024], F32)
    dbgf_d = S.dram("dbgf", [24, 128, 512], F32)
    dbgb_d = S.dram("dbgb", [24, 128, 512], BF16)
    dbgn = {"f": 0, "b": 0, "names": []}

    def dump(name, v, bf):
        k = "b" if bf else "f"
        i = dbgn[k]
        dbgn[k] += 1
        dbgn["names"].append((name, k, i, tuple(v.ap.shape)))
        dst = (dbgb_d if bf else dbgf_d)
        p = v.ap.shape[0]
        n = 1
        for d_ in v.ap.shape[1:]:
            n *= d_
        dv = dst[i, 0:p, 0:n]
        if len(v.ap.shape) == 3:
            dv = dv.re("p (a c) -> p a c", a=v.ap.shape[1])
        S.dma("sp", dv, v)
    DEBUG["names"] = dbgn["names"]

    cb = S.sb("cb", [128, 1664], BF16)
    S.dma("pool", cb, cbf_d)
    ident, mUs, mUi, mLs, mLi = (cb[:, i * 128:(i + 1) * 128] for i in range(5))
    ones_bf = cb[:, 640:768]
    smask = [cb[:, 768 + l * 128:896 + l * 128] for l in range(7)]
    cf = S.sb("cf", [128, 8 * 128 + 4 + 256], F32)
    S.dma("sp", cf, cf_d)
    fm = [cf[:, i * 128:(i + 1) * 128] for i in range(8)]
    negcol = cf[:, 1024:1025]
    g16col = cf[:, 1025:1026]
    iota_p0 = cf[:, 1026:1027]
    iota_p1 = cf[:, 1027:1028]
    iota_f = cf[:, 1028:1284]
    modT = S.sb("modT", [128, 16, 3], F32)

    pbanks = [S.ps("pb%d" % i, [128, 512], F32) for i in range(6)]
    ptbanks = [S.ps("ptb%d" % i, [128, 1024], BF16) for i in range(2)]
    rot = {"p": 0, "t": 0}

    def P():
        rot["p"] = (rot["p"] + 1) % 6
        return pbanks[rot["p"]]

    def PT():
        rot["t"] = (rot["t"] + 1) % 2
        return ptbanks[rot["t"]]

    def layer_norm_stats(xt, st, mv, rstd, eps=1e-5):
        for c in range(2):
            o, i = st.ap[:, c, :], xt.ap[:, c * 512:(c + 1) * 512]
            S.op("dve", lambda e, o=o, i=i: e.bn_stats(o, i), reads=[xt.buf], writes=[st.buf])
        S.op("dve", lambda e: e.bn_aggr(mv.ap, st.ap), reads=[st.buf], writes=[mv.buf])
        S.act(rstd, mv[:, 1:2], AF.Sqrt, bias=eps)
        S.op("dve", lambda e: e.reciprocal(rstd.ap, rstd.ap), reads=[rstd.buf], writes=[rstd.buf])

    S.push()
    cT = S.sb("cT", [128, 8, 3], F32)
    S.dma("sp", cT, cT_d)
    scb = S.sb("scb", [128, 8, 3], BF16)
    S.act(scb, cT, AF.Silu)
    rep = []
    for b in range(NB):
        r_ = S.sb("rep%d" % b, [128, 8, 128], BF16)
        S.cp("dve", r_, scb[:, :, b:b + 1].bc([128, 8, 128]))
        rep.append(r_)
    adabT = S.sb("adabT", [128, 48], F32)
    S.dma("sp", adabT, adabT_d)
    blk = [S.sb("adablk%d" % i, [128, 8, 512], BF16) for i in range(2)]
    brow = [S.sb("adabrow%d" % i, [128, 512], F32) for i in range(2)]
    mrow = [S.sb("mrow%d" % i, [128, 512], F32) for i in range(2)]
    psT = P()
    psTv = psT[:, 0:48].re("p (j c) -> p j c", c=3)
    adaw_v = adaw_d.re("(kc p) c -> p kc c", p=128)
    for jb in range(12):
        bk = blk[jb % 2]
        S.dma("pool", bk, adaw_v[:, :, jb * 512:(jb + 1) * 512])
        if jb < 4:
            for jj in range(4):
                j = jb * 4 + jj
                for kc in range(8):
                    S.mm(psTv[:, j, :], bk[:, kc, jj * 128:(jj + 1) * 128], scb[:, kc, :],
                         start=(kc == 0), stop=(kc == 7))
            if jb == 3:
                for c in range(3):
                    S.tt("dve", modT[:, 0:8, c], psTv[:, 0:8, c], adabT[:, 0:8], ALU.add)
                    S.stt("dve", modT[:, 8:16, c], psTv[:, 8:16, c], 1.0, adabT[:, 8:16], ALU.add, ALU.add)
        else:
            which = (jb - 4) // 2
            half = (jb - 4) % 2
            br = brow[jb % 2]
            S.dma("sp", br, adab_d[:, jb * 512:(jb + 1) * 512].pb(128))
            for b in range(NB):
                pr = P()
                for kc in range(8):
                    S.mm(pr, rep[b][:, kc, :], bk[:, kc, :], start=(kc == 0), stop=(kc == 7))
                mr = mrow[b]
                if which == 2:
                    S.stt("dve", mr, pr, 1.0, br, ALU.add, ALU.add)
                else:
                    S.tt("dve", mr, pr, br, ALU.add)
                S.dma("sp", modrow_d[b, which:which + 1, half * 512:(half + 1) * 512], mr[0:1, :])
    S.pop()
    if DEBUG["stop_after"] == "A":
        S.finish()
        return nc

    S.push()
    Wa = S.sb("Wa", [128, 8, 3072], BF16)
    Wb = S.sb("Wb", [128, 8, 1536], BF16)
    Wl = [S.sb("Wl%d" % d, [128, 8, 96], BF16) for d in range(2)]
    Wlb = [S.sb("Wlb%d" % d, [128, 8, 96], BF16) for d in range(2)]
    Wgd = S.sb("Wgd", [128, 8, 96], BF16)
    Wgdb = S.sb("Wgdb", [128, 8, 96], BF16)
    sw = [S.sb("sw%d" % d, [96, 512], BF16) for d in range(2)]
    g2w = S.sb("g2w", [96, 512], BF16)
    br3 = S.sb("br3", [96, 2, 512], F32)
    onesf = S.sb("onesf", [96, 128], F32)
    S.memset("dve", onesf, 1.0)
    for d in range(2):
        S.dma("sp", br3[0:1, d, :], w0_d[d:d + 1, :])
        S.dma("sp", br3[32:33, d, :], a0_d[d:d + 1, :])
        S.dma("sp", br3[64:65, d, 0:256], gab_d[d:d + 1, :])
    rows = S.sb("rows", [128, 6, 512], F32)
    kkrow, karow, rkrow, gnwrow, gnbrow, gnormrow = (rows[:, i, :] for i in range(6))
    for i, dsrc in enumerate((kk_d, ka_d, rk_d, gnw_d, gnb_d, gnorm_d)):
        S.dma("sp", rows[:, i, :], dsrc.pb(128))
    for d in range(2):
        S.dma("pool", sw[d][0:32, :], w2_d[d])
        S.dma("pool", sw[d][32:64, :], a2_d[d])
        S.dma("pool", sw[d][64:80, 0:256], ga2_d[d])
    S.dma("pool", g2w, g2_d)

    S.push()
    mub = S.sb("mub", [128, 3328], F32)
    omm = S.sb("omm", [128, 3328], F32)
    S.dma("sp", mub, mu_d.pb(128))
    S.ts("dve", omm, mub, -1.0, ALU.mult, 1.0, ALU.add)
    stg = [S.sb("stg%d" % i, [128, 3328], F32) for i in range(2)]
    for kc in range(8):
        st_ = stg[kc % 2]
        S.dma("sp", st_, win_d[kc * 128:(kc + 1) * 128, :])
        S.tt("dve", Wa[:, kc, 0:1536], st_[:, 0:1536], omm[:, 0:1536], ALU.mult)
        S.tt("dve", Wa[:, kc, 1536:3072], st_[:, 1760:3296], omm[:, 1760:3296], ALU.mult)
        S.tt("pool", Wb[:, kc, :], st_[:, 0:1536], mub[:, 0:1536], ALU.mult)
        for d in range(2):
            for (o0, c0, n) in ((0, 1536 + 32 * d, 32), (32, 1600 + 32 * d, 32), (64, 3296 + 16 * d, 16)):
                S.tt("pool", Wl[d][:, kc, o0:o0 + n], st_[:, c0:c0 + n], omm[:, c0:c0 + n], ALU.mult)
            for (o0, c0, n) in ((0, 1536 + 32 * d, 32), (32, 1600 + 32 * d, 32)):
                S.tt("pool", Wlb[d][:, kc, o0:o0 + n], st_[:, c0:c0 + n], mub[:, c0:c0 + n], ALU.mult)
        S.tt("pool", Wgd[:, kc, :], st_[:, 1664:1760], omm[:, 1664:1760], ALU.mult)
        S.tt("pool", Wgdb[:, kc, :], st_[:, 1664:1760], mub[:, 1664:1760], ALU.mult)
    S.pop()
    for d in range(2):
        S.memset("pool", Wl[d][:, :, 80:96], 0.0)
        S.memset("pool", Wlb[d][:, :, 64:96], 0.0)

    def f32t(name, n=512, p=128):
        return S.sb(name, [p, n], F32)

    def bft(name, n=512, p=128):
        return S.sb(name, [p, n], BF16)

    xring = [f32t("xr%d" % i, 1024) for i in range(1)]
    hring = [S.sb("hT%d" % i, [128, 8, 128], BF16) for i in range(3)]
    hnb = S.sb("hnb", [128, 8, 128], BF16)
    st6 = S.sb("st6", [128, 2, 6], F32)
    mv2 = S.sb("mv2", [128, 2], F32)
    rstd1 = S.sb("rstd1", [128, 1], F32)
    r_sb, k_sb, v_sb, qk_sb = f32t("r_sb"), f32t("k_sb"), f32t("v_sb"), f32t("qk_sb")
    v_bf, vg_bf = bft("v_bf"), bft("vg_bf")
    lo = S.sb("lo", [96, 128], BF16)
    lo_gd = S.sb("lo_gd", [96, 128], BF16)
    sg, a_sb, lg = f32t("sg"), f32t("a_sb"), f32t("lg", 256)
    E1, E2, E3, E4 = f32t("E1"), f32t("E2"), f32t("E3"), f32t("E4")
    t0, t1, kk, kka, kh = f32t("t0"), f32t("t1"), f32t("kk"), f32t("kka"), f32t("kh")
    xn_bf = V(t0.buf, t0.ap.bitcast(BF16))
    nbacc = V(t1.buf, t1.ap.bitcast(BF16)).re("p (k t) -> p k t", k=8)
    ss = S.sb("ss", [128, 8], F32)
    bs = S.sb("bs", [128, 8], F32)
    pc_sb = S.sb("pc_sb", [64, 8], F32)
    pcg_sb = S.sb("pcg_sb", [64, 4], F32)
    At_bf, Bt_bf, Kt_bf, Rt_bf, Bh_bf, Kh_bf = (bft(n) for n in ("At", "Bt", "Kt", "Rt", "Bh", "Kh"))
    qt_bf, kt_bf, khg_bf = bft("qt", 256), bft("kt", 256), bft("khg", 256)

    def ht(name, p=128):
        return S.sb(name, [p, 4, 128], BF16)

    AtT, BtT, KtT, RtT = ht("AtT", 64), ht("BtT", 64), ht("KtT", 64), ht("RtT", 64)
    LakT, ArbT, ArkT, AqkT = ht("LakT"), ht("ArbT"), ht("ArkT"), ht("AqkT")
    Xr = [ht("Xr0"), ht("Xr1")]
    XTr = [ht("XTr0"), ht("XTr1")]
    Zr = [ht("Zr0"), ht("Zr1")]
    def half_aps(t):
        tb = t.ap.bitcast(BF16)
        return [tb[:, i * 512:(i + 1) * 512].rearrange("p (h t) -> p h t", h=4) for i in range(2)]
    QeffT, qtT, ktT = ht("QeffT", 64), ht("qtT", 64), ht("ktT", 64)
    HT = S.sb("HT", [64, 4, 64], BF16)
    M_f = [S.sb("M_f%d" % d, [64, 8, 64], F32) for d in range(2)]
    M_bf = [S.sb("M_bf%d" % d, [64, 8, 64], BF16) for d in range(2)]
    Mg_f = [S.sb("Mg_f%d" % d, [64, 4, 128], F32) for d in range(2)]
    Mg_bf = [S.sb("Mg_bf%d" % d, [64, 4, 128], BF16) for d in range(2)]
    y_sb, bo_sb, o_sb = f32t("y_sb"), f32t("bo_sb"), f32t("o_sb")
    yfl = f32t("yfl", 1536)
    gout_sb, gsil = sg, a_sb
    mix_bf = bft("mix_bf", 1024)
    s8a, s8b, s8c, s8d = (S.sb("s8%s" % n, [128, 8], F32) for n in "abcd")

    if DEBUG.get("verbose"):
        print("mixer SBUF bytes remaining per partition:", nc.sbuf_bytes_remaining)

    def v4(pbank, p=128, w=128):
        return pbank[0:p, 0:4 * w].re("p (h t) -> p h t", h=4)

    def proj_block(dst, hT, hn, c0, ncols, rw):
        n = 16 if rw else 8
        i = 0
        for kc in range(8):
            S.mm(dst[:, 0:ncols], hT[:, kc, :], Wa[:, kc, c0:c0 + ncols], start=(i == 0), stop=(i == n - 1))
            i += 1
        if rw:
            for kc in range(8):
                S.mm(dst[:, 0:ncols], hn[:, kc, :], Wb[:, kc, c0:c0 + ncols], start=False, stop=(i == n - 1))
                i += 1

    def produce(b, kind, idx):
        xt = xring[0]
        src = ctx_d[b, idx * 128:(idx + 1) * 128, :] if kind == "c" else x_d[b, idx * 128:(idx + 1) * 128, :]
        S.dma("sp", xt, src)
        layer_norm_stats(xt, st6, mv2, rstd1)
        S.ts("dve", xn_bf, xt, mv2[:, 0:1], ALU.subtract, rstd1, ALU.mult)
        pt = PT()
        ptv = pt.re("p (k t) -> p k t", k=8)
        col = 2 if kind == "c" else b
        hs = hring[idx % 3]
        for kc in range(8):
            S.tr(ptv[:, kc, :], xn_bf[:, kc * 128:(kc + 1) * 128], ident)
        for kc in range(8):
            S.act(hs[:, kc, :], ptv[:, kc, :], AF.Identity, bias=modT[:, kc, col:col + 1],
                  scale=modT[:, 8 + kc, col:col + 1])

    def neighbor(kind, idx, n_tiles):
        C = hring[idx % 3]
        Pv = hring[(idx - 1) % 3] if idx > 0 else None
        Nx = hring[(idx + 1) % 3] if idx < n_tiles - 1 else None
        if kind == "l":
            if Pv is not None:
                S.tt("dve", nbacc[:, :, 0:64], C[:, :, 64:128], Pv[:, :, 64:128], ALU.add)
            else:
                S.cp("dve", nbacc[:, :, 0:64], C[:, :, 64:128])
            if Nx is not None:
                S.tt("dve", nbacc[:, :, 64:128], C[:, :, 0:64], Nx[:, :, 0:64], ALU.add)
            else:
                S.cp("dve", nbacc[:, :, 64:128], C[:, :, 0:64])
            a4 = nbacc.re("p k (r c) -> p k r c", r=2)
            c4 = C.re("p k (r c) -> p k r c", r=2)
            S.tt("dve", a4[:, :, :, 1:64], a4[:, :, :, 1:64], c4[:, :, :, 0:63], ALU.add)
            S.tt("dve", a4[:, :, :, 0:63], a4[:, :, :, 0:63], c4[:, :, :, 1:64], ALU.add)
            S.ts("dve", hnb, nbacc, 0.25, ALU.mult)
        else:
            S.cp("dve", nbacc[:, :, 1:128], C[:, :, 0:127])
            if Pv is not None:
                S.cp("dve", nbacc[:, :, 0:1], Pv[:, :, 127:128])
            else:
                S.memset("dve", nbacc[:, :, 0:1], 0.0)
            S.tt("dve", nbacc[:, :, 0:127], nbacc[:, :, 0:127], C[:, :, 1:128], ALU.add)
            if Nx is not None:
                S.tt("dve", nbacc[:, :, 127:128], nbacc[:, :, 127:128], Nx[:, :, 0:1], ALU.add)
            S.ts("dve", hnb, nbacc, 0.5, ALU.mult)

    def mix_tile(b, kind, idx, d, want_out):
        hT = hring[idx % 3]
        final = want_out and d == 1
        p = P(); proj_block(p, hT, hnb, 0, 512, True); S.cp("act", r_sb, p)
        p = P(); proj_block(p, hT, hnb, 512, 512, True); S.cp("act", k_sb, p)
        p = P(); proj_block(p, hT, hnb, 1024, 512, True); S.cp("act", v_sb, p); S.cp("pool", v_bf, v_sb)
        p = P(); proj_block(p, hT, hnb, 1536, 512, False); S.cp("act", qk_sb, p)
        p = P(); proj_block(p, hT, hnb, 2048, 512, False); S.cp("act", vg_bf, p)
        if DEBUG["cut"] <= 0:
            return
        pl = P()
        for kc in range(8):
            S.mm(pl[0:96, 0:128], Wl[d][:, kc, :], hT[:, kc, :], start=(kc == 0), stop=False)
        for kc in range(8):
            S.mm(pl[0:96, 0:128], Wlb[d][:, kc, :], hnb[:, kc, :], start=False, stop=(kc == 7))
        S.act(lo[0:32, :], pl[0:32, 0:128], AF.Tanh)
        S.act(lo[32:64, :], pl[32:64, 0:128], AF.Copy)
        S.act(lo[64:80, :], pl[64:80, 0:128], AF.Copy)
        pw = P()
        S.mm(pw, lo[0:32, :], sw[d][0:32, :], start=True, stop=False)
        S.mm(pw, onesf[0:1, :], br3[0:1, d, :], start=False, stop=True)
        S.act(sg, pw, AF.Sigmoid)
        pa = P()
        S.mm(pa, lo[32:64, :], sw[d][32:64, :], start=True, stop=False)
        S.mm(pa, onesf[32:33, :], br3[32:33, d, :], start=False, stop=True)
        S.act(a_sb, pa, AF.Sigmoid)
        pg = P()
        S.mm(pg[:, 0:256], lo[64:80, :], sw[d][64:80, 0:256], start=True, stop=False)
        S.mm(pg[:, 0:256], onesf[64:65, :], br3[64:65, d, 0:256], start=False, stop=True)
        S.act(lg, pg[:, 0:256], AF.Sigmoid)
        S.act(lg, lg, AF.Ln)
        if DEBUG["cut"] <= 1:
            return
        if d == 0:
            mi, me, mr = fm[0], fm[1], fm[2]
            gmi, gmr = fm[4], fm[6]
            m_ij_s, m_ji_s, m_ji_i = mLs, mUs, mUi
        else:
            mi, me, mr = fm[3], fm[2], fm[1]
            gmi, gmr = fm[7], fm[5]
            m_ij_s, m_ji_s, m_ji_i = mUs, mLs, mLi
        pc1 = P(); S.mm(pc1, mi, sg)
        S.act(E1, pc1, AF.Exp); S.act(E2, pc1, AF.Exp, scale=-1.0)
        pc2 = P(); S.mm(pc2, me, sg)
        S.act(E3, pc2, AF.Exp)
        pc3 = P(); S.mm(pc3, mr, sg)
        S.act(E4, pc3, AF.Exp)
        pp = P()
        for h in range(8):
            S.mm(pp[0:64, h:h + 1], sg[:, h * 64:(h + 1) * 64], negcol)
        for h in range(4):
            S.mm(pp[0:64, 8 + h:9 + h], lg[:, h * 64:(h + 1) * 64], g16col)
        S.act(pc_sb, pp[0:64, 0:8], AF.Exp)
        S.act(pcg_sb, pp[0:64, 8:12], AF.Exp)
        if DEBUG["cut"] <= 2:
            return
        S.tt("dve", t0, k_sb, kkrow, ALU.mult)
        S.tt("pool", t1, t0, t0, ALU.mult)
        S.red("dve", ss, t1.re("p (h c) -> p h c", h=8))
        S.act(ss, ss, AF.Sqrt, bias=1e-12)
        S.op("dve", lambda e: e.reciprocal(ss.ap, ss.ap), reads=[ss.buf], writes=[ss.buf])
        h8 = lambda t: t.re("p (h c) -> p h c", h=8)
        S.tt("dve", h8(kk), h8(t0), ss.us(2).bc([128, 8, 64]), ALU.mult)
        S.tt("pool", kka, kk, a_sb, ALU.mult)
        S.stt("pool", t1, a_sb, -1.0, karow, ALU.add, ALU.mult)
        S.stt("pool", kh, t1, 1.0, k_sb, ALU.add, ALU.mult)
        S.stt("dve", At_bf, kk, -1.0, E3, ALU.mult, ALU.mult)
        S.tt("dve", Bt_bf, kka, E2, ALU.mult)
        S.tt("dve", Kt_bf, kh, E2, ALU.mult)
        S.tt("dve", Rt_bf, r_sb, E1, ALU.mult)
        S.tt("pool", Bh_bf, kka, E4, ALU.mult)
        S.tt("pool", Kh_bf, kh, E4, ALU.mult)
        if want_out:
            S.tt("pool", t0, r_sb, kh, ALU.mult)
            S.tt("pool", t0, t0, rkrow, ALU.mult)
            S.red("dve", bs, h8(t0))
            S.tt("pool", h8(bo_sb), h8(v_sb), bs.us(2).bc([128, 8, 64]), ALU.mult)
        if DEBUG["cut"] <= 3:
            return
        dbg = DEBUG["dbg"] and b == 0 and kind == "c" and idx == 0 and d == 0
        if dbg:
            for nm, t_ in (("E1", E1), ("E2", E2), ("E3", E3), ("E4", E4), ("kk", kk), ("a", a_sb), ("sg", sg), ("kh", kh),
                           ("r", r_sb), ("v", v_sb)):
                dump(nm, t_, False)
            for nm, t_ in (("At", At_bf), ("Bt", Bt_bf), ("Kt", Kt_bf), ("Rt", Rt_bf), ("Bh", Bh_bf), ("Kh", Kh_bf), ("vb", v_bf)):
                dump(nm, t_, True)
            dump("pc", pc_sb, False)
        G1, G2, G4 = E1, E2, E3
        pg1 = P(); S.mm(pg1[:, 0:256], gmi, lg)
        pg3 = P(); S.mm(pg3[:, 0:256], gmr, lg)
        S.act(G1[:, 0:256], pg1[:, 0:256], AF.Exp)
        S.act(G2[:, 0:256], pg1[:, 0:256], AF.Exp, scale=-1.0)
        S.act(G4[:, 0:256], pg3[:, 0:256], AF.Exp)
        S.stt("dve", qt_bf, qk_sb[:, 0:256], 0.125, G1[:, 0:256], ALU.mult, ALU.mult)
        S.tt("dve", kt_bf, qk_sb[:, 256:512], G2[:, 0:256], ALU.mult)
        S.tt("pool", khg_bf, qk_sb[:, 256:512], G4[:, 0:256], ALU.mult)
        if DEBUG["cut"] <= 4:
            return
        parents = [E1, E2, E3, E4, sg, a_sb, t0, t1, kk, kka, kh, k_sb, qk_sb, r_sb]
        kids = [S.alias_acquire(pt_, half_aps(pt_)) for pt_ in parents]
        flat = [c for pair in kids[3:] for c in pair]
        G = [dict(AtT=AtT, BtT=BtT, KtT=KtT, RtT=RtT, X0=Xr[0], XT0=XTr[0], Lm=Xr[1], LTm=XTr[1],
                  LakT=LakT, ArbT=ArbT, ArkT=ArkT, Z0=Zr[0], Zf=Zr[1], D=kids[0], DT=kids[1],
                  Y1=kids[2][0], Y1t=kids[2][1], QeffT=QeffT, HT=HT),
             dict(AtT=flat[0][0:64], BtT=flat[1][0:64], KtT=flat[2][0:64], RtT=flat[3][0:64],
                  X0=flat[4], XT0=flat[5], Lm=flat[6], LTm=flat[7], LakT=flat[8], ArbT=flat[9], ArkT=flat[10],
                  Z0=flat[11], Zf=flat[12], D=[flat[13], flat[14]], DT=[flat[15], flat[16]],
                  Y1=flat[17], Y1t=flat[18], QeffT=flat[19][0:64], HT=flat[20][0:64, :, 0:64])]
        identb = ident.us(1).bc([128, 4, 128])

        def score(dst, L, R, mask):
            p_ = P()
            pv = v4(p_)
            for hh in range(4):
                S.mm(pv[:, hh, :], L[0:64, hh, :], R[0:64, hh, :])
            S.tt("dve", dst, pv, mask.us(1).bc([128, 4, 128]), ALU.mult)

        for g in range(2):
            T = G[g]
            for src, key, eng in ((At_bf, "AtT", "act"), (Bt_bf, "BtT", "dve"), (Kt_bf, "KtT", "act"), (Rt_bf, "RtT", "dve")):
                pt = PT()
                ptv = v4(pt, 64)
                for hh in range(4):
                    h = 4 * g + hh
                    S.tr(ptv[:, hh, :], src[:, h * 64:(h + 1) * 64], ident)
                S.cp(eng, T[key], ptv)
        for g in range(2):
            T = G[g]
            score(T["X0"], T["AtT"], T["BtT"], m_ij_s)
            score(T["XT0"], T["BtT"], T["AtT"], m_ji_s)
            score(T["LakT"], T["KtT"], T["AtT"], m_ji_s)
            score(T["ArbT"], T["BtT"], T["RtT"], m_ji_i)
            score(T["ArkT"], T["KtT"], T["RtT"], m_ji_i)
        for g in range(2):
            T = G[g]
            p = P()
            pv = p[:, 0:256].re("p (h v) -> p h v", h=4)
            for hh in range(4):
                h = 4 * g + hh
                S.mm(pv[:, hh, :], T["LakT"][:, hh, :], v_bf[:, h * 64:(h + 1) * 64])
            S.cp("act", T["Z0"][:, :, 0:64], pv)
            S.cp("pool", T["Z0"][:, :, 64:128], At_bf[:, g * 256:(g + 1) * 256].re("p (h c) -> p h c", h=4))
            m0 = smask[0].us(1).bc([128, 4, 128])
            S.tt("pool", T["Lm"], T["X0"], m0, ALU.mult)
            S.tt("pool", T["LTm"], T["XT0"], m0, ALU.mult)
            S.tt("dve", T["D"][0], T["Lm"], identb, ALU.add)
            S.tt("dve", T["DT"][0], T["LTm"], identb, ALU.add)
        for l in range(1, 7):
            ml = smask[l].us(1).bc([128, 4, 128])
            for g in range(2):
                T = G[g]
                Dc, DTc = T["D"][(l - 1) % 2], T["DT"][(l - 1) % 2]
                S.tt("pool", T["Lm"], T["X0"], ml, ALU.mult)
                S.tt("pool", T["LTm"], T["XT0"], ml, ALU.mult)
                p = P(); pv = v4(p)
                for hh in range(4):
                    S.mm(pv[:, hh, :], T["LTm"][:, hh, :], Dc[:, hh, :])
                S.cp("act", T["Y1"], pv)
                p = P(); pv = v4(p)
                for hh in range(4):
                    S.mm(pv[:, hh, :], T["Lm"][:, hh, :], DTc[:, hh, :])
                S.cp("dve", T["Y1t"], pv)
            for g in range(2):
                T = G[g]
                Dc, DTc = T["D"][(l - 1) % 2], T["DT"][(l - 1) % 2]
                Dn, DTn = T["D"][l % 2], T["DT"][l % 2]
                p = P(); pv = v4(p)
                for hh in range(4):
                    S.mm(pv[:, hh, :], ident, Dc[:, hh, :], start=True, stop=False)
                    S.mm(pv[:, hh, :], DTc[:, hh, :], T["Y1"][:, hh, :], start=False, stop=True)
                S.cp("act", Dn, pv)
                p = P(); pv = v4(p)
                for hh in range(4):
                    S.mm(pv[:, hh, :], ident, DTc[:, hh, :], start=True, stop=False)
                    S.mm(pv[:, hh, :], Dc[:, hh, :], T["Y1t"][:, hh, :], start=False, stop=True)
                S.cp("dve", DTn, pv)
        for g in range(2):
            T = G[g]
            DTc = T["DT"][0]
            p = P(); pv = v4(p)
            for hh in range(4):
                S.mm(pv[:, hh, :], DTc[:, hh, :], T["Z0"][:, hh, :])
            S.cp("act" if g == 0 else "dve", T["Zf"], pv)
        for g in range(2):
            T = G[g]
            Zf = T["Zf"]
            p = P()
            pv = v4(p, 64)
            for hh in range(4):
                h = 4 * g + hh
                S.mm(pv[:, hh, :], Rt_bf[:, h * 64:(h + 1) * 64], ident, start=True, stop=False)
                S.mm(pv[:, hh, :], Zf[:, hh, 64:128], T["ArbT"][:, hh, :], start=False, stop=True)
            S.cp("act", T["QeffT"], pv)
            p = P()
            pvh = p[0:64, 0:256].re("p (h k) -> p h k", h=4)
            for hh in range(4):
                h = 4 * g + hh
                S.mm(pvh[:, hh, :], Zf[:, hh, 64:128], Bh_bf[:, h * 64:(h + 1) * 64])
            S.cp("dve", T["HT"], pvh)
        for g in range(2):
            T = G[g]
            Zf = T["Zf"]
            if want_out:
                p = P()
                pv = p[:, 0:256].re("p (h v) -> p h v", h=4)
                for hh in range(4):
                    h = 4 * g + hh
                    S.mm(pv[:, hh, :], T["QeffT"][:, hh, :], M_bf[d][:, h, :], start=True, stop=False)
                    S.mm(pv[:, hh, :], T["ArbT"][:, hh, :], Zf[:, hh, 0:64], start=False, stop=False)
                    S.mm(pv[:, hh, :], T["ArkT"][:, hh, :], v_bf[:, h * 64:(h + 1) * 64], start=False, stop=True)
                S.cp("act", y_sb[:, g * 256:(g + 1) * 256], p[:, 0:256])
            p = P()
            pvm = p[0:64, 0:256].re("p (h v) -> p h v", h=4)
            for hh in range(4):
                h = 4 * g + hh
                S.mm(pvm[:, hh, :], T["HT"][:, hh, :], M_bf[d][:, h, :], start=True, stop=False)
                S.mm(pvm[:, hh, :], Bh_bf[:, h * 64:(h + 1) * 64], Zf[:, hh, 0:64], start=False, stop=False)
                S.mm(pvm[:, hh, :], Kh_bf[:, h * 64:(h + 1) * 64], v_bf[:, h * 64:(h + 1) * 64], start=False, stop=True)
            Mv = M_f[d][:, 4 * g:4 * g + 4, :]
            S.tt("dve", Mv, Mv, pc_sb[:, 4 * g:4 * g + 4].us(2).bc([64, 4, 64]), ALU.mult)
            S.tt("dve", Mv, Mv, pvm, ALU.add)
            S.cp("dve", M_bf[d][:, 4 * g:4 * g + 4, :], Mv)
        for pt_, ch in zip(parents, kids):
            S.alias_release(pt_, ch)
        for src, dstT, eng in ((qt_bf, qtT, "act"), (kt_bf, ktT, "dve")):
            pt = PT()
            ptv = v4(pt, 64)
            for hh in range(4):
                S.tr(ptv[:, hh, :], src[:, hh * 64:(hh + 1) * 64], ident)
            S.cp(eng, dstT, ptv)
        p = P()
        pv = v4(p)
        for hh in range(4):
            S.mm(pv[:, hh, :], ktT[:, hh, :], qtT[:, hh, :])
        S.tt("dve", AqkT, pv, m_ji_i.us(1).bc([128, 4, 128]), ALU.mult)
        if want_out:
            p = P()
            pv = v4(p)
            for hh in range(4):
                S.mm(pv[:, hh, :], AqkT[:, hh, :], vg_bf[:, hh * 128:(hh + 1) * 128], start=True, stop=False)
                S.mm(pv[:, hh, :], qtT[:, hh, :], Mg_bf[d][:, hh, :], start=False, stop=True)
            S.cp("act", o_sb, p)
        p = P()
        pvg = v4(p, 64)
        for hh in range(4):
            S.mm(pvg[:, hh, :], khg_bf[:, hh * 64:(hh + 1) * 64], vg_bf[:, hh * 128:(hh + 1) * 128])
        S.tt("dve", Mg_f[d], Mg_f[d], pcg_sb.us(2).bc([64, 4, 128]), ALU.mult)
        S.tt("dve", Mg_f[d], Mg_f[d], pvg, ALU.add)
        S.cp("dve", Mg_bf[d], Mg_f[d])
        if not want_out:
            return
        if d == 0:
            S.dma("sp", yf_d[idx, :, 0:512], y_sb)
            S.dma("sp", yf_d[idx, :, 512:1024], bo_sb)
            S.dma("sp", yf_d[idx, :, 1024:1536], o_sb)
            return
        pl2 = P()
        for kc in range(8):
            S.mm(pl2[0:96, 0:128], Wgd[:, kc, :], hT[:, kc, :], start=(kc == 0), stop=False)
        for kc in range(8):
            S.mm(pl2[0:96, 0:128], Wgdb[:, kc, :], hnb[:, kc, :], start=False, stop=(kc == 7))
        S.act(lo_gd, pl2[0:96, 0:128], AF.Sigmoid)
        pgo = P()
        S.mm(pgo, lo_gd, g2w)
        S.cp("act", gout_sb, pgo)
        p = P(); proj_block(p, hT, hnb, 2560, 512, False)
        S.act(gsil, p, AF.Silu)
        S.dma("sp", yfl, yf_d[idx])
        S.tt("dve", y_sb, y_sb, yfl[:, 0:512], ALU.add)
        S.red("dve", s8a, h8(y_sb))
        S.tt("pool", t0, y_sb, y_sb, ALU.mult)
        S.red("dve", s8b, h8(t0))
        S.ts("dve", s8a, s8a, 1.0 / 64.0, ALU.mult)
        S.tt("dve", s8c, s8a, s8a, ALU.mult)
        S.stt("dve", s8b, s8b, 1.0 / 64.0, s8c, ALU.mult, ALU.subtract)
        S.act(s8b, s8b, AF.Sqrt, bias=GN_EPS)
        S.op("dve", lambda e: e.reciprocal(s8b.ap, s8b.ap), reads=[s8b.buf], writes=[s8b.buf])
        S.tt("dve", h8(y_sb), h8(y_sb), s8a.us(2).bc([128, 8, 64]), ALU.subtract)
        S.tt("dve", h8(y_sb), h8(y_sb), s8b.us(2).bc([128, 8, 64]), ALU.mult)
        S.tt("dve", y_sb, y_sb, gnwrow, ALU.mult)
        S.tt("dve", y_sb, y_sb, gnbrow, ALU.add)
        S.tt("dve", y_sb, y_sb, bo_sb, ALU.add)
        S.tt("dve", y_sb, y_sb, yfl[:, 512:1024], ALU.add)
        S.tt("dve", mix_bf[:, 0:512], y_sb, gout_sb, ALU.mult)
        S.tt("dve", o_sb, o_sb, yfl[:, 1024:1536], ALU.add)
        S.tt("pool", t0, o_sb, o_sb, ALU.mult)
        S.red("dve", s8d[:, 0:4], t0.re("p (h c) -> p h c", h=4))
        S.act(s8d[:, 0:4], s8d[:, 0:4], AF.Sqrt, bias=1e-5, scale=1.0 / 128.0)
        S.op("dve", lambda e: e.reciprocal(s8d.ap[:, 0:4], s8d.ap[:, 0:4]), reads=[s8d.buf], writes=[s8d.buf])
        o4 = o_sb.re("p (h c) -> p h c", h=4)
        S.tt("dve", o4, o4, s8d[:, 0:4].us(2).bc([128, 4, 128]), ALU.mult)
        S.tt("dve", o_sb, o_sb, gnormrow, ALU.mult)
        S.tt("dve", mix_bf[:, 512:1024], o_sb, gsil, ALU.mult)
        S.dma("sp", mix_d[b, idx * 128:(idx + 1) * 128, :], mix_bf)

    if DEBUG["stop_after"] == "Bs":
        S.pop()
        S.finish()
        return nc
    for b in range(NB):
        for d in range(2):
            S.memset("dve", M_f[d], 0.0)
            S.memset("dve", M_bf[d], 0.0)
            S.memset("pool", Mg_f[d], 0.0)
            S.memset("pool", Mg_bf[d], 0.0)
        for d in range(2):
            for kind, n_t in (("c", 2), ("l", 16)):
                order = list(range(n_t)) if d == 0 else list(range(n_t - 1, -1, -1))
                step = 1 if d == 0 else -1
                produce(b, kind, order[0])
                for i, idx in enumerate(order):
                    nxt = idx + step
                    if 0 <= nxt < n_t:
                        produce(b, kind, nxt)
                    neighbor(kind, idx, n_t)
                    mix_tile(b, kind, idx, d, kind == "l")
                    if DEBUG["stop_after"] == "B1":
                        break
                if DEBUG["stop_after"] == "B1":
                    break
    S.pop()
    if DEBUG["stop_after"] in ("B", "B1"):
        S.finish()
        return nc
    return moe_phase(nc, S, locals())


def moe_phase(nc, S, L):
    out_d, modrow_d = L["out_d"], L["modrow_d"]
    rtr_d, exg_d, exu_d, exd_d = L["rtr_d"], L["exg_d"], L["exu_d"], L["exd_d"]
    ln2g_d, ln2b_d, eoh_d = L["ln2g_d"], L["ln2b_d"], L["eoh_d"]
    ident, mUi, ones_bf = L["ident"], L["mUi"], L["ones_bf"]
    iota_f = L["iota_f"]
    pbanks, ptbanks = L["pbanks"], L["ptbanks"]
    layer_norm_stats = L["layer_norm_stats"]
    rot = {"p": 0, "t": 0}

    def P():
        rot["p"] = (rot["p"] + 1) % 2
        return pbanks[4 + rot["p"]]

    def PT():
        rot["t"] = (rot["t"] + 1) % 2
        return ptbanks[rot["t"]]

    Y = pbanks[0:4]
    exg_v = [exg_d[e].re("(kc p) f -> p kc f", p=128) for e in range(16)]
    exu_v = [exu_d[e].re("(kc p) f -> p kc f", p=128) for e in range(16)]
    exd_v = [exd_d[e].re("(fc p) d -> p fc d", p=128) for e in range(16)]

    x_d, mix_d, wout_d, ln1g_d, ln1b_d = L["x_d"], L["mix_d"], L["wout_d"], L["ln1g_d"], L["ln1b_d"]
    for b in range(NB):
        S.epoch()
        S.push()
        h2 = S.sb("h2", [128, 16, 1024], BF16)
        acc = S.sb("acc", [128, 16, 1024], F32)
        mrows = S.sb("mrows", [128, 3, 1024], F32)
        S.dma("sp", mrows[:, 0, :], modrow_d[b, 1:2, :].pb(128))
        S.dma("sp", mrows[:, 1, :], modrow_d[b, 2:3, :].pb(128))
        S.dma("sp", mrows[:, 2, :], modrow_d[b, 3:4, :].pb(128))
        identf = S.sb("identf", [128, 128], F32)
        S.cp("dve", identf, ident)
        aff_tok = S.sb("aff_tok", [128, 16, 16], F32)
        mask_tok = S.sb("mask_tok", [128, 16, 16], BF16)
        gate_tok = S.sb("gate_tok", [128, 16, 16], BF16)
        slot_tok = S.sb("slot_tok", [128, 16, 16], F32)
        m8 = S.sb("m8", [16, 8], F32)
        st6 = S.sb("mst6", [128, 2, 6], F32)
        mv2 = S.sb("mmv2", [128, 2], F32)
        rstd1 = S.sb("mrstd1", [128, 1], F32)
        xt = [S.sb("mx%d" % i, [128, 1024], F32) for i in range(2)]

        S.push()
        affT = S.sb("affT", [16, 2048], F32)
        wk = S.sb("wk", [16, 2048], F32)
        S.push()
        woutg = S.sb("woutg", [128, 8, 1024], BF16)
        g1row = S.sb("g1row", [128, 1024], F32)
        wstg = [S.sb("wstg%d" % i, [128, 1024], F32) for i in range(2)]
        lnrows = S.sb("lnrows", [128, 2, 1024], F32)
        S.dma("sp", lnrows[:, 0, :], ln1g_d.pb(128))
        S.dma("sp", lnrows[:, 1, :], ln1b_d.pb(128))
        S.dma("sp", g1row, modrow_d[b, 0:1, :].pb(128))
        for kc in range(8):
            ws = wstg[kc % 2]
            S.dma("sp", ws, wout_d[kc * 128:(kc + 1) * 128, :])
            S.tt("pool", woutg[:, kc, :], ws, g1row, ALU.mult)
        rtr = S.sb("rtr", [128, 8, 16], BF16)
        S.dma("pool", rtr, rtr_d.re("(kc p) e -> p kc e", p=128))
        mixt = [S.sb("mixt%d" % i, [128, 1024], BF16) for i in range(2)]
        mixT = S.sb("mixT", [128, 8, 128], BF16)
        x1t = S.sb("x1t", [128, 1024], F32)
        xnf = S.sb("xnf", [128, 1024], F32)
        h2T = S.sb("h2T", [128, 8, 128], BF16)
        sm1 = S.sb("sm1", [128, 1], F32)
        sm2 = S.sb("sm2", [128, 1], F32)
        lgt = S.sb("lgt", [128, 16], F32)
        for t in range(16):
            x = xt[t % 2]
            mt = mixt[t % 2]
            S.dma("sp", x, x_d[b, t * 128:(t + 1) * 128, :])
            S.dma("sp", mt, mix_d[b, t * 128:(t + 1) * 128, :])
            pt = PT()
            ptv = pt.re("p (k t) -> p k t", k=8)
            for kc in range(8):
                S.tr(ptv[:, kc, :], mt[:, kc * 128:(kc + 1) * 128], ident)
            S.cp("act", mixT, ptv)
            for half in range(2):
                pp_ = P()
                for kc in range(8):
                    S.mm(pp_, mixT[:, kc, :], woutg[:, kc, half * 512:(half + 1) * 512], start=(kc == 0), stop=(kc == 7))
                S.stt("dve", x1t[:, half * 512:(half + 1) * 512], x[:, half * 512:(half + 1) * 512], ALPHA, pp_,
                      ALU.mult, ALU.add)
            layer_norm_stats(x1t, st6, mv2, rstd1)
            S.ts("dve", x1t, x1t, mv2[:, 0:1], ALU.subtract, rstd1, ALU.mult)
            S.tt("dve", x1t, x1t, lnrows[:, 0, :], ALU.mult)
            S.tt("dve", x1t, x1t, lnrows[:, 1, :], ALU.add)
            S.ts("pool", acc[:, t, :], x1t, ALPHA, ALU.mult)
            layer_norm_stats(x1t, st6, mv2, rstd1)
            S.ts("dve", xnf, x1t, mv2[:, 0:1], ALU.subtract, rstd1, ALU.mult)
            S.tt("dve", xnf, xnf, mrows[:, 1, :], ALU.mult)
            S.tt("dve", h2[:, t, :], xnf, mrows[:, 0, :], ALU.add)
            pt = PT()
            ptv = pt.re("p (k t) -> p k t", k=8)
            for kc in range(8):
                S.tr(ptv[:, kc, :], h2[:, t, kc * 128:(kc + 1) * 128], ident)
            S.cp("act", h2T, ptv)
            p = P()
            for kc in range(8):
                S.mm(p[:, 0:16], h2T[:, kc, :], rtr[:, kc, :], start=(kc == 0), stop=(kc == 7))
            S.op("dve", lambda e, p=p: e.reduce_max(sm1.ap, p.ap[:, 0:16], AX.X), reads=[p.buf], writes=[sm1.buf])
            S.ts("dve", sm1, sm1, -1.0, ALU.mult)
            S.act(lgt, p[:, 0:16], AF.Exp, bias=sm1)
            S.red("dve", sm2, lgt)
            S.op("dve", lambda e: e.reciprocal(sm2.ap, sm2.ap), reads=[sm2.buf], writes=[sm2.buf])
            S.ts("dve", aff_tok[:, t, :], lgt, sm2, ALU.mult)
            p2 = P()
            S.mm(p2[0:16, 0:128], aff_tok[:, t, :], identf)
            S.cp("act", affT[:, t * 128:(t + 1) * 128], p2[0:16, 0:128])
        S.pop()
        if DEBUG["stop_after"] == "C0":
            S.pop()
            S.pop()
            S.finish()
            return nc
        S.cp("dve", wk, affT)
        for r in range(32):
            S.op("dve", lambda e: e.max(m8.ap, wk.ap), reads=[wk.buf], writes=[m8.buf])
            if r < 31:
                S.op("dve", lambda e: e.match_replace(wk.ap, m8.ap, wk.ap, -1.0), reads=[wk.buf, m8.buf], writes=[wk.buf])
        S.ts("dve", wk, affT, m8[:, 7:8], ALU.is_ge)
        for t in range(16):
            p = P()
            S.mm(p[:, 0:16], wk[:, t * 128:(t + 1) * 128], identf[0:16, 0:16])
            S.cp("act", mask_tok[:, t, :], p[:, 0:16])
        for t in range(16):
            S.tt("dve", gate_tok[:, t, :], aff_tok[:, t, :], mask_tok[:, t, :], ALU.mult)
            p = P()
            for t2 in range(t + 1):
                S.mm(p[:, 0:16], mUi if t2 == t else ones_bf, mask_tok[:, t2, :], start=(t2 == 0), stop=(t2 == t))
            S.tt("dve", slot_tok[:, t, :], p[:, 0:16], mask_tok[:, t, :], ALU.mult)
            S.ts("dve", slot_tok[:, t, :], slot_tok[:, t, :], -1.0, ALU.add)
        S.pop()

        S.push()
        sel = S.sb("sel", [128, 16, 256], BF16)
        selT = [S.sb("selT%d" % i, [128, 2, 2048], BF16) for i in range(2)]
        xeT = [S.sb("xeT%d" % i, [128, 8, 256], BF16) for i in range(2)]
        actT = S.sb("actT", [128, 2, 256], BF16)
        sl = S.sb("sl", [128, 256], F32)
        y_bf = S.sb("y_bf", [128, 2, 1024], BF16)
        gslot = [S.sb("gslot%d" % i, [128, 2], F32) for i in range(2)]
        NW = 3
        wg_t = [S.sb("wg%d" % i, [128, 8, 256], BF16) for i in range(NW)]
        wu_t = [S.sb("wu%d" % i, [128, 8, 256], BF16) for i in range(NW)]
        wd_t = [S.sb("wd%d" % i, [128, 2, 1024], BF16) for i in range(NW)]
        paux = V(ptbanks[0].buf, ptbanks[0].ap.bitcast(F32))
        paux2 = V(ptbanks[1].buf, ptbanks[1].ap.bitcast(F32))
        ptr = ptbanks[1]

        def prep_pieces(e):
            k = e % 2
            pieces = []

            def mk_sel(t0_):
                def f():
                    for t in range(t0_, t0_ + 4):
                        S.ts("dve", sel[:, t, :], iota_f, slot_tok[:, t, e:e + 1], ALU.is_equal)
                return f
            for t0_ in range(0, 16, 4):
                pieces.append(mk_sel(t0_))

            def f_gslot():
                for jc in range(2):
                    for t in range(16):
                        S.mm(paux[:, jc:jc + 1], sel[:, t, jc * 128:(jc + 1) * 128], gate_tok[:, t, e:e + 1],
                             start=(t == 0), stop=(t == 15))
                S.cp("act", gslot[k], paux[:, 0:2])
            pieces.append(f_gslot)

            def mk_tr(jc, tq):
                def f():
                    ptv = ptr.re("p (k t) -> p k t", k=8)
                    for tt_ in range(8):
                        t = tq * 8 + tt_
                        S.tr(ptv[:, tt_, :], sel[:, t, jc * 128:(jc + 1) * 128], ident)
                    S.cp("act", selT[k][:, jc, tq * 1024:(tq + 1) * 1024], ptr)
                return f
            tail = []
            for jc in range(2):
                for tq in range(2):
                    tail.append(mk_tr(jc, tq))

            def mk_gather(dp):
                def f():
                    pv = paux.re("p (c j) -> p c j", c=2)
                    for c2 in range(2):
                        dc = dp * 2 + c2
                        for t in range(16):
                            S.mm(pv[:, c2, :], h2[:, t, dc * 128:(dc + 1) * 128], sel[:, t, :], start=(t == 0), stop=(t == 15))
                    S.cp("act", xeT[k][:, dp * 2:dp * 2 + 2, :], pv)
                return f
            for dp in range(4):
                pieces.append(mk_gather(dp))
            return pieces, tail

        def scatter_pieces(e):
            k = e % 2
            pieces = []

            def mk(t, dh):
                def f():
                    pa = paux if dh == 0 else paux2
                    for jc in range(2):
                        S.mm(pa, selT[k][:, jc, t * 128:(t + 1) * 128], y_bf[:, jc, dh * 512:(dh + 1) * 512],
                             start=(jc == 0), stop=(jc == 1))
                    a_ = acc[:, t, dh * 512:(dh + 1) * 512]
                    S.tt("dve", a_, a_, pa, ALU.add)
                return f
            for t in range(16):
                for dh in range(2):
                    pieces.append(mk(t, dh))
            return pieces

        pm_, pt_ = prep_pieces(0)
        for f in pm_ + pt_:
            f()
        gcount = 0
        for e in range(16):
            k = e % 2
            pend_sc = scatter_pieces(e - 1) if e > 0 else []
            pend_pr, pend_tr = prep_pieces(e + 1) if e < 15 else ([], [])
            chunk_i = 0
            wsel = {}

            def issue_w(fg):
                nonlocal gcount
                wi = gcount % NW
                gcount += 1
                f0 = fg * 256
                S.dma("pool", wg_t[wi], exg_v[e][:, :, f0:f0 + 256])
                S.dma("pool", wu_t[wi], exu_v[e][:, :, f0:f0 + 256])
                S.dma("pool", wd_t[wi], exd_v[e][:, fg * 2:fg * 2 + 2, :])
                wsel[fg] = wi

            def GU(n):
                fg, fc = n // 2, n % 2
                if fg not in wsel:
                    issue_w(fg)
                wi = wsel[fg]
                p = P()
                for kc in range(8):
                    S.mm(p[:, 0:256], wg_t[wi][:, kc, fc * 128:(fc + 1) * 128], xeT[k][:, kc, :], start=(kc == 0), stop=(kc == 7))
                for kc in range(8):
                    S.mm(p[:, 256:512], wu_t[wi][:, kc, fc * 128:(fc + 1) * 128], xeT[k][:, kc, :], start=(kc == 0), stop=(kc == 7))
                return p

            pcur = GU(0)
            for n in range(22):
                fg, fc = n // 2, n % 2
                wi = wsel[fg]
                pnext = GU(n + 1) if n < 21 else None
                S.act(sl, pcur[:, 0:256], AF.Silu)
                S.tt("dve", actT[:, fc, :], sl, pcur[:, 256:512], ALU.mult)
                for jc in range(2):
                    for dh in range(2):
                        S.mm(Y[jc * 2 + dh], actT[:, fc, jc * 128:(jc + 1) * 128], wd_t[wi][:, fc, dh * 512:(dh + 1) * 512],
                             start=(n == 0), stop=(n == 21))
                pcur = pnext
                chunk_i += 1
                if chunk_i <= 16:
                    for _ in range(min(2, len(pend_sc))):
                        pend_sc.pop(0)()
                if chunk_i <= 18 and chunk_i % 2 == 0 and pend_pr:
                    pend_pr.pop(0)()
                if chunk_i > 18:
                    while pend_sc:
                        pend_sc.pop(0)()
                    while pend_pr:
                        pend_pr.pop(0)()
                    if pend_tr:
                        pend_tr.pop(0)()
            while pend_sc:
                pend_sc.pop(0)()
            while pend_pr:
                pend_pr.pop(0)()
            while pend_tr:
                pend_tr.pop(0)()
            for jc in range(2):
                for dh in range(2):
                    S.stt("dve", y_bf[:, jc, dh * 512:(dh + 1) * 512], Y[jc * 2 + dh], gslot[k][:, jc:jc + 1],
                          mrows[:, 2, dh * 512:(dh + 1) * 512], ALU.mult, ALU.mult)
        for f in scatter_pieces(15):
            f()
        S.pop()
        S.push()
        ln2rows = S.sb("ln2rows", [128, 2, 1024], F32)
        S.dma("sp", ln2rows[:, 0, :], ln2g_d.pb(128))
        S.dma("sp", ln2rows[:, 1, :], ln2b_d.pb(128))
        for t in range(16):
            a_ = acc[:, t, :]
            layer_norm_stats(a_, st6, mv2, rstd1)
            o_ = xt[t % 2]
            S.ts("dve", o_, a_, mv2[:, 0:1], ALU.subtract, rstd1, ALU.mult)
            S.tt("dve", o_, o_, ln2rows[:, 0, :], ALU.mult)
            S.tt("dve", o_, o_, ln2rows[:, 1, :], ALU.add)
            S.dma("sp", out_d[b, t * 128:(t + 1) * 128, :], o_)
        S.pop()
        S.pop()
    S.finish()
    return nc


def kernel(x, c, ctx, c_ctx, ada_w, ada_b, w_in, rw_mu, rw_w0, rw_w2, rw_a0, rw_a2, rw_g2,
           rw_k_k, rw_k_a, rw_r_k, rw_gn_w, rw_gn_b, gla_a2, gla_a_b, gla_norm_w, w_out,
           ln1_g, ln1_b, router_w, ex_gate, ex_up, ex_down, ln2_g, ln2_b):
    f = lambda a: np.ascontiguousarray(np.asarray(a, dtype=np.float32))
    x, c, ctx, c_ctx = f(x), f(c), f(ctx), f(c_ctx)
    cbf, cfc = host_consts()
    eoh = np.zeros((16, 16 * 128), np.float32)
    for e in range(16):
        eoh[e, e * 128:(e + 1) * 128] = 1.0
    mu_ext = np.zeros((1, 3328), np.float32)
    mu_ext[0, :1760] = f(rw_mu)[0]
    shared = {
        "ada_w": f(ada_w)[0], "ada_b": f(ada_b)[0][None, :],
        "ada_bT": np.ascontiguousarray(f(ada_b)[0].reshape(48, 128).T),
        "w_in": f(w_in)[0], "mu_ext": mu_ext,
        "rw_w0": f(rw_w0)[0], "rw_w2": f(rw_w2)[0], "rw_a0": f(rw_a0)[0], "rw_a2": f(rw_a2)[0],
        "rw_g2": f(rw_g2)[0], "rw_k_k": f(rw_k_k)[0][None, :], "rw_k_a": f(rw_k_a)[0][None, :],
        "rw_r_k": f(rw_r_k)[0].reshape(1, 512), "rw_gn_w": f(rw_gn_w)[0][None, :], "rw_gn_b": f(rw_gn_b)[0][None, :],
        "gla_a2": f(gla_a2)[0], "gla_a_b": f(gla_a_b)[0], "gla_norm_w": f(gla_norm_w)[0][None, :],
        "w_out": f(w_out)[0], "ln1_g": f(ln1_g)[0][None, :], "ln1_b": f(ln1_b)[0][None, :],
        "router_w": f(router_w)[0], "ex_gate": f(ex_gate)[0], "ex_up": f(ex_up)[0], "ex_down": f(ex_down)[0],
        "ln2_g": f(ln2_g)[0][None, :], "ln2_b": f(ln2_b)[0][None, :],
        "cbf": cbf, "cf": cfc, "eoh": eoh,
    }
    in_maps = []
    for core in range(8):
        bs = slice(core * NB, (core + 1) * NB)
        cc = np.stack([c[core * NB], c[core * NB + 1], c_ctx], axis=1)
        cT = np.ascontiguousarray(cc.reshape(8, 128, 3).transpose(1, 0, 2))
        m = dict(shared)
        m["x"] = np.ascontiguousarray(x[bs])
        m["ctx"] = np.ascontiguousarray(ctx[bs])
        m["cT"] = cT
        in_maps.append(m)
    nc = build()
    res = run_bass_kernel_spmd(nc, in_maps, core_ids=list(range(8)))
    if DEBUG["dump"]:
        DEBUG["res"] = res.results
    return np.concatenate([np.asarray(r["out"], dtype=np.float32) for r in res.results], axis=0)
```

```python
from contextlib import ExitStack
import numpy as np
import concourse.bass as bass
import concourse.mybir as mybir
from concourse.bass_utils import run_bass_kernel_spmd

F32 = mybir.dt.float32
BF16 = mybir.dt.bfloat16
AF = mybir.ActivationFunctionType
ALU = mybir.AluOpType
AX = mybir.AxisListType


class Buf:
    __slots__ = ("name", "lw", "rd")

    def __init__(self, name):
        self.name = name
        self.lw = None
        self.rd = {}


class V:
    __slots__ = ("buf", "ap")

    def __init__(self, buf, ap):
        self.buf = buf
        self.ap = ap

    def __getitem__(self, k):
        return V(self.buf, self.ap[k])

    def bc(self, shape):
        return V(self.buf, self.ap.to_broadcast(list(shape)))

    def us(self, axis):
        return V(self.buf, self.ap.unsqueeze(axis))

    def re(self, pat, **kw):
        return V(self.buf, self.ap.rearrange(pat, **kw))

    def pb(self, n=128):
        return V(self.buf, self.ap.partition_broadcast(n))


class Sched:
    ENGS = ("pe", "act", "dve", "pool", "sp")

    def __init__(self, nc, ndma=12):
        self.nc = nc
        self.stack = ExitStack()
        self.scopes = [self.stack]
        self.sem = {}
        self.cnt = {}
        self.prog = {}
        self.seen = {}
        for e in self.ENGS:
            self.sem[e] = self.stack.enter_context(nc.semaphore("s_" + e))
            self.cnt[e] = 0
            self.prog[e] = []
            self.seen[e] = {}
        self.ep = 0
        self.dq = {}
        for q in ("sp", "pool", "act"):
            sems = [self.stack.enter_context(nc.semaphore("d_%s%d" % (q, i))) for i in range(ndma)]
            self.dq[q] = dict(sems=sems, vals=[0] * ndma, nxt=0)
        self.n_ops = 0

    def sb(self, name, shape, dtype):
        self.n_alloc = getattr(self, "n_alloc", 0) + 1
        t = self.scopes[-1].enter_context(self.nc.sbuf_tensor("t%d_%s" % (self.n_alloc, name), list(shape), dtype))
        return V(Buf(name), t[:])

    def ps(self, name, shape, dtype):
        t = self.scopes[-1].enter_context(self.nc.psum_tensor("p_" + name, list(shape), dtype))
        return V(Buf(name), t[:])

    def dram(self, name, shape, dtype, kind="Internal"):
        if DEBUG["dump"]:
            kind = "ExternalOutput"
        t = self.nc.dram_tensor(name, list(shape), dtype, kind=kind)
        return V(Buf(name), t.ap())

    def epoch(self):
        self.barrier()
        self.ep += 1
        for e in self.ENGS:
            self.sem[e] = self.stack.enter_context(self.nc.semaphore("s_%s_%d" % (e, self.ep)))
            self.cnt[e] = 0
            for k in [k for k in self.seen[e] if isinstance(k, str)]:
                del self.seen[e][k]

    def alias_acquire(self, parent, aps, name="al"):
        out = []
        for i, ap in enumerate(aps):
            b = Buf("%s_%s%d" % (parent.buf.name, name, i))
            b.lw = parent.buf.lw
            b.rd = dict(parent.buf.rd)
            out.append(V(b, ap))
        return out

    def alias_release(self, parent, children):
        for c in children:
            evs = list(c.buf.rd.values())
            if c.buf.lw is not None:
                evs.append(c.buf.lw)
            for ev in evs:
                old = parent.buf.rd.get(ev[0])
                if old is None or (old[2], old[1]) < (ev[2], ev[1]):
                    parent.buf.rd[ev[0]] = ev

    def push(self):
        self.scopes.append(ExitStack())

    def pop(self):
        self.barrier()
        self.flush()
        self.scopes.pop().close()

    def _semof(self, key):
        if isinstance(key, str):
            return self.sem[key]
        q, slot = key
        return self.dq[q]["sems"][slot]

    def _collect(self, eng, reads, writes, is_dma):
        waits = {}

        def need(ev, war=False):
            if ev is None:
                return
            key, val, dep_ep = ev
            if dep_ep < self.ep:
                return
            if not is_dma and isinstance(key, str) and key == eng:
                if eng == "pe" or war:
                    return
            if self.seen[eng].get(key, 0) >= val:
                return
            if waits.get(key, 0) < val:
                waits[key] = val

        for b in reads:
            need(b.lw)
        for b in writes:
            need(b.lw)
            for ev in b.rd.values():
                need(ev, war=True)
        return waits

    def _commit(self, eng, waits):
        for k, v in waits.items():
            self.seen[eng][k] = v
        return [(self._semof(k), v) for k, v in waits.items()]

    def _record(self, ev, reads, writes):
        for b in reads:
            b.rd[ev[0]] = ev
        for b in writes:
            b.lw = ev
            b.rd = {}

    def op(self, eng, fn, reads=(), writes=()):
        waits = self._collect(eng, reads, writes, False)
        wl = self._commit(eng, waits)
        self.cnt[eng] += 1
        ev = (eng, self.cnt[eng], self.ep)
        self.prog[eng].append((wl, fn, (self.sem[eng], 1)))
        self._record(ev, reads, writes)
        self.n_ops += 1

    def dma(self, q, out, in_, **kw):
        oap, iap = out.ap, in_.ap
        d = self.dq[q]
        slot = d["nxt"]
        d["nxt"] = (slot + 1) % len(d["sems"])
        waits = self._collect(q, [in_.buf], [out.buf], True)
        key = (q, slot)
        prev = d["vals"][slot]
        if prev and self.seen[q].get(key, 0) < prev:
            waits[key] = max(waits.get(key, 0), prev)
        wl = self._commit(q, waits)
        d["vals"][slot] = prev + 16
        ev = (key, prev + 16, self.ep)
        sem = d["sems"][slot]
        self.prog[q].append((wl, lambda e: e.dma_start(out=oap, in_=iap, **kw), (sem, 16)))
        self._record(ev, [in_.buf], [out.buf])
        self.n_ops += 1

    @staticmethod
    def _bufs(*xs):
        return [x.buf for x in xs if isinstance(x, V)]

    @staticmethod
    def _a(x):
        return x.ap if isinstance(x, V) else x

    def mm(self, out, lhsT, rhs, start=True, stop=True):
        o, l, r = out.ap, lhsT.ap, rhs.ap
        self.op("pe", lambda e: e.matmul(o, l, r, start=start, stop=stop),
                reads=self._bufs(lhsT, rhs), writes=[out.buf])

    def tr(self, out, in_, ident):
        o, i, d = out.ap, in_.ap, ident.ap
        self.op("pe", lambda e: e.transpose(o, i, d), reads=self._bufs(in_, ident), writes=[out.buf])

    def act(self, out, in_, func, bias=None, scale=None):
        kw = {}
        if bias is not None:
            kw["bias"] = self._a(bias)
        if scale is not None:
            kw["scale"] = self._a(scale)
        o, i = out.ap, in_.ap
        self.op("act", lambda e: e.activation(o, i, func, **kw),
                reads=self._bufs(in_, bias, scale), writes=[out.buf])

    def tt(self, eng, out, a, b, op):
        o, x, y = out.ap, a.ap, b.ap
        self.op(eng, lambda e: e.tensor_tensor(o, x, y, op), reads=self._bufs(a, b), writes=[out.buf])

    def ts(self, eng, out, a, s1, op0, s2=None, op1=None):
        o, x, p1, p2 = out.ap, a.ap, self._a(s1), self._a(s2)
        if op1 is None:
            self.op(eng, lambda e: e.tensor_scalar(o, x, p1, None, op0),
                    reads=self._bufs(a, s1), writes=[out.buf])
        else:
            self.op(eng, lambda e: e.tensor_scalar(o, x, p1, p2, op0, op1),
                    reads=self._bufs(a, s1, s2), writes=[out.buf])

    def stt(self, eng, out, a, sc, b, op0, op1):
        o, x, p, y = out.ap, a.ap, self._a(sc), b.ap
        eng = "dve"
        self.op(eng, lambda e: e.scalar_tensor_tensor(o, x, p, y, op0, op1),
                reads=self._bufs(a, sc, b), writes=[out.buf])

    def cp(self, eng, out, a):
        o, x = out.ap, a.ap
        if eng == "act":
            self.op("act", lambda e: e.activation(o, x, AF.Copy), reads=[a.buf], writes=[out.buf])
        else:
            self.op(eng, lambda e: e.tensor_copy(o, x), reads=[a.buf], writes=[out.buf])

    def red(self, eng, out, a, op=None):
        o, x = out.ap, a.ap
        op = ALU.add if op is None else op
        self.op(eng, lambda e: e.tensor_reduce(o, x, AX.X, op), reads=[a.buf], writes=[out.buf])

    def memset(self, eng, out, val):
        o = out.ap
        self.op(eng, lambda e: e.memset(o, val), writes=[out.buf])

    def barrier(self):
        for e in self.ENGS:
            waits = {}
            for e2 in self.ENGS:
                if e2 != e and self.cnt[e2] > self.seen[e].get(e2, 0):
                    waits[e2] = self.cnt[e2]
            if e not in ("pe",) and self.cnt[e] > self.seen[e].get(e, 0):
                waits[e] = self.cnt[e]
            for q, d in self.dq.items():
                for slot, v in enumerate(d["vals"]):
                    if v and self.seen[e].get((q, slot), 0) < v:
                        waits[(q, slot)] = v
            wl = self._commit(e, waits)
            if wl:
                self.prog[e].append((wl, None, None))

    def flush(self):
        nc = self.nc
        prog = self.prog
        self.prog = {e: [] for e in self.ENGS}

        def replay(name, e):
            for wl, fn, inc in prog[name]:
                for sem, val in wl:
                    e.wait_ge(sem, val)
                if fn is not None:
                    ins = fn(e)
                    ins.then_inc(inc[0], inc[1])

        with nc.Block() as block:
            @block.tensor
            def _(e):
                replay("pe", e)

            @block.scalar
            def _(e):
                replay("act", e)

            @block.vector
            def _(e):
                replay("dve", e)

            @block.gpsimd
            def _(e):
                replay("pool", e)

            @block.sync
            def _(e):
                replay("sp", e)

    def finish(self):
        self.barrier()
        self.flush()
        self.stack.close()


NB = 2
ALPHA = float(2.0 ** 0.25)
CRW = -float(np.exp(-0.5))
GN_EPS = 64e-5
DEBUG = {"stop_after": None, "cut": 99, "dump": False, "res": None, "dbg": False}


def host_consts():
    p = np.arange(128)[:, None]
    q = np.arange(128)[None, :]
    Us = (p < q).astype(np.float32)
    Ui = (p <= q).astype(np.float32)
    Ls = (p > q).astype(np.float32)
    Li = (p >= q).astype(np.float32)
    ident = (p == q).astype(np.float32)
    sm = []
    for l in range(7):
        sz = 2 ** l
        same = (p // (2 * sz)) == (q // (2 * sz))
        lo = same & ((p % (2 * sz)) >= sz) & ((q % (2 * sz)) < sz)
        sm.append((lo | lo.T).astype(np.float32))
    cbf = np.concatenate([ident, Us, Ui, Ls, Li, np.ones((128, 128), np.float32)] + sm, axis=1)
    cf = np.concatenate([Ui * CRW, Us * CRW, Ls * CRW, Li * CRW,
                         Ui / 16.0, Us / 16.0, Ls / 16.0, Li / 16.0,
                         np.full((128, 1), CRW, np.float32), np.full((128, 1), 1.0 / 16.0, np.float32),
                         np.arange(128, dtype=np.float32)[:, None], np.arange(128, dtype=np.float32)[:, None] + 128.0,
                         np.tile(np.arange(256, dtype=np.float32)[None, :], (128, 1))], axis=1)
    return np.ascontiguousarray(cbf), np.ascontiguousarray(cf.astype(np.float32))


def build():
    nc = bass.Bass("TRN2", target_bir_lowering=False)
    S = Sched(nc)

    def din(name, shape):
        return V(Buf(name), nc.dram_tensor(name, list(shape), F32, kind="ExternalInput").ap())

    x_d = din("x", [NB, 2048, 1024])
    ctx_d = din("ctx", [NB, 256, 1024])
    cT_d = din("cT", [128, 8, 3])
    adaw_d = din("ada_w", [1024, 6144])
    adab_d = din("ada_b", [1, 6144])
    adabT_d = din("ada_bT", [128, 48])
    win_d = din("w_in", [1024, 3328])
    mu_d = din("mu_ext", [1, 3328])
    w0_d = din("rw_w0", [2, 512])
    w2_d = din("rw_w2", [2, 32, 512])
    a0_d = din("rw_a0", [2, 512])
    a2_d = din("rw_a2", [2, 32, 512])
    g2_d = din("rw_g2", [96, 512])
    kk_d = din("rw_k_k", [1, 512])
    ka_d = din("rw_k_a", [1, 512])
    rk_d = din("rw_r_k", [1, 512])
    gnw_d = din("rw_gn_w", [1, 512])
    gnb_d = din("rw_gn_b", [1, 512])
    ga2_d = din("gla_a2", [2, 16, 256])
    gab_d = din("gla_a_b", [2, 256])
    gnorm_d = din("gla_norm_w", [1, 512])
    wout_d = din("w_out", [1024, 1024])
    ln1g_d = din("ln1_g", [1, 1024])
    ln1b_d = din("ln1_b", [1, 1024])
    rtr_d = din("router_w", [1024, 16])
    exg_d = din("ex_gate", [16, 1024, 2816])
    exu_d = din("ex_up", [16, 1024, 2816])
    exd_d = din("ex_down", [16, 2816, 1024])
    ln2g_d = din("ln2_g", [1, 1024])
    ln2b_d = din("ln2_b", [1, 1024])
    cbf_d = din("cbf", [128, 1664])
    cf_d = din("cf", [128, 8 * 128 + 4 + 256])
    eoh_d = din("eoh", [16, 16 * 128])
    out_d = V(Buf("out"), nc.dram_tensor("out", [NB, 2048, 1024], F32, kind="ExternalOutput").ap())
    mix_d = S.dram("mixs", [NB, 2048, 1024], BF16)
    yf_d = S.dram("yfs", [16, 128, 1536], F32)
    modrow_d = S.dram("modrows", [NB, 4, 1024], F32)
    dbgf_d = S.dram("dbgf", [24, 128, 512], F32)
    dbgb_d = S.dram("dbgb", [24, 128, 512], BF16)
    dbgn = {"f": 0, "b": 0, "names": []}

    def dump(name, v, bf):
        k = "b" if bf else "f"
        i = dbgn[k]
        dbgn[k] += 1
        dbgn["names"].append((name, k, i, tuple(v.ap.shape)))
        dst = (dbgb_d if bf else dbgf_d)
        p = v.ap.shape[0]
        n = 1
        for d_ in v.ap.shape[1:]:
            n *= d_
        dv = dst[i, 0:p, 0:n]
        if len(v.ap.shape) == 3:
            dv = dv.re("p (a c) -> p a c", a=v.ap.shape[1])
        S.dma("sp", dv, v)
    DEBUG["names"] = dbgn["names"]

    cb = S.sb("cb", [128, 1664], BF16)
    S.dma("pool", cb, cbf_d)
    ident, mUs, mUi, mLs, mLi = (cb[:, i * 128:(i + 1) * 128] for i in range(5))
    ones_bf = cb[:, 640:768]
    smask = [cb[:, 768 + l * 128:896 + l * 128] for l in range(7)]
    cf = S.sb("cf", [128, 8 * 128 + 4 + 256], F32)
    S.dma("sp", cf, cf_d)
    fm = [cf[:, i * 128:(i + 1) * 128] for i in range(8)]
    negcol = cf[:, 1024:1025]
    g16col = cf[:, 1025:1026]
    iota_p0 = cf[:, 1026:1027]
    iota_p1 = cf[:, 1027:1028]
    iota_f = cf[:, 1028:1284]
    modT = S.sb("modT", [128, 16, 3], F32)

    pbanks = [S.ps("pb%d" % i, [128, 512], F32) for i in range(6)]
    ptbanks = [S.ps("ptb%d" % i, [128, 1024], BF16) for i in range(2)]
    rot = {"p": 0, "t": 0}

    def P():
        rot["p"] = (rot["p"] + 1) % 6
        return pbanks[rot["p"]]

    def PT():
        rot["t"] = (rot["t"] + 1) % 2
        return ptbanks[rot["t"]]

    def layer_norm_stats(xt, st, mv, rstd, eps=1e-5):
        for c in range(2):
            o, i = st.ap[:, c, :], xt.ap[:, c * 512:(c + 1) * 512]
            S.op("dve", lambda e, o=o, i=i: e.bn_stats(o, i), reads=[xt.buf], writes=[st.buf])
        S.op("dve", lambda e: e.bn_aggr(mv.ap, st.ap), reads=[st.buf], writes=[mv.buf])
        S.act(rstd, mv[:, 1:2], AF.Sqrt, bias=eps)
        S.op("dve", lambda e: e.reciprocal(rstd.ap, rstd.ap), reads=[rstd.buf], writes=[rstd.buf])

    S.push()
    cT = S.sb("cT", [128, 8, 3], F32)
    S.dma("sp", cT, cT_d)
    scb = S.sb("scb", [128, 8, 3], BF16)
    S.act(scb, cT, AF.Silu)
    rep = []
    for b in range(NB):
        r_ = S.sb("rep%d" % b, [128, 8, 128], BF16)
        S.cp("dve", r_, scb[:, :, b:b + 1].bc([128, 8, 128]))
        rep.append(r_)
    adabT = S.sb("adabT", [128, 48], F32)
    S.dma("sp", adabT, adabT_d)
    blk = [S.sb("adablk%d" % i, [128, 8, 512], BF16) for i in range(2)]
    brow = [S.sb("adabrow%d" % i, [128, 512], F32) for i in range(2)]
    mrow = [S.sb("mrow%d" % i, [128, 512], F32) for i in range(2)]
    psT = P()
    psTv = psT[:, 0:48].re("p (j c) -> p j c", c=3)
    adaw_v = adaw_d.re("(kc p) c -> p kc c", p=128)
    for jb in range(12):
        bk = blk[jb % 2]
        S.dma("pool", bk, adaw_v[:, :, jb * 512:(jb + 1) * 512])
        if jb < 4:
            for jj in range(4):
                j = jb * 4 + jj
                for kc in range(8):
                    S.mm(psTv[:, j, :], bk[:, kc, jj * 128:(jj + 1) * 128], scb[:, kc, :],
                         start=(kc == 0), stop=(kc == 7))
            if jb == 3:
                for c in range(3):
                    S.tt("dve", modT[:, 0:8, c], psTv[:, 0:8, c], adabT[:, 0:8], ALU.add)
                    S.stt("dve", modT[:, 8:16, c], psTv[:, 8:16, c], 1.0, adabT[:, 8:16], ALU.add, ALU.add)
        else:
            which = (jb - 4) // 2
            half = (jb - 4) % 2
            br = brow[jb % 2]
            S.dma("sp", br, adab_d[:, jb * 512:(jb + 1) * 512].pb(128))
            for b in range(NB):
                pr = P()
                for kc in range(8):
                    S.mm(pr, rep[b][:, kc, :], bk[:, kc, :], start=(kc == 0), stop=(kc == 7))
                mr = mrow[b]
                if which == 2:
                    S.stt("dve", mr, pr, 1.0, br, ALU.add, ALU.add)
                else:
                    S.tt("dve", mr, pr, br, ALU.add)
                S.dma("sp", modrow_d[b, which:which + 1, half * 512:(half + 1) * 512], mr[0:1, :])
    S.pop()
    if DEBUG["stop_after"] == "A":
        S.finish()
        return nc

    S.push()
    Wa = S.sb("Wa", [128, 8, 3072], BF16)
    Wb = S.sb("Wb", [128, 8, 1536], BF16)
    Wl = [S.sb("Wl%d" % d, [128, 8, 96], BF16) for d in range(2)]
    Wlb = [S.sb("Wlb%d" % d, [128, 8, 96], BF16) for d in range(2)]
    Wgd = S.sb("Wgd", [128, 8, 96], BF16)
    Wgdb = S.sb("Wgdb", [128, 8, 96], BF16)
    sw = [S.sb("sw%d" % d, [96, 512], BF16) for d in range(2)]
    g2w = S.sb("g2w", [96, 512], BF16)
    br3 = S.sb("br3", [96, 2, 512], F32)
    onesf = S.sb("onesf", [96, 128], F32)
    S.memset("dve", onesf, 1.0)
    for d in range(2):
        S.dma("sp", br3[0:1, d, :], w0_d[d:d + 1, :])
        S.dma("sp", br3[32:33, d, :], a0_d[d:d + 1, :])
        S.dma("sp", br3[64:65, d, 0:256], gab_d[d:d + 1, :])
    rows = S.sb("rows", [128, 6, 512], F32)
    kkrow, karow, rkrow, gnwrow, gnbrow, gnormrow = (rows[:, i, :] for i in range(6))
    for i, dsrc in enumerate((kk_d, ka_d, rk_d, gnw_d, gnb_d, gnorm_d)):
        S.dma("sp", rows[:, i, :], dsrc.pb(128))
    for d in range(2):
        S.dma("pool", sw[d][0:32, :], w2_d[d])
        S.dma("pool", sw[d][32:64, :], a2_d[d])
        S.dma("pool", sw[d][64:80, 0:256], ga2_d[d])
    S.dma("pool", g2w, g2_d)

    S.push()
    mub = S.sb("mub", [128, 3328], F32)
    omm = S.sb("omm", [128, 3328], F32)
    S.dma("sp", mub, mu_d.pb(128))
    S.ts("dve", omm, mub, -1.0, ALU.mult, 1.0, ALU.add)
    stg = [S.sb("stg%d" % i, [128, 3328], F32) for i in range(2)]
    for kc in range(8):
        st_ = stg[kc % 2]
        S.dma("sp", st_, win_d[kc * 128:(kc + 1) * 128, :])
        S.tt("dve", Wa[:, kc, 0:1536], st_[:, 0:1536], omm[:, 0:1536], ALU.mult)
        S.tt("dve", Wa[:, kc, 1536:3072], st_[:, 1760:3296], omm[:, 1760:3296], ALU.mult)
        S.tt("pool", Wb[:, kc, :], st_[:, 0:1536], mub[:, 0:1536], ALU.mult)
        for d in range(2):
            for (o0, c0, n) in ((0, 1536 + 32 * d, 32), (32, 1600 + 32 * d, 32), (64, 3296 + 16 * d, 16)):
                S.tt("pool", Wl[d][:, kc, o0:o0 + n], st_[:, c0:c0 + n], omm[:, c0:c0 + n], ALU.mult)
            for (o0, c0, n) in ((0, 1536 + 32 * d, 32), (32, 1600 + 32 * d, 32)):
                S.tt("pool", Wlb[d][:, kc, o0:o0 + n], st_[:, c0:c0 + n], mub[:, c0:c0 + n], ALU.mult)
        S.tt("pool", Wgd[:, kc, :], st_[:, 1664:1760], omm[:, 1664:1760], ALU.mult)
        S.tt("pool", Wgdb[:, kc, :], st_[:, 1664:1760], mub[:, 1664:1760], ALU.mult)
    S.pop()
    for d in range(2):
        S.memset("pool", Wl[d][:, :, 80:96], 0.0)
        S.memset("pool", Wlb[d][:, :, 64:96], 0.0)

    def f32t(name, n=512, p=128):
        return S.sb(name, [p, n], F32)

    def bft(name, n=512, p=128):
        return S.sb(name, [p, n], BF16)

    xring = [f32t("xr%d" % i, 1024) for i in range(1)]
    hring = [S.sb("hT%d" % i, [128, 8, 128], BF16) for i in range(3)]
    hnb = S.sb("hnb", [128, 8, 128], BF16)
    st6 = S.sb("st6", [128, 2, 6], F32)
    mv2 = S.sb("mv2", [128, 2], F32)
    rstd1 = S.sb("rstd1", [128, 1], F32)
    r_sb, k_sb, v_sb, qk_sb = f32t("r_sb"), f32t("k_sb"), f32t("v_sb"), f32t("qk_sb")
    v_bf, vg_bf = bft("v_bf"), bft("vg_bf")
    lo = S.sb("lo", [96, 128], BF16)
    lo_gd = S.sb("lo_gd", [96, 128], BF16)
    sg, a_sb, lg = f32t("sg"), f32t("a_sb"), f32t("lg", 256)
    E1, E2, E3, E4 = f32t("E1"), f32t("E2"), f32t("E3"), f32t("E4")
    t0, t1, kk, kka, kh = f32t("t0"), f32t("t1"), f32t("kk"), f32t("kka"), f32t("kh")
    xn_bf = V(t0.buf, t0.ap.bitcast(BF16))
    nbacc = V(t1.buf, t1.ap.bitcast(BF16)).re("p (k t) -> p k t", k=8)
    ss = S.sb("ss", [128, 8], F32)
    bs = S.sb("bs", [128, 8], F32)
    pc_sb = S.sb("pc_sb", [64, 8], F32)
    pcg_sb = S.sb("pcg_sb", [64, 4], F32)
    At_bf, Bt_bf, Kt_bf, Rt_bf, Bh_bf, Kh_bf = (bft(n) for n in ("At", "Bt", "Kt", "Rt", "Bh", "Kh"))
    qt_bf, kt_bf, khg_bf = bft("qt", 256), bft("kt", 256), bft("khg", 256)

    def ht(name, p=128):
        return S.sb(name, [p, 4, 128], BF16)

    AtT, BtT, KtT, RtT = ht("AtT", 64), ht("BtT", 64), ht("KtT", 64), ht("RtT", 64)
    LakT, ArbT, ArkT, AqkT = ht("LakT"), ht("ArbT"), ht("ArkT"), ht("AqkT")
    Xr = [ht("Xr0"), ht("Xr1")]
    XTr = [ht("XTr0"), ht("XTr1")]
    Zr = [ht("Zr0"), ht("Zr1")]
    def half_aps(t):
        tb = t.ap.bitcast(BF16)
        return [tb[:, i * 512:(i + 1) * 512].rearrange("p (h t) -> p h t", h=4) for i in range(2)]
    QeffT, qtT, ktT = ht("QeffT", 64), ht("qtT", 64), ht("ktT", 64)
    HT = S.sb("HT", [64, 4, 64], BF16)
    M_f = [S.sb("M_f%d" % d, [64, 8, 64], F32) for d in range(2)]
    M_bf = [S.sb("M_bf%d" % d, [64, 8, 64], BF16) for d in range(2)]
    Mg_f = [S.sb("Mg_f%d" % d, [64, 4, 128], F32) for d in range(2)]
    Mg_bf = [S.sb("Mg_bf%d" % d, [64, 4, 128], BF16) for d in range(2)]
    y_sb, bo_sb, o_sb = f32t("y_sb"), f32t("bo_sb"), f32t("o_sb")
    yfl = f32t("yfl", 1536)
    gout_sb, gsil = sg, a_sb
    mix_bf = bft("mix_bf", 1024)
    s8a, s8b, s8c, s8d = (S.sb("s8%s" % n, [128, 8], F32) for n in "abcd")

    if DEBUG.get("verbose"):
        print("mixer SBUF bytes remaining per partition:", nc.sbuf_bytes_remaining)

    def v4(pbank, p=128, w=128):
        return pbank[0:p, 0:4 * w].re("p (h t) -> p h t", h=4)

    def proj_block(dst, hT, hn, c0, ncols, rw):
        n = 16 if rw else 8
        i = 0
        for kc in range(8):
            S.mm(dst[:, 0:ncols], hT[:, kc, :], Wa[:, kc, c0:c0 + ncols], start=(i == 0), stop=(i == n - 1))
            i += 1
        if rw:
            for kc in range(8):
                S.mm(dst[:, 0:ncols], hn[:, kc, :], Wb[:, kc, c0:c0 + ncols], start=False, stop=(i == n - 1))
                i += 1

    def produce(b, kind, idx):
        xt = xring[0]
        src = ctx_d[b, idx * 128:(idx + 1) * 128, :] if kind == "c" else x_d[b, idx * 128:(idx + 1) * 128, :]
        S.dma("sp", xt, src)
        layer_norm_stats(xt, st6, mv2, rstd1)
        S.ts("dve", xn_bf, xt, mv2[:, 0:1], ALU.subtract, rstd1, ALU.mult)
        pt = PT()
        ptv = pt.re("p (k t) -> p k t", k=8)
        col = 2 if kind == "c" else b
        hs = hring[idx % 3]
        for kc in range(8):
            S.tr(ptv[:, kc, :], xn_bf[:, kc * 128:(kc + 1) * 128], ident)
        for kc in range(8):
            S.act(hs[:, kc, :], ptv[:, kc, :], AF.Identity, bias=modT[:, kc, col:col + 1],
                  scale=modT[:, 8 + kc, col:col + 1])

    def neighbor(kind, idx, n_tiles):
        C = hring[idx % 3]
        Pv = hring[(idx - 1) % 3] if idx > 0 else None
        Nx = hring[(idx + 1) % 3] if idx < n_tiles - 1 else None
        if kind == "l":
            if Pv is not None:
                S.tt("dve", nbacc[:, :, 0:64], C[:, :, 64:128], Pv[:, :, 64:128], ALU.add)
            else:
                S.cp("dve", nbacc[:, :, 0:64], C[:, :, 64:128])
            if Nx is not None:
                S.tt("dve", nbacc[:, :, 64:128], C[:, :, 0:64], Nx[:, :, 0:64], ALU.add)
            else:
                S.cp("dve", nbacc[:, :, 64:128], C[:, :, 0:64])
            a4 = nbacc.re("p k (r c) -> p k r c", r=2)
            c4 = C.re("p k (r c) -> p k r c", r=2)
            S.tt("dve", a4[:, :, :, 1:64], a4[:, :, :, 1:64], c4[:, :, :, 0:63], ALU.add)
            S.tt("dve", a4[:, :, :, 0:63], a4[:, :, :, 0:63], c4[:, :, :, 1:64], ALU.add)
            S.ts("dve", hnb, nbacc, 0.25, ALU.mult)
        else:
            S.cp("dve", nbacc[:, :, 1:128], C[:, :, 0:127])
            if Pv is not None:
                S.cp("dve", nbacc[:, :, 0:1], Pv[:, :, 127:128])
            else:
                S.memset("dve", nbacc[:, :, 0:1], 0.0)
            S.tt("dve", nbacc[:, :, 0:127], nbacc[:, :, 0:127], C[:, :, 1:128], ALU.add)
            if Nx is not None:
                S.tt("dve", nbacc[:, :, 127:128], nbacc[:, :, 127:128], Nx[:, :, 0:1], ALU.add)
            S.ts("dve", hnb, nbacc, 0.5, ALU.mult)

    def mix_tile(b, kind, idx, d, want_out):
        hT = hring[idx % 3]
        final = want_out and d == 1
        if d == 0:
            mi, me, mr = fm[0], fm[1], fm[2]
            gmi, gmr = fm[4], fm[6]
            m_ij_s, m_ji_s, m_ji_i = mLs, mUs, mUi
        else:
            mi, me, mr = fm[3], fm[2], fm[1]
            gmi, gmr = fm[7], fm[5]
            m_ij_s, m_ji_s, m_ji_i = mUs, mLs, mLi
        pl = P()
        for kc in range(8):
            S.mm(pl[0:96, 0:128], Wl[d][:, kc, :], hT[:, kc, :], start=(kc == 0), stop=False)
        for kc in range(8):
            S.mm(pl[0:96, 0:128], Wlb[d][:, kc, :], hnb[:, kc, :], start=False, stop=(kc == 7))
        S.act(lo[0:32, :], pl[0:32, 0:128], AF.Tanh)
        S.act(lo[32:64, :], pl[32:64, 0:128], AF.Copy)
        S.act(lo[64:80, :], pl[64:80, 0:128], AF.Copy)
        pr_ = P(); proj_block(pr_, hT, hnb, 0, 512, True)
        pw = P()
        S.mm(pw, lo[0:32, :], sw[d][0:32, :], start=True, stop=False)
        S.mm(pw, onesf[0:1, :], br3[0:1, d, :], start=False, stop=True)
        pa = P()
        S.mm(pa, lo[32:64, :], sw[d][32:64, :], start=True, stop=False)
        S.mm(pa, onesf[32:33, :], br3[32:33, d, :], start=False, stop=True)
        pg = P()
        S.mm(pg[:, 0:256], lo[64:80, :], sw[d][64:80, 0:256], start=True, stop=False)
        S.mm(pg[:, 0:256], onesf[64:65, :], br3[64:65, d, 0:256], start=False, stop=True)
        S.act(sg, pw, AF.Sigmoid)
        S.act(a_sb, pa, AF.Sigmoid)
        S.act(lg, pg[:, 0:256], AF.Sigmoid)
        S.cp("act", r_sb, pr_)
        pk_ = P(); proj_block(pk_, hT, hnb, 512, 512, True)
        pc1 = P(); S.mm(pc1, mi, sg)
        pc2 = P(); S.mm(pc2, me, sg)
        S.act(lg, lg, AF.Ln)
        S.act(E1, pc1, AF.Exp); S.act(E2, pc1, AF.Exp, scale=-1.0)
        S.act(E3, pc2, AF.Exp)
        S.cp("act", k_sb, pk_)
        pc3 = P(); S.mm(pc3, mr, sg)
        pp = P()
        for h in range(8):
            S.mm(pp[0:64, h:h + 1], sg[:, h * 64:(h + 1) * 64], negcol)
        for h in range(4):
            S.mm(pp[0:64, 8 + h:9 + h], lg[:, h * 64:(h + 1) * 64], g16col)
        pv_ = P(); proj_block(pv_, hT, hnb, 1024, 512, True)
        S.act(E4, pc3, AF.Exp)
        S.act(pc_sb, pp[0:64, 0:8], AF.Exp)
        S.act(pcg_sb, pp[0:64, 8:12], AF.Exp)
        S.cp("act", v_sb, pv_); S.cp("pool", v_bf, v_sb)
        p = P(); proj_block(p, hT, hnb, 1536, 512, False); S.cp("act", qk_sb, p)
        p = P(); proj_block(p, hT, hnb, 2048, 512, False); S.cp("act", vg_bf, p)
        if DEBUG["cut"] <= 2:
            return
        S.tt("dve", t0, k_sb, kkrow, ALU.mult)
        S.tt("pool", t1, t0, t0, ALU.mult)
        S.red("dve", ss, t1.re("p (h c) -> p h c", h=8))
        S.act(ss, ss, AF.Sqrt, bias=1e-12)
        S.op("dve", lambda e: e.reciprocal(ss.ap, ss.ap), reads=[ss.buf], writes=[ss.buf])
        h8 = lambda t: t.re("p (h c) -> p h c", h=8)
        S.tt("dve", h8(kk), h8(t0), ss.us(2).bc([128, 8, 64]), ALU.mult)
        S.tt("pool", kka, kk, a_sb, ALU.mult)
        S.stt("pool", t1, a_sb, -1.0, karow, ALU.add, ALU.mult)
        S.stt("pool", kh, t1, 1.0, k_sb, ALU.add, ALU.mult)
        S.stt("dve", At_bf, kk, -1.0, E3, ALU.mult, ALU.mult)
        S.tt("dve", Bt_bf, kka, E2, ALU.mult)
        S.tt("dve", Kt_bf, kh, E2, ALU.mult)
        S.tt("dve", Rt_bf, r_sb, E1, ALU.mult)
        S.tt("pool", Bh_bf, kka, E4, ALU.mult)
        S.tt("pool", Kh_bf, kh, E4, ALU.mult)
        if want_out:
            S.tt("pool", t0, r_sb, kh, ALU.mult)
            S.tt("pool", t0, t0, rkrow, ALU.mult)
            S.red("dve", bs, h8(t0))
            S.tt("pool", h8(bo_sb), h8(v_sb), bs.us(2).bc([128, 8, 64]), ALU.mult)
        if DEBUG["cut"] <= 3:
            return
        dbg = DEBUG["dbg"] and b == 0 and kind == "c" and idx == 0 and d == 0
        if dbg:
            for nm, t_ in (("E1", E1), ("E2", E2), ("E3", E3), ("E4", E4), ("kk", kk), ("a", a_sb), ("sg", sg), ("kh", kh),
                           ("r", r_sb), ("v", v_sb)):
                dump(nm, t_, False)
            for nm, t_ in (("At", At_bf), ("Bt", Bt_bf), ("Kt", Kt_bf), ("Rt", Rt_bf), ("Bh", Bh_bf), ("Kh", Kh_bf), ("vb", v_bf)):
                dump(nm, t_, True)
            dump("pc", pc_sb, False)
        G1, G2, G4 = E1, E2, E3
        pg1 = P(); S.mm(pg1[:, 0:256], gmi, lg)
        pg3 = P(); S.mm(pg3[:, 0:256], gmr, lg)
        S.act(G1[:, 0:256], pg1[:, 0:256], AF.Exp)
        S.act(G2[:, 0:256], pg1[:, 0:256], AF.Exp, scale=-1.0)
        S.act(G4[:, 0:256], pg3[:, 0:256], AF.Exp)
        S.stt("dve", qt_bf, qk_sb[:, 0:256], 0.125, G1[:, 0:256], ALU.mult, ALU.mult)
        S.tt("dve", kt_bf, qk_sb[:, 256:512], G2[:, 0:256], ALU.mult)
        S.tt("pool", khg_bf, qk_sb[:, 256:512], G4[:, 0:256], ALU.mult)
        if DEBUG["cut"] <= 4:
            return
        parents = [E1, E2, E3, E4, sg, a_sb, t0, t1, kk, kka, kh, k_sb, qk_sb, r_sb]
        kids = [S.alias_acquire(pt_, half_aps(pt_)) for pt_ in parents]
        flat = [c for pair in kids[3:] for c in pair]
        G = [dict(AtT=AtT, BtT=BtT, KtT=KtT, RtT=RtT, X0=Xr[0], XT0=XTr[0], Lm=Xr[1], LTm=XTr[1],
                  LakT=LakT, ArbT=ArbT, ArkT=ArkT, Z0=Zr[0], Zf=Zr[1], D=kids[0], DT=kids[1],
                  Y1=kids[2][0], Y1t=kids[2][1], QeffT=QeffT, HT=HT),
             dict(AtT=flat[0][0:64], BtT=flat[1][0:64], KtT=flat[2][0:64], RtT=flat[3][0:64],
                  X0=flat[4], XT0=flat[5], Lm=flat[6], LTm=flat[7], LakT=flat[8], ArbT=flat[9], ArkT=flat[10],
                  Z0=flat[11], Zf=flat[12], D=[flat[13], flat[14]], DT=[flat[15], flat[16]],
                  Y1=flat[17], Y1t=flat[18], QeffT=flat[19][0:64], HT=flat[20][0:64, :, 0:64])]
        identb = ident.us(1).bc([128, 4, 128])

        def score(dst, L, R, mask):
            p_ = P()
            pv = v4(p_)
            for hh in range(4):
                S.mm(pv[:, hh, :], L[0:64, hh, :], R[0:64, hh, :])
            S.tt("dve", dst, pv, mask.us(1).bc([128, 4, 128]), ALU.mult)

        for g in range(2):
            T = G[g]
            for src, key, eng in ((At_bf, "AtT", "act"), (Bt_bf, "BtT", "dve"), (Kt_bf, "KtT", "act"), (Rt_bf, "RtT", "dve")):
                pt = PT()
                ptv = v4(pt, 64)
                for hh in range(4):
                    h = 4 * g + hh
                    S.tr(ptv[:, hh, :], src[:, h * 64:(h + 1) * 64], ident)
                S.cp(eng, T[key], ptv)
        for g in range(2):
            T = G[g]
            score(T["X0"], T["AtT"], T["BtT"], m_ij_s)
            score(T["XT0"], T["BtT"], T["AtT"], m_ji_s)
            score(T["LakT"], T["KtT"], T["AtT"], m_ji_s)
            score(T["ArbT"], T["BtT"], T["RtT"], m_ji_i)
            score(T["ArkT"], T["KtT"], T["RtT"], m_ji_i)
        for g in range(2):
            T = G[g]
            p = P()
            pv = p[:, 0:256].re("p (h v) -> p h v", h=4)
            for hh in range(4):
                h = 4 * g + hh
                S.mm(pv[:, hh, :], T["LakT"][:, hh, :], v_bf[:, h * 64:(h + 1) * 64])
            S.cp("act", T["Z0"][:, :, 0:64], pv)
            S.cp("pool", T["Z0"][:, :, 64:128], At_bf[:, g * 256:(g + 1) * 256].re("p (h c) -> p h c", h=4))
            m0 = smask[0].us(1).bc([128, 4, 128])
            S.tt("pool", T["Lm"], T["X0"], m0, ALU.mult)
            S.tt("pool", T["LTm"], T["XT0"], m0, ALU.mult)
            S.tt("dve", T["D"][0], T["Lm"], identb, ALU.add)
            S.tt("dve", T["DT"][0], T["LTm"], identb, ALU.add)
        U16 = mybir.dt.uint16
        for l in range(1, 7):
            mk = V(cb.buf, smask[l].ap.bitcast(U16)).us(1).bc([128, 4, 128])
            for g in range(2):
                T = G[g]
                Dc, DTc = T["D"][0], T["DT"][0]
                p = P(); pv = v4(p)
                for hh in range(4):
                    S.mm(pv[:, hh, :], T["XT0"][:, hh, :], Dc[:, hh, :])
                S.cp("act", T["Y1"], pv)
                p = P(); pv = v4(p)
                for hh in range(4):
                    S.mm(pv[:, hh, :], T["X0"][:, hh, :], DTc[:, hh, :])
                S.cp("act", T["Y1t"], pv)
            for g in range(2):
                T = G[g]
                Dc, DTc = T["D"][0], T["DT"][0]
                p1 = P(); pv1 = v4(p1)
                for hh in range(4):
                    S.mm(pv1[:, hh, :], DTc[:, hh, :], T["Y1"][:, hh, :])
                p2 = P(); pv2 = v4(p2)
                for hh in range(4):
                    S.mm(pv2[:, hh, :], Dc[:, hh, :], T["Y1t"][:, hh, :])
                for dst, pvx in ((Dc, pv1), (DTc, pv2)):
                    o_, m_, d_ = dst.ap, mk.ap, pvx.ap
                    S.op("dve", lambda e, o_=o_, m_=m_, d_=d_: e.copy_predicated(o_, m_, d_),
                         reads=[pvx.buf, mk.buf, dst.buf], writes=[dst.buf])
        for g in range(2):
            T = G[g]
            DTc = T["DT"][0]
            p = P(); pv = v4(p)
            for hh in range(4):
                S.mm(pv[:, hh, :], DTc[:, hh, :], T["Z0"][:, hh, :])
            S.cp("act" if g == 0 else "dve", T["Zf"], pv)
        for g in range(2):
            T = G[g]
            Zf = T["Zf"]
            p = P()
            pv = v4(p, 64)
            for hh in range(4):
                h = 4 * g + hh
                S.mm(pv[:, hh, :], Rt_bf[:, h * 64:(h + 1) * 64], ident, start=True, stop=False)
                S.mm(pv[:, hh, :], Zf[:, hh, 64:128], T["ArbT"][:, hh, :], start=False, stop=True)
            S.cp("act", T["QeffT"], pv)
            p = P()
            pvh = p[0:64, 0:256].re("p (h k) -> p h k", h=4)
            for hh in range(4):
                h = 4 * g + hh
                S.mm(pvh[:, hh, :], Zf[:, hh, 64:128], Bh_bf[:, h * 64:(h + 1) * 64])
            S.cp("dve", T["HT"], pvh)
        for g in range(2):
            T = G[g]
            Zf = T["Zf"]
            if want_out:
                p = P()
                pv = p[:, 0:256].re("p (h v) -> p h v", h=4)
                for hh in range(4):
                    h = 4 * g + hh
                    S.mm(pv[:, hh, :], T["QeffT"][:, hh, :], M_bf[d][:, h, :], start=True, stop=False)
                    S.mm(pv[:, hh, :], T["ArbT"][:, hh, :], Zf[:, hh, 0:64], start=False, stop=False)
                    S.mm(pv[:, hh, :], T["ArkT"][:, hh, :], v_bf[:, h * 64:(h + 1) * 64], start=False, stop=True)
                S.cp("act", y_sb[:, g * 256:(g + 1) * 256], p[:, 0:256])
            p = P()
            pvm = p[0:64, 0:256].re("p (h v) -> p h v", h=4)
            for hh in range(4):
                h = 4 * g + hh
                S.mm(pvm[:, hh, :], T["HT"][:, hh, :], M_bf[d][:, h, :], start=True, stop=False)
                S.mm(pvm[:, hh, :], Bh_bf[:, h * 64:(h + 1) * 64], Zf[:, hh, 0:64], start=False, stop=False)
                S.mm(pvm[:, hh, :], Kh_bf[:, h * 64:(h + 1) * 64], v_bf[:, h * 64:(h + 1) * 64], start=False, stop=True)
            Mv = M_f[d][:, 4 * g:4 * g + 4, :]
            S.tt("dve", Mv, Mv, pc_sb[:, 4 * g:4 * g + 4].us(2).bc([64, 4, 64]), ALU.mult)
            S.tt("dve", Mv, Mv, pvm, ALU.add)
            S.cp("dve", M_bf[d][:, 4 * g:4 * g + 4, :], Mv)
        for pt_, ch in zip(parents, kids):
            S.alias_release(pt_, ch)
        for src, dstT, eng in ((qt_bf, qtT, "act"), (kt_bf, ktT, "dve")):
            pt = PT()
            ptv = v4(pt, 64)
            for hh in range(4):
                S.tr(ptv[:, hh, :], src[:, hh * 64:(hh + 1) * 64], ident)
            S.cp(eng, dstT, ptv)
        p = P()
        pv = v4(p)
        for hh in range(4):
            S.mm(pv[:, hh, :], ktT[:, hh, :], qtT[:, hh, :])
        S.tt("dve", AqkT, pv, m_ji_i.us(1).bc([128, 4, 128]), ALU.mult)
        if want_out:
            p = P()
            pv = v4(p)
            for hh in range(4):
                S.mm(pv[:, hh, :], AqkT[:, hh, :], vg_bf[:, hh * 128:(hh + 1) * 128], start=True, stop=False)
                S.mm(pv[:, hh, :], qtT[:, hh, :], Mg_bf[d][:, hh, :], start=False, stop=True)
            S.cp("act", o_sb, p)
        p = P()
        pvg = v4(p, 64)
        for hh in range(4):
            S.mm(pvg[:, hh, :], khg_bf[:, hh * 64:(hh + 1) * 64], vg_bf[:, hh * 128:(hh + 1) * 128])
        S.tt("dve", Mg_f[d], Mg_f[d], pcg_sb.us(2).bc([64, 4, 128]), ALU.mult)
        S.tt("dve", Mg_f[d], Mg_f[d], pvg, ALU.add)
        S.cp("dve", Mg_bf[d], Mg_f[d])
        if not want_out:
            return
        if d == 0:
            S.dma("sp", yf_d[idx, :, 0:512], y_sb)
            S.dma("sp", yf_d[idx, :, 512:1024], bo_sb)
            S.dma("sp", yf_d[idx, :, 1024:1536], o_sb)
            return
        pl2 = P()
        for kc in range(8):
            S.mm(pl2[0:96, 0:128], Wgd[:, kc, :], hT[:, kc, :], start=(kc == 0), stop=False)
        for kc in range(8):
            S.mm(pl2[0:96, 0:128], Wgdb[:, kc, :], hnb[:, kc, :], start=False, stop=(kc == 7))
        S.act(lo_gd, pl2[0:96, 0:128], AF.Sigmoid)
        pgo = P()
        S.mm(pgo, lo_gd, g2w)
        S.cp("act", gout_sb, pgo)
        p = P(); proj_block(p, hT, hnb, 2560, 512, False)
        S.act(gsil, p, AF.Silu)
        S.dma("sp", yfl, yf_d[idx])
        S.tt("dve", y_sb, y_sb, yfl[:, 0:512], ALU.add)
        S.red("dve", s8a, h8(y_sb))
        S.tt("pool", t0, y_sb, y_sb, ALU.mult)
        S.red("dve", s8b, h8(t0))
        S.ts("dve", s8a, s8a, 1.0 / 64.0, ALU.mult)
        S.tt("dve", s8c, s8a, s8a, ALU.mult)
        S.stt("dve", s8b, s8b, 1.0 / 64.0, s8c, ALU.mult, ALU.subtract)
        S.act(s8b, s8b, AF.Sqrt, bias=GN_EPS)
        S.op("dve", lambda e: e.reciprocal(s8b.ap, s8b.ap), reads=[s8b.buf], writes=[s8b.buf])
        S.tt("dve", h8(y_sb), h8(y_sb), s8a.us(2).bc([128, 8, 64]), ALU.subtract)
        S.tt("dve", h8(y_sb), h8(y_sb), s8b.us(2).bc([128, 8, 64]), ALU.mult)
        S.tt("dve", y_sb, y_sb, gnwrow, ALU.mult)
        S.tt("dve", y_sb, y_sb, gnbrow, ALU.add)
        S.tt("dve", y_sb, y_sb, bo_sb, ALU.add)
        S.tt("dve", y_sb, y_sb, yfl[:, 512:1024], ALU.add)
        S.tt("dve", mix_bf[:, 0:512], y_sb, gout_sb, ALU.mult)
        S.tt("dve", o_sb, o_sb, yfl[:, 1024:1536], ALU.add)
        S.tt("pool", t0, o_sb, o_sb, ALU.mult)
        S.red("dve", s8d[:, 0:4], t0.re("p (h c) -> p h c", h=4))
        S.act(s8d[:, 0:4], s8d[:, 0:4], AF.Sqrt, bias=1e-5, scale=1.0 / 128.0)
        S.op("dve", lambda e: e.reciprocal(s8d.ap[:, 0:4], s8d.ap[:, 0:4]), reads=[s8d.buf], writes=[s8d.buf])
        o4 = o_sb.re("p (h c) -> p h c", h=4)
        S.tt("dve", o4, o4, s8d[:, 0:4].us(2).bc([128, 4, 128]), ALU.mult)
        S.tt("dve", o_sb, o_sb, gnormrow, ALU.mult)
        S.tt("dve", mix_bf[:, 512:1024], o_sb, gsil, ALU.mult)
        S.dma("sp", mix_d[b, idx * 128:(idx + 1) * 128, :], mix_bf)

    if DEBUG["stop_after"] == "Bs":
        S.pop()
        S.finish()
        return nc
    for b in range(NB):
        for d in range(2):
            S.memset("dve", M_f[d], 0.0)
            S.memset("dve", M_bf[d], 0.0)
            S.memset("pool", Mg_f[d], 0.0)
            S.memset("pool", Mg_bf[d], 0.0)
        for d in range(2):
            for kind, n_t in (("c", 2), ("l", 16)):
                order = list(range(n_t)) if d == 0 else list(range(n_t - 1, -1, -1))
                step = 1 if d == 0 else -1
                produce(b, kind, order[0])
                for i, idx in enumerate(order):
                    nxt = idx + step
                    if 0 <= nxt < n_t:
                        produce(b, kind, nxt)
                    neighbor(kind, idx, n_t)
                    mix_tile(b, kind, idx, d, kind == "l")
                    if DEBUG["stop_after"] == "B1":
                        break
                if DEBUG["stop_after"] == "B1":
                    break
    S.pop()
    if DEBUG["stop_after"] in ("B", "B1"):
        S.finish()
        return nc
    return moe_phase(nc, S, locals())


def moe_phase(nc, S, L):
    out_d, modrow_d = L["out_d"], L["modrow_d"]
    rtr_d, exg_d, exu_d, exd_d = L["rtr_d"], L["exg_d"], L["exu_d"], L["exd_d"]
    ln2g_d, ln2b_d, eoh_d = L["ln2g_d"], L["ln2b_d"], L["eoh_d"]
    ident, mUi, ones_bf = L["ident"], L["mUi"], L["ones_bf"]
    iota_f = L["iota_f"]
    pbanks, ptbanks = L["pbanks"], L["ptbanks"]
    layer_norm_stats = L["layer_norm_stats"]
    rot = {"p": 0, "t": 0}

    def P():
        rot["p"] = (rot["p"] + 1) % 2
        return pbanks[4 + rot["p"]]

    def PT():
        rot["t"] = (rot["t"] + 1) % 2
        return ptbanks[rot["t"]]

    Y = pbanks[0:4]
    exg_v = [exg_d[e].re("(kc p) f -> p kc f", p=128) for e in range(16)]
    exu_v = [exu_d[e].re("(kc p) f -> p kc f", p=128) for e in range(16)]
    exd_v = [exd_d[e].re("(fc p) d -> p fc d", p=128) for e in range(16)]

    x_d, mix_d, wout_d, ln1g_d, ln1b_d = L["x_d"], L["mix_d"], L["wout_d"], L["ln1g_d"], L["ln1b_d"]
    for b in range(NB):
        S.epoch()
        S.push()
        h2 = S.sb("h2", [128, 16, 1024], BF16)
        acc = S.sb("acc", [128, 16, 1024], F32)
        mrows = S.sb("mrows", [128, 3, 1024], F32)
        S.dma("sp", mrows[:, 0, :], modrow_d[b, 1:2, :].pb(128))
        S.dma("sp", mrows[:, 1, :], modrow_d[b, 2:3, :].pb(128))
        S.dma("sp", mrows[:, 2, :], modrow_d[b, 3:4, :].pb(128))
        identf = S.sb("identf", [128, 128], F32)
        S.cp("dve", identf, ident)
        aff_tok = S.sb("aff_tok", [128, 16, 16], F32)
        mask_tok = S.sb("mask_tok", [128, 16, 16], BF16)
        gate_tok = S.sb("gate_tok", [128, 16, 16], BF16)
        slot_tok = S.sb("slot_tok", [128, 16, 16], F32)
        m8 = S.sb("m8", [16, 8], F32)
        st6 = S.sb("mst6", [128, 2, 6], F32)
        mv2 = S.sb("mmv2", [128, 2], F32)
        rstd1 = S.sb("mrstd1", [128, 1], F32)
        xt = [S.sb("mx%d" % i, [128, 1024], F32) for i in range(2)]

        S.push()
        affT = S.sb("affT", [16, 2048], F32)
        wk = S.sb("wk", [16, 2048], F32)
        S.push()
        woutg = S.sb("woutg", [128, 8, 1024], BF16)
        g1row = S.sb("g1row", [128, 1024], F32)
        wstg = [S.sb("wstg%d" % i, [128, 1024], F32) for i in range(2)]
        lnrows = S.sb("lnrows", [128, 2, 1024], F32)
        S.dma("sp", lnrows[:, 0, :], ln1g_d.pb(128))
        S.dma("sp", lnrows[:, 1, :], ln1b_d.pb(128))
        S.dma("sp", g1row, modrow_d[b, 0:1, :].pb(128))
        for kc in range(8):
            ws = wstg[kc % 2]
            S.dma("sp", ws, wout_d[kc * 128:(kc + 1) * 128, :])
            S.tt("pool", woutg[:, kc, :], ws, g1row, ALU.mult)
        rtr = S.sb("rtr", [128, 8, 16], BF16)
        S.dma("pool", rtr, rtr_d.re("(kc p) e -> p kc e", p=128))
        mixt = [S.sb("mixt%d" % i, [128, 1024], BF16) for i in range(2)]
        mixT = S.sb("mixT", [128, 8, 128], BF16)
        x1t = S.sb("x1t", [128, 1024], F32)
        xnf = S.sb("xnf", [128, 1024], F32)
        h2T = S.sb("h2T", [128, 8, 128], BF16)
        sm1 = S.sb("sm1", [128, 1], F32)
        sm2 = S.sb("sm2", [128, 1], F32)
        nmr = S.sb("nmr", [128, 1], F32)
        lgt = S.sb("lgt", [128, 16], F32)
        for t in range(16):
            x = xt[t % 2]
            mt = mixt[t % 2]
            S.dma("sp", x, x_d[b, t * 128:(t + 1) * 128, :])
            S.dma("sp", mt, mix_d[b, t * 128:(t + 1) * 128, :])
            pt = PT()
            ptv = pt.re("p (k t) -> p k t", k=8)
            for kc in range(8):
                S.tr(ptv[:, kc, :], mt[:, kc * 128:(kc + 1) * 128], ident)
            S.cp("act", mixT, ptv)
            for half in range(2):
                pp_ = P()
                for kc in range(8):
                    S.mm(pp_, mixT[:, kc, :], woutg[:, kc, half * 512:(half + 1) * 512], start=(kc == 0), stop=(kc == 7))
                S.stt("dve", x1t[:, half * 512:(half + 1) * 512], x[:, half * 512:(half + 1) * 512], ALPHA, pp_,
                      ALU.mult, ALU.add)
            layer_norm_stats(x1t, st6, mv2, rstd1)
            S.stt("dve", nmr, mv2[:, 0:1], -1.0, rstd1, ALU.mult, ALU.mult)
            S.act(x1t, x1t, AF.Identity, bias=nmr, scale=rstd1)
            S.tt("dve", x1t, x1t, lnrows[:, 0, :], ALU.mult)
            S.tt("dve", x1t, x1t, lnrows[:, 1, :], ALU.add)
            S.act(acc[:, t, :], x1t, AF.Copy, scale=ALPHA)
            layer_norm_stats(x1t, st6, mv2, rstd1)
            S.stt("dve", nmr, mv2[:, 0:1], -1.0, rstd1, ALU.mult, ALU.mult)
            S.act(xnf, x1t, AF.Identity, bias=nmr, scale=rstd1)
            S.tt("dve", xnf, xnf, mrows[:, 1, :], ALU.mult)
            S.tt("dve", h2[:, t, :], xnf, mrows[:, 0, :], ALU.add)
            pt = PT()
            ptv = pt.re("p (k t) -> p k t", k=8)
            for kc in range(8):
                S.tr(ptv[:, kc, :], h2[:, t, kc * 128:(kc + 1) * 128], ident)
            S.cp("act", h2T, ptv)
            p = P()
            for kc in range(8):
                S.mm(p[:, 0:16], h2T[:, kc, :], rtr[:, kc, :], start=(kc == 0), stop=(kc == 7))
            S.op("dve", lambda e, p=p: e.reduce_max(sm1.ap, p.ap[:, 0:16], AX.X), reads=[p.buf], writes=[sm1.buf])
            S.ts("dve", sm1, sm1, -1.0, ALU.mult)
            S.act(lgt, p[:, 0:16], AF.Exp, bias=sm1)
            S.red("dve", sm2, lgt)
            S.op("dve", lambda e: e.reciprocal(sm2.ap, sm2.ap), reads=[sm2.buf], writes=[sm2.buf])
            S.ts("dve", aff_tok[:, t, :], lgt, sm2, ALU.mult)
            p2 = P()
            S.mm(p2[0:16, 0:128], aff_tok[:, t, :], identf)
            S.cp("act", affT[:, t * 128:(t + 1) * 128], p2[0:16, 0:128])
        S.pop()
        if DEBUG["stop_after"] == "C0":
            S.pop()
            S.pop()
            S.finish()
            return nc
        S.cp("dve", wk, affT)
        for r in range(32):
            S.op("dve", lambda e: e.max(m8.ap, wk.ap), reads=[wk.buf], writes=[m8.buf])
            if r < 31:
                S.op("dve", lambda e: e.match_replace(wk.ap, m8.ap, wk.ap, -1.0), reads=[wk.buf, m8.buf], writes=[wk.buf])
        S.ts("dve", wk, affT, m8[:, 7:8], ALU.is_ge)
        for t in range(16):
            p = P()
            S.mm(p[:, 0:16], wk[:, t * 128:(t + 1) * 128], identf[0:16, 0:16])
            S.cp("act", mask_tok[:, t, :], p[:, 0:16])
        for t in range(16):
            S.tt("dve", gate_tok[:, t, :], aff_tok[:, t, :], mask_tok[:, t, :], ALU.mult)
            p = P()
            for t2 in range(t + 1):
                S.mm(p[:, 0:16], mUi if t2 == t else ones_bf, mask_tok[:, t2, :], start=(t2 == 0), stop=(t2 == t))
            S.tt("dve", slot_tok[:, t, :], p[:, 0:16], mask_tok[:, t, :], ALU.mult)
            S.ts("dve", slot_tok[:, t, :], slot_tok[:, t, :], -1.0, ALU.add)
        S.pop()

        S.push()
        sel = S.sb("sel", [128, 16, 256], BF16)
        selT = [S.sb("selT%d" % i, [128, 2, 2048], BF16) for i in range(2)]
        xeT = [S.sb("xeT%d" % i, [128, 8, 256], BF16) for i in range(2)]
        actT = S.sb("actT", [128, 2, 256], BF16)
        sl = S.sb("sl", [128, 256], F32)
        y_bf = S.sb("y_bf", [128, 2, 1024], BF16)
        gslot = [S.sb("gslot%d" % i, [128, 2], F32) for i in range(2)]
        NW = 3
        wg_t = [S.sb("wg%d" % i, [128, 8, 256], BF16) for i in range(NW)]
        wu_t = [S.sb("wu%d" % i, [128, 8, 256], BF16) for i in range(NW)]
        wd_t = [S.sb("wd%d" % i, [128, 2, 1024], BF16) for i in range(NW)]
        paux = V(ptbanks[0].buf, ptbanks[0].ap.bitcast(F32))
        paux2 = V(ptbanks[1].buf, ptbanks[1].ap.bitcast(F32))
        ptr = ptbanks[1]

        def prep_pieces(e):
            k = e % 2
            pieces = []

            def mk_sel(t0_):
                def f():
                    for t in range(t0_, t0_ + 4):
                        S.ts("dve", sel[:, t, :], iota_f, slot_tok[:, t, e:e + 1], ALU.is_equal)
                return f
            for t0_ in range(0, 16, 4):
                pieces.append(mk_sel(t0_))

            def f_gslot():
                for jc in range(2):
                    for t in range(16):
                        S.mm(paux[:, jc:jc + 1], sel[:, t, jc * 128:(jc + 1) * 128], gate_tok[:, t, e:e + 1],
                             start=(t == 0), stop=(t == 15))
                S.cp("act", gslot[k], paux[:, 0:2])
            pieces.append(f_gslot)

            def mk_tr(jc, tq):
                def f():
                    ptv = ptr.re("p (k t) -> p k t", k=8)
                    for tt_ in range(8):
                        t = tq * 8 + tt_
                        S.tr(ptv[:, tt_, :], sel[:, t, jc * 128:(jc + 1) * 128], ident)
                    S.cp("act", selT[k][:, jc, tq * 1024:(tq + 1) * 1024], ptr)
                return f
            tail = []
            for jc in range(2):
                for tq in range(2):
                    tail.append(mk_tr(jc, tq))

            def mk_gather(dp):
                def f():
                    pv = paux.re("p (c j) -> p c j", c=2)
                    for c2 in range(2):
                        dc = dp * 2 + c2
                        for t in range(16):
                            S.mm(pv[:, c2, :], h2[:, t, dc * 128:(dc + 1) * 128], sel[:, t, :], start=(t == 0), stop=(t == 15))
                    S.cp("act", xeT[k][:, dp * 2:dp * 2 + 2, :], pv)
                return f
            for dp in range(4):
                pieces.append(mk_gather(dp))
            return pieces, tail

        def scatter_pieces(e):
            k = e % 2
            pieces = []

            def mk(t, dh):
                def f():
                    pa = paux if dh == 0 else paux2
                    for jc in range(2):
                        S.mm(pa, selT[k][:, jc, t * 128:(t + 1) * 128], y_bf[:, jc, dh * 512:(dh + 1) * 512],
                             start=(jc == 0), stop=(jc == 1))
                    a_ = acc[:, t, dh * 512:(dh + 1) * 512]
                    S.tt("dve", a_, a_, pa, ALU.add)
                return f
            for t in range(16):
                for dh in range(2):
                    pieces.append(mk(t, dh))
            return pieces

        pm_, pt_ = prep_pieces(0)
        for f in pm_ + pt_:
            f()
        gcount = 0
        for e in range(16):
            k = e % 2
            pend_sc = scatter_pieces(e - 1) if e > 0 else []
            pend_pr, pend_tr = prep_pieces(e + 1) if e < 15 else ([], [])
            chunk_i = 0
            wsel = {}

            def issue_w(fg):
                nonlocal gcount
                wi = gcount % NW
                gcount += 1
                f0 = fg * 256
                S.dma("pool", wg_t[wi], exg_v[e][:, :, f0:f0 + 256])
                S.dma("pool", wu_t[wi], exu_v[e][:, :, f0:f0 + 256])
                S.dma("pool", wd_t[wi], exd_v[e][:, fg * 2:fg * 2 + 2, :])
                wsel[fg] = wi

            def GU(n):
                fg, fc = n // 2, n % 2
                if fg not in wsel:
                    issue_w(fg)
                wi = wsel[fg]
                p = P()
                for kc in range(8):
                    S.mm(p[:, 0:256], wg_t[wi][:, kc, fc * 128:(fc + 1) * 128], xeT[k][:, kc, :], start=(kc == 0), stop=(kc == 7))
                for kc in range(8):
                    S.mm(p[:, 256:512], wu_t[wi][:, kc, fc * 128:(fc + 1) * 128], xeT[k][:, kc, :], start=(kc == 0), stop=(kc == 7))
                return p

            pcur = GU(0)
            for n in range(22):
                fg, fc = n // 2, n % 2
                wi = wsel[fg]
                pnext = GU(n + 1) if n < 21 else None
                S.act(sl, pcur[:, 0:256], AF.Silu)
                S.tt("dve", actT[:, fc, :], sl, pcur[:, 256:512], ALU.mult)
                for jc in range(2):
                    for dh in range(2):
                        S.mm(Y[jc * 2 + dh], actT[:, fc, jc * 128:(jc + 1) * 128], wd_t[wi][:, fc, dh * 512:(dh + 1) * 512],
                             start=(n == 0), stop=(n == 21))
                pcur = pnext
                chunk_i += 1
                if chunk_i <= 16:
                    for _ in range(min(2, len(pend_sc))):
                        pend_sc.pop(0)()
                if chunk_i <= 18 and chunk_i % 2 == 0 and pend_pr:
                    pend_pr.pop(0)()
                if chunk_i > 18:
                    while pend_sc:
                        pend_sc.pop(0)()
                    while pend_pr:
                        pend_pr.pop(0)()
                    if pend_tr:
                        pend_tr.pop(0)()
            while pend_sc:
                pend_sc.pop(0)()
            while pend_pr:
                pend_pr.pop(0)()
            while pend_tr:
                pend_tr.pop(0)()
            for jc in range(2):
                for dh in range(2):
                    S.stt("dve", y_bf[:, jc, dh * 512:(dh + 1) * 512], Y[jc * 2 + dh], gslot[k][:, jc:jc + 1],
                          mrows[:, 2, dh * 512:(dh + 1) * 512], ALU.mult, ALU.mult)
        for f in scatter_pieces(15):
            f()
        S.pop()
        S.push()
        ln2rows = S.sb("ln2rows", [128, 2, 1024], F32)
        S.dma("sp", ln2rows[:, 0, :], ln2g_d.pb(128))
        S.dma("sp", ln2rows[:, 1, :], ln2b_d.pb(128))
        for t in range(16):
            a_ = acc[:, t, :]
            layer_norm_stats(a_, st6, mv2, rstd1)
            o_ = xt[t % 2]
            S.ts("dve", o_, a_, mv2[:, 0:1], ALU.subtract, rstd1, ALU.mult)
            S.tt("dve", o_, o_, ln2rows[:, 0, :], ALU.mult)
            S.tt("dve", o_, o_, ln2rows[:, 1, :], ALU.add)
            S.dma("sp", out_d[b, t * 128:(t + 1) * 128, :], o_)
        S.pop()
        S.pop()
    S.finish()
    return nc


def kernel(x, c, ctx, c_ctx, ada_w, ada_b, w_in, rw_mu, rw_w0, rw_w2, rw_a0, rw_a2, rw_g2,
           rw_k_k, rw_k_a, rw_r_k, rw_gn_w, rw_gn_b, gla_a2, gla_a_b, gla_norm_w, w_out,
           ln1_g, ln1_b, router_w, ex_gate, ex_up, ex_down, ln2_g, ln2_b):
    f = lambda a: np.ascontiguousarray(np.asarray(a, dtype=np.float32))
    x, c, ctx, c_ctx = f(x), f(c), f(ctx), f(c_ctx)
    cbf, cfc = host_consts()
    eoh = np.zeros((16, 16 * 128), np.float32)
    for e in range(16):
        eoh[e, e * 128:(e + 1) * 128] = 1.0
    mu_ext = np.zeros((1, 3328), np.float32)
    mu_ext[0, :1760] = f(rw_mu)[0]
    shared = {
        "ada_w": f(ada_w)[0], "ada_b": f(ada_b)[0][None, :],
        "ada_bT": np.ascontiguousarray(f(ada_b)[0].reshape(48, 128).T),
        "w_in": f(w_in)[0], "mu_ext": mu_ext,
        "rw_w0": f(rw_w0)[0], "rw_w2": f(rw_w2)[0], "rw_a0": f(rw_a0)[0], "rw_a2": f(rw_a2)[0],
        "rw_g2": f(rw_g2)[0], "rw_k_k": f(rw_k_k)[0][None, :], "rw_k_a": f(rw_k_a)[0][None, :],
        "rw_r_k": f(rw_r_k)[0].reshape(1, 512), "rw_gn_w": f(rw_gn_w)[0][None, :], "rw_gn_b": f(rw_gn_b)[0][None, :],
        "gla_a2": f(gla_a2)[0], "gla_a_b": f(gla_a_b)[0], "gla_norm_w": f(gla_norm_w)[0][None, :],
        "w_out": f(w_out)[0], "ln1_g": f(ln1_g)[0][None, :], "ln1_b": f(ln1_b)[0][None, :],
        "router_w": f(router_w)[0], "ex_gate": f(ex_gate)[0], "ex_up": f(ex_up)[0], "ex_down": f(ex_down)[0],
        "ln2_g": f(ln2_g)[0][None, :], "ln2_b": f(ln2_b)[0][None, :],
        "cbf": cbf, "cf": cfc, "eoh": eoh,
    }
    in_maps = []
    for core in range(8):
        bs = slice(core * NB, (core + 1) * NB)
        cc = np.stack([c[core * NB], c[core * NB + 1], c_ctx], axis=1)
        cT = np.ascontiguousarray(cc.reshape(8, 128, 3).transpose(1, 0, 2))
        m = dict(shared)
        m["x"] = np.ascontiguousarray(x[bs])
        m["ctx"] = np.ascontiguousarray(ctx[bs])
        m["cT"] = cT
        in_maps.append(m)
    nc = build()
    res = run_bass_kernel_spmd(nc, in_maps, core_ids=list(range(8)))
    if DEBUG["dump"]:
        DEBUG["res"] = res.results
    return np.concatenate([np.asarray(r["out"], dtype=np.float32) for r in res.results], axis=0)
```

```python
from contextlib import ExitStack
import numpy as np
import concourse.bass as bass
import concourse.mybir as mybir
from concourse.bass_utils import run_bass_kernel_spmd

F32 = mybir.dt.float32
BF16 = mybir.dt.bfloat16
AF = mybir.ActivationFunctionType
ALU = mybir.AluOpType
AX = mybir.AxisListType


class Buf:
    __slots__ = ("name", "lw", "rd")

    def __init__(self, name):
        self.name = name
        self.lw = None
        self.rd = {}


class V:
    __slots__ = ("buf", "ap")

    def __init__(self, buf, ap):
        self.buf = buf
        self.ap = ap

    def __getitem__(self, k):
        return V(self.buf, self.ap[k])

    def bc(self, shape):
        return V(self.buf, self.ap.to_broadcast(list(shape)))

    def us(self, axis):
        return V(self.buf, self.ap.unsqueeze(axis))

    def re(self, pat, **kw):
        return V(self.buf, self.ap.rearrange(pat, **kw))

    def pb(self, n=128):
        return V(self.buf, self.ap.partition_broadcast(n))


class Sched:
    ENGS = ("pe", "act", "dve", "pool", "sp")

    def __init__(self, nc, ndma=12):
        self.nc = nc
        self.stack = ExitStack()
        self.scopes = [self.stack]
        self.sem = {}
        self.cnt = {}
        self.prog = {}
        self.seen = {}
        for e in self.ENGS:
            self.sem[e] = self.stack.enter_context(nc.semaphore("s_" + e))
            self.cnt[e] = 0
            self.prog[e] = []
            self.seen[e] = {}
        self.ep = 0
        self.dq = {}
        for q in ("sp", "pool", "act"):
            sems = [self.stack.enter_context(nc.semaphore("d_%s%d" % (q, i))) for i in range(ndma)]
            self.dq[q] = dict(sems=sems, vals=[0] * ndma, nxt=0)
        self.n_ops = 0

    def sb(self, name, shape, dtype):
        self.n_alloc = getattr(self, "n_alloc", 0) + 1
        t = self.scopes[-1].enter_context(self.nc.sbuf_tensor("t%d_%s" % (self.n_alloc, name), list(shape), dtype))
        return V(Buf(name), t[:])

    def ps(self, name, shape, dtype):
        t = self.scopes[-1].enter_context(self.nc.psum_tensor("p_" + name, list(shape), dtype))
        return V(Buf(name), t[:])

    def dram(self, name, shape, dtype, kind="Internal"):
        if DEBUG["dump"]:
            kind = "ExternalOutput"
        t = self.nc.dram_tensor(name, list(shape), dtype, kind=kind)
        return V(Buf(name), t.ap())

    def epoch(self):
        self.barrier()
        self.ep += 1
        for e in self.ENGS:
            self.sem[e] = self.stack.enter_context(self.nc.semaphore("s_%s_%d" % (e, self.ep)))
            self.cnt[e] = 0
            for k in [k for k in self.seen[e] if isinstance(k, str)]:
                del self.seen[e][k]

    def alias_acquire(self, parent, aps, name="al"):
        out = []
        for i, ap in enumerate(aps):
            b = Buf("%s_%s%d" % (parent.buf.name, name, i))
            b.lw = parent.buf.lw
            b.rd = dict(parent.buf.rd)
            out.append(V(b, ap))
        return out

    def alias_release(self, parent, children):
        for c in children:
            evs = list(c.buf.rd.values())
            if c.buf.lw is not None:
                evs.append(c.buf.lw)
            for ev in evs:
                old = parent.buf.rd.get(ev[0])
                if old is None or (old[2], old[1]) < (ev[2], ev[1]):
                    parent.buf.rd[ev[0]] = ev

    def push(self):
        self.scopes.append(ExitStack())

    def pop(self):
        self.barrier()
        self.flush()
        self.scopes.pop().close()

    def _semof(self, key):
        if isinstance(key, str):
            return self.sem[key]
        q, slot = key
        return self.dq[q]["sems"][slot]

    def _collect(self, eng, reads, writes, is_dma):
        waits = {}

        def need(ev, war=False):
            if ev is None:
                return
            key, val, dep_ep = ev
            if dep_ep < self.ep:
                return
            if not is_dma and isinstance(key, str) and key == eng:
                if eng == "pe" or war:
                    return
            if self.seen[eng].get(key, 0) >= val:
                return
            if waits.get(key, 0) < val:
                waits[key] = val

        for b in reads:
            need(b.lw)
        for b in writes:
            need(b.lw)
            for ev in b.rd.values():
                need(ev, war=True)
        return waits

    def _commit(self, eng, waits):
        for k, v in waits.items():
            self.seen[eng][k] = v
        return [(self._semof(k), v) for k, v in waits.items()]

    def _record(self, ev, reads, writes):
        for b in reads:
            b.rd[ev[0]] = ev
        for b in writes:
            b.lw = ev
            b.rd = {}

    def op(self, eng, fn, reads=(), writes=()):
        waits = self._collect(eng, reads, writes, False)
        wl = self._commit(eng, waits)
        self.cnt[eng] += 1
        ev = (eng, self.cnt[eng], self.ep)
        self.prog[eng].append((wl, fn, (self.sem[eng], 1)))
        self._record(ev, reads, writes)
        self.n_ops += 1

    def dma(self, q, out, in_, **kw):
        oap, iap = out.ap, in_.ap
        d = self.dq[q]
        slot = d["nxt"]
        d["nxt"] = (slot + 1) % len(d["sems"])
        waits = self._collect(q, [in_.buf], [out.buf], True)
        key = (q, slot)
        prev = d["vals"][slot]
        if prev and self.seen[q].get(key, 0) < prev:
            waits[key] = max(waits.get(key, 0), prev)
        wl = self._commit(q, waits)
        d["vals"][slot] = prev + 16
        ev = (key, prev + 16, self.ep)
        sem = d["sems"][slot]
        self.prog[q].append((wl, lambda e: e.dma_start(out=oap, in_=iap, **kw), (sem, 16)))
        self._record(ev, [in_.buf], [out.buf])
        self.n_ops += 1

    @staticmethod
    def _bufs(*xs):
        return [x.buf for x in xs if isinstance(x, V)]

    @staticmethod
    def _a(x):
        return x.ap if isinstance(x, V) else x

    def mm(self, out, lhsT, rhs, start=True, stop=True):
        o, l, r = out.ap, lhsT.ap, rhs.ap
        self.op("pe", lambda e: e.matmul(o, l, r, start=start, stop=stop),
                reads=self._bufs(lhsT, rhs), writes=[out.buf])

    def tr(self, out, in_, ident):
        o, i, d = out.ap, in_.ap, ident.ap
        self.op("pe", lambda e: e.transpose(o, i, d), reads=self._bufs(in_, ident), writes=[out.buf])

    def act(self, out, in_, func, bias=None, scale=None):
        kw = {}
        if bias is not None:
            kw["bias"] = self._a(bias)
        if scale is not None:
            kw["scale"] = self._a(scale)
        o, i = out.ap, in_.ap
        self.op("act", lambda e: e.activation(o, i, func, **kw),
                reads=self._bufs(in_, bias, scale), writes=[out.buf])

    def tt(self, eng, out, a, b, op):
        o, x, y = out.ap, a.ap, b.ap
        self.op(eng, lambda e: e.tensor_tensor(o, x, y, op), reads=self._bufs(a, b), writes=[out.buf])

    def ts(self, eng, out, a, s1, op0, s2=None, op1=None):
        o, x, p1, p2 = out.ap, a.ap, self._a(s1), self._a(s2)
        if op1 is None:
            self.op(eng, lambda e: e.tensor_scalar(o, x, p1, None, op0),
                    reads=self._bufs(a, s1), writes=[out.buf])
        else:
            self.op(eng, lambda e: e.tensor_scalar(o, x, p1, p2, op0, op1),
                    reads=self._bufs(a, s1, s2), writes=[out.buf])

    def stt(self, eng, out, a, sc, b, op0, op1):
        o, x, p, y = out.ap, a.ap, self._a(sc), b.ap
        eng = "dve"
        self.op(eng, lambda e: e.scalar_tensor_tensor(o, x, p, y, op0, op1),
                reads=self._bufs(a, sc, b), writes=[out.buf])

    def cp(self, eng, out, a):
        o, x = out.ap, a.ap
        if eng == "act":
            self.op("act", lambda e: e.activation(o, x, AF.Copy), reads=[a.buf], writes=[out.buf])
        else:
            self.op(eng, lambda e: e.tensor_copy(o, x), reads=[a.buf], writes=[out.buf])

    def red(self, eng, out, a, op=None):
        o, x = out.ap, a.ap
        op = ALU.add if op is None else op
        self.op(eng, lambda e: e.tensor_reduce(o, x, AX.X, op), reads=[a.buf], writes=[out.buf])

    def memset(self, eng, out, val):
        o = out.ap
        self.op(eng, lambda e: e.memset(o, val), writes=[out.buf])

    def barrier(self):
        for e in self.ENGS:
            waits = {}
            for e2 in self.ENGS:
                if e2 != e and self.cnt[e2] > self.seen[e].get(e2, 0):
                    waits[e2] = self.cnt[e2]
            if e not in ("pe",) and self.cnt[e] > self.seen[e].get(e, 0):
                waits[e] = self.cnt[e]
            for q, d in self.dq.items():
                for slot, v in enumerate(d["vals"]):
                    if v and self.seen[e].get((q, slot), 0) < v:
                        waits[(q, slot)] = v
            wl = self._commit(e, waits)
            if wl:
                self.prog[e].append((wl, None, None))

    def flush(self):
        nc = self.nc
        prog = self.prog
        self.prog = {e: [] for e in self.ENGS}

        def replay(name, e):
            for wl, fn, inc in prog[name]:
                for sem, val in wl:
                    e.wait_ge(sem, val)
                if fn is not None:
                    ins = fn(e)
                    ins.then_inc(inc[0], inc[1])

        with nc.Block() as block:
            @block.tensor
            def _(e):
                replay("pe", e)

            @block.scalar
            def _(e):
                replay("act", e)

            @block.vector
            def _(e):
                replay("dve", e)

            @block.gpsimd
            def _(e):
                replay("pool", e)

            @block.sync
            def _(e):
                replay("sp", e)

    def finish(self):
        self.barrier()
        self.flush()
        self.stack.close()


NB = 2
ALPHA = float(2.0 ** 0.25)
CRW = -float(np.exp(-0.5))
GN_EPS = 64e-5
DEBUG = {"stop_after": None, "cut": 99, "dump": False, "res": None, "dbg": False}


def host_consts():
    p = np.arange(128)[:, None]
    q = np.arange(128)[None, :]
    Us = (p < q).astype(np.float32)
    Ui = (p <= q).astype(np.float32)
    Ls = (p > q).astype(np.float32)
    Li = (p >= q).astype(np.float32)
    ident = (p == q).astype(np.float32)
    sm = []
    for l in range(7):
        sz = 2 ** l
        same = (p // (2 * sz)) == (q // (2 * sz))
        lo = same & ((p % (2 * sz)) >= sz) & ((q % (2 * sz)) < sz)
        sm.append((lo | lo.T).astype(np.float32))
    cbf = np.concatenate([ident, Us, Ui, Ls, Li, np.ones((128, 128), np.float32)] + sm, axis=1)
    cf = np.concatenate([Ui * CRW, Us * CRW, Ls * CRW, Li * CRW,
                         Ui / 16.0, Us / 16.0, Ls / 16.0, Li / 16.0,
                         np.full((128, 1), CRW, np.float32), np.full((128, 1), 1.0 / 16.0, np.float32),
                         np.arange(128, dtype=np.float32)[:, None], np.arange(128, dtype=np.float32)[:, None] + 128.0,
                         np.tile(np.arange(256, dtype=np.float32)[None, :], (128, 1))], axis=1)
    return np.ascontiguousarray(cbf), np.ascontiguousarray(cf.astype(np.float32))


def build():
    nc = bass.Bass("TRN2", target_bir_lowering=False)
    S = Sched(nc)

    def din(name, shape):
        return V(Buf(name), nc.dram_tensor(name, list(shape), F32, kind="ExternalInput").ap())

    x_d = din("x", [NB, 2048, 1024])
    ctx_d = din("ctx", [NB, 256, 1024])
    cT_d = din("cT", [128, 8, 3])
    adaw_d = din("ada_w", [1024, 6144])
    adab_d = din("ada_b", [1, 6144])
    adabT_d = din("ada_bT", [128, 48])
    win_d = din("w_in", [1024, 3328])
    mu_d = din("mu_ext", [1, 3328])
    w0_d = din("rw_w0", [2, 512])
    w2_d = din("rw_w2", [2, 32, 512])
    a0_d = din("rw_a0", [2, 512])
    a2_d = din("rw_a2", [2, 32, 512])
    g2_d = din("rw_g2", [96, 512])
    kk_d = din("rw_k_k", [1, 512])
    ka_d = din("rw_k_a", [1, 512])
    rk_d = din("rw_r_k", [1, 512])
    gnw_d = din("rw_gn_w", [1, 512])
    gnb_d = din("rw_gn_b", [1, 512])
    ga2_d = din("gla_a2", [2, 16, 256])
    gab_d = din("gla_a_b", [2, 256])
    gnorm_d = din("gla_norm_w", [1, 512])
    wout_d = din("w_out", [1024, 1024])
    ln1g_d = din("ln1_g", [1, 1024])
    ln1b_d = din("ln1_b", [1, 1024])
    rtr_d = din("router_w", [1024, 16])
    exg_d = din("ex_gate", [16, 1024, 2816])
    exu_d = din("ex_up", [16, 1024, 2816])
    exd_d = din("ex_down", [16, 2816, 1024])
    ln2g_d = din("ln2_g", [1, 1024])
    ln2b_d = din("ln2_b", [1, 1024])
    cbf_d = din("cbf", [128, 1664])
    cf_d = din("cf", [128, 8 * 128 + 4 + 256])
    eoh_d = din("eoh", [16, 16 * 128])
    out_d = V(Buf("out"), nc.dram_tensor("out", [NB, 2048, 1024], F32, kind="ExternalOutput").ap())
    mix_d = S.dram("mixs", [NB, 2048, 1024], BF16)
    yf_d = S.dram("yfs", [16, 128, 1536], F32)
    modrow_d = S.dram("modrows", [NB, 4, 1024], F32)
    dbgf_d = S.dram("dbgf", [24, 128, 512], F32)
    dbgb_d = S.dram("dbgb", [24, 128, 512], BF16)
    dbgn = {"f": 0, "b": 0, "names": []}

    def dump(name, v, bf):
        k = "b" if bf else "f"
        i = dbgn[k]
        dbgn[k] += 1
        dbgn["names"].append((name, k, i, tuple(v.ap.shape)))
        dst = (dbgb_d if bf else dbgf_d)
        p = v.ap.shape[0]
        n = 1
        for d_ in v.ap.shape[1:]:
            n *= d_
        dv = dst[i, 0:p, 0:n]
        if len(v.ap.shape) == 3:
            dv = dv.re("p (a c) -> p a c", a=v.ap.shape[1])
        S.dma("sp", dv, v)
    DEBUG["names"] = dbgn["names"]

    cb = S.sb("cb", [128, 1664], BF16)
    S.dma("pool", cb, cbf_d)
    ident, mUs, mUi, mLs, mLi = (cb[:, i * 128:(i + 1) * 128] for i in range(5))
    ones_bf = cb[:, 640:768]
    smask = [cb[:, 768 + l * 128:896 + l * 128] for l in range(7)]
    cf = S.sb("cf", [128, 8 * 128 + 4 + 256], F32)
    S.dma("sp", cf, cf_d)
    fm = [cf[:, i * 128:(i + 1) * 128] for i in range(8)]
    negcol = cf[:, 1024:1025]
    g16col = cf[:, 1025:1026]
    iota_p0 = cf[:, 1026:1027]
    iota_p1 = cf[:, 1027:1028]
    iota_f = cf[:, 1028:1284]
    modT = S.sb("modT", [128, 16, 3], F32)

    pbanks = [S.ps("pb%d" % i, [128, 512], F32) for i in range(6)]
    ptbanks = [S.ps("ptb%d" % i, [128, 1024], BF16) for i in range(2)]
    rot = {"p": 0, "t": 0}

    allbanks = pbanks + [V(t_.buf, t_.ap.bitcast(F32)) for t_ in ptbanks]

    def P():
        rot["p"] = (rot["p"] + 1) % 8
        return allbanks[rot["p"]]

    def PT():
        p_ = P()
        return V(p_.buf, p_.ap.bitcast(BF16))

    def layer_norm_stats(xt, st, mv, rstd, eps=1e-5):
        for c in range(2):
            o, i = st.ap[:, c, :], xt.ap[:, c * 512:(c + 1) * 512]
            S.op("dve", lambda e, o=o, i=i: e.bn_stats(o, i), reads=[xt.buf], writes=[st.buf])
        S.op("dve", lambda e: e.bn_aggr(mv.ap, st.ap), reads=[st.buf], writes=[mv.buf])
        S.act(rstd, mv[:, 1:2], AF.Sqrt, bias=eps)
        S.op("dve", lambda e: e.reciprocal(rstd.ap, rstd.ap), reads=[rstd.buf], writes=[rstd.buf])

    S.push()
    cT = S.sb("cT", [128, 8, 3], F32)
    S.dma("sp", cT, cT_d)
    scb = S.sb("scb", [128, 8, 3], BF16)
    S.act(scb, cT, AF.Silu)
    rep = []
    for b in range(NB):
        r_ = S.sb("rep%d" % b, [128, 8, 128], BF16)
        S.cp("dve", r_, scb[:, :, b:b + 1].bc([128, 8, 128]))
        rep.append(r_)
    adabT = S.sb("adabT", [128, 48], F32)
    S.dma("sp", adabT, adabT_d)
    blk = [S.sb("adablk%d" % i, [128, 8, 512], BF16) for i in range(2)]
    brow = [S.sb("adabrow%d" % i, [128, 512], F32) for i in range(2)]
    mrow = [S.sb("mrow%d" % i, [128, 512], F32) for i in range(2)]
    psT = P()
    psTv = psT[:, 0:48].re("p (j c) -> p j c", c=3)
    adaw_v = adaw_d.re("(kc p) c -> p kc c", p=128)
    for jb in range(12):
        bk = blk[jb % 2]
        S.dma("pool", bk, adaw_v[:, :, jb * 512:(jb + 1) * 512])
        if jb < 4:
            for jj in range(4):
                j = jb * 4 + jj
                for kc in range(8):
                    S.mm(psTv[:, j, :], bk[:, kc, jj * 128:(jj + 1) * 128], scb[:, kc, :],
                         start=(kc == 0), stop=(kc == 7))
            if jb == 3:
                for c in range(3):
                    S.tt("dve", modT[:, 0:8, c], psTv[:, 0:8, c], adabT[:, 0:8], ALU.add)
                    S.stt("dve", modT[:, 8:16, c], psTv[:, 8:16, c], 1.0, adabT[:, 8:16], ALU.add, ALU.add)
        else:
            which = (jb - 4) // 2
            half = (jb - 4) % 2
            br = brow[jb % 2]
            S.dma("sp", br, adab_d[:, jb * 512:(jb + 1) * 512].pb(128))
            for b in range(NB):
                pr = P()
                for kc in range(8):
                    S.mm(pr, rep[b][:, kc, :], bk[:, kc, :], start=(kc == 0), stop=(kc == 7))
                mr = mrow[b]
                if which == 2:
                    S.stt("dve", mr, pr, 1.0, br, ALU.add, ALU.add)
                else:
                    S.tt("dve", mr, pr, br, ALU.add)
                S.dma("sp", modrow_d[b, which:which + 1, half * 512:(half + 1) * 512], mr[0:1, :])
    S.pop()
    if DEBUG["stop_after"] == "A":
        S.finish()
        return nc

    S.push()
    Wa = S.sb("Wa", [128, 8, 3072], BF16)
    Wb = S.sb("Wb", [128, 8, 1536], BF16)
    Wl = [S.sb("Wl%d" % d, [128, 8, 96], BF16) for d in range(2)]
    Wlb = [S.sb("Wlb%d" % d, [128, 8, 96], BF16) for d in range(2)]
    Wgd = S.sb("Wgd", [128, 8, 96], BF16)
    Wgdb = S.sb("Wgdb", [128, 8, 96], BF16)
    sw = [S.sb("sw%d" % d, [96, 512], BF16) for d in range(2)]
    g2w = S.sb("g2w", [96, 512], BF16)
    br3 = S.sb("br3", [96, 2, 512], F32)
    onesf = S.sb("onesf", [96, 128], F32)
    S.memset("dve", onesf, 1.0)
    for d in range(2):
        S.dma("sp", br3[0:1, d, :], w0_d[d:d + 1, :])
        S.dma("sp", br3[32:33, d, :], a0_d[d:d + 1, :])
        S.dma("sp", br3[64:65, d, 0:256], gab_d[d:d + 1, :])
    rows = S.sb("rows", [128, 6, 512], F32)
    kkrow, karow, rkrow, gnwrow, gnbrow, gnormrow = (rows[:, i, :] for i in range(6))
    for i, dsrc in enumerate((kk_d, ka_d, rk_d, gnw_d, gnb_d, gnorm_d)):
        S.dma("sp", rows[:, i, :], dsrc.pb(128))
    for d in range(2):
        S.dma("pool", sw[d][0:32, :], w2_d[d])
        S.dma("pool", sw[d][32:64, :], a2_d[d])
        S.dma("pool", sw[d][64:80, 0:256], ga2_d[d])
    S.dma("pool", g2w, g2_d)

    S.push()
    mub = S.sb("mub", [128, 3328], F32)
    omm = S.sb("omm", [128, 3328], F32)
    S.dma("sp", mub, mu_d.pb(128))
    S.ts("dve", omm, mub, -1.0, ALU.mult, 1.0, ALU.add)
    stg = [S.sb("stg%d" % i, [128, 3328], F32) for i in range(2)]
    for kc in range(8):
        st_ = stg[kc % 2]
        S.dma("sp", st_, win_d[kc * 128:(kc + 1) * 128, :])
        S.tt("dve", Wa[:, kc, 0:1536], st_[:, 0:1536], omm[:, 0:1536], ALU.mult)
        S.tt("dve", Wa[:, kc, 1536:3072], st_[:, 1760:3296], omm[:, 1760:3296], ALU.mult)
        S.tt("pool", Wb[:, kc, :], st_[:, 0:1536], mub[:, 0:1536], ALU.mult)
        for d in range(2):
            for (o0, c0, n) in ((0, 1536 + 32 * d, 32), (32, 1600 + 32 * d, 32), (64, 3296 + 16 * d, 16)):
                S.tt("pool", Wl[d][:, kc, o0:o0 + n], st_[:, c0:c0 + n], omm[:, c0:c0 + n], ALU.mult)
            for (o0, c0, n) in ((0, 1536 + 32 * d, 32), (32, 1600 + 32 * d, 32)):
                S.tt("pool", Wlb[d][:, kc, o0:o0 + n], st_[:, c0:c0 + n], mub[:, c0:c0 + n], ALU.mult)
        S.tt("pool", Wgd[:, kc, :], st_[:, 1664:1760], omm[:, 1664:1760], ALU.mult)
        S.tt("pool", Wgdb[:, kc, :], st_[:, 1664:1760], mub[:, 1664:1760], ALU.mult)
    S.pop()
    for d in range(2):
        S.memset("pool", Wl[d][:, :, 80:96], 0.0)
        S.memset("pool", Wlb[d][:, :, 64:96], 0.0)

    def f32t(name, n=512, p=128):
        return S.sb(name, [p, n], F32)

    def bft(name, n=512, p=128):
        return S.sb(name, [p, n], BF16)

    xring = [f32t("xr%d" % i, 1024) for i in range(1)]
    hring = [S.sb("hT%d" % i, [128, 8, 128], BF16) for i in range(3)]
    hnb = S.sb("hnb", [128, 8, 128], BF16)
    st6 = S.sb("st6", [128, 2, 6], F32)
    mv2 = S.sb("mv2", [128, 2], F32)
    rstd1 = S.sb("rstd1", [128, 1], F32)
    r_sb, k_sb, v_sb, qk_sb = f32t("r_sb"), f32t("k_sb"), f32t("v_sb"), f32t("qk_sb")
    v_bf, vg_bf = bft("v_bf"), bft("vg_bf")
    lo = S.sb("lo", [96, 128], BF16)
    lo_gd = S.sb("lo_gd", [96, 128], BF16)
    sg, a_sb, lg = f32t("sg"), f32t("a_sb"), f32t("lg", 256)
    E1, E2, E3, E4 = f32t("E1"), f32t("E2"), f32t("E3"), f32t("E4")
    t0, t1, kk, kka, kh = f32t("t0"), f32t("t1"), f32t("kk"), f32t("kka"), f32t("kh")
    xn_bf = V(t0.buf, t0.ap.bitcast(BF16))
    nbacc = V(t1.buf, t1.ap.bitcast(BF16)).re("p (k t) -> p k t", k=8)
    ss = S.sb("ss", [128, 8], F32)
    bs = S.sb("bs", [128, 8], F32)
    pc_sb = S.sb("pc_sb", [64, 8], F32)
    pcg_sb = S.sb("pcg_sb", [64, 4], F32)
    At_bf, Bt_bf, Kt_bf, Rt_bf, Bh_bf, Kh_bf = (bft(n) for n in ("At", "Bt", "Kt", "Rt", "Bh", "Kh"))
    qt_bf, kt_bf, khg_bf = bft("qt", 256), bft("kt", 256), bft("khg", 256)

    def ht(name, p=128):
        return S.sb(name, [p, 4, 128], BF16)

    AtT, BtT, KtT, RtT = ht("AtT", 64), ht("BtT", 64), ht("KtT", 64), ht("RtT", 64)
    LakT, ArbT, ArkT, AqkT = ht("LakT"), ht("ArbT"), ht("ArkT"), ht("AqkT")
    Xr = [ht("Xr0"), ht("Xr1")]
    XTr = [ht("XTr0"), ht("XTr1")]
    Zr = [ht("Zr0"), ht("Zr1")]
    def half_aps(t):
        tb = t.ap.bitcast(BF16)
        return [tb[:, i * 512:(i + 1) * 512].rearrange("p (h t) -> p h t", h=4) for i in range(2)]
    QeffT, qtT, ktT = ht("QeffT", 64), ht("qtT", 64), ht("ktT", 64)
    HT = S.sb("HT", [64, 4, 64], BF16)
    M_f = [S.sb("M_f%d" % d, [64, 8, 64], F32) for d in range(2)]
    M_bf = [S.sb("M_bf%d" % d, [64, 8, 64], BF16) for d in range(2)]
    Mg_f = [S.sb("Mg_f%d" % d, [64, 4, 128], F32) for d in range(2)]
    Mg_bf = [S.sb("Mg_bf%d" % d, [64, 4, 128], BF16) for d in range(2)]
    y_sb, bo_sb, o_sb = f32t("y_sb"), f32t("bo_sb"), f32t("o_sb")
    yfl = f32t("yfl", 1536)
    gout_sb, gsil = sg, a_sb
    mix_bf = bft("mix_bf", 1024)
    s8a, s8b, s8c, s8d = (S.sb("s8%s" % n, [128, 8], F32) for n in "abcd")

    if DEBUG.get("verbose"):
        print("mixer SBUF bytes remaining per partition:", nc.sbuf_bytes_remaining)

    def v4(pbank, p=128, w=128):
        return pbank[0:p, 0:4 * w].re("p (h t) -> p h t", h=4)

    def proj_block(dst, hT, hn, c0, ncols, rw):
        n = 16 if rw else 8
        i = 0
        for kc in range(8):
            S.mm(dst[:, 0:ncols], hT[:, kc, :], Wa[:, kc, c0:c0 + ncols], start=(i == 0), stop=(i == n - 1))
            i += 1
        if rw:
            for kc in range(8):
                S.mm(dst[:, 0:ncols], hn[:, kc, :], Wb[:, kc, c0:c0 + ncols], start=False, stop=(i == n - 1))
                i += 1

    def produce(b, kind, idx):
        xt = xring[0]
        src = ctx_d[b, idx * 128:(idx + 1) * 128, :] if kind == "c" else x_d[b, idx * 128:(idx + 1) * 128, :]
        S.dma("sp", xt, src)
        layer_norm_stats(xt, st6, mv2, rstd1)
        S.ts("dve", xn_bf, xt, mv2[:, 0:1], ALU.subtract, rstd1, ALU.mult)
        pt = PT()
        ptv = pt.re("p (k t) -> p k t", k=8)
        col = 2 if kind == "c" else b
        hs = hring[idx % 3]
        for kc in range(8):
            S.tr(ptv[:, kc, :], xn_bf[:, kc * 128:(kc + 1) * 128], ident)
        for kc in range(8):
            S.act(hs[:, kc, :], ptv[:, kc, :], AF.Identity, bias=modT[:, kc, col:col + 1],
                  scale=modT[:, 8 + kc, col:col + 1])

    def neighbor(kind, idx, n_tiles):
        C = hring[idx % 3]
        Pv = hring[(idx - 1) % 3] if idx > 0 else None
        Nx = hring[(idx + 1) % 3] if idx < n_tiles - 1 else None
        if kind == "l":
            if Pv is not None:
                S.tt("dve", nbacc[:, :, 0:64], C[:, :, 64:128], Pv[:, :, 64:128], ALU.add)
            else:
                S.cp("dve", nbacc[:, :, 0:64], C[:, :, 64:128])
            if Nx is not None:
                S.tt("dve", nbacc[:, :, 64:128], C[:, :, 0:64], Nx[:, :, 0:64], ALU.add)
            else:
                S.cp("dve", nbacc[:, :, 64:128], C[:, :, 0:64])
            a4 = nbacc.re("p k (r c) -> p k r c", r=2)
            c4 = C.re("p k (r c) -> p k r c", r=2)
            S.tt("dve", a4[:, :, :, 1:64], a4[:, :, :, 1:64], c4[:, :, :, 0:63], ALU.add)
            S.tt("dve", a4[:, :, :, 0:63], a4[:, :, :, 0:63], c4[:, :, :, 1:64], ALU.add)
            S.ts("dve", hnb, nbacc, 0.25, ALU.mult)
        else:
            S.cp("dve", nbacc[:, :, 1:128], C[:, :, 0:127])
            if Pv is not None:
                S.cp("dve", nbacc[:, :, 0:1], Pv[:, :, 127:128])
            else:
                S.memset("dve", nbacc[:, :, 0:1], 0.0)
            S.tt("dve", nbacc[:, :, 0:127], nbacc[:, :, 0:127], C[:, :, 1:128], ALU.add)
            if Nx is not None:
                S.tt("dve", nbacc[:, :, 127:128], nbacc[:, :, 127:128], Nx[:, :, 0:1], ALU.add)
            S.ts("dve", hnb, nbacc, 0.5, ALU.mult)

    def mix_tile(b, kind, idx, d, want_out):
        hT = hring[idx % 3]
        final = want_out and d == 1
        if d == 0:
            mi, me, mr = fm[0], fm[1], fm[2]
            gmi, gmr = fm[4], fm[6]
            m_ij_s, m_ji_s, m_ji_i = mLs, mUs, mUi
        else:
            mi, me, mr = fm[3], fm[2], fm[1]
            gmi, gmr = fm[7], fm[5]
            m_ij_s, m_ji_s, m_ji_i = mUs, mLs, mLi
        pl = P()
        for kc in range(8):
            S.mm(pl[0:96, 0:128], Wl[d][:, kc, :], hT[:, kc, :], start=(kc == 0), stop=False)
        for kc in range(8):
            S.mm(pl[0:96, 0:128], Wlb[d][:, kc, :], hnb[:, kc, :], start=False, stop=(kc == 7))
        S.act(lo[0:32, :], pl[0:32, 0:128], AF.Tanh)
        S.act(lo[32:64, :], pl[32:64, 0:128], AF.Copy)
        S.act(lo[64:80, :], pl[64:80, 0:128], AF.Copy)
        pr_ = P(); proj_block(pr_, hT, hnb, 0, 512, True)
        pw = P()
        S.mm(pw, lo[0:32, :], sw[d][0:32, :], start=True, stop=False)
        S.mm(pw, onesf[0:1, :], br3[0:1, d, :], start=False, stop=True)
        pa = P()
        S.mm(pa, lo[32:64, :], sw[d][32:64, :], start=True, stop=False)
        S.mm(pa, onesf[32:33, :], br3[32:33, d, :], start=False, stop=True)
        pg = P()
        S.mm(pg[:, 0:256], lo[64:80, :], sw[d][64:80, 0:256], start=True, stop=False)
        S.mm(pg[:, 0:256], onesf[64:65, :], br3[64:65, d, 0:256], start=False, stop=True)
        S.act(sg, pw, AF.Sigmoid)
        S.act(a_sb, pa, AF.Sigmoid)
        S.act(lg, pg[:, 0:256], AF.Sigmoid)
        S.cp("act", r_sb, pr_)
        pk_ = P(); proj_block(pk_, hT, hnb, 512, 512, True)
        pc1 = P(); S.mm(pc1, mi, sg)
        pc2 = P(); S.mm(pc2, me, sg)
        S.act(lg, lg, AF.Ln)
        S.act(E1, pc1, AF.Exp); S.act(E2, pc1, AF.Exp, scale=-1.0)
        S.act(E3, pc2, AF.Exp)
        S.cp("act", k_sb, pk_)
        S.tt("dve", t0, k_sb, kkrow, ALU.mult)
        S.tt("pool", t1, t0, t0, ALU.mult)
        S.red("dve", ss, t1.re("p (h c) -> p h c", h=8))
        S.act(ss, ss, AF.Ln, bias=1e-12)
        S.act(ss, ss, AF.Exp, scale=-0.5)
        S.tt("dve", kk.re("p (h c) -> p h c", h=8), t0.re("p (h c) -> p h c", h=8), ss.us(2).bc([128, 8, 64]), ALU.mult)
        pc3 = P(); S.mm(pc3, mr, sg)
        pp = P()
        for h in range(8):
            S.mm(pp[0:64, h:h + 1], sg[:, h * 64:(h + 1) * 64], negcol)
        for h in range(4):
            S.mm(pp[0:64, 8 + h:9 + h], lg[:, h * 64:(h + 1) * 64], g16col)
        pv_ = P(); proj_block(pv_, hT, hnb, 1024, 512, True)
        S.act(E4, pc3, AF.Exp)
        S.act(pc_sb, pp[0:64, 0:8], AF.Exp)
        S.act(pcg_sb, pp[0:64, 8:12], AF.Exp)
        S.cp("act", v_sb, pv_); S.cp("pool", v_bf, v_sb)
        p = P(); proj_block(p, hT, hnb, 1536, 512, False); S.cp("act", qk_sb, p)
        p = P(); proj_block(p, hT, hnb, 2048, 512, False); S.cp("act", vg_bf, p)
        if DEBUG["cut"] <= 2:
            return
        h8 = lambda t: t.re("p (h c) -> p h c", h=8)
        S.tt("pool", kka, kk, a_sb, ALU.mult)
        S.stt("pool", t1, a_sb, -1.0, karow, ALU.add, ALU.mult)
        S.stt("pool", kh, t1, 1.0, k_sb, ALU.add, ALU.mult)
        S.stt("dve", At_bf, kk, -1.0, E3, ALU.mult, ALU.mult)
        S.tt("dve", Bt_bf, kka, E2, ALU.mult)
        S.tt("dve", Kt_bf, kh, E2, ALU.mult)
        S.tt("dve", Rt_bf, r_sb, E1, ALU.mult)
        S.tt("pool", Bh_bf, kka, E4, ALU.mult)
        S.tt("pool", Kh_bf, kh, E4, ALU.mult)
        if want_out:
            S.tt("pool", t0, r_sb, kh, ALU.mult)
            S.tt("pool", t0, t0, rkrow, ALU.mult)
            S.red("dve", bs, h8(t0))
            S.tt("pool", h8(bo_sb), h8(v_sb), bs.us(2).bc([128, 8, 64]), ALU.mult)
        if DEBUG["cut"] <= 3:
            return
        dbg = DEBUG["dbg"] and b == 0 and kind == "c" and idx == 0 and d == 0
        if dbg:
            for nm, t_ in (("E1", E1), ("E2", E2), ("E3", E3), ("E4", E4), ("kk", kk), ("a", a_sb), ("sg", sg), ("kh", kh),
                           ("r", r_sb), ("v", v_sb)):
                dump(nm, t_, False)
            for nm, t_ in (("At", At_bf), ("Bt", Bt_bf), ("Kt", Kt_bf), ("Rt", Rt_bf), ("Bh", Bh_bf), ("Kh", Kh_bf), ("vb", v_bf)):
                dump(nm, t_, True)
            dump("pc", pc_sb, False)
        G1, G2, G4 = E1, E2, E3
        pg1 = P(); S.mm(pg1[:, 0:256], gmi, lg)
        pg3 = P(); S.mm(pg3[:, 0:256], gmr, lg)
        S.act(G1[:, 0:256], pg1[:, 0:256], AF.Exp)
        S.act(G2[:, 0:256], pg1[:, 0:256], AF.Exp, scale=-1.0)
        S.act(G4[:, 0:256], pg3[:, 0:256], AF.Exp)
        S.stt("dve", qt_bf, qk_sb[:, 0:256], 0.125, G1[:, 0:256], ALU.mult, ALU.mult)
        S.tt("dve", kt_bf, qk_sb[:, 256:512], G2[:, 0:256], ALU.mult)
        S.tt("pool", khg_bf, qk_sb[:, 256:512], G4[:, 0:256], ALU.mult)
        if DEBUG["cut"] <= 4:
            return
        parents = [E1, E2, E3, E4, sg, a_sb, t0, t1, kk, kka, kh, k_sb, qk_sb, r_sb]
        kids = [S.alias_acquire(pt_, half_aps(pt_)) for pt_ in parents]
        flat = [c for pair in kids[3:] for c in pair]
        G = [dict(AtT=AtT, BtT=BtT, KtT=KtT, RtT=RtT, X0=Xr[0], XT0=XTr[0], Lm=Xr[1], LTm=XTr[1],
                  LakT=LakT, ArbT=ArbT, ArkT=ArkT, Z0=Zr[0], Zf=Zr[1], D=kids[0], DT=kids[1],
                  Y1=kids[2][0], Y1t=kids[2][1], QeffT=QeffT, HT=HT),
             dict(AtT=flat[0][0:64], BtT=flat[1][0:64], KtT=flat[2][0:64], RtT=flat[3][0:64],
                  X0=flat[4], XT0=flat[5], Lm=flat[6], LTm=flat[7], LakT=flat[8], ArbT=flat[9], ArkT=flat[10],
                  Z0=flat[11], Zf=flat[12], D=[flat[13], flat[14]], DT=[flat[15], flat[16]],
                  Y1=flat[17], Y1t=flat[18], QeffT=flat[19][0:64], HT=flat[20][0:64, :, 0:64])]
        identb = ident.us(1).bc([128, 4, 128])

        def score(dst, L, R, mask):
            p_ = P()
            pv = v4(p_)
            for hh in range(4):
                S.mm(pv[:, hh, :], L[0:64, hh, :], R[0:64, hh, :])
            S.tt("dve", dst, pv, mask.us(1).bc([128, 4, 128]), ALU.mult)

        for g in range(2):
            T = G[g]
            for src, key, eng in ((At_bf, "AtT", "act"), (Bt_bf, "BtT", "dve"), (Kt_bf, "KtT", "act"), (Rt_bf, "RtT", "dve")):
                pt = PT()
                ptv = v4(pt, 64)
                for hh in range(4):
                    h = 4 * g + hh
                    S.tr(ptv[:, hh, :], src[:, h * 64:(h + 1) * 64], ident)
                S.cp(eng, T[key], ptv)
        for g in range(2):
            T = G[g]
            score(T["X0"], T["AtT"], T["BtT"], m_ij_s)
            score(T["XT0"], T["BtT"], T["AtT"], m_ji_s)
            score(T["LakT"], T["KtT"], T["AtT"], m_ji_s)
            score(T["ArbT"], T["BtT"], T["RtT"], m_ji_i)
            score(T["ArkT"], T["KtT"], T["RtT"], m_ji_i)
        for g in range(2):
            T = G[g]
            p = P()
            pv = p[:, 0:256].re("p (h v) -> p h v", h=4)
            for hh in range(4):
                h = 4 * g + hh
                S.mm(pv[:, hh, :], T["LakT"][:, hh, :], v_bf[:, h * 64:(h + 1) * 64])
            S.cp("act", T["Z0"][:, :, 0:64], pv)
            S.cp("pool", T["Z0"][:, :, 64:128], At_bf[:, g * 256:(g + 1) * 256].re("p (h c) -> p h c", h=4))
            m0 = smask[0].us(1).bc([128, 4, 128])
            S.tt("pool", T["Lm"], T["X0"], m0, ALU.mult)
            S.tt("pool", T["LTm"], T["XT0"], m0, ALU.mult)
            S.tt("dve", T["D"][0], T["Lm"], identb, ALU.add)
            S.tt("dve", T["DT"][0], T["LTm"], identb, ALU.add)
        U16 = mybir.dt.uint16
        mk_bf = flat[21]
        mk = V(mk_bf.buf, mk_bf.ap.bitcast(U16).rearrange("p h t -> p (h t)"))
        for l in range(1, 7):
            S.cp("pool", mk_bf, smask[l].us(1).bc([128, 4, 128]))
            for g in range(2):
                T = G[g]
                Dc, DTc = T["D"][0], T["DT"][0]
                p = P(); pv = v4(p)
                for hh in range(4):
                    S.mm(pv[:, hh, :], T["XT0"][:, hh, :], Dc[:, hh, :])
                S.cp("act", T["Y1"], pv)
                p = P(); pv = v4(p)
                for hh in range(4):
                    S.mm(pv[:, hh, :], T["X0"][:, hh, :], DTc[:, hh, :])
                S.cp("act", T["Y1t"], pv)
            for g in range(2):
                T = G[g]
                Dc, DTc = T["D"][0], T["DT"][0]
                p1 = P(); pv1 = v4(p1)
                for hh in range(4):
                    S.mm(pv1[:, hh, :], DTc[:, hh, :], T["Y1"][:, hh, :])
                p2 = P(); pv2 = v4(p2)
                for hh in range(4):
                    S.mm(pv2[:, hh, :], Dc[:, hh, :], T["Y1t"][:, hh, :])
                for dst, pvx in ((Dc, p1), (DTc, p2)):
                    o_, m_, d_ = dst.ap.rearrange("p h t -> p (h t)"), mk.ap, pvx.ap[:, 0:512]
                    S.op("dve", lambda e, o_=o_, m_=m_, d_=d_: e.copy_predicated(o_, m_, d_),
                         reads=[pvx.buf, mk.buf, dst.buf], writes=[dst.buf])
        for g in range(2):
            T = G[g]
            DTc = T["DT"][0]
            p = P(); pv = v4(p)
            for hh in range(4):
                S.mm(pv[:, hh, :], DTc[:, hh, :], T["Z0"][:, hh, :])
            S.cp("act" if g == 0 else "dve", T["Zf"], pv)
        for g in range(2):
            T = G[g]
            Zf = T["Zf"]
            p = P()
            pv = v4(p, 64)
            for hh in range(4):
                h = 4 * g + hh
                S.mm(pv[:, hh, :], Rt_bf[:, h * 64:(h + 1) * 64], ident, start=True, stop=False)
                S.mm(pv[:, hh, :], Zf[:, hh, 64:128], T["ArbT"][:, hh, :], start=False, stop=True)
            S.cp("act", T["QeffT"], pv)
            p = P()
            pvh = p[0:64, 0:256].re("p (h k) -> p h k", h=4)
            for hh in range(4):
                h = 4 * g + hh
                S.mm(pvh[:, hh, :], Zf[:, hh, 64:128], Bh_bf[:, h * 64:(h + 1) * 64])
            S.cp("dve", T["HT"], pvh)
        for g in range(2):
            T = G[g]
            Zf = T["Zf"]
            if want_out:
                p = P()
                pv = p[:, 0:256].re("p (h v) -> p h v", h=4)
                for hh in range(4):
                    h = 4 * g + hh
                    S.mm(pv[:, hh, :], T["QeffT"][:, hh, :], M_bf[d][:, h, :], start=True, stop=False)
                    S.mm(pv[:, hh, :], T["ArbT"][:, hh, :], Zf[:, hh, 0:64], start=False, stop=False)
                    S.mm(pv[:, hh, :], T["ArkT"][:, hh, :], v_bf[:, h * 64:(h + 1) * 64], start=False, stop=True)
                S.cp("act", y_sb[:, g * 256:(g + 1) * 256], p[:, 0:256])
            p = P()
            pvm = p[0:64, 0:256].re("p (h v) -> p h v", h=4)
            for hh in range(4):
                h = 4 * g + hh
                S.mm(pvm[:, hh, :], T["HT"][:, hh, :], M_bf[d][:, h, :], start=True, stop=False)
                S.mm(pvm[:, hh, :], Bh_bf[:, h * 64:(h + 1) * 64], Zf[:, hh, 0:64], start=False, stop=False)
                S.mm(pvm[:, hh, :], Kh_bf[:, h * 64:(h + 1) * 64], v_bf[:, h * 64:(h + 1) * 64], start=False, stop=True)
            Mv = M_f[d][:, 4 * g:4 * g + 4, :]
            S.tt("dve", Mv, Mv, pc_sb[:, 4 * g:4 * g + 4].us(2).bc([64, 4, 64]), ALU.mult)
            S.tt("dve", Mv, Mv, pvm, ALU.add)
            S.cp("dve", M_bf[d][:, 4 * g:4 * g + 4, :], Mv)
        for pt_, ch in zip(parents, kids):
            S.alias_release(pt_, ch)
        for src, dstT, eng in ((qt_bf, qtT, "act"), (kt_bf, ktT, "dve")):
            pt = PT()
            ptv = v4(pt, 64)
            for hh in range(4):
                S.tr(ptv[:, hh, :], src[:, hh * 64:(hh + 1) * 64], ident)
            S.cp(eng, dstT, ptv)
        p = P()
        pv = v4(p)
        for hh in range(4):
            S.mm(pv[:, hh, :], ktT[:, hh, :], qtT[:, hh, :])
        S.tt("dve", AqkT, pv, m_ji_i.us(1).bc([128, 4, 128]), ALU.mult)
        if want_out:
            p = P()
            pv = v4(p)
            for hh in range(4):
                S.mm(pv[:, hh, :], AqkT[:, hh, :], vg_bf[:, hh * 128:(hh + 1) * 128], start=True, stop=False)
                S.mm(pv[:, hh, :], qtT[:, hh, :], Mg_bf[d][:, hh, :], start=False, stop=True)
            S.cp("act", o_sb, p)
        p = P()
        pvg = v4(p, 64)
        for hh in range(4):
            S.mm(pvg[:, hh, :], khg_bf[:, hh * 64:(hh + 1) * 64], vg_bf[:, hh * 128:(hh + 1) * 128])
        S.tt("dve", Mg_f[d], Mg_f[d], pcg_sb.us(2).bc([64, 4, 128]), ALU.mult)
        S.tt("dve", Mg_f[d], Mg_f[d], pvg, ALU.add)
        S.cp("dve", Mg_bf[d], Mg_f[d])
        if not want_out:
            return
        if d == 0:
            S.dma("sp", yf_d[idx, :, 0:512], y_sb)
            S.dma("sp", yf_d[idx, :, 512:1024], bo_sb)
            S.dma("sp", yf_d[idx, :, 1024:1536], o_sb)
            return
        pl2 = P()
        for kc in range(8):
            S.mm(pl2[0:96, 0:128], Wgd[:, kc, :], hT[:, kc, :], start=(kc == 0), stop=False)
        for kc in range(8):
            S.mm(pl2[0:96, 0:128], Wgdb[:, kc, :], hnb[:, kc, :], start=False, stop=(kc == 7))
        S.act(lo_gd, pl2[0:96, 0:128], AF.Sigmoid)
        pgo = P()
        S.mm(pgo, lo_gd, g2w)
        S.cp("act", gout_sb, pgo)
        p = P(); proj_block(p, hT, hnb, 2560, 512, False)
        S.act(gsil, p, AF.Silu)
        S.dma("sp", yfl, yf_d[idx])
        S.tt("dve", y_sb, y_sb, yfl[:, 0:512], ALU.add)
        S.red("dve", s8a, h8(y_sb))
        S.tt("pool", t0, y_sb, y_sb, ALU.mult)
        S.red("dve", s8b, h8(t0))
        S.ts("dve", s8a, s8a, 1.0 / 64.0, ALU.mult)
        S.tt("dve", s8c, s8a, s8a, ALU.mult)
        S.stt("dve", s8b, s8b, 1.0 / 64.0, s8c, ALU.mult, ALU.subtract)
        S.act(s8b, s8b, AF.Sqrt, bias=GN_EPS)
        S.op("dve", lambda e: e.reciprocal(s8b.ap, s8b.ap), reads=[s8b.buf], writes=[s8b.buf])
        S.tt("dve", h8(y_sb), h8(y_sb), s8a.us(2).bc([128, 8, 64]), ALU.subtract)
        S.tt("dve", h8(y_sb), h8(y_sb), s8b.us(2).bc([128, 8, 64]), ALU.mult)
        S.tt("dve", y_sb, y_sb, gnwrow, ALU.mult)
        S.tt("dve", y_sb, y_sb, gnbrow, ALU.add)
        S.tt("dve", y_sb, y_sb, bo_sb, ALU.add)
        S.tt("dve", y_sb, y_sb, yfl[:, 512:1024], ALU.add)
        S.tt("dve", mix_bf[:, 0:512], y_sb, gout_sb, ALU.mult)
        S.tt("dve", o_sb, o_sb, yfl[:, 1024:1536], ALU.add)
        S.tt("pool", t0, o_sb, o_sb, ALU.mult)
        S.red("dve", s8d[:, 0:4], t0.re("p (h c) -> p h c", h=4))
        S.act(s8d[:, 0:4], s8d[:, 0:4], AF.Sqrt, bias=1e-5, scale=1.0 / 128.0)
        S.op("dve", lambda e: e.reciprocal(s8d.ap[:, 0:4], s8d.ap[:, 0:4]), reads=[s8d.buf], writes=[s8d.buf])
        o4 = o_sb.re("p (h c) -> p h c", h=4)
        S.tt("dve", o4, o4, s8d[:, 0:4].us(2).bc([128, 4, 128]), ALU.mult)
        S.tt("dve", o_sb, o_sb, gnormrow, ALU.mult)
        S.tt("dve", mix_bf[:, 512:1024], o_sb, gsil, ALU.mult)
        S.dma("sp", mix_d[b, idx * 128:(idx + 1) * 128, :], mix_bf)

    if DEBUG["stop_after"] == "Bs":
        S.pop()
        S.finish()
        return nc
    for b in range(NB):
        for d in range(2):
            S.memset("dve", M_f[d], 0.0)
            S.memset("dve", M_bf[d], 0.0)
            S.memset("pool", Mg_f[d], 0.0)
            S.memset("pool", Mg_bf[d], 0.0)
        for d in range(2):
            for kind, n_t in (("c", 2), ("l", 16)):
                order = list(range(n_t)) if d == 0 else list(range(n_t - 1, -1, -1))
                step = 1 if d == 0 else -1
                produce(b, kind, order[0])
                for i, idx in enumerate(order):
                    nxt = idx + step
                    if 0 <= nxt < n_t:
                        produce(b, kind, nxt)
                    neighbor(kind, idx, n_t)
                    mix_tile(b, kind, idx, d, kind == "l")
                    if DEBUG["stop_after"] == "B1":
                        break
                if DEBUG["stop_after"] == "B1":
                    break
    S.pop()
    if DEBUG["stop_after"] in ("B", "B1"):
        S.finish()
        return nc
    return moe_phase(nc, S, locals())


def moe_phase(nc, S, L):
    out_d, modrow_d = L["out_d"], L["modrow_d"]
    rtr_d, exg_d, exu_d, exd_d = L["rtr_d"], L["exg_d"], L["exu_d"], L["exd_d"]
    ln2g_d, ln2b_d, eoh_d = L["ln2g_d"], L["ln2b_d"], L["eoh_d"]
    ident, mUi, ones_bf = L["ident"], L["mUi"], L["ones_bf"]
    iota_f = L["iota_f"]
    pbanks, ptbanks = L["pbanks"], L["ptbanks"]
    layer_norm_stats = L["layer_norm_stats"]
    rot = {"p": 0, "t": 0}

    def P():
        rot["p"] = (rot["p"] + 1) % 2
        return pbanks[4 + rot["p"]]

    def PT():
        rot["t"] = (rot["t"] + 1) % 2
        return ptbanks[rot["t"]]

    Y = pbanks[0:4]
    exg_v = [exg_d[e].re("(kc p) f -> p kc f", p=128) for e in range(16)]
    exu_v = [exu_d[e].re("(kc p) f -> p kc f", p=128) for e in range(16)]
    exd_v = [exd_d[e].re("(fc p) d -> p fc d", p=128) for e in range(16)]

    x_d, mix_d, wout_d, ln1g_d, ln1b_d = L["x_d"], L["mix_d"], L["wout_d"], L["ln1g_d"], L["ln1b_d"]
    for b in range(NB):
        S.epoch()
        S.push()
        h2 = S.sb("h2", [128, 16, 1024], BF16)
        acc = S.sb("acc", [128, 16, 1024], F32)
        mrows = S.sb("mrows", [128, 3, 1024], F32)
        S.dma("sp", mrows[:, 0, :], modrow_d[b, 1:2, :].pb(128))
        S.dma("sp", mrows[:, 1, :], modrow_d[b, 2:3, :].pb(128))
        S.dma("sp", mrows[:, 2, :], modrow_d[b, 3:4, :].pb(128))
        identf = S.sb("identf", [128, 128], F32)
        S.cp("dve", identf, ident)
        aff_tok = S.sb("aff_tok", [128, 16, 16], F32)
        mask_tok = S.sb("mask_tok", [128, 16, 16], BF16)
        gate_tok = S.sb("gate_tok", [128, 16, 16], BF16)
        slot_tok = S.sb("slot_tok", [128, 16, 16], F32)
        m8 = S.sb("m8", [16, 8], F32)
        st6 = S.sb("mst6", [128, 2, 6], F32)
        mv2 = S.sb("mmv2", [128, 2], F32)
        rstd1 = S.sb("mrstd1", [128, 1], F32)
        xt = [S.sb("mx%d" % i, [128, 1024], F32) for i in range(2)]

        S.push()
        affT = S.sb("affT", [16, 2048], F32)
        wk = S.sb("wk", [16, 2048], F32)
        S.push()
        woutg = S.sb("woutg", [128, 8, 1024], BF16)
        g1row = S.sb("g1row", [128, 1024], F32)
        wstg = [S.sb("wstg%d" % i, [128, 1024], F32) for i in range(2)]
        lnrows = S.sb("lnrows", [128, 2, 1024], F32)
        S.dma("sp", lnrows[:, 0, :], ln1g_d.pb(128))
        S.dma("sp", lnrows[:, 1, :], ln1b_d.pb(128))
        S.dma("sp", g1row, modrow_d[b, 0:1, :].pb(128))
        for kc in range(8):
            ws = wstg[kc % 2]
            S.dma("sp", ws, wout_d[kc * 128:(kc + 1) * 128, :])
            S.tt("pool", woutg[:, kc, :], ws, g1row, ALU.mult)
        rtr = S.sb("rtr", [128, 8, 16], BF16)
        S.dma("pool", rtr, rtr_d.re("(kc p) e -> p kc e", p=128))
        mixt = [S.sb("mixt%d" % i, [128, 1024], BF16) for i in range(2)]
        mixT = S.sb("mixT", [128, 8, 128], BF16)
        x1t = S.sb("x1t", [128, 1024], F32)
        xnf = S.sb("xnf", [128, 1024], F32)
        h2T = S.sb("h2T", [128, 8, 128], BF16)
        sm1 = S.sb("sm1", [128, 1], F32)
        sm2 = S.sb("sm2", [128, 1], F32)
        nmr = S.sb("nmr", [128, 1], F32)
        lgt = S.sb("lgt", [128, 16], F32)
        for t in range(16):
            x = xt[t % 2]
            mt = mixt[t % 2]
            S.dma("sp", x, x_d[b, t * 128:(t + 1) * 128, :])
            S.dma("sp", mt, mix_d[b, t * 128:(t + 1) * 128, :])
            pt = PT()
            ptv = pt.re("p (k t) -> p k t", k=8)
            for kc in range(8):
                S.tr(ptv[:, kc, :], mt[:, kc * 128:(kc + 1) * 128], ident)
            S.cp("act", mixT, ptv)
            for half in range(2):
                pp_ = P()
                for kc in range(8):
                    S.mm(pp_, mixT[:, kc, :], woutg[:, kc, half * 512:(half + 1) * 512], start=(kc == 0), stop=(kc == 7))
                S.stt("dve", x1t[:, half * 512:(half + 1) * 512], x[:, half * 512:(half + 1) * 512], ALPHA, pp_,
                      ALU.mult, ALU.add)
            layer_norm_stats(x1t, st6, mv2, rstd1)
            S.stt("dve", nmr, mv2[:, 0:1], -1.0, rstd1, ALU.mult, ALU.mult)
            S.act(x1t, x1t, AF.Identity, bias=nmr, scale=rstd1)
            S.tt("dve", x1t, x1t, lnrows[:, 0, :], ALU.mult)
            S.tt("dve", x1t, x1t, lnrows[:, 1, :], ALU.add)
            S.act(acc[:, t, :], x1t, AF.Copy, scale=ALPHA)
            layer_norm_stats(x1t, st6, mv2, rstd1)
            S.stt("dve", nmr, mv2[:, 0:1], -1.0, rstd1, ALU.mult, ALU.mult)
            S.act(xnf, x1t, AF.Identity, bias=nmr, scale=rstd1)
            S.tt("dve", xnf, xnf, mrows[:, 1, :], ALU.mult)
            S.tt("dve", h2[:, t, :], xnf, mrows[:, 0, :], ALU.add)
            pt = PT()
            ptv = pt.re("p (k t) -> p k t", k=8)
            for kc in range(8):
                S.tr(ptv[:, kc, :], h2[:, t, kc * 128:(kc + 1) * 128], ident)
            S.cp("act", h2T, ptv)
            p = P()
            for kc in range(8):
                S.mm(p[:, 0:16], h2T[:, kc, :], rtr[:, kc, :], start=(kc == 0), stop=(kc == 7))
            S.op("dve", lambda e, p=p: e.reduce_max(sm1.ap, p.ap[:, 0:16], AX.X), reads=[p.buf], writes=[sm1.buf])
            S.ts("dve", sm1, sm1, -1.0, ALU.mult)
            S.act(lgt, p[:, 0:16], AF.Exp, bias=sm1)
            S.red("dve", sm2, lgt)
            S.op("dve", lambda e: e.reciprocal(sm2.ap, sm2.ap), reads=[sm2.buf], writes=[sm2.buf])
            S.ts("dve", aff_tok[:, t, :], lgt, sm2, ALU.mult)
            p2 = P()
            S.mm(p2[0:16, 0:128], aff_tok[:, t, :], identf)
            S.cp("act", affT[:, t * 128:(t + 1) * 128], p2[0:16, 0:128])
        S.pop()
        if DEBUG["stop_after"] == "C0":
            S.pop()
            S.pop()
            S.finish()
            return nc
        S.cp("dve", wk, affT)
        for r in range(32):
            S.op("dve", lambda e: e.max(m8.ap, wk.ap), reads=[wk.buf], writes=[m8.buf])
            if r < 31:
                S.op("dve", lambda e: e.match_replace(wk.ap, m8.ap, wk.ap, -1.0), reads=[wk.buf, m8.buf], writes=[wk.buf])
        S.ts("dve", wk, affT, m8[:, 7:8], ALU.is_ge)
        for t in range(16):
            p = P()
            S.mm(p[:, 0:16], wk[:, t * 128:(t + 1) * 128], identf[0:16, 0:16])
            S.cp("act", mask_tok[:, t, :], p[:, 0:16])
        for t in range(16):
            S.tt("dve", gate_tok[:, t, :], aff_tok[:, t, :], mask_tok[:, t, :], ALU.mult)
            p = P()
            for t2 in range(t + 1):
                S.mm(p[:, 0:16], mUi if t2 == t else ones_bf, mask_tok[:, t2, :], start=(t2 == 0), stop=(t2 == t))
            S.tt("dve", slot_tok[:, t, :], p[:, 0:16], mask_tok[:, t, :], ALU.mult)
            S.ts("dve", slot_tok[:, t, :], slot_tok[:, t, :], -1.0, ALU.add)
        S.pop()

        S.push()
        sel = S.sb("sel", [128, 16, 256], BF16)
        selT = [S.sb("selT%d" % i, [128, 2, 2048], BF16) for i in range(2)]
        xeT = [S.sb("xeT%d" % i, [128, 8, 256], BF16) for i in range(2)]
        actT = S.sb("actT", [128, 2, 256], BF16)
        sl = S.sb("sl", [128, 256], F32)
        y_bf = S.sb("y_bf", [128, 2, 1024], BF16)
        gslot = [S.sb("gslot%d" % i, [128, 2], F32) for i in range(2)]
        NW = 3
        wg_t = [S.sb("wg%d" % i, [128, 8, 256], BF16) for i in range(NW)]
        wu_t = [S.sb("wu%d" % i, [128, 8, 256], BF16) for i in range(NW)]
        wd_t = [S.sb("wd%d" % i, [128, 2, 1024], BF16) for i in range(NW)]
        paux = V(ptbanks[0].buf, ptbanks[0].ap.bitcast(F32))
        paux2 = V(ptbanks[1].buf, ptbanks[1].ap.bitcast(F32))
        ptr = ptbanks[1]

        def prep_pieces(e):
            k = e % 2
            pieces = []

            def mk_sel(t0_):
                def f():
                    for t in range(t0_, t0_ + 4):
                        S.ts("dve", sel[:, t, :], iota_f, slot_tok[:, t, e:e + 1], ALU.is_equal)
                return f
            for t0_ in range(0, 16, 4):
                pieces.append(mk_sel(t0_))

            def f_gslot():
                for jc in range(2):
                    for t in range(16):
                        S.mm(paux[:, jc:jc + 1], sel[:, t, jc * 128:(jc + 1) * 128], gate_tok[:, t, e:e + 1],
                             start=(t == 0), stop=(t == 15))
                S.cp("act", gslot[k], paux[:, 0:2])
            pieces.append(f_gslot)

            def mk_tr(jc, tq):
                def f():
                    ptv = ptr.re("p (k t) -> p k t", k=8)
                    for tt_ in range(8):
                        t = tq * 8 + tt_
                        S.tr(ptv[:, tt_, :], sel[:, t, jc * 128:(jc + 1) * 128], ident)
                    S.cp("act", selT[k][:, jc, tq * 1024:(tq + 1) * 1024], ptr)
                return f
            tail = []
            for jc in range(2):
                for tq in range(2):
                    tail.append(mk_tr(jc, tq))

            def mk_gather(dp):
                def f():
                    pv = paux.re("p (c j) -> p c j", c=2)
                    for c2 in range(2):
                        dc = dp * 2 + c2
                        for t in range(16):
                            S.mm(pv[:, c2, :], h2[:, t, dc * 128:(dc + 1) * 128], sel[:, t, :], start=(t == 0), stop=(t == 15))
                    S.cp("act", xeT[k][:, dp * 2:dp * 2 + 2, :], pv)
                return f
            for dp in range(4):
                pieces.append(mk_gather(dp))
            return pieces, tail

        def scatter_pieces(e):
            k = e % 2
            pieces = []

            def mk(t, dh):
                def f():
                    pa = paux if dh == 0 else paux2
                    for jc in range(2):
                        S.mm(pa, selT[k][:, jc, t * 128:(t + 1) * 128], y_bf[:, jc, dh * 512:(dh + 1) * 512],
                             start=(jc == 0), stop=(jc == 1))
                    a_ = acc[:, t, dh * 512:(dh + 1) * 512]
                    S.tt("dve", a_, a_, pa, ALU.add)
                return f
            for t in range(16):
                for dh in range(2):
                    pieces.append(mk(t, dh))
            return pieces

        pm_, pt_ = prep_pieces(0)
        for f in pm_ + pt_:
            f()
        gcount = 0
        for e in range(16):
            k = e % 2
            pend_sc = scatter_pieces(e - 1) if e > 0 else []
            pend_pr, pend_tr = prep_pieces(e + 1) if e < 15 else ([], [])
            chunk_i = 0
            wsel = {}

            def issue_w(fg):
                nonlocal gcount
                wi = gcount % NW
                gcount += 1
                f0 = fg * 256
                S.dma("pool", wg_t[wi], exg_v[e][:, :, f0:f0 + 256])
                S.dma("pool", wu_t[wi], exu_v[e][:, :, f0:f0 + 256])
                S.dma("pool", wd_t[wi], exd_v[e][:, fg * 2:fg * 2 + 2, :])
                wsel[fg] = wi

            def GU(n):
                fg, fc = n // 2, n % 2
                if fg not in wsel:
                    issue_w(fg)
                wi = wsel[fg]
                p = P()
                for kc in range(8):
                    S.mm(p[:, 0:256], wg_t[wi][:, kc, fc * 128:(fc + 1) * 128], xeT[k][:, kc, :], start=(kc == 0), stop=(kc == 7))
                for kc in range(8):
                    S.mm(p[:, 256:512], wu_t[wi][:, kc, fc * 128:(fc + 1) * 128], xeT[k][:, kc, :], start=(kc == 0), stop=(kc == 7))
                return p

            pcur = GU(0)
            for n in range(22):
                fg, fc = n // 2, n % 2
                wi = wsel[fg]
                pnext = GU(n + 1) if n < 21 else None
                S.act(sl, pcur[:, 0:256], AF.Silu)
                S.tt("dve", actT[:, fc, :], sl, pcur[:, 256:512], ALU.mult)
                for jc in range(2):
                    for dh in range(2):
                        S.mm(Y[jc * 2 + dh], actT[:, fc, jc * 128:(jc + 1) * 128], wd_t[wi][:, fc, dh * 512:(dh + 1) * 512],
                             start=(n == 0), stop=(n == 21))
                pcur = pnext
                chunk_i += 1
                if chunk_i <= 16:
                    for _ in range(min(2, len(pend_sc))):
                        pend_sc.pop(0)()
                if chunk_i <= 18 and chunk_i % 2 == 0 and pend_pr:
                    pend_pr.pop(0)()
                if chunk_i > 18:
                    while pend_sc:
                        pend_sc.pop(0)()
                    while pend_pr:
                        pend_pr.pop(0)()
                    if pend_tr:
                        pend_tr.pop(0)()
            while pend_sc:
                pend_sc.pop(0)()
            while pend_pr:
                pend_pr.pop(0)()
            while pend_tr:
                pend_tr.pop(0)()
            for jc in range(2):
                for dh in range(2):
                    S.stt("dve", y_bf[:, jc, dh * 512:(dh + 1) * 512], Y[jc * 2 + dh], gslot[k][:, jc:jc + 1],
                          mrows[:, 2, dh * 512:(dh + 1) * 512], ALU.mult, ALU.mult)
        for f in scatter_pieces(15):
            f()
        S.pop()
        S.push()
        ln2rows = S.sb("ln2rows", [128, 2, 1024], F32)
        S.dma("sp", ln2rows[:, 0, :], ln2g_d.pb(128))
        S.dma("sp", ln2rows[:, 1, :], ln2b_d.pb(128))
        for t in range(16):
            a_ = acc[:, t, :]
            layer_norm_stats(a_, st6, mv2, rstd1)
            o_ = xt[t % 2]
            S.ts("dve", o_, a_, mv2[:, 0:1], ALU.subtract, rstd1, ALU.mult)
            S.tt("dve", o_, o_, ln2rows[:, 0, :], ALU.mult)
            S.tt("dve", o_, o_, ln2rows[:, 1, :], ALU.add)
            S.dma("sp", out_d[b, t * 128:(t + 1) * 128, :], o_)
        S.pop()
        S.pop()
    S.finish()
    return nc


def kernel(x, c, ctx, c_ctx, ada_w, ada_b, w_in, rw_mu, rw_w0, rw_w2, rw_a0, rw_a2, rw_g2,
           rw_k_k, rw_k_a, rw_r_k, rw_gn_w, rw_gn_b, gla_a2, gla_a_b, gla_norm_w, w_out,
           ln1_g, ln1_b, router_w, ex_gate, ex_up, ex_down, ln2_g, ln2_b):
    f = lambda a: np.ascontiguousarray(np.asarray(a, dtype=np.float32))
    x, c, ctx, c_ctx = f(x), f(c), f(ctx), f(c_ctx)
    cbf, cfc = host_consts()
    eoh = np.zeros((16, 16 * 128), np.float32)
    for e in range(16):
        eoh[e, e * 128:(e + 1) * 128] = 1.0
    mu_ext = np.zeros((1, 3328), np.float32)
    mu_ext[0, :1760] = f(rw_mu)[0]
    shared = {
        "ada_w": f(ada_w)[0], "ada_b": f(ada_b)[0][None, :],
        "ada_bT": np.ascontiguousarray(f(ada_b)[0].reshape(48, 128).T),
        "w_in": f(w_in)[0], "mu_ext": mu_ext,
        "rw_w0": f(rw_w0)[0], "rw_w2": f(rw_w2)[0], "rw_a0": f(rw_a0)[0], "rw_a2": f(rw_a2)[0],
        "rw_g2": f(rw_g2)[0], "rw_k_k": f(rw_k_k)[0][None, :], "rw_k_a": f(rw_k_a)[0][None, :],
        "rw_r_k": f(rw_r_k)[0].reshape(1, 512), "rw_gn_w": f(rw_gn_w)[0][None, :], "rw_gn_b": f(rw_gn_b)[0][None, :],
        "gla_a2": f(gla_a2)[0], "gla_a_b": f(gla_a_b)[0], "gla_norm_w": f(gla_norm_w)[0][None, :],
        "w_out": f(w_out)[0], "ln1_g": f(ln1_g)[0][None, :], "ln1_b": f(ln1_b)[0][None, :],
        "router_w": f(router_w)[0], "ex_gate": f(ex_gate)[0], "ex_up": f(ex_up)[0], "ex_down": f(ex_down)[0],
        "ln2_g": f(ln2_g)[0][None, :], "ln2_b": f(ln2_b)[0][None, :],
        "cbf": cbf, "cf": cfc, "eoh": eoh,
    }
    in_maps = []
    for core in range(8):
        bs = slice(core * NB, (core + 1) * NB)
        cc = np.stack([c[core * NB], c[core * NB + 1], c_ctx], axis=1)
        cT = np.ascontiguousarray(cc.reshape(8, 128, 3).transpose(1, 0, 2))
        m = dict(shared)
        m["x"] = np.ascontiguousarray(x[bs])
        m["ctx"] = np.ascontiguousarray(ctx[bs])
        m["cT"] = cT
        in_maps.append(m)
    nc = build()
    res = run_bass_kernel_spmd(nc, in_maps, core_ids=list(range(8)))
    if DEBUG["dump"]:
        DEBUG["res"] = res.results
    return np.concatenate([np.asarray(r["out"], dtype=np.float32) for r in res.results], axis=0)
```

```python
from contextlib import ExitStack
import numpy as np
import concourse.bass as bass
import concourse.mybir as mybir
from concourse.bass_utils import run_bass_kernel_spmd

F32 = mybir.dt.float32
BF16 = mybir.dt.bfloat16
AF = mybir.ActivationFunctionType
ALU = mybir.AluOpType
AX = mybir.AxisListType


class Buf:
    __slots__ = ("name", "lw", "rd")

    def __init__(self, name):
        self.name = name
        self.lw = None
        self.rd = {}


class V:
    __slots__ = ("buf", "ap")

    def __init__(self, buf, ap):
        self.buf = buf
        self.ap = ap

    def __getitem__(self, k):
        return V(self.buf, self.ap[k])

    def bc(self, shape):
        return V(self.buf, self.ap.to_broadcast(list(shape)))

    def us(self, axis):
        return V(self.buf, self.ap.unsqueeze(axis))

    def re(self, pat, **kw):
        return V(self.buf, self.ap.rearrange(pat, **kw))

    def pb(self, n=128):
        return V(self.buf, self.ap.partition_broadcast(n))


class Sched:
    ENGS = ("pe", "act", "dve", "pool", "sp")

    def __init__(self, nc, ndma=12):
        self.nc = nc
        self.stack = ExitStack()
        self.scopes = [self.stack]
        self.sem = {}
        self.cnt = {}
        self.prog = {}
        self.seen = {}
        for e in self.ENGS:
            self.sem[e] = self.stack.enter_context(nc.semaphore("s_" + e))
            self.cnt[e] = 0
            self.prog[e] = []
            self.seen[e] = {}
        self.ep = 0
        self.dq = {}
        for q in ("sp", "pool", "act"):
            sems = [self.stack.enter_context(nc.semaphore("d_%s%d" % (q, i))) for i in range(ndma)]
            self.dq[q] = dict(sems=sems, vals=[0] * ndma, nxt=0)
        self.n_ops = 0

    def sb(self, name, shape, dtype):
        self.n_alloc = getattr(self, "n_alloc", 0) + 1
        t = self.scopes[-1].enter_context(self.nc.sbuf_tensor("t%d_%s" % (self.n_alloc, name), list(shape), dtype))
        return V(Buf(name), t[:])

    def ps(self, name, shape, dtype):
        t = self.scopes[-1].enter_context(self.nc.psum_tensor("p_" + name, list(shape), dtype))
        return V(Buf(name), t[:])

    def dram(self, name, shape, dtype, kind="Internal"):
        if DEBUG["dump"]:
            kind = "ExternalOutput"
        t = self.nc.dram_tensor(name, list(shape), dtype, kind=kind)
        return V(Buf(name), t.ap())

    def epoch(self):
        self.barrier()
        self.ep += 1
        for e in self.ENGS:
            self.sem[e] = self.stack.enter_context(self.nc.semaphore("s_%s_%d" % (e, self.ep)))
            self.cnt[e] = 0
            for k in [k for k in self.seen[e] if isinstance(k, str)]:
                del self.seen[e][k]

    def alias_acquire(self, parent, aps, name="al"):
        out = []
        for i, ap in enumerate(aps):
            b = Buf("%s_%s%d" % (parent.buf.name, name, i))
            b.lw = parent.buf.lw
            b.rd = dict(parent.buf.rd)
            out.append(V(b, ap))
        return out

    def alias_release(self, parent, children):
        for c in children:
            evs = list(c.buf.rd.values())
            if c.buf.lw is not None:
                evs.append(c.buf.lw)
            for ev in evs:
                old = parent.buf.rd.get(ev[0])
                if old is None or (old[2], old[1]) < (ev[2], ev[1]):
                    parent.buf.rd[ev[0]] = ev

    def push(self):
        self.scopes.append(ExitStack())

    def pop(self):
        self.barrier()
        self.flush()
        self.scopes.pop().close()

    def _semof(self, key):
        if isinstance(key, str):
            return self.sem[key]
        q, slot = key
        return self.dq[q]["sems"][slot]

    def _collect(self, eng, reads, writes, is_dma):
        waits = {}

        def need(ev, war=False):
            if ev is None:
                return
            key, val, dep_ep = ev
            if dep_ep < self.ep:
                return
            if not is_dma and isinstance(key, str) and key == eng:
                if eng == "pe" or war:
                    return
            if self.seen[eng].get(key, 0) >= val:
                return
            if waits.get(key, 0) < val:
                waits[key] = val

        for b in reads:
            need(b.lw)
        for b in writes:
            need(b.lw)
            for ev in b.rd.values():
                need(ev, war=True)
        return waits

    def _commit(self, eng, waits):
        for k, v in waits.items():
            self.seen[eng][k] = v
        return [(self._semof(k), v) for k, v in waits.items()]

    def _record(self, ev, reads, writes):
        for b in reads:
            b.rd[ev[0]] = ev
        for b in writes:
            b.lw = ev
            b.rd = {}

    def op(self, eng, fn, reads=(), writes=()):
        waits = self._collect(eng, reads, writes, False)
        wl = self._commit(eng, waits)
        self.cnt[eng] += 1
        ev = (eng, self.cnt[eng], self.ep)
        self.prog[eng].append((wl, fn, (self.sem[eng], 1)))
        self._record(ev, reads, writes)
        self.n_ops += 1

    def dma(self, q, out, in_, **kw):
        oap, iap = out.ap, in_.ap
        d = self.dq[q]
        slot = d["nxt"]
        d["nxt"] = (slot + 1) % len(d["sems"])
        waits = self._collect(q, [in_.buf], [out.buf], True)
        key = (q, slot)
        prev = d["vals"][slot]
        if prev and self.seen[q].get(key, 0) < prev:
            waits[key] = max(waits.get(key, 0), prev)
        wl = self._commit(q, waits)
        d["vals"][slot] = prev + 16
        ev = (key, prev + 16, self.ep)
        sem = d["sems"][slot]
        self.prog[q].append((wl, lambda e: e.dma_start(out=oap, in_=iap, **kw), (sem, 16)))
        self._record(ev, [in_.buf], [out.buf])
        self.n_ops += 1

    @staticmethod
    def _bufs(*xs):
        return [x.buf for x in xs if isinstance(x, V)]

    @staticmethod
    def _a(x):
        return x.ap if isinstance(x, V) else x

    def mm(self, out, lhsT, rhs, start=True, stop=True):
        o, l, r = out.ap, lhsT.ap, rhs.ap
        self.op("pe", lambda e: e.matmul(o, l, r, start=start, stop=stop),
                reads=self._bufs(lhsT, rhs), writes=[out.buf])

    def tr(self, out, in_, ident):
        o, i, d = out.ap, in_.ap, ident.ap
        self.op("pe", lambda e: e.transpose(o, i, d), reads=self._bufs(in_, ident), writes=[out.buf])

    def act(self, out, in_, func, bias=None, scale=None):
        kw = {}
        if bias is not None:
            kw["bias"] = self._a(bias)
        if scale is not None:
            kw["scale"] = self._a(scale)
        o, i = out.ap, in_.ap
        self.op("act", lambda e: e.activation(o, i, func, **kw),
                reads=self._bufs(in_, bias, scale), writes=[out.buf])

    def tt(self, eng, out, a, b, op):
        o, x, y = out.ap, a.ap, b.ap
        self.op(eng, lambda e: e.tensor_tensor(o, x, y, op), reads=self._bufs(a, b), writes=[out.buf])

    def ts(self, eng, out, a, s1, op0, s2=None, op1=None):
        o, x, p1, p2 = out.ap, a.ap, self._a(s1), self._a(s2)
        if op1 is None:
            self.op(eng, lambda e: e.tensor_scalar(o, x, p1, None, op0),
                    reads=self._bufs(a, s1), writes=[out.buf])
        else:
            self.op(eng, lambda e: e.tensor_scalar(o, x, p1, p2, op0, op1),
                    reads=self._bufs(a, s1, s2), writes=[out.buf])

    def stt(self, eng, out, a, sc, b, op0, op1):
        o, x, p, y = out.ap, a.ap, self._a(sc), b.ap
        eng = "dve"
        self.op(eng, lambda e: e.scalar_tensor_tensor(o, x, p, y, op0, op1),
                reads=self._bufs(a, sc, b), writes=[out.buf])

    def cp(self, eng, out, a):
        o, x = out.ap, a.ap
        if eng == "act":
            self.op("act", lambda e: e.activation(o, x, AF.Copy), reads=[a.buf], writes=[out.buf])
        else:
            self.op(eng, lambda e: e.tensor_copy(o, x), reads=[a.buf], writes=[out.buf])

    def red(self, eng, out, a, op=None):
        o, x = out.ap, a.ap
        op = ALU.add if op is None else op
        self.op(eng, lambda e: e.tensor_reduce(o, x, AX.X, op), reads=[a.buf], writes=[out.buf])

    def memset(self, eng, out, val):
        o = out.ap
        self.op(eng, lambda e: e.memset(o, val), writes=[out.buf])

    def barrier(self):
        for e in self.ENGS:
            waits = {}
            for e2 in self.ENGS:
                if e2 != e and self.cnt[e2] > self.seen[e].get(e2, 0):
                    waits[e2] = self.cnt[e2]
            if e not in ("pe",) and self.cnt[e] > self.seen[e].get(e, 0):
                waits[e] = self.cnt[e]
            for q, d in self.dq.items():
                for slot, v in enumerate(d["vals"]):
                    if v and self.seen[e].get((q, slot), 0) < v:
                        waits[(q, slot)] = v
            wl = self._commit(e, waits)
            if wl:
                self.prog[e].append((wl, None, None))

    def flush(self):
        nc = self.nc
        prog = self.prog
        self.prog = {e: [] for e in self.ENGS}

        def replay(name, e):
            for wl, fn, inc in prog[name]:
                for sem, val in wl:
                    e.wait_ge(sem, val)
                if fn is not None:
                    ins = fn(e)
                    ins.then_inc(inc[0], inc[1])

        with nc.Block() as block:
            @block.tensor
            def _(e):
                replay("pe", e)

            @block.scalar
            def _(e):
                replay("act", e)

            @block.vector
            def _(e):
                replay("dve", e)

            @block.gpsimd
            def _(e):
                replay("pool", e)

            @block.sync
            def _(e):
                replay("sp", e)

    def finish(self):
        self.barrier()
        self.flush()
        self.stack.close()


NB = 2
ALPHA = float(2.0 ** 0.25)
CRW = -float(np.exp(-0.5))
GN_EPS = 64e-5
DEBUG = {"stop_after": None, "cut": 99, "dump": False, "res": None, "dbg": False}


def host_consts():
    p = np.arange(128)[:, None]
    q = np.arange(128)[None, :]
    Us = (p < q).astype(np.float32)
    Ui = (p <= q).astype(np.float32)
    Ls = (p > q).astype(np.float32)
    Li = (p >= q).astype(np.float32)
    ident = (p == q).astype(np.float32)
    sm = []
    for l in range(7):
        sz = 2 ** l
        same = (p // (2 * sz)) == (q // (2 * sz))
        lo = same & ((p % (2 * sz)) >= sz) & ((q % (2 * sz)) < sz)
        sm.append((lo | lo.T).astype(np.float32))
    cbf = np.concatenate([ident, Us, Ui, Ls, Li, np.ones((128, 128), np.float32)] + sm, axis=1)
    cf = np.concatenate([Ui * CRW, Us * CRW, Ls * CRW, Li * CRW,
                         Ui / 16.0, Us / 16.0, Ls / 16.0, Li / 16.0,
                         np.full((128, 1), CRW, np.float32), np.full((128, 1), 1.0 / 16.0, np.float32),
                         np.arange(128, dtype=np.float32)[:, None], np.arange(128, dtype=np.float32)[:, None] + 128.0,
                         np.tile(np.arange(256, dtype=np.float32)[None, :], (128, 1))], axis=1)
    return np.ascontiguousarray(cbf), np.ascontiguousarray(cf.astype(np.float32))


def build():
    nc = bass.Bass("TRN2", target_bir_lowering=False)
    S = Sched(nc)

    def din(name, shape):
        return V(Buf(name), nc.dram_tensor(name, list(shape), F32, kind="ExternalInput").ap())

    x_d = din("x", [NB, 2048, 1024])
    ctx_d = din("ctx", [NB, 256, 1024])
    cT_d = din("cT", [128, 8, 3])
    adaw_d = din("ada_w", [1024, 6144])
    adab_d = din("ada_b", [1, 6144])
    adabT_d = din("ada_bT", [128, 48])
    win_d = din("w_in", [1024, 3328])
    mu_d = din("mu_ext", [1, 3328])
    w0_d = din("rw_w0", [2, 512])
    w2_d = din("rw_w2", [2, 32, 512])
    a0_d = din("rw_a0", [2, 512])
    a2_d = din("rw_a2", [2, 32, 512])
    g2_d = din("rw_g2", [96, 512])
    kk_d = din("rw_k_k", [1, 512])
    ka_d = din("rw_k_a", [1, 512])
    rk_d = din("rw_r_k", [1, 512])
    gnw_d = din("rw_gn_w", [1, 512])
    gnb_d = din("rw_gn_b", [1, 512])
    ga2_d = din("gla_a2", [2, 16, 256])
    gab_d = din("gla_a_b", [2, 256])
    gnorm_d = din("gla_norm_w", [1, 512])
    wout_d = din("w_out", [1024, 1024])
    ln1g_d = din("ln1_g", [1, 1024])
    ln1b_d = din("ln1_b", [1, 1024])
    rtr_d = din("router_w", [1024, 16])
    exg_d = din("ex_gate", [16, 1024, 2816])
    exu_d = din("ex_up", [16, 1024, 2816])
    exd_d = din("ex_down", [16, 2816, 1024])
    ln2g_d = din("ln2_g", [1, 1024])
    ln2b_d = din("ln2_b", [1, 1024])
    cbf_d = din("cbf", [128, 1664])
    cf_d = din("cf", [128, 8 * 128 + 4 + 256])
    eoh_d = din("eoh", [16, 16 * 128])
    out_d = V(Buf("out"), nc.dram_tensor("out", [NB, 2048, 1024], F32, kind="ExternalOutput").ap())
    mix_d = S.dram("mixs", [NB, 2048, 1024], BF16)
    yf_d = S.dram("yfs", [16, 128, 1536], F32)
    modrow_d = S.dram("modrows", [NB, 4, 1024], F32)
    dbgf_d = S.dram("dbgf", [24, 128, 512], F32)
    dbgb_d = S.dram("dbgb", [24, 128, 512], BF16)
    dbgn = {"f": 0, "b": 0, "names": []}

    def dump(name, v, bf):
        k = "b" if bf else "f"
        i = dbgn[k]
        dbgn[k] += 1
        dbgn["names"].append((name, k, i, tuple(v.ap.shape)))
        dst = (dbgb_d if bf else dbgf_d)
        p = v.ap.shape[0]
        n = 1
        for d_ in v.ap.shape[1:]:
            n *= d_
        dv = dst[i, 0:p, 0:n]
        if len(v.ap.shape) == 3:
            dv = dv.re("p (a c) -> p a c", a=v.ap.shape[1])
        S.dma("sp", dv, v)
    DEBUG["names"] = dbgn["names"]

    cb = S.sb("cb", [128, 1664], BF16)
    S.dma("pool", cb, cbf_d)
    ident, mUs, mUi, mLs, mLi = (cb[:, i * 128:(i + 1) * 128] for i in range(5))
    ones_bf = cb[:, 640:768]
    smask = [cb[:, 768 + l * 128:896 + l * 128] for l in range(7)]
    cf = S.sb("cf", [128, 8 * 128 + 4 + 256], F32)
    S.dma("sp", cf, cf_d)
    fm = [cf[:, i * 128:(i + 1) * 128] for i in range(8)]
    negcol = cf[:, 1024:1025]
    g16col = cf[:, 1025:1026]
    iota_p0 = cf[:, 1026:1027]
    iota_p1 = cf[:, 1027:1028]
    iota_f = cf[:, 1028:1284]
    modT = S.sb("modT", [128, 16, 3], F32)

    pbanks = [S.ps("pb%d" % i, [128, 512], F32) for i in range(6)]
    ptbanks = [S.ps("ptb%d" % i, [128, 1024], BF16) for i in range(2)]
    rot = {"p": 0, "t": 0}

    allbanks = pbanks + [V(t_.buf, t_.ap.bitcast(F32)) for t_ in ptbanks]

    def P():
        rot["p"] = (rot["p"] + 1) % 8
        return allbanks[rot["p"]]

    def PT():
        p_ = P()
        return V(p_.buf, p_.ap.bitcast(BF16))

    def layer_norm_stats(xt, st, mv, rstd, eps=1e-5):
        for c in range(2):
            o, i = st.ap[:, c, :], xt.ap[:, c * 512:(c + 1) * 512]
            S.op("dve", lambda e, o=o, i=i: e.bn_stats(o, i), reads=[xt.buf], writes=[st.buf])
        S.op("dve", lambda e: e.bn_aggr(mv.ap, st.ap), reads=[st.buf], writes=[mv.buf])
        S.act(rstd, mv[:, 1:2], AF.Sqrt, bias=eps)
        S.op("dve", lambda e: e.reciprocal(rstd.ap, rstd.ap), reads=[rstd.buf], writes=[rstd.buf])

    S.push()
    cT = S.sb("cT", [128, 8, 3], F32)
    S.dma("sp", cT, cT_d)
    scb = S.sb("scb", [128, 8, 3], BF16)
    S.act(scb, cT, AF.Silu)
    rep = []
    for b in range(NB):
        r_ = S.sb("rep%d" % b, [128, 8, 128], BF16)
        S.cp("dve", r_, scb[:, :, b:b + 1].bc([128, 8, 128]))
        rep.append(r_)
    adabT = S.sb("adabT", [128, 48], F32)
    S.dma("sp", adabT, adabT_d)
    blk = [S.sb("adablk%d" % i, [128, 8, 512], BF16) for i in range(2)]
    brow = [S.sb("adabrow%d" % i, [128, 512], F32) for i in range(2)]
    mrow = [S.sb("mrow%d" % i, [128, 512], F32) for i in range(2)]
    psT = P()
    psTv = psT[:, 0:48].re("p (j c) -> p j c", c=3)
    adaw_v = adaw_d.re("(kc p) c -> p kc c", p=128)
    for jb in range(12):
        bk = blk[jb % 2]
        S.dma("pool", bk, adaw_v[:, :, jb * 512:(jb + 1) * 512])
        if jb < 4:
            for jj in range(4):
                j = jb * 4 + jj
                for kc in range(8):
                    S.mm(psTv[:, j, :], bk[:, kc, jj * 128:(jj + 1) * 128], scb[:, kc, :],
                         start=(kc == 0), stop=(kc == 7))
            if jb == 3:
                for c in range(3):
                    S.tt("dve", modT[:, 0:8, c], psTv[:, 0:8, c], adabT[:, 0:8], ALU.add)
                    S.stt("dve", modT[:, 8:16, c], psTv[:, 8:16, c], 1.0, adabT[:, 8:16], ALU.add, ALU.add)
        else:
            which = (jb - 4) // 2
            half = (jb - 4) % 2
            br = brow[jb % 2]
            S.dma("sp", br, adab_d[:, jb * 512:(jb + 1) * 512].pb(128))
            for b in range(NB):
                pr = P()
                for kc in range(8):
                    S.mm(pr, rep[b][:, kc, :], bk[:, kc, :], start=(kc == 0), stop=(kc == 7))
                mr = mrow[b]
                if which == 2:
                    S.stt("dve", mr, pr, 1.0, br, ALU.add, ALU.add)
                else:
                    S.tt("dve", mr, pr, br, ALU.add)
                S.dma("sp", modrow_d[b, which:which + 1, half * 512:(half + 1) * 512], mr[0:1, :])
    S.pop()
    if DEBUG["stop_after"] == "A":
        S.finish()
        return nc

    S.push()
    Wa = S.sb("Wa", [128, 8, 3072], BF16)
    Wb = S.sb("Wb", [128, 8, 1536], BF16)
    Wl = [S.sb("Wl%d" % d, [128, 8, 96], BF16) for d in range(2)]
    Wlb = [S.sb("Wlb%d" % d, [128, 8, 96], BF16) for d in range(2)]
    Wgd = S.sb("Wgd", [128, 8, 96], BF16)
    Wgdb = S.sb("Wgdb", [128, 8, 96], BF16)
    sw = [S.sb("sw%d" % d, [96, 512], BF16) for d in range(2)]
    g2w = S.sb("g2w", [96, 512], BF16)
    br3 = S.sb("br3", [96, 2, 512], F32)
    onesf = S.sb("onesf", [96, 128], F32)
    S.memset("dve", onesf, 1.0)
    for d in range(2):
        S.dma("sp", br3[0:1, d, :], w0_d[d:d + 1, :])
        S.dma("sp", br3[32:33, d, :], a0_d[d:d + 1, :])
        S.dma("sp", br3[64:65, d, 0:256], gab_d[d:d + 1, :])
    rows = S.sb("rows", [128, 6, 512], F32)
    kkrow, karow, rkrow, gnwrow, gnbrow, gnormrow = (rows[:, i, :] for i in range(6))
    for i, dsrc in enumerate((kk_d, ka_d, rk_d, gnw_d, gnb_d, gnorm_d)):
        S.dma("sp", rows[:, i, :], dsrc.pb(128))
    for d in range(2):
        S.dma("pool", sw[d][0:32, :], w2_d[d])
        S.dma("pool", sw[d][32:64, :], a2_d[d])
        S.dma("pool", sw[d][64:80, 0:256], ga2_d[d])
    S.dma("pool", g2w, g2_d)

    S.push()
    mub = S.sb("mub", [128, 3328], F32)
    omm = S.sb("omm", [128, 3328], F32)
    S.dma("sp", mub, mu_d.pb(128))
    S.ts("dve", omm, mub, -1.0, ALU.mult, 1.0, ALU.add)
    stg = [S.sb("stg%d" % i, [128, 3328], F32) for i in range(2)]
    for kc in range(8):
        st_ = stg[kc % 2]
        S.dma("sp", st_, win_d[kc * 128:(kc + 1) * 128, :])
        S.tt("dve", Wa[:, kc, 0:1536], st_[:, 0:1536], omm[:, 0:1536], ALU.mult)
        S.tt("dve", Wa[:, kc, 1536:3072], st_[:, 1760:3296], omm[:, 1760:3296], ALU.mult)
        S.tt("pool", Wb[:, kc, :], st_[:, 0:1536], mub[:, 0:1536], ALU.mult)
        for d in range(2):
            for (o0, c0, n) in ((0, 1536 + 32 * d, 32), (32, 1600 + 32 * d, 32), (64, 3296 + 16 * d, 16)):
                S.tt("pool", Wl[d][:, kc, o0:o0 + n], st_[:, c0:c0 + n], omm[:, c0:c0 + n], ALU.mult)
            for (o0, c0, n) in ((0, 1536 + 32 * d, 32), (32, 1600 + 32 * d, 32)):
                S.tt("pool", Wlb[d][:, kc, o0:o0 + n], st_[:, c0:c0 + n], mub[:, c0:c0 + n], ALU.mult)
        S.tt("pool", Wgd[:, kc, :], st_[:, 1664:1760], omm[:, 1664:1760], ALU.mult)
        S.tt("pool", Wgdb[:, kc, :], st_[:, 1664:1760], mub[:, 1664:1760], ALU.mult)
    S.pop()
    for d in range(2):
        S.memset("pool", Wl[d][:, :, 80:96], 0.0)
        S.memset("pool", Wlb[d][:, :, 64:96], 0.0)

    def f32t(name, n=512, p=128):
        return S.sb(name, [p, n], F32)

    def bft(name, n=512, p=128):
        return S.sb(name, [p, n], BF16)

    xring = [f32t("xr%d" % i, 1024) for i in range(1)]
    hring = [S.sb("hT%d" % i, [128, 8, 128], BF16) for i in range(3)]
    hnb = S.sb("hnb", [128, 8, 128], BF16)
    st6 = S.sb("st6", [128, 2, 6], F32)
    mv2 = S.sb("mv2", [128, 2], F32)
    rstd1 = S.sb("rstd1", [128, 1], F32)
    r_sb, k_sb, v_sb, qk_sb = f32t("r_sb"), f32t("k_sb"), f32t("v_sb"), f32t("qk_sb")
    v_bf, vg_bf = bft("v_bf"), bft("vg_bf")
    lo = S.sb("lo", [96, 128], BF16)
    lo_gd = S.sb("lo_gd", [96, 128], BF16)
    sg, a_sb, lg = f32t("sg"), f32t("a_sb"), f32t("lg", 256)
    E1, E2, E3, E4 = f32t("E1"), f32t("E2"), f32t("E3"), f32t("E4")
    t0, t1, kk, kka, kh = f32t("t0"), f32t("t1"), f32t("kk"), f32t("kka"), f32t("kh")
    xn_bf = V(t0.buf, t0.ap.bitcast(BF16))
    nbacc = V(t1.buf, t1.ap.bitcast(BF16)).re("p (k t) -> p k t", k=8)
    ss = S.sb("ss", [128, 8], F32)
    bs = S.sb("bs", [128, 8], F32)
    pc_sb = S.sb("pc_sb", [64, 8], F32)
    pcg_sb = S.sb("pcg_sb", [64, 4], F32)
    At_bf, Bt_bf, Kt_bf, Rt_bf, Bh_bf, Kh_bf = (bft(n) for n in ("At", "Bt", "Kt", "Rt", "Bh", "Kh"))
    qt_bf, kt_bf, khg_bf = bft("qt", 256), bft("kt", 256), bft("khg", 256)

    def ht(name, p=128):
        return S.sb(name, [p, 4, 128], BF16)

    AtT, BtT, KtT, RtT = ht("AtT", 64), ht("BtT", 64), ht("KtT", 64), ht("RtT", 64)
    LakT, ArbT, ArkT, AqkT = ht("LakT"), ht("ArbT"), ht("ArkT"), ht("AqkT")
    Xr = [ht("Xr0"), ht("Xr1")]
    XTr = [ht("XTr0"), ht("XTr1")]
    Zr = [ht("Zr0"), ht("Zr1")]
    def half_aps(t):
        tb = t.ap.bitcast(BF16)
        return [tb[:, i * 512:(i + 1) * 512].rearrange("p (h t) -> p h t", h=4) for i in range(2)]
    QeffT, qtT, ktT = ht("QeffT", 64), ht("qtT", 64), ht("ktT", 64)
    mkB = ht("mkB")
    HT = S.sb("HT", [64, 4, 64], BF16)
    M_f = [S.sb("M_f%d" % d, [64, 8, 64], F32) for d in range(2)]
    M_bf = [S.sb("M_bf%d" % d, [64, 8, 64], BF16) for d in range(2)]
    Mg_f = [S.sb("Mg_f%d" % d, [64, 4, 128], F32) for d in range(2)]
    Mg_bf = [S.sb("Mg_bf%d" % d, [64, 4, 128], BF16) for d in range(2)]
    y_sb, bo_sb, o_sb = f32t("y_sb"), f32t("bo_sb"), f32t("o_sb")
    yfl = f32t("yfl", 1536)
    gout_sb, gsil = sg, a_sb
    mix_bf = bft("mix_bf", 1024)
    s8a, s8b, s8c, s8d = (S.sb("s8%s" % n, [128, 8], F32) for n in "abcd")

    if DEBUG.get("verbose"):
        print("mixer SBUF bytes remaining per partition:", nc.sbuf_bytes_remaining)

    def v4(pbank, p=128, w=128):
        return pbank[0:p, 0:4 * w].re("p (h t) -> p h t", h=4)

    def proj_block(dst, hT, hn, c0, ncols, rw):
        n = 16 if rw else 8
        i = 0
        for kc in range(8):
            S.mm(dst[:, 0:ncols], hT[:, kc, :], Wa[:, kc, c0:c0 + ncols], start=(i == 0), stop=(i == n - 1))
            i += 1
        if rw:
            for kc in range(8):
                S.mm(dst[:, 0:ncols], hn[:, kc, :], Wb[:, kc, c0:c0 + ncols], start=False, stop=(i == n - 1))
                i += 1

    def produce(b, kind, idx):
        xt = xring[0]
        src = ctx_d[b, idx * 128:(idx + 1) * 128, :] if kind == "c" else x_d[b, idx * 128:(idx + 1) * 128, :]
        S.dma("sp", xt, src)
        layer_norm_stats(xt, st6, mv2, rstd1)
        S.ts("dve", xn_bf, xt, mv2[:, 0:1], ALU.subtract, rstd1, ALU.mult)
        pt = PT()
        ptv = pt.re("p (k t) -> p k t", k=8)
        col = 2 if kind == "c" else b
        hs = hring[idx % 3]
        for kc in range(8):
            S.tr(ptv[:, kc, :], xn_bf[:, kc * 128:(kc + 1) * 128], ident)
        for kc in range(8):
            S.act(hs[:, kc, :], ptv[:, kc, :], AF.Identity, bias=modT[:, kc, col:col + 1],
                  scale=modT[:, 8 + kc, col:col + 1])

    def neighbor(kind, idx, n_tiles):
        C = hring[idx % 3]
        Pv = hring[(idx - 1) % 3] if idx > 0 else None
        Nx = hring[(idx + 1) % 3] if idx < n_tiles - 1 else None
        if kind == "l":
            if Pv is not None:
                S.tt("dve", nbacc[:, :, 0:64], C[:, :, 64:128], Pv[:, :, 64:128], ALU.add)
            else:
                S.cp("dve", nbacc[:, :, 0:64], C[:, :, 64:128])
            if Nx is not None:
                S.tt("dve", nbacc[:, :, 64:128], C[:, :, 0:64], Nx[:, :, 0:64], ALU.add)
            else:
                S.cp("dve", nbacc[:, :, 64:128], C[:, :, 0:64])
            a4 = nbacc.re("p k (r c) -> p k r c", r=2)
            c4 = C.re("p k (r c) -> p k r c", r=2)
            S.tt("dve", a4[:, :, :, 1:64], a4[:, :, :, 1:64], c4[:, :, :, 0:63], ALU.add)
            S.tt("dve", a4[:, :, :, 0:63], a4[:, :, :, 0:63], c4[:, :, :, 1:64], ALU.add)
            S.ts("dve", hnb, nbacc, 0.25, ALU.mult)
        else:
            S.cp("dve", nbacc[:, :, 1:128], C[:, :, 0:127])
            if Pv is not None:
                S.cp("dve", nbacc[:, :, 0:1], Pv[:, :, 127:128])
            else:
                S.memset("dve", nbacc[:, :, 0:1], 0.0)
            S.tt("dve", nbacc[:, :, 0:127], nbacc[:, :, 0:127], C[:, :, 1:128], ALU.add)
            if Nx is not None:
                S.tt("dve", nbacc[:, :, 127:128], nbacc[:, :, 127:128], Nx[:, :, 0:1], ALU.add)
            S.ts("dve", hnb, nbacc, 0.5, ALU.mult)

    def mix_tile(b, kind, idx, d, want_out):
        hT = hring[idx % 3]
        final = want_out and d == 1
        if d == 0:
            mi, me, mr = fm[0], fm[1], fm[2]
            gmi, gmr = fm[4], fm[6]
            m_ij_s, m_ji_s, m_ji_i = mLs, mUs, mUi
        else:
            mi, me, mr = fm[3], fm[2], fm[1]
            gmi, gmr = fm[7], fm[5]
            m_ij_s, m_ji_s, m_ji_i = mUs, mLs, mLi
        pl = P()
        for kc in range(8):
            S.mm(pl[0:96, 0:128], Wl[d][:, kc, :], hT[:, kc, :], start=(kc == 0), stop=False)
        for kc in range(8):
            S.mm(pl[0:96, 0:128], Wlb[d][:, kc, :], hnb[:, kc, :], start=False, stop=(kc == 7))
        S.act(lo[0:32, :], pl[0:32, 0:128], AF.Tanh)
        S.act(lo[32:64, :], pl[32:64, 0:128], AF.Copy)
        S.act(lo[64:80, :], pl[64:80, 0:128], AF.Copy)
        pr_ = P(); proj_block(pr_, hT, hnb, 0, 512, True)
        pw = P()
        S.mm(pw, lo[0:32, :], sw[d][0:32, :], start=True, stop=False)
        S.mm(pw, onesf[0:1, :], br3[0:1, d, :], start=False, stop=True)
        pa = P()
        S.mm(pa, lo[32:64, :], sw[d][32:64, :], start=True, stop=False)
        S.mm(pa, onesf[32:33, :], br3[32:33, d, :], start=False, stop=True)
        pg = P()
        S.mm(pg[:, 0:256], lo[64:80, :], sw[d][64:80, 0:256], start=True, stop=False)
        S.mm(pg[:, 0:256], onesf[64:65, :], br3[64:65, d, 0:256], start=False, stop=True)
        S.act(sg, pw, AF.Sigmoid)
        S.act(a_sb, pa, AF.Sigmoid)
        S.act(lg, pg[:, 0:256], AF.Sigmoid)
        S.cp("act", r_sb, pr_)
        pk_ = P(); proj_block(pk_, hT, hnb, 512, 512, True)
        pc1 = P(); S.mm(pc1, mi, sg)
        pc2 = P(); S.mm(pc2, me, sg)
        S.act(lg, lg, AF.Ln)
        S.act(E1, pc1, AF.Exp); S.act(E2, pc1, AF.Exp, scale=-1.0)
        S.act(E3, pc2, AF.Exp)
        S.cp("act", k_sb, pk_)
        S.tt("dve", t0, k_sb, kkrow, ALU.mult)
        S.tt("pool", t1, t0, t0, ALU.mult)
        S.red("dve", ss, t1.re("p (h c) -> p h c", h=8))
        S.act(ss, ss, AF.Ln, bias=1e-12)
        S.act(ss, ss, AF.Exp, scale=-0.5)
        S.tt("dve", kk.re("p (h c) -> p h c", h=8), t0.re("p (h c) -> p h c", h=8), ss.us(2).bc([128, 8, 64]), ALU.mult)
        pc3 = P(); S.mm(pc3, mr, sg)
        pp = P()
        for h in range(8):
            S.mm(pp[0:64, h:h + 1], sg[:, h * 64:(h + 1) * 64], negcol)
        for h in range(4):
            S.mm(pp[0:64, 8 + h:9 + h], lg[:, h * 64:(h + 1) * 64], g16col)
        pv_ = P(); proj_block(pv_, hT, hnb, 1024, 512, True)
        S.act(E4, pc3, AF.Exp)
        S.act(pc_sb, pp[0:64, 0:8], AF.Exp)
        S.act(pcg_sb, pp[0:64, 8:12], AF.Exp)
        S.cp("act", v_sb, pv_); S.cp("pool", v_bf, v_sb)
        p = P(); proj_block(p, hT, hnb, 1536, 512, False); S.cp("act", qk_sb, p)
        p = P(); proj_block(p, hT, hnb, 2048, 512, False); S.cp("act", vg_bf, p)
        if DEBUG["cut"] <= 2:
            return
        h8 = lambda t: t.re("p (h c) -> p h c", h=8)
        S.tt("pool", kka, kk, a_sb, ALU.mult)
        S.stt("pool", t1, a_sb, -1.0, karow, ALU.add, ALU.mult)
        S.stt("pool", kh, t1, 1.0, k_sb, ALU.add, ALU.mult)
        S.stt("dve", At_bf, kk, -1.0, E3, ALU.mult, ALU.mult)
        S.tt("dve", Bt_bf, kka, E2, ALU.mult)
        S.tt("dve", Kt_bf, kh, E2, ALU.mult)
        S.tt("dve", Rt_bf, r_sb, E1, ALU.mult)
        S.tt("pool", Bh_bf, kka, E4, ALU.mult)
        S.tt("pool", Kh_bf, kh, E4, ALU.mult)
        if want_out:
            S.tt("pool", t0, r_sb, kh, ALU.mult)
            S.tt("pool", t0, t0, rkrow, ALU.mult)
            S.red("dve", bs, h8(t0))
            S.tt("pool", h8(bo_sb), h8(v_sb), bs.us(2).bc([128, 8, 64]), ALU.mult)
        if DEBUG["cut"] <= 3:
            return
        dbg = DEBUG["dbg"] and b == 0 and kind == "c" and idx == 0 and d == 0
        if dbg:
            for nm, t_ in (("E1", E1), ("E2", E2), ("E3", E3), ("E4", E4), ("kk", kk), ("a", a_sb), ("sg", sg), ("kh", kh),
                           ("r", r_sb), ("v", v_sb)):
                dump(nm, t_, False)
            for nm, t_ in (("At", At_bf), ("Bt", Bt_bf), ("Kt", Kt_bf), ("Rt", Rt_bf), ("Bh", Bh_bf), ("Kh", Kh_bf), ("vb", v_bf)):
                dump(nm, t_, True)
            dump("pc", pc_sb, False)
        G1, G2, G4 = E1, E2, E3
        pg1 = P(); S.mm(pg1[:, 0:256], gmi, lg)
        pg3 = P(); S.mm(pg3[:, 0:256], gmr, lg)
        S.act(G1[:, 0:256], pg1[:, 0:256], AF.Exp)
        S.act(G2[:, 0:256], pg1[:, 0:256], AF.Exp, scale=-1.0)
        S.act(G4[:, 0:256], pg3[:, 0:256], AF.Exp)
        S.stt("dve", qt_bf, qk_sb[:, 0:256], 0.125, G1[:, 0:256], ALU.mult, ALU.mult)
        S.tt("dve", kt_bf, qk_sb[:, 256:512], G2[:, 0:256], ALU.mult)
        S.tt("pool", khg_bf, qk_sb[:, 256:512], G4[:, 0:256], ALU.mult)
        if DEBUG["cut"] <= 4:
            return
        parents = [E1, E2, E3, E4, sg, a_sb, t0, t1, kk, kka, kh, k_sb, qk_sb, r_sb]
        kids = [S.alias_acquire(pt_, half_aps(pt_)) for pt_ in parents]
        flat = [c for pair in kids[3:] for c in pair]
        G = [dict(AtT=AtT, BtT=BtT, KtT=KtT, RtT=RtT, X0=Xr[0], XT0=XTr[0], Lm=Xr[1], LTm=XTr[1],
                  LakT=LakT, ArbT=ArbT, ArkT=ArkT, Z0=Zr[0], Zf=Zr[1], D=kids[0], DT=kids[1],
                  Y1=kids[2][0], Y1t=kids[2][1], QeffT=QeffT, HT=HT),
             dict(AtT=flat[0][0:64], BtT=flat[1][0:64], KtT=flat[2][0:64], RtT=flat[3][0:64],
                  X0=flat[4], XT0=flat[5], Lm=flat[6], LTm=flat[7], LakT=flat[8], ArbT=flat[9], ArkT=flat[10],
                  Z0=flat[11], Zf=flat[12], D=[flat[13], flat[14]], DT=[flat[15], flat[16]],
                  Y1=flat[17], Y1t=flat[18], QeffT=flat[19][0:64], HT=flat[20][0:64, :, 0:64])]
        identb = ident.us(1).bc([128, 4, 128])

        def score(dst, L, R, mask):
            p_ = P()
            pv = v4(p_)
            for hh in range(4):
                S.mm(pv[:, hh, :], L[0:64, hh, :], R[0:64, hh, :])
            S.tt("dve", dst, pv, mask.us(1).bc([128, 4, 128]), ALU.mult)

        for g in range(2):
            T = G[g]
            for src, key, eng in ((At_bf, "AtT", "act"), (Bt_bf, "BtT", "dve"), (Kt_bf, "KtT", "act"), (Rt_bf, "RtT", "dve")):
                pt = PT()
                ptv = v4(pt, 64)
                for hh in range(4):
                    h = 4 * g + hh
                    S.tr(ptv[:, hh, :], src[:, h * 64:(h + 1) * 64], ident)
                S.cp(eng, T[key], ptv)
        for g in range(2):
            T = G[g]
            score(T["X0"], T["AtT"], T["BtT"], m_ij_s)
            score(T["XT0"], T["BtT"], T["AtT"], m_ji_s)
            score(T["LakT"], T["KtT"], T["AtT"], m_ji_s)
            score(T["ArbT"], T["BtT"], T["RtT"], m_ji_i)
            score(T["ArkT"], T["KtT"], T["RtT"], m_ji_i)
        for g in range(2):
            T = G[g]
            p = P()
            pv = p[:, 0:256].re("p (h v) -> p h v", h=4)
            for hh in range(4):
                h = 4 * g + hh
                S.mm(pv[:, hh, :], T["LakT"][:, hh, :], v_bf[:, h * 64:(h + 1) * 64])
            S.cp("act", T["Z0"][:, :, 0:64], pv)
            S.cp("pool", T["Z0"][:, :, 64:128], At_bf[:, g * 256:(g + 1) * 256].re("p (h c) -> p h c", h=4))
            m0 = smask[0].us(1).bc([128, 4, 128])
            S.tt("pool", T["Lm"], T["X0"], m0, ALU.mult)
            S.tt("pool", T["LTm"], T["XT0"], m0, ALU.mult)
            S.tt("dve", T["D"][0], T["Lm"], identb, ALU.add)
            S.tt("dve", T["DT"][0], T["LTm"], identb, ALU.add)
        U16 = mybir.dt.uint16
        mk_tiles = [flat[21], mkB]
        for l in range(1, 7):
            mk_bf = mk_tiles[l % 2]
            mk = V(mk_bf.buf, mk_bf.ap.bitcast(U16).rearrange("p h t -> p (h t)"))
            S.cp("pool", mk_bf, smask[l].us(1).bc([128, 4, 128]))
            for g in range(2):
                T = G[g]
                Dc, DTc = T["D"][0], T["DT"][0]
                p = P(); pv = v4(p)
                for hh in range(4):
                    S.mm(pv[:, hh, :], T["XT0"][:, hh, :], Dc[:, hh, :])
                S.cp("act", T["Y1"], pv)
                p = P(); pv = v4(p)
                for hh in range(4):
                    S.mm(pv[:, hh, :], T["X0"][:, hh, :], DTc[:, hh, :])
                S.cp("act", T["Y1t"], pv)
            for g in range(2):
                T = G[g]
                Dc, DTc = T["D"][0], T["DT"][0]
                p1 = P(); pv1 = v4(p1)
                for hh in range(4):
                    S.mm(pv1[:, hh, :], DTc[:, hh, :], T["Y1"][:, hh, :])
                p2 = P(); pv2 = v4(p2)
                for hh in range(4):
                    S.mm(pv2[:, hh, :], Dc[:, hh, :], T["Y1t"][:, hh, :])
                for dst, pvx in ((Dc, p1), (DTc, p2)):
                    o_, m_, d_ = dst.ap.rearrange("p h t -> p (h t)"), mk.ap, pvx.ap[:, 0:512]
                    S.op("dve", lambda e, o_=o_, m_=m_, d_=d_: e.copy_predicated(o_, m_, d_),
                         reads=[pvx.buf, mk.buf, dst.buf], writes=[dst.buf])
        for g in range(2):
            T = G[g]
            DTc = T["DT"][0]
            p = P(); pv = v4(p)
            for hh in range(4):
                S.mm(pv[:, hh, :], DTc[:, hh, :], T["Z0"][:, hh, :])
            S.cp("act" if g == 0 else "dve", T["Zf"], pv)
        for g in range(2):
            T = G[g]
            Zf = T["Zf"]
            p = P()
            pv = v4(p, 64)
            for hh in range(4):
                h = 4 * g + hh
                S.mm(pv[:, hh, :], Rt_bf[:, h * 64:(h + 1) * 64], ident, start=True, stop=False)
                S.mm(pv[:, hh, :], Zf[:, hh, 64:128], T["ArbT"][:, hh, :], start=False, stop=True)
            S.cp("act", T["QeffT"], pv)
            p = P()
            pvh = p[0:64, 0:256].re("p (h k) -> p h k", h=4)
            for hh in range(4):
                h = 4 * g + hh
                S.mm(pvh[:, hh, :], Zf[:, hh, 64:128], Bh_bf[:, h * 64:(h + 1) * 64])
            S.cp("dve", T["HT"], pvh)
        for g in range(2):
            T = G[g]
            Zf = T["Zf"]
            if want_out:
                p = P()
                pv = p[:, 0:256].re("p (h v) -> p h v", h=4)
                for hh in range(4):
                    h = 4 * g + hh
                    S.mm(pv[:, hh, :], T["QeffT"][:, hh, :], M_bf[d][:, h, :], start=True, stop=False)
                    S.mm(pv[:, hh, :], T["ArbT"][:, hh, :], Zf[:, hh, 0:64], start=False, stop=False)
                    S.mm(pv[:, hh, :], T["ArkT"][:, hh, :], v_bf[:, h * 64:(h + 1) * 64], start=False, stop=True)
                S.cp("act", y_sb[:, g * 256:(g + 1) * 256], p[:, 0:256])
            p = P()
            pvm = p[0:64, 0:256].re("p (h v) -> p h v", h=4)
            for hh in range(4):
                h = 4 * g + hh
                S.mm(pvm[:, hh, :], T["HT"][:, hh, :], M_bf[d][:, h, :], start=True, stop=False)
                S.mm(pvm[:, hh, :], Bh_bf[:, h * 64:(h + 1) * 64], Zf[:, hh, 0:64], start=False, stop=False)
                S.mm(pvm[:, hh, :], Kh_bf[:, h * 64:(h + 1) * 64], v_bf[:, h * 64:(h + 1) * 64], start=False, stop=True)
            Mv = M_f[d][:, 4 * g:4 * g + 4, :]
            S.tt("dve", Mv, Mv, pc_sb[:, 4 * g:4 * g + 4].us(2).bc([64, 4, 64]), ALU.mult)
            S.tt("dve", Mv, Mv, pvm, ALU.add)
            S.cp("dve", M_bf[d][:, 4 * g:4 * g + 4, :], Mv)
        for pt_, ch in zip(parents, kids):
            S.alias_release(pt_, ch)
        for src, dstT, eng in ((qt_bf, qtT, "act"), (kt_bf, ktT, "dve")):
            pt = PT()
            ptv = v4(pt, 64)
            for hh in range(4):
                S.tr(ptv[:, hh, :], src[:, hh * 64:(hh + 1) * 64], ident)
            S.cp(eng, dstT, ptv)
        p = P()
        pv = v4(p)
        for hh in range(4):
            S.mm(pv[:, hh, :], ktT[:, hh, :], qtT[:, hh, :])
        S.tt("dve", AqkT, pv, m_ji_i.us(1).bc([128, 4, 128]), ALU.mult)
        if want_out:
            p = P()
            pv = v4(p)
            for hh in range(4):
                S.mm(pv[:, hh, :], AqkT[:, hh, :], vg_bf[:, hh * 128:(hh + 1) * 128], start=True, stop=False)
                S.mm(pv[:, hh, :], qtT[:, hh, :], Mg_bf[d][:, hh, :], start=False, stop=True)
            S.cp("act", o_sb, p)
        p = P()
        pvg = v4(p, 64)
        for hh in range(4):
            S.mm(pvg[:, hh, :], khg_bf[:, hh * 64:(hh + 1) * 64], vg_bf[:, hh * 128:(hh + 1) * 128])
        S.tt("dve", Mg_f[d], Mg_f[d], pcg_sb.us(2).bc([64, 4, 128]), ALU.mult)
        S.tt("dve", Mg_f[d], Mg_f[d], pvg, ALU.add)
        S.cp("dve", Mg_bf[d], Mg_f[d])
        if not want_out:
            return
        if d == 0:
            S.dma("sp", yf_d[idx, :, 0:512], y_sb)
            S.dma("sp", yf_d[idx, :, 512:1024], bo_sb)
            S.dma("sp", yf_d[idx, :, 1024:1536], o_sb)
            return
        pl2 = P()
        for kc in range(8):
            S.mm(pl2[0:96, 0:128], Wgd[:, kc, :], hT[:, kc, :], start=(kc == 0), stop=False)
        for kc in range(8):
            S.mm(pl2[0:96, 0:128], Wgdb[:, kc, :], hnb[:, kc, :], start=False, stop=(kc == 7))
        S.act(lo_gd, pl2[0:96, 0:128], AF.Sigmoid)
        pgo = P()
        S.mm(pgo, lo_gd, g2w)
        S.cp("act", gout_sb, pgo)
        p = P(); proj_block(p, hT, hnb, 2560, 512, False)
        S.act(gsil, p, AF.Silu)
        S.dma("sp", yfl, yf_d[idx])
        S.tt("dve", y_sb, y_sb, yfl[:, 0:512], ALU.add)
        S.red("dve", s8a, h8(y_sb))
        S.tt("pool", t0, y_sb, y_sb, ALU.mult)
        S.red("dve", s8b, h8(t0))
        S.ts("dve", s8a, s8a, 1.0 / 64.0, ALU.mult)
        S.tt("dve", s8c, s8a, s8a, ALU.mult)
        S.stt("dve", s8b, s8b, 1.0 / 64.0, s8c, ALU.mult, ALU.subtract)
        S.act(s8b, s8b, AF.Sqrt, bias=GN_EPS)
        S.op("dve", lambda e: e.reciprocal(s8b.ap, s8b.ap), reads=[s8b.buf], writes=[s8b.buf])
        S.tt("dve", h8(y_sb), h8(y_sb), s8a.us(2).bc([128, 8, 64]), ALU.subtract)
        S.tt("dve", h8(y_sb), h8(y_sb), s8b.us(2).bc([128, 8, 64]), ALU.mult)
        S.tt("dve", y_sb, y_sb, gnwrow, ALU.mult)
        S.tt("dve", y_sb, y_sb, gnbrow, ALU.add)
        S.tt("dve", y_sb, y_sb, bo_sb, ALU.add)
        S.tt("dve", y_sb, y_sb, yfl[:, 512:1024], ALU.add)
        S.tt("dve", mix_bf[:, 0:512], y_sb, gout_sb, ALU.mult)
        S.tt("dve", o_sb, o_sb, yfl[:, 1024:1536], ALU.add)
        S.tt("pool", t0, o_sb, o_sb, ALU.mult)
        S.red("dve", s8d[:, 0:4], t0.re("p (h c) -> p h c", h=4))
        S.act(s8d[:, 0:4], s8d[:, 0:4], AF.Sqrt, bias=1e-5, scale=1.0 / 128.0)
        S.op("dve", lambda e: e.reciprocal(s8d.ap[:, 0:4], s8d.ap[:, 0:4]), reads=[s8d.buf], writes=[s8d.buf])
        o4 = o_sb.re("p (h c) -> p h c", h=4)
        S.tt("dve", o4, o4, s8d[:, 0:4].us(2).bc([128, 4, 128]), ALU.mult)
        S.tt("dve", o_sb, o_sb, gnormrow, ALU.mult)
        S.tt("dve", mix_bf[:, 512:1024], o_sb, gsil, ALU.mult)
        S.dma("sp", mix_d[b, idx * 128:(idx + 1) * 128, :], mix_bf)

    if DEBUG["stop_after"] == "Bs":
        S.pop()
        S.finish()
        return nc
    for b in range(NB):
        for d in range(2):
            S.memset("dve", M_f[d], 0.0)
            S.memset("dve", M_bf[d], 0.0)
            S.memset("pool", Mg_f[d], 0.0)
            S.memset("pool", Mg_bf[d], 0.0)
        for d in range(2):
            for kind, n_t in (("c", 2), ("l", 16)):
                order = list(range(n_t)) if d == 0 else list(range(n_t - 1, -1, -1))
                step = 1 if d == 0 else -1
                produce(b, kind, order[0])
                for i, idx in enumerate(order):
                    nxt = idx + step
                    if 0 <= nxt < n_t:
                        produce(b, kind, nxt)
                    neighbor(kind, idx, n_t)
                    mix_tile(b, kind, idx, d, kind == "l")
                    if DEBUG["stop_after"] == "B1":
                        break
                if DEBUG["stop_after"] == "B1":
                    break
    S.pop()
    if DEBUG["stop_after"] in ("B", "B1"):
        S.finish()
        return nc
    return moe_phase(nc, S, locals())


def moe_phase(nc, S, L):
    out_d, modrow_d = L["out_d"], L["modrow_d"]
    rtr_d, exg_d, exu_d, exd_d = L["rtr_d"], L["exg_d"], L["exu_d"], L["exd_d"]
    ln2g_d, ln2b_d, eoh_d = L["ln2g_d"], L["ln2b_d"], L["eoh_d"]
    ident, mUi, ones_bf = L["ident"], L["mUi"], L["ones_bf"]
    iota_f = L["iota_f"]
    pbanks, ptbanks = L["pbanks"], L["ptbanks"]
    layer_norm_stats = L["layer_norm_stats"]
    rot = {"p": 0, "t": 0}

    def P():
        rot["p"] = (rot["p"] + 1) % 2
        return pbanks[4 + rot["p"]]

    def PT():
        rot["t"] = (rot["t"] + 1) % 2
        return ptbanks[rot["t"]]

    Y = pbanks[0:4]
    exg_v = [exg_d[e].re("(kc p) f -> p kc f", p=128) for e in range(16)]
    exu_v = [exu_d[e].re("(kc p) f -> p kc f", p=128) for e in range(16)]
    exd_v = [exd_d[e].re("(fc p) d -> p fc d", p=128) for e in range(16)]

    x_d, mix_d, wout_d, ln1g_d, ln1b_d = L["x_d"], L["mix_d"], L["wout_d"], L["ln1g_d"], L["ln1b_d"]
    for b in range(NB):
        S.epoch()
        S.push()
        h2 = S.sb("h2", [128, 16, 1024], BF16)
        acc = S.sb("acc", [128, 16, 1024], F32)
        mrows = S.sb("mrows", [128, 3, 1024], F32)
        S.dma("sp", mrows[:, 0, :], modrow_d[b, 1:2, :].pb(128))
        S.dma("sp", mrows[:, 1, :], modrow_d[b, 2:3, :].pb(128))
        S.dma("sp", mrows[:, 2, :], modrow_d[b, 3:4, :].pb(128))
        identf = S.sb("identf", [128, 128], F32)
        S.cp("dve", identf, ident)
        aff_tok = S.sb("aff_tok", [128, 16, 16], F32)
        mask_tok = S.sb("mask_tok", [128, 16, 16], BF16)
        gate_tok = S.sb("gate_tok", [128, 16, 16], BF16)
        slot_tok = S.sb("slot_tok", [128, 16, 16], F32)
        m8 = S.sb("m8", [16, 8], F32)
        st6 = S.sb("mst6", [128, 2, 6], F32)
        mv2 = S.sb("mmv2", [128, 2], F32)
        rstd1 = S.sb("mrstd1", [128, 1], F32)
        xt = [S.sb("mx%d" % i, [128, 1024], F32) for i in range(2)]

        S.push()
        affT = S.sb("affT", [16, 2048], F32)
        wk = S.sb("wk", [16, 2048], F32)
        S.push()
        woutg = S.sb("woutg", [128, 8, 1024], BF16)
        g1row = S.sb("g1row", [128, 1024], F32)
        wstg = [S.sb("wstg%d" % i, [128, 1024], F32) for i in range(2)]
        lnrows = S.sb("lnrows", [128, 2, 1024], F32)
        S.dma("sp", lnrows[:, 0, :], ln1g_d.pb(128))
        S.dma("sp", lnrows[:, 1, :], ln1b_d.pb(128))
        S.dma("sp", g1row, modrow_d[b, 0:1, :].pb(128))
        for kc in range(8):
            ws = wstg[kc % 2]
            S.dma("sp", ws, wout_d[kc * 128:(kc + 1) * 128, :])
            S.tt("pool", woutg[:, kc, :], ws, g1row, ALU.mult)
        rtr = S.sb("rtr", [128, 8, 16], BF16)
        S.dma("pool", rtr, rtr_d.re("(kc p) e -> p kc e", p=128))
        mixt = [S.sb("mixt%d" % i, [128, 1024], BF16) for i in range(2)]
        mixT = S.sb("mixT", [128, 8, 128], BF16)
        x1t = S.sb("x1t", [128, 1024], F32)
        xnf = S.sb("xnf", [128, 1024], F32)
        h2T = S.sb("h2T", [128, 8, 128], BF16)
        sm1 = S.sb("sm1", [128, 1], F32)
        sm2 = S.sb("sm2", [128, 1], F32)
        nmr = S.sb("nmr", [128, 1], F32)
        lgt = S.sb("lgt", [128, 16], F32)
        for t in range(16):
            x = xt[t % 2]
            mt = mixt[t % 2]
            S.dma("sp", x, x_d[b, t * 128:(t + 1) * 128, :])
            S.dma("sp", mt, mix_d[b, t * 128:(t + 1) * 128, :])
            pt = PT()
            ptv = pt.re("p (k t) -> p k t", k=8)
            for kc in range(8):
                S.tr(ptv[:, kc, :], mt[:, kc * 128:(kc + 1) * 128], ident)
            S.cp("act", mixT, ptv)
            for half in range(2):
                pp_ = P()
                for kc in range(8):
                    S.mm(pp_, mixT[:, kc, :], woutg[:, kc, half * 512:(half + 1) * 512], start=(kc == 0), stop=(kc == 7))
                S.stt("dve", x1t[:, half * 512:(half + 1) * 512], x[:, half * 512:(half + 1) * 512], ALPHA, pp_,
                      ALU.mult, ALU.add)
            layer_norm_stats(x1t, st6, mv2, rstd1)
            S.stt("dve", nmr, mv2[:, 0:1], -1.0, rstd1, ALU.mult, ALU.mult)
            S.act(x1t, x1t, AF.Identity, bias=nmr, scale=rstd1)
            S.tt("dve", x1t, x1t, lnrows[:, 0, :], ALU.mult)
            S.tt("dve", x1t, x1t, lnrows[:, 1, :], ALU.add)
            S.act(acc[:, t, :], x1t, AF.Copy, scale=ALPHA)
            layer_norm_stats(x1t, st6, mv2, rstd1)
            S.stt("dve", nmr, mv2[:, 0:1], -1.0, rstd1, ALU.mult, ALU.mult)
            S.act(xnf, x1t, AF.Identity, bias=nmr, scale=rstd1)
            S.tt("dve", xnf, xnf, mrows[:, 1, :], ALU.mult)
            S.tt("dve", h2[:, t, :], xnf, mrows[:, 0, :], ALU.add)
            pt = PT()
            ptv = pt.re("p (k t) -> p k t", k=8)
            for kc in range(8):
                S.tr(ptv[:, kc, :], h2[:, t, kc * 128:(kc + 1) * 128], ident)
            S.cp("act", h2T, ptv)
            p = P()
            for kc in range(8):
                S.mm(p[:, 0:16], h2T[:, kc, :], rtr[:, kc, :], start=(kc == 0), stop=(kc == 7))
            S.op("dve", lambda e, p=p: e.reduce_max(sm1.ap, p.ap[:, 0:16], AX.X), reads=[p.buf], writes=[sm1.buf])
            S.ts("dve", sm1, sm1, -1.0, ALU.mult)
            S.act(lgt, p[:, 0:16], AF.Exp, bias=sm1)
            S.red("dve", sm2, lgt)
            S.op("dve", lambda e: e.reciprocal(sm2.ap, sm2.ap), reads=[sm2.buf], writes=[sm2.buf])
            S.ts("dve", aff_tok[:, t, :], lgt, sm2, ALU.mult)
            p2 = P()
            S.mm(p2[0:16, 0:128], aff_tok[:, t, :], identf)
            S.cp("act", affT[:, t * 128:(t + 1) * 128], p2[0:16, 0:128])
        S.pop()
        if DEBUG["stop_after"] == "C0":
            S.pop()
            S.pop()
            S.finish()
            return nc
        S.cp("dve", wk, affT)
        for r in range(32):
            S.op("dve", lambda e: e.max(m8.ap, wk.ap), reads=[wk.buf], writes=[m8.buf])
            if r < 31:
                S.op("dve", lambda e: e.match_replace(wk.ap, m8.ap, wk.ap, -1.0), reads=[wk.buf, m8.buf], writes=[wk.buf])
        S.ts("dve", wk, affT, m8[:, 7:8], ALU.is_ge)
        for t in range(16):
            p = P()
            S.mm(p[:, 0:16], wk[:, t * 128:(t + 1) * 128], identf[0:16, 0:16])
            S.cp("act", mask_tok[:, t, :], p[:, 0:16])
        for t in range(16):
            S.tt("dve", gate_tok[:, t, :], aff_tok[:, t, :], mask_tok[:, t, :], ALU.mult)
            p = P()
            for t2 in range(t + 1):
                S.mm(p[:, 0:16], mUi if t2 == t else ones_bf, mask_tok[:, t2, :], start=(t2 == 0), stop=(t2 == t))
            S.tt("dve", slot_tok[:, t, :], p[:, 0:16], mask_tok[:, t, :], ALU.mult)
            S.ts("dve", slot_tok[:, t, :], slot_tok[:, t, :], -1.0, ALU.add)
        S.pop()

        S.push()
        sel = S.sb("sel", [128, 16, 256], BF16)
        selT = [S.sb("selT%d" % i, [128, 2, 2048], BF16) for i in range(2)]
        xeT = [S.sb("xeT%d" % i, [128, 8, 256], BF16) for i in range(2)]
        actT = S.sb("actT", [128, 2, 256], BF16)
        sl = S.sb("sl", [128, 256], F32)
        y_bf = S.sb("y_bf", [128, 2, 1024], BF16)
        gslot = [S.sb("gslot%d" % i, [128, 2], F32) for i in range(2)]
        NW = 3
        wg_t = [S.sb("wg%d" % i, [128, 8, 256], BF16) for i in range(NW)]
        wu_t = [S.sb("wu%d" % i, [128, 8, 256], BF16) for i in range(NW)]
        wd_t = [S.sb("wd%d" % i, [128, 2, 1024], BF16) for i in range(NW)]
        paux = V(ptbanks[0].buf, ptbanks[0].ap.bitcast(F32))
        paux2 = V(ptbanks[1].buf, ptbanks[1].ap.bitcast(F32))
        ptr = ptbanks[1]

        def prep_pieces(e):
            k = e % 2
            pieces = []

            def mk_sel(t0_):
                def f():
                    for t in range(t0_, t0_ + 4):
                        S.ts("dve", sel[:, t, :], iota_f, slot_tok[:, t, e:e + 1], ALU.is_equal)
                return f
            for t0_ in range(0, 16, 4):
                pieces.append(mk_sel(t0_))

            def f_gslot():
                for jc in range(2):
                    for t in range(16):
                        S.mm(paux[:, jc:jc + 1], sel[:, t, jc * 128:(jc + 1) * 128], gate_tok[:, t, e:e + 1],
                             start=(t == 0), stop=(t == 15))
                S.cp("act", gslot[k], paux[:, 0:2])
            pieces.append(f_gslot)

            def mk_tr(jc, tq):
                def f():
                    ptv = ptr.re("p (k t) -> p k t", k=8)
                    for tt_ in range(8):
                        t = tq * 8 + tt_
                        S.tr(ptv[:, tt_, :], sel[:, t, jc * 128:(jc + 1) * 128], ident)
                    S.cp("act", selT[k][:, jc, tq * 1024:(tq + 1) * 1024], ptr)
                return f
            tail = []
            for jc in range(2):
                for tq in range(2):
                    tail.append(mk_tr(jc, tq))

            def mk_gather(dp):
                def f():
                    pv = paux.re("p (c j) -> p c j", c=2)
                    for c2 in range(2):
                        dc = dp * 2 + c2
                        for t in range(16):
                            S.mm(pv[:, c2, :], h2[:, t, dc * 128:(dc + 1) * 128], sel[:, t, :], start=(t == 0), stop=(t == 15))
                    S.cp("act", xeT[k][:, dp * 2:dp * 2 + 2, :], pv)
                return f
            for dp in range(4):
                pieces.append(mk_gather(dp))
            return pieces, tail

        def scatter_pieces(e):
            k = e % 2
            pieces = []

            def mk(t, dh):
                def f():
                    pa = paux if dh == 0 else paux2
                    for jc in range(2):
                        S.mm(pa, selT[k][:, jc, t * 128:(t + 1) * 128], y_bf[:, jc, dh * 512:(dh + 1) * 512],
                             start=(jc == 0), stop=(jc == 1))
                    a_ = acc[:, t, dh * 512:(dh + 1) * 512]
                    S.tt("dve", a_, a_, pa, ALU.add)
                return f
            for t in range(16):
                for dh in range(2):
                    pieces.append(mk(t, dh))
            return pieces

        pm_, pt_ = prep_pieces(0)
        for f in pm_ + pt_:
            f()
        gcount = 0
        for e in range(16):
            k = e % 2
            pend_sc = scatter_pieces(e - 1) if e > 0 else []
            pend_pr, pend_tr = prep_pieces(e + 1) if e < 15 else ([], [])
            chunk_i = 0
            wsel = {}

            def issue_w(fg):
                nonlocal gcount
                wi = gcount % NW
                gcount += 1
                f0 = fg * 256
                S.dma("pool", wg_t[wi], exg_v[e][:, :, f0:f0 + 256])
                S.dma("pool", wu_t[wi], exu_v[e][:, :, f0:f0 + 256])
                S.dma("pool", wd_t[wi], exd_v[e][:, fg * 2:fg * 2 + 2, :])
                wsel[fg] = wi

            def GU(n):
                fg, fc = n // 2, n % 2
                if fg not in wsel:
                    issue_w(fg)
                wi = wsel[fg]
                p = P()
                for kc in range(8):
                    S.mm(p[:, 0:256], wg_t[wi][:, kc, fc * 128:(fc + 1) * 128], xeT[k][:, kc, :], start=(kc == 0), stop=(kc == 7))
                for kc in range(8):
                    S.mm(p[:, 256:512], wu_t[wi][:, kc, fc * 128:(fc + 1) * 128], xeT[k][:, kc, :], start=(kc == 0), stop=(kc == 7))
                return p

            pcur = GU(0)
            for n in range(22):
                fg, fc = n // 2, n % 2
                wi = wsel[fg]
                pnext = GU(n + 1) if n < 21 else None
                S.act(sl, pcur[:, 0:256], AF.Silu)
                S.tt("dve", actT[:, fc, :], sl, pcur[:, 256:512], ALU.mult)
                for jc in range(2):
                    for dh in range(2):
                        S.mm(Y[jc * 2 + dh], actT[:, fc, jc * 128:(jc + 1) * 128], wd_t[wi][:, fc, dh * 512:(dh + 1) * 512],
                             start=(n == 0), stop=(n == 21))
                pcur = pnext
                chunk_i += 1
                if chunk_i <= 16:
                    for _ in range(min(2, len(pend_sc))):
                        pend_sc.pop(0)()
                if chunk_i <= 18 and chunk_i % 2 == 0 and pend_pr:
                    pend_pr.pop(0)()
                if chunk_i > 18:
                    while pend_sc:
                        pend_sc.pop(0)()
                    while pend_pr:
                        pend_pr.pop(0)()
                    if pend_tr:
                        pend_tr.pop(0)()
            while pend_sc:
                pend_sc.pop(0)()
            while pend_pr:
                pend_pr.pop(0)()
            while pend_tr:
                pend_tr.pop(0)()
            for jc in range(2):
                for dh in range(2):
                    S.stt("dve", y_bf[:, jc, dh * 512:(dh + 1) * 512], Y[jc * 2 + dh], gslot[k][:, jc:jc + 1],
                          mrows[:, 2, dh * 512:(dh + 1) * 512], ALU.mult, ALU.mult)
        for f in scatter_pieces(15):
            f()
        S.pop()
        S.push()
        ln2rows = S.sb("ln2rows", [128, 2, 1024], F32)
        S.dma("sp", ln2rows[:, 0, :], ln2g_d.pb(128))
        S.dma("sp", ln2rows[:, 1, :], ln2b_d.pb(128))
        for t in range(16):
            a_ = acc[:, t, :]
            layer_norm_stats(a_, st6, mv2, rstd1)
            o_ = xt[t % 2]
            S.ts("dve", o_, a_, mv2[:, 0:1], ALU.subtract, rstd1, ALU.mult)
            S.tt("dve", o_, o_, ln2rows[:, 0, :], ALU.mult)
            S.tt("dve", o_, o_, ln2rows[:, 1, :], ALU.add)
            S.dma("sp", out_d[b, t * 128:(t + 1) * 128, :], o_)
        S.pop()
        S.pop()
    S.finish()
    return nc


def kernel(x, c, ctx, c_ctx, ada_w, ada_b, w_in, rw_mu, rw_w0, rw_w2, rw_a0, rw_a2, rw_g2,
           rw_k_k, rw_k_a, rw_r_k, rw_gn_w, rw_gn_b, gla_a2, gla_a_b, gla_norm_w, w_out,
           ln1_g, ln1_b, router_w, ex_gate, ex_up, ex_down, ln2_g, ln2_b):
    f = lambda a: np.ascontiguousarray(np.asarray(a, dtype=np.float32))
    x, c, ctx, c_ctx = f(x), f(c), f(ctx), f(c_ctx)
    cbf, cfc = host_consts()
    eoh = np.zeros((16, 16 * 128), np.float32)
    for e in range(16):
        eoh[e, e * 128:(e + 1) * 128] = 1.0
    mu_ext = np.zeros((1, 3328), np.float32)
    mu_ext[0, :1760] = f(rw_mu)[0]
    shared = {
        "ada_w": f(ada_w)[0], "ada_b": f(ada_b)[0][None, :],
        "ada_bT": np.ascontiguousarray(f(ada_b)[0].reshape(48, 128).T),
        "w_in": f(w_in)[0], "mu_ext": mu_ext,
        "rw_w0": f(rw_w0)[0], "rw_w2": f(rw_w2)[0], "rw_a0": f(rw_a0)[0], "rw_a2": f(rw_a2)[0],
        "rw_g2": f(rw_g2)[0], "rw_k_k": f(rw_k_k)[0][None, :], "rw_k_a": f(rw_k_a)[0][None, :],
        "rw_r_k": f(rw_r_k)[0].reshape(1, 512), "rw_gn_w": f(rw_gn_w)[0][None, :], "rw_gn_b": f(rw_gn_b)[0][None, :],
        "gla_a2": f(gla_a2)[0], "gla_a_b": f(gla_a_b)[0], "gla_norm_w": f(gla_norm_w)[0][None, :],
        "w_out": f(w_out)[0], "ln1_g": f(ln1_g)[0][None, :], "ln1_b": f(ln1_b)[0][None, :],
        "router_w": f(router_w)[0], "ex_gate": f(ex_gate)[0], "ex_up": f(ex_up)[0], "ex_down": f(ex_down)[0],
        "ln2_g": f(ln2_g)[0][None, :], "ln2_b": f(ln2_b)[0][None, :],
        "cbf": cbf, "cf": cfc, "eoh": eoh,
    }
    in_maps = []
    for core in range(8):
        bs = slice(core * NB, (core + 1) * NB)
        cc = np.stack([c[core * NB], c[core * NB + 1], c_ctx], axis=1)
        cT = np.ascontiguousarray(cc.reshape(8, 128, 3).transpose(1, 0, 2))
        m = dict(shared)
        m["x"] = np.ascontiguousarray(x[bs])
        m["ctx"] = np.ascontiguousarray(ctx[bs])
        m["cT"] = cT
        in_maps.append(m)
    nc = build()
    res = run_bass_kernel_spmd(nc, in_maps, core_ids=list(range(8)))
    if DEBUG["dump"]:
        DEBUG["res"] = res.results
    return np.concatenate([np.asarray(r["out"], dtype=np.float32) for r in res.results], axis=0)
```

```python
from contextlib import ExitStack
import numpy as np
import concourse.bass as bass
import concourse.mybir as mybir
from concourse.bass_utils import run_bass_kernel_spmd

F32 = mybir.dt.float32
BF16 = mybir.dt.bfloat16
AF = mybir.ActivationFunctionType
ALU = mybir.AluOpType
AX = mybir.AxisListType


class Buf:
    __slots__ = ("name", "lw", "rd")

    def __init__(self, name):
        self.name = name
        self.lw = None
        self.rd = {}


class V:
    __slots__ = ("buf", "ap")

    def __init__(self, buf, ap):
        self.buf = buf
        self.ap = ap

    def __getitem__(self, k):
        return V(self.buf, self.ap[k])

    def bc(self, shape):
        return V(self.buf, self.ap.to_broadcast(list(shape)))

    def us(self, axis):
        return V(self.buf, self.ap.unsqueeze(axis))

    def re(self, pat, **kw):
        return V(self.buf, self.ap.rearrange(pat, **kw))

    def pb(self, n=128):
        return V(self.buf, self.ap.partition_broadcast(n))


class Sched:
    ENGS = ("pe", "act", "dve", "pool", "sp")

    def __init__(self, nc, ndma=12):
        self.nc = nc
        self.stack = ExitStack()
        self.scopes = [self.stack]
        self.sem = {}
        self.cnt = {}
        self.prog = {}
        self.seen = {}
        for e in self.ENGS:
            self.sem[e] = self.stack.enter_context(nc.semaphore("s_" + e))
            self.cnt[e] = 0
            self.prog[e] = []
            self.seen[e] = {}
        self.ep = 0
        self.dq = {}
        for q in ("sp", "pool", "act"):
            sems = [self.stack.enter_context(nc.semaphore("d_%s%d" % (q, i))) for i in range(ndma)]
            self.dq[q] = dict(sems=sems, vals=[0] * ndma, nxt=0)
        self.n_ops = 0

    def sb(self, name, shape, dtype):
        self.n_alloc = getattr(self, "n_alloc", 0) + 1
        t = self.scopes[-1].enter_context(self.nc.sbuf_tensor("t%d_%s" % (self.n_alloc, name), list(shape), dtype))
        return V(Buf(name), t[:])

    def ps(self, name, shape, dtype):
        t = self.scopes[-1].enter_context(self.nc.psum_tensor("p_" + name, list(shape), dtype))
        return V(Buf(name), t[:])

    def dram(self, name, shape, dtype, kind="Internal"):
        if DEBUG["dump"]:
            kind = "ExternalOutput"
        t = self.nc.dram_tensor(name, list(shape), dtype, kind=kind)
        return V(Buf(name), t.ap())

    def epoch(self):
        self.barrier()
        self.ep += 1
        for e in self.ENGS:
            self.sem[e] = self.stack.enter_context(self.nc.semaphore("s_%s_%d" % (e, self.ep)))
            self.cnt[e] = 0
            for k in [k for k in self.seen[e] if isinstance(k, str)]:
                del self.seen[e][k]

    def alias_acquire(self, parent, aps, name="al"):
        out = []
        for i, ap in enumerate(aps):
            b = Buf("%s_%s%d" % (parent.buf.name, name, i))
            b.lw = parent.buf.lw
            b.rd = dict(parent.buf.rd)
            out.append(V(b, ap))
        return out

    def alias_release(self, parent, children):
        for c in children:
            evs = list(c.buf.rd.values())
            if c.buf.lw is not None:
                evs.append(c.buf.lw)
            for ev in evs:
                old = parent.buf.rd.get(ev[0])
                if old is None or (old[2], old[1]) < (ev[2], ev[1]):
                    parent.buf.rd[ev[0]] = ev

    def push(self):
        self.scopes.append(ExitStack())

    def pop(self):
        self.barrier()
        self.flush()
        self.scopes.pop().close()

    def _semof(self, key):
        if isinstance(key, str):
            return self.sem[key]
        q, slot = key
        return self.dq[q]["sems"][slot]

    def _collect(self, eng, reads, writes, is_dma):
        waits = {}

        def need(ev, war=False):
            if ev is None:
                return
            key, val, dep_ep = ev
            if dep_ep < self.ep:
                return
            if not is_dma and isinstance(key, str) and key == eng:
                if eng == "pe" or war:
                    return
            if self.seen[eng].get(key, 0) >= val:
                return
            if waits.get(key, 0) < val:
                waits[key] = val

        for b in reads:
            need(b.lw)
        for b in writes:
            need(b.lw)
            for ev in b.rd.values():
                need(ev, war=True)
        return waits

    def _commit(self, eng, waits):
        for k, v in waits.items():
            self.seen[eng][k] = v
        return [(self._semof(k), v) for k, v in waits.items()]

    def _record(self, ev, reads, writes):
        for b in reads:
            b.rd[ev[0]] = ev
        for b in writes:
            b.lw = ev
            b.rd = {}

    def op(self, eng, fn, reads=(), writes=()):
        waits = self._collect(eng, reads, writes, False)
        wl = self._commit(eng, waits)
        self.cnt[eng] += 1
        ev = (eng, self.cnt[eng], self.ep)
        self.prog[eng].append((wl, fn, (self.sem[eng], 1)))
        self._record(ev, reads, writes)
        self.n_ops += 1

    def dma(self, q, out, in_, **kw):
        oap, iap = out.ap, in_.ap
        d = self.dq[q]
        slot = d["nxt"]
        d["nxt"] = (slot + 1) % len(d["sems"])
        waits = self._collect(q, [in_.buf], [out.buf], True)
        key = (q, slot)
        prev = d["vals"][slot]
        if prev and self.seen[q].get(key, 0) < prev:
            waits[key] = max(waits.get(key, 0), prev)
        wl = self._commit(q, waits)
        d["vals"][slot] = prev + 16
        ev = (key, prev + 16, self.ep)
        sem = d["sems"][slot]
        self.prog[q].append((wl, lambda e: e.dma_start(out=oap, in_=iap, **kw), (sem, 16)))
        self._record(ev, [in_.buf], [out.buf])
        self.n_ops += 1

    @staticmethod
    def _bufs(*xs):
        return [x.buf for x in xs if isinstance(x, V)]

    @staticmethod
    def _a(x):
        return x.ap if isinstance(x, V) else x

    def mm(self, out, lhsT, rhs, start=True, stop=True):
        o, l, r = out.ap, lhsT.ap, rhs.ap
        self.op("pe", lambda e: e.matmul(o, l, r, start=start, stop=stop),
                reads=self._bufs(lhsT, rhs), writes=[out.buf])

    def tr(self, out, in_, ident):
        o, i, d = out.ap, in_.ap, ident.ap
        self.op("pe", lambda e: e.transpose(o, i, d), reads=self._bufs(in_, ident), writes=[out.buf])

    def act(self, out, in_, func, bias=None, scale=None):
        kw = {}
        if bias is not None:
            kw["bias"] = self._a(bias)
        if scale is not None:
            kw["scale"] = self._a(scale)
        o, i = out.ap, in_.ap
        self.op("act", lambda e: e.activation(o, i, func, **kw),
                reads=self._bufs(in_, bias, scale), writes=[out.buf])

    def tt(self, eng, out, a, b, op):
        o, x, y = out.ap, a.ap, b.ap
        self.op(eng, lambda e: e.tensor_tensor(o, x, y, op), reads=self._bufs(a, b), writes=[out.buf])

    def ts(self, eng, out, a, s1, op0, s2=None, op1=None):
        o, x, p1, p2 = out.ap, a.ap, self._a(s1), self._a(s2)
        if op1 is None:
            self.op(eng, lambda e: e.tensor_scalar(o, x, p1, None, op0),
                    reads=self._bufs(a, s1), writes=[out.buf])
        else:
            self.op(eng, lambda e: e.tensor_scalar(o, x, p1, p2, op0, op1),
                    reads=self._bufs(a, s1, s2), writes=[out.buf])

    def stt(self, eng, out, a, sc, b, op0, op1):
        o, x, p, y = out.ap, a.ap, self._a(sc), b.ap
        eng = "dve"
        self.op(eng, lambda e: e.scalar_tensor_tensor(o, x, p, y, op0, op1),
                reads=self._bufs(a, sc, b), writes=[out.buf])

    def cp(self, eng, out, a):
        o, x = out.ap, a.ap
        if eng == "act":
            self.op("act", lambda e: e.activation(o, x, AF.Copy), reads=[a.buf], writes=[out.buf])
        else:
            self.op(eng, lambda e: e.tensor_copy(o, x), reads=[a.buf], writes=[out.buf])

    def red(self, eng, out, a, op=None):
        o, x = out.ap, a.ap
        op = ALU.add if op is None else op
        self.op(eng, lambda e: e.tensor_reduce(o, x, AX.X, op), reads=[a.buf], writes=[out.buf])

    def memset(self, eng, out, val):
        o = out.ap
        self.op(eng, lambda e: e.memset(o, val), writes=[out.buf])

    def barrier(self):
        for e in self.ENGS:
            waits = {}
            for e2 in self.ENGS:
                if e2 != e and self.cnt[e2] > self.seen[e].get(e2, 0):
                    waits[e2] = self.cnt[e2]
            if e not in ("pe",) and self.cnt[e] > self.seen[e].get(e, 0):
                waits[e] = self.cnt[e]
            for q, d in self.dq.items():
                for slot, v in enumerate(d["vals"]):
                    if v and self.seen[e].get((q, slot), 0) < v:
                        waits[(q, slot)] = v
            wl = self._commit(e, waits)
            if wl:
                self.prog[e].append((wl, None, None))

    def flush(self):
        nc = self.nc
        prog = self.prog
        self.prog = {e: [] for e in self.ENGS}

        def replay(name, e):
            for wl, fn, inc in prog[name]:
                for sem, val in wl:
                    e.wait_ge(sem, val)
                if fn is not None:
                    ins = fn(e)
                    ins.then_inc(inc[0], inc[1])

        with nc.Block() as block:
            @block.tensor
            def _(e):
                replay("pe", e)

            @block.scalar
            def _(e):
                replay("act", e)

            @block.vector
            def _(e):
                replay("dve", e)

            @block.gpsimd
            def _(e):
                replay("pool", e)

            @block.sync
            def _(e):
                replay("sp", e)

    def finish(self):
        self.barrier()
        self.flush()
        self.stack.close()


NB = 2
ALPHA = float(2.0 ** 0.25)
CRW = -float(np.exp(-0.5))
GN_EPS = 64e-5
DEBUG = {"stop_after": None, "cut": 99, "dump": False, "res": None, "dbg": False}


def host_consts():
    p = np.arange(128)[:, None]
    q = np.arange(128)[None, :]
    Us = (p < q).astype(np.float32)
    Ui = (p <= q).astype(np.float32)
    Ls = (p > q).astype(np.float32)
    Li = (p >= q).astype(np.float32)
    ident = (p == q).astype(np.float32)
    sm = []
    for l in range(7):
        sz = 2 ** l
        same = (p // (2 * sz)) == (q // (2 * sz))
        lo = same & ((p % (2 * sz)) >= sz) & ((q % (2 * sz)) < sz)
        sm.append((lo | lo.T).astype(np.float32))
    cbf = np.concatenate([ident, Us, Ui, Ls, Li, np.ones((128, 128), np.float32)] + sm, axis=1)
    cf = np.concatenate([Ui * CRW, Us * CRW, Ls * CRW, Li * CRW,
                         Ui / 16.0, Us / 16.0, Ls / 16.0, Li / 16.0,
                         np.full((128, 1), CRW, np.float32), np.full((128, 1), 1.0 / 16.0, np.float32),
                         np.arange(128, dtype=np.float32)[:, None], np.arange(128, dtype=np.float32)[:, None] + 128.0,
                         np.tile(np.arange(256, dtype=np.float32)[None, :], (128, 1))], axis=1)
    return np.ascontiguousarray(cbf), np.ascontiguousarray(cf.astype(np.float32))


def build():
    nc = bass.Bass("TRN2", target_bir_lowering=False)
    S = Sched(nc)

    def din(name, shape):
        return V(Buf(name), nc.dram_tensor(name, list(shape), F32, kind="ExternalInput").ap())

    x_d = din("x", [NB, 2048, 1024])
    ctx_d = din("ctx", [NB, 256, 1024])
    cT_d = din("cT", [128, 8, 3])
    adaw_d = din("ada_w", [1024, 6144])
    adab_d = din("ada_b", [1, 6144])
    adabT_d = din("ada_bT", [128, 48])
    win_d = din("w_in", [1024, 3328])
    mu_d = din("mu_ext", [1, 3328])
    w0_d = din("rw_w0", [2, 512])
    w2_d = din("rw_w2", [2, 32, 512])
    a0_d = din("rw_a0", [2, 512])
    a2_d = din("rw_a2", [2, 32, 512])
    g2_d = din("rw_g2", [96, 512])
    kk_d = din("rw_k_k", [1, 512])
    ka_d = din("rw_k_a", [1, 512])
    rk_d = din("rw_r_k", [1, 512])
    gnw_d = din("rw_gn_w", [1, 512])
    gnb_d = din("rw_gn_b", [1, 512])
    ga2_d = din("gla_a2", [2, 16, 256])
    gab_d = din("gla_a_b", [2, 256])
    gnorm_d = din("gla_norm_w", [1, 512])
    wout_d = din("w_out", [1024, 1024])
    ln1g_d = din("ln1_g", [1, 1024])
    ln1b_d = din("ln1_b", [1, 1024])
    rtr_d = din("router_w", [1024, 16])
    exg_d = din("ex_gate", [16, 1024, 2816])
    exu_d = din("ex_up", [16, 1024, 2816])
    exd_d = din("ex_down", [16, 2816, 1024])
    ln2g_d = din("ln2_g", [1, 1024])
    ln2b_d = din("ln2_b", [1, 1024])
    cbf_d = din("cbf", [128, 1664])
    cf_d = din("cf", [128, 8 * 128 + 4 + 256])
    eoh_d = din("eoh", [16, 16 * 128])
    out_d = V(Buf("out"), nc.dram_tensor("out", [NB, 2048, 1024], F32, kind="ExternalOutput").ap())
    mix_d = S.dram("mixs", [NB, 2048, 1024], BF16)
    yf_d = S.dram("yfs", [16, 128, 1536], F32)
    modrow_d = S.dram("modrows", [NB, 4, 1024], F32)
    dbgf_d = S.dram("dbgf", [24, 128, 512], F32)
    dbgb_d = S.dram("dbgb", [24, 128, 512], BF16)
    dbgn = {"f": 0, "b": 0, "names": []}

    def dump(name, v, bf):
        k = "b" if bf else "f"
        i = dbgn[k]
        dbgn[k] += 1
        dbgn["names"].append((name, k, i, tuple(v.ap.shape)))
        dst = (dbgb_d if bf else dbgf_d)
        p = v.ap.shape[0]
        n = 1
        for d_ in v.ap.shape[1:]:
            n *= d_
        dv = dst[i, 0:p, 0:n]
        if len(v.ap.shape) == 3:
            dv = dv.re("p (a c) -> p a c", a=v.ap.shape[1])
        S.dma("sp", dv, v)
    DEBUG["names"] = dbgn["names"]

    cb = S.sb("cb", [128, 1664], BF16)
    S.dma("pool", cb, cbf_d)
    ident, mUs, mUi, mLs, mLi = (cb[:, i * 128:(i + 1) * 128] for i in range(5))
    ones_bf = cb[:, 640:768]
    smask = [cb[:, 768 + l * 128:896 + l * 128] for l in range(7)]
    cf = S.sb("cf", [128, 8 * 128 + 4 + 256], F32)
    S.dma("sp", cf, cf_d)
    fm = [cf[:, i * 128:(i + 1) * 128] for i in range(8)]
    negcol = cf[:, 1024:1025]
    g16col = cf[:, 1025:1026]
    iota_p0 = cf[:, 1026:1027]
    iota_p1 = cf[:, 1027:1028]
    iota_f = cf[:, 1028:1284]
    modT = S.sb("modT", [128, 16, 3], F32)

    pbanks = [S.ps("pb%d" % i, [128, 512], F32) for i in range(6)]
    ptbanks = [S.ps("ptb%d" % i, [128, 1024], BF16) for i in range(2)]
    rot = {"p": 0, "t": 0}

    allbanks = pbanks + [V(t_.buf, t_.ap.bitcast(F32)) for t_ in ptbanks]

    def P():
        rot["p"] = (rot["p"] + 1) % 8
        return allbanks[rot["p"]]

    def PT():
        p_ = P()
        return V(p_.buf, p_.ap.bitcast(BF16))

    def layer_norm_stats(xt, st, mv, rstd, eps=1e-5):
        for c in range(2):
            o, i = st.ap[:, c, :], xt.ap[:, c * 512:(c + 1) * 512]
            S.op("dve", lambda e, o=o, i=i: e.bn_stats(o, i), reads=[xt.buf], writes=[st.buf])
        S.op("dve", lambda e: e.bn_aggr(mv.ap, st.ap), reads=[st.buf], writes=[mv.buf])
        S.act(rstd, mv[:, 1:2], AF.Sqrt, bias=eps)
        S.op("dve", lambda e: e.reciprocal(rstd.ap, rstd.ap), reads=[rstd.buf], writes=[rstd.buf])

    S.push()
    cT = S.sb("cT", [128, 8, 3], F32)
    S.dma("sp", cT, cT_d)
    scb = S.sb("scb", [128, 8, 3], BF16)
    S.act(scb, cT, AF.Silu)
    rep = []
    for b in range(NB):
        r_ = S.sb("rep%d" % b, [128, 8, 128], BF16)
        S.cp("dve", r_, scb[:, :, b:b + 1].bc([128, 8, 128]))
        rep.append(r_)
    adabT = S.sb("adabT", [128, 48], F32)
    S.dma("sp", adabT, adabT_d)
    blk = [S.sb("adablk%d" % i, [128, 8, 512], BF16) for i in range(2)]
    brow = [S.sb("adabrow%d" % i, [128, 512], F32) for i in range(2)]
    mrow = [S.sb("mrow%d" % i, [128, 512], F32) for i in range(2)]
    psT = P()
    psTv = psT[:, 0:48].re("p (j c) -> p j c", c=3)
    adaw_v = adaw_d.re("(kc p) c -> p kc c", p=128)
    for jb in range(12):
        bk = blk[jb % 2]
        S.dma("pool", bk, adaw_v[:, :, jb * 512:(jb + 1) * 512])
        if jb < 4:
            for jj in range(4):
                j = jb * 4 + jj
                for kc in range(8):
                    S.mm(psTv[:, j, :], bk[:, kc, jj * 128:(jj + 1) * 128], scb[:, kc, :],
                         start=(kc == 0), stop=(kc == 7))
            if jb == 3:
                for c in range(3):
                    S.tt("dve", modT[:, 0:8, c], psTv[:, 0:8, c], adabT[:, 0:8], ALU.add)
                    S.stt("dve", modT[:, 8:16, c], psTv[:, 8:16, c], 1.0, adabT[:, 8:16], ALU.add, ALU.add)
        else:
            which = (jb - 4) // 2
            half = (jb - 4) % 2
            br = brow[jb % 2]
            S.dma("sp", br, adab_d[:, jb * 512:(jb + 1) * 512].pb(128))
            for b in range(NB):
                pr = P()
                for kc in range(8):
                    S.mm(pr, rep[b][:, kc, :], bk[:, kc, :], start=(kc == 0), stop=(kc == 7))
                mr = mrow[b]
                if which == 2:
                    S.stt("dve", mr, pr, 1.0, br, ALU.add, ALU.add)
                else:
                    S.tt("dve", mr, pr, br, ALU.add)
                S.dma("sp", modrow_d[b, which:which + 1, half * 512:(half + 1) * 512], mr[0:1, :])
    S.pop()
    if DEBUG["stop_after"] == "A":
        S.finish()
        return nc

    S.push()
    Wa = S.sb("Wa", [128, 8, 3072], BF16)
    Wb = S.sb("Wb", [128, 8, 1536], BF16)
    Wl = [S.sb("Wl%d" % d, [128, 8, 96], BF16) for d in range(2)]
    Wlb = [S.sb("Wlb%d" % d, [128, 8, 96], BF16) for d in range(2)]
    Wgd = S.sb("Wgd", [128, 8, 96], BF16)
    Wgdb = S.sb("Wgdb", [128, 8, 96], BF16)
    sw = [S.sb("sw%d" % d, [96, 512], BF16) for d in range(2)]
    g2w = S.sb("g2w", [96, 512], BF16)
    br3 = S.sb("br3", [96, 2, 512], F32)
    onesf = S.sb("onesf", [96, 128], F32)
    S.memset("dve", onesf, 1.0)
    for d in range(2):
        S.dma("sp", br3[0:1, d, :], w0_d[d:d + 1, :])
        S.dma("sp", br3[32:33, d, :], a0_d[d:d + 1, :])
        S.dma("sp", br3[64:65, d, 0:256], gab_d[d:d + 1, :])
    rows = S.sb("rows", [128, 6, 512], F32)
    kkrow, karow, rkrow, gnwrow, gnbrow, gnormrow = (rows[:, i, :] for i in range(6))
    for i, dsrc in enumerate((kk_d, ka_d, rk_d, gnw_d, gnb_d, gnorm_d)):
        S.dma("sp", rows[:, i, :], dsrc.pb(128))
    for d in range(2):
        S.dma("pool", sw[d][0:32, :], w2_d[d])
        S.dma("pool", sw[d][32:64, :], a2_d[d])
        S.dma("pool", sw[d][64:80, 0:256], ga2_d[d])
    S.dma("pool", g2w, g2_d)

    S.push()
    mub = S.sb("mub", [128, 3328], F32)
    omm = S.sb("omm", [128, 3328], F32)
    S.dma("sp", mub, mu_d.pb(128))
    S.ts("dve", omm, mub, -1.0, ALU.mult, 1.0, ALU.add)
    stg = [S.sb("stg%d" % i, [128, 3328], F32) for i in range(2)]
    for kc in range(8):
        st_ = stg[kc % 2]
        S.dma("sp", st_, win_d[kc * 128:(kc + 1) * 128, :])
        S.tt("dve", Wa[:, kc, 0:1536], st_[:, 0:1536], omm[:, 0:1536], ALU.mult)
        S.tt("dve", Wa[:, kc, 1536:3072], st_[:, 1760:3296], omm[:, 1760:3296], ALU.mult)
        S.tt("pool", Wb[:, kc, :], st_[:, 0:1536], mub[:, 0:1536], ALU.mult)
        for d in range(2):
            for (o0, c0, n) in ((0, 1536 + 32 * d, 32), (32, 1600 + 32 * d, 32), (64, 3296 + 16 * d, 16)):
                S.tt("pool", Wl[d][:, kc, o0:o0 + n], st_[:, c0:c0 + n], omm[:, c0:c0 + n], ALU.mult)
            for (o0, c0, n) in ((0, 1536 + 32 * d, 32), (32, 1600 + 32 * d, 32)):
                S.tt("pool", Wlb[d][:, kc, o0:o0 + n], st_[:, c0:c0 + n], mub[:, c0:c0 + n], ALU.mult)
        S.tt("pool", Wgd[:, kc, :], st_[:, 1664:1760], omm[:, 1664:1760], ALU.mult)
        S.tt("pool", Wgdb[:, kc, :], st_[:, 1664:1760], mub[:, 1664:1760], ALU.mult)
    S.pop()
    for d in range(2):
        S.memset("pool", Wl[d][:, :, 80:96], 0.0)
        S.memset("pool", Wlb[d][:, :, 64:96], 0.0)

    def f32t(name, n=512, p=128):
        return S.sb(name, [p, n], F32)

    def bft(name, n=512, p=128):
        return S.sb(name, [p, n], BF16)

    xring = [f32t("xr%d" % i, 1024) for i in range(1)]
    hring = [S.sb("hT%d" % i, [128, 8, 128], BF16) for i in range(3)]
    hnb = S.sb("hnb", [128, 8, 128], BF16)
    st6 = S.sb("st6", [128, 2, 6], F32)
    mv2 = S.sb("mv2", [128, 2], F32)
    rstd1 = S.sb("rstd1", [128, 1], F32)
    r_sb, k_sb, v_sb, qk_sb = f32t("r_sb"), f32t("k_sb"), f32t("v_sb"), f32t("qk_sb")
    v_bf, vg_bf = bft("v_bf"), bft("vg_bf")
    lo = S.sb("lo", [96, 128], BF16)
    lo_gd = S.sb("lo_gd", [96, 128], BF16)
    sg, a_sb, lg = f32t("sg"), f32t("a_sb"), f32t("lg", 256)
    E1, E2, E3, E4 = f32t("E1"), f32t("E2"), f32t("E3"), f32t("E4")
    t0, t1, kk, kka, kh = f32t("t0"), f32t("t1"), f32t("kk"), f32t("kka"), f32t("kh")
    xn_bf = V(t0.buf, t0.ap.bitcast(BF16))
    nbacc = V(t1.buf, t1.ap.bitcast(BF16)).re("p (k t) -> p k t", k=8)
    ss = S.sb("ss", [128, 8], F32)
    bs = S.sb("bs", [128, 8], F32)
    pc_sb = S.sb("pc_sb", [64, 8], F32)
    pcg_sb = S.sb("pcg_sb", [64, 4], F32)
    At_bf, Bt_bf, Kt_bf, Rt_bf, Bh_bf, Kh_bf = (bft(n) for n in ("At", "Bt", "Kt", "Rt", "Bh", "Kh"))
    qt_bf, kt_bf, khg_bf = bft("qt", 256), bft("kt", 256), bft("khg", 256)

    def ht(name, p=128):
        return S.sb(name, [p, 4, 128], BF16)

    AtT, BtT, KtT, RtT = ht("AtT", 64), ht("BtT", 64), ht("KtT", 64), ht("RtT", 64)
    LakT, ArbT, ArkT, AqkT = ht("LakT"), ht("ArbT"), ht("ArkT"), ht("AqkT")
    Xr = [ht("Xr0"), ht("Xr1")]
    XTr = [ht("XTr0"), ht("XTr1")]
    Zr = [ht("Zr0"), ht("Zr1")]
    def half_aps(t):
        tb = t.ap.bitcast(BF16)
        return [tb[:, i * 512:(i + 1) * 512].rearrange("p (h t) -> p h t", h=4) for i in range(2)]
    QeffT, qtT, ktT = ht("QeffT", 64), ht("qtT", 64), ht("ktT", 64)
    mkB = ht("mkB")
    HT = S.sb("HT", [64, 4, 64], BF16)
    M_f = [S.sb("M_f%d" % d, [64, 8, 64], F32) for d in range(2)]
    M_bf = [S.sb("M_bf%d" % d, [64, 8, 64], BF16) for d in range(2)]
    Mg_f = [S.sb("Mg_f%d" % d, [64, 4, 128], F32) for d in range(2)]
    Mg_bf = [S.sb("Mg_bf%d" % d, [64, 4, 128], BF16) for d in range(2)]
    y_sb, bo_sb, o_sb = f32t("y_sb"), f32t("bo_sb"), f32t("o_sb")
    yfl = f32t("yfl", 1536)
    gout_sb, gsil = sg, a_sb
    mix_bf = bft("mix_bf", 1024)
    s8a, s8b, s8c, s8d = (S.sb("s8%s" % n, [128, 8], F32) for n in "abcd")

    if DEBUG.get("verbose"):
        print("mixer SBUF bytes remaining per partition:", nc.sbuf_bytes_remaining)

    def v4(pbank, p=128, w=128):
        return pbank[0:p, 0:4 * w].re("p (h t) -> p h t", h=4)

    def proj_block(dst, hT, hn, c0, ncols, rw):
        n = 16 if rw else 8
        i = 0
        for kc in range(8):
            S.mm(dst[:, 0:ncols], hT[:, kc, :], Wa[:, kc, c0:c0 + ncols], start=(i == 0), stop=(i == n - 1))
            i += 1
        if rw:
            for kc in range(8):
                S.mm(dst[:, 0:ncols], hn[:, kc, :], Wb[:, kc, c0:c0 + ncols], start=False, stop=(i == n - 1))
                i += 1

    def produce(b, kind, idx):
        xt = xring[0]
        src = ctx_d[b, idx * 128:(idx + 1) * 128, :] if kind == "c" else x_d[b, idx * 128:(idx + 1) * 128, :]
        S.dma("sp", xt, src)
        layer_norm_stats(xt, st6, mv2, rstd1)
        S.ts("dve", xn_bf, xt, mv2[:, 0:1], ALU.subtract, rstd1, ALU.mult)
        pt = PT()
        ptv = pt.re("p (k t) -> p k t", k=8)
        col = 2 if kind == "c" else b
        hs = hring[idx % 3]
        for kc in range(8):
            S.tr(ptv[:, kc, :], xn_bf[:, kc * 128:(kc + 1) * 128], ident)
        for kc in range(8):
            S.act(hs[:, kc, :], ptv[:, kc, :], AF.Identity, bias=modT[:, kc, col:col + 1],
                  scale=modT[:, 8 + kc, col:col + 1])

    def neighbor(kind, idx, n_tiles):
        C = hring[idx % 3]
        Pv = hring[(idx - 1) % 3] if idx > 0 else None
        Nx = hring[(idx + 1) % 3] if idx < n_tiles - 1 else None
        if kind == "l":
            if Pv is not None:
                S.tt("dve", nbacc[:, :, 0:64], C[:, :, 64:128], Pv[:, :, 64:128], ALU.add)
            else:
                S.cp("dve", nbacc[:, :, 0:64], C[:, :, 64:128])
            if Nx is not None:
                S.tt("dve", nbacc[:, :, 64:128], C[:, :, 0:64], Nx[:, :, 0:64], ALU.add)
            else:
                S.cp("dve", nbacc[:, :, 64:128], C[:, :, 0:64])
            a4 = nbacc.re("p k (r c) -> p k r c", r=2)
            c4 = C.re("p k (r c) -> p k r c", r=2)
            S.tt("dve", a4[:, :, :, 1:64], a4[:, :, :, 1:64], c4[:, :, :, 0:63], ALU.add)
            S.tt("dve", a4[:, :, :, 0:63], a4[:, :, :, 0:63], c4[:, :, :, 1:64], ALU.add)
            S.ts("dve", hnb, nbacc, 0.25, ALU.mult)
        else:
            S.cp("dve", nbacc[:, :, 1:128], C[:, :, 0:127])
            if Pv is not None:
                S.cp("dve", nbacc[:, :, 0:1], Pv[:, :, 127:128])
            else:
                S.memset("dve", nbacc[:, :, 0:1], 0.0)
            S.tt("dve", nbacc[:, :, 0:127], nbacc[:, :, 0:127], C[:, :, 1:128], ALU.add)
            if Nx is not None:
                S.tt("dve", nbacc[:, :, 127:128], nbacc[:, :, 127:128], Nx[:, :, 0:1], ALU.add)
            S.ts("dve", hnb, nbacc, 0.5, ALU.mult)

    def mix_tile(b, kind, idx, d, want_out, hoist=None):
        hT = hring[idx % 3]
        final = want_out and d == 1
        if d == 0:
            mi, me, mr = fm[0], fm[1], fm[2]
            gmi, gmr = fm[4], fm[6]
            m_ij_s, m_ji_s, m_ji_i = mLs, mUs, mUi
        else:
            mi, me, mr = fm[3], fm[2], fm[1]
            gmi, gmr = fm[7], fm[5]
            m_ij_s, m_ji_s, m_ji_i = mUs, mLs, mLi
        pl = P()
        for kc in range(8):
            S.mm(pl[0:96, 0:128], Wl[d][:, kc, :], hT[:, kc, :], start=(kc == 0), stop=False)
        for kc in range(8):
            S.mm(pl[0:96, 0:128], Wlb[d][:, kc, :], hnb[:, kc, :], start=False, stop=(kc == 7))
        S.act(lo[0:32, :], pl[0:32, 0:128], AF.Tanh)
        S.act(lo[32:64, :], pl[32:64, 0:128], AF.Copy)
        S.act(lo[64:80, :], pl[64:80, 0:128], AF.Copy)
        pr_ = P(); proj_block(pr_, hT, hnb, 0, 512, True)
        pw = P()
        S.mm(pw, lo[0:32, :], sw[d][0:32, :], start=True, stop=False)
        S.mm(pw, onesf[0:1, :], br3[0:1, d, :], start=False, stop=True)
        pa = P()
        S.mm(pa, lo[32:64, :], sw[d][32:64, :], start=True, stop=False)
        S.mm(pa, onesf[32:33, :], br3[32:33, d, :], start=False, stop=True)
        pg = P()
        S.mm(pg[:, 0:256], lo[64:80, :], sw[d][64:80, 0:256], start=True, stop=False)
        S.mm(pg[:, 0:256], onesf[64:65, :], br3[64:65, d, 0:256], start=False, stop=True)
        S.act(sg, pw, AF.Sigmoid)
        S.act(a_sb, pa, AF.Sigmoid)
        S.act(lg, pg[:, 0:256], AF.Sigmoid)
        S.cp("act", r_sb, pr_)
        pk_ = P(); proj_block(pk_, hT, hnb, 512, 512, True)
        pc1 = P(); S.mm(pc1, mi, sg)
        pc2 = P(); S.mm(pc2, me, sg)
        S.act(lg, lg, AF.Ln)
        S.act(E1, pc1, AF.Exp); S.act(E2, pc1, AF.Exp, scale=-1.0)
        S.act(E3, pc2, AF.Exp)
        S.cp("act", k_sb, pk_)
        S.tt("dve", t0, k_sb, kkrow, ALU.mult)
        S.tt("pool", t1, t0, t0, ALU.mult)
        S.red("dve", ss, t1.re("p (h c) -> p h c", h=8))
        S.act(ss, ss, AF.Ln, bias=1e-12)
        S.act(ss, ss, AF.Exp, scale=-0.5)
        S.tt("dve", kk.re("p (h c) -> p h c", h=8), t0.re("p (h c) -> p h c", h=8), ss.us(2).bc([128, 8, 64]), ALU.mult)
        pc3 = P(); S.mm(pc3, mr, sg)
        pp = P()
        for h in range(8):
            S.mm(pp[0:64, h:h + 1], sg[:, h * 64:(h + 1) * 64], negcol)
        for h in range(4):
            S.mm(pp[0:64, 8 + h:9 + h], lg[:, h * 64:(h + 1) * 64], g16col)
        pv_ = P(); proj_block(pv_, hT, hnb, 1024, 512, True)
        S.act(E4, pc3, AF.Exp)
        S.act(pc_sb, pp[0:64, 0:8], AF.Exp)
        S.act(pcg_sb, pp[0:64, 8:12], AF.Exp)
        S.cp("act", v_sb, pv_); S.cp("pool", v_bf, v_sb)
        p = P(); proj_block(p, hT, hnb, 1536, 512, False); S.cp("act", qk_sb, p)
        p = P(); proj_block(p, hT, hnb, 2048, 512, False); S.cp("act", vg_bf, p)
        if DEBUG["cut"] <= 2:
            return
        h8 = lambda t: t.re("p (h c) -> p h c", h=8)
        S.tt("pool", kka, kk, a_sb, ALU.mult)
        S.stt("pool", t1, a_sb, -1.0, karow, ALU.add, ALU.mult)
        S.stt("pool", kh, t1, 1.0, k_sb, ALU.add, ALU.mult)
        S.stt("dve", At_bf, kk, -1.0, E3, ALU.mult, ALU.mult)
        S.tt("dve", Bt_bf, kka, E2, ALU.mult)
        S.tt("dve", Kt_bf, kh, E2, ALU.mult)
        S.tt("dve", Rt_bf, r_sb, E1, ALU.mult)
        S.tt("pool", Bh_bf, kka, E4, ALU.mult)
        S.tt("pool", Kh_bf, kh, E4, ALU.mult)
        if want_out:
            S.tt("pool", t0, r_sb, kh, ALU.mult)
            S.tt("pool", t0, t0, rkrow, ALU.mult)
            S.red("dve", bs, h8(t0))
            S.tt("pool", h8(bo_sb), h8(v_sb), bs.us(2).bc([128, 8, 64]), ALU.mult)
        if DEBUG["cut"] <= 3:
            return
        dbg = DEBUG["dbg"] and b == 0 and kind == "c" and idx == 0 and d == 0
        if dbg:
            for nm, t_ in (("E1", E1), ("E2", E2), ("E3", E3), ("E4", E4), ("kk", kk), ("a", a_sb), ("sg", sg), ("kh", kh),
                           ("r", r_sb), ("v", v_sb)):
                dump(nm, t_, False)
            for nm, t_ in (("At", At_bf), ("Bt", Bt_bf), ("Kt", Kt_bf), ("Rt", Rt_bf), ("Bh", Bh_bf), ("Kh", Kh_bf), ("vb", v_bf)):
                dump(nm, t_, True)
            dump("pc", pc_sb, False)
        G1, G2, G4 = E1, E2, E3
        pg1 = P(); S.mm(pg1[:, 0:256], gmi, lg)
        pg3 = P(); S.mm(pg3[:, 0:256], gmr, lg)
        S.act(G1[:, 0:256], pg1[:, 0:256], AF.Exp)
        S.act(G2[:, 0:256], pg1[:, 0:256], AF.Exp, scale=-1.0)
        S.act(G4[:, 0:256], pg3[:, 0:256], AF.Exp)
        S.stt("dve", qt_bf, qk_sb[:, 0:256], 0.125, G1[:, 0:256], ALU.mult, ALU.mult)
        S.tt("dve", kt_bf, qk_sb[:, 256:512], G2[:, 0:256], ALU.mult)
        S.tt("pool", khg_bf, qk_sb[:, 256:512], G4[:, 0:256], ALU.mult)
        if DEBUG["cut"] <= 4:
            return
        if hoist is not None:
            hoist()
        parents = [E1, E2, E3, E4, sg, a_sb, t0, t1, kk, kka, kh, k_sb, qk_sb, r_sb]
        kids = [S.alias_acquire(pt_, half_aps(pt_)) for pt_ in parents]
        flat = [c for pair in kids[3:] for c in pair]
        G = [dict(AtT=AtT, BtT=BtT, KtT=KtT, RtT=RtT, X0=Xr[0], XT0=XTr[0], Lm=Xr[1], LTm=XTr[1],
                  LakT=LakT, ArbT=ArbT, ArkT=ArkT, Z0=Zr[0], Zf=Zr[1], D=kids[0], DT=kids[1],
                  Y1=kids[2][0], Y1t=kids[2][1], QeffT=QeffT, HT=HT),
             dict(AtT=flat[0][0:64], BtT=flat[1][0:64], KtT=flat[2][0:64], RtT=flat[3][0:64],
                  X0=flat[4], XT0=flat[5], Lm=flat[6], LTm=flat[7], LakT=flat[8], ArbT=flat[9], ArkT=flat[10],
                  Z0=flat[11], Zf=flat[12], D=[flat[13], flat[14]], DT=[flat[15], flat[16]],
                  Y1=flat[17], Y1t=flat[18], QeffT=flat[19][0:64], HT=flat[20][0:64, :, 0:64])]
        identb = ident.us(1).bc([128, 4, 128])

        def score(dst, L, R, mask):
            p_ = P()
            pv = v4(p_)
            for hh in range(4):
                S.mm(pv[:, hh, :], L[0:64, hh, :], R[0:64, hh, :])
            S.tt("dve", dst, pv, mask.us(1).bc([128, 4, 128]), ALU.mult)

        for g in range(2):
            T = G[g]
            for src, key, eng in ((At_bf, "AtT", "act"), (Bt_bf, "BtT", "dve"), (Kt_bf, "KtT", "act"), (Rt_bf, "RtT", "dve")):
                pt = PT()
                ptv = v4(pt, 64)
                for hh in range(4):
                    h = 4 * g + hh
                    S.tr(ptv[:, hh, :], src[:, h * 64:(h + 1) * 64], ident)
                S.cp(eng, T[key], ptv)
        for g in range(2):
            T = G[g]
            score(T["X0"], T["AtT"], T["BtT"], m_ij_s)
            score(T["XT0"], T["BtT"], T["AtT"], m_ji_s)
            score(T["LakT"], T["KtT"], T["AtT"], m_ji_s)
            score(T["ArbT"], T["BtT"], T["RtT"], m_ji_i)
            score(T["ArkT"], T["KtT"], T["RtT"], m_ji_i)
        for g in range(2):
            T = G[g]
            p = P()
            pv = p[:, 0:256].re("p (h v) -> p h v", h=4)
            for hh in range(4):
                h = 4 * g + hh
                S.mm(pv[:, hh, :], T["LakT"][:, hh, :], v_bf[:, h * 64:(h + 1) * 64])
            S.cp("act", T["Z0"][:, :, 0:64], pv)
            S.cp("pool", T["Z0"][:, :, 64:128], At_bf[:, g * 256:(g + 1) * 256].re("p (h c) -> p h c", h=4))
            m0 = smask[0].us(1).bc([128, 4, 128])
            S.tt("pool", T["Lm"], T["X0"], m0, ALU.mult)
            S.tt("pool", T["LTm"], T["XT0"], m0, ALU.mult)
            S.tt("dve", T["D"][0], T["Lm"], identb, ALU.add)
            S.tt("dve", T["DT"][0], T["LTm"], identb, ALU.add)
        U16 = mybir.dt.uint16
        mk_tiles = [flat[21], mkB]
        for l in range(1, 7):
            mk_bf = mk_tiles[l % 2]
            mk = V(mk_bf.buf, mk_bf.ap.bitcast(U16).rearrange("p h t -> p (h t)"))
            S.cp("pool", mk_bf, smask[l].us(1).bc([128, 4, 128]))
            for g in range(2):
                T = G[g]
                Dc, DTc = T["D"][0], T["DT"][0]
                p = P(); pv = v4(p)
                for hh in range(4):
                    S.mm(pv[:, hh, :], T["XT0"][:, hh, :], Dc[:, hh, :])
                S.cp("act", T["Y1"], pv)
                p = P(); pv = v4(p)
                for hh in range(4):
                    S.mm(pv[:, hh, :], T["X0"][:, hh, :], DTc[:, hh, :])
                S.cp("act", T["Y1t"], pv)
            for g in range(2):
                T = G[g]
                Dc, DTc = T["D"][0], T["DT"][0]
                p1 = P(); pv1 = v4(p1)
                for hh in range(4):
                    S.mm(pv1[:, hh, :], DTc[:, hh, :], T["Y1"][:, hh, :])
                p2 = P(); pv2 = v4(p2)
                for hh in range(4):
                    S.mm(pv2[:, hh, :], Dc[:, hh, :], T["Y1t"][:, hh, :])
                for dst, pvx in ((Dc, p1), (DTc, p2)):
                    o_, m_, d_ = dst.ap.rearrange("p h t -> p (h t)"), mk.ap, pvx.ap[:, 0:512]
                    S.op("dve", lambda e, o_=o_, m_=m_, d_=d_: e.copy_predicated(o_, m_, d_),
                         reads=[pvx.buf, mk.buf, dst.buf], writes=[dst.buf])
        for g in range(2):
            T = G[g]
            DTc = T["DT"][0]
            p = P(); pv = v4(p)
            for hh in range(4):
                S.mm(pv[:, hh, :], DTc[:, hh, :], T["Z0"][:, hh, :])
            S.cp("act" if g == 0 else "dve", T["Zf"], pv)
        for g in range(2):
            T = G[g]
            Zf = T["Zf"]
            p = P()
            pv = v4(p, 64)
            for hh in range(4):
                h = 4 * g + hh
                S.mm(pv[:, hh, :], Rt_bf[:, h * 64:(h + 1) * 64], ident, start=True, stop=False)
                S.mm(pv[:, hh, :], Zf[:, hh, 64:128], T["ArbT"][:, hh, :], start=False, stop=True)
            S.cp("act", T["QeffT"], pv)
            p = P()
            pvh = p[0:64, 0:256].re("p (h k) -> p h k", h=4)
            for hh in range(4):
                h = 4 * g + hh
                S.mm(pvh[:, hh, :], Zf[:, hh, 64:128], Bh_bf[:, h * 64:(h + 1) * 64])
            S.cp("dve", T["HT"], pvh)
        for g in range(2):
            T = G[g]
            Zf = T["Zf"]
            if want_out:
                p = P()
                pv = p[:, 0:256].re("p (h v) -> p h v", h=4)
                for hh in range(4):
                    h = 4 * g + hh
                    S.mm(pv[:, hh, :], T["QeffT"][:, hh, :], M_bf[d][:, h, :], start=True, stop=False)
                    S.mm(pv[:, hh, :], T["ArbT"][:, hh, :], Zf[:, hh, 0:64], start=False, stop=False)
                    S.mm(pv[:, hh, :], T["ArkT"][:, hh, :], v_bf[:, h * 64:(h + 1) * 64], start=False, stop=True)
                S.cp("act", y_sb[:, g * 256:(g + 1) * 256], p[:, 0:256])
            p = P()
            pvm = p[0:64, 0:256].re("p (h v) -> p h v", h=4)
            for hh in range(4):
                h = 4 * g + hh
                S.mm(pvm[:, hh, :], T["HT"][:, hh, :], M_bf[d][:, h, :], start=True, stop=False)
                S.mm(pvm[:, hh, :], Bh_bf[:, h * 64:(h + 1) * 64], Zf[:, hh, 0:64], start=False, stop=False)
                S.mm(pvm[:, hh, :], Kh_bf[:, h * 64:(h + 1) * 64], v_bf[:, h * 64:(h + 1) * 64], start=False, stop=True)
            Mv = M_f[d][:, 4 * g:4 * g + 4, :]
            S.tt("dve", Mv, Mv, pc_sb[:, 4 * g:4 * g + 4].us(2).bc([64, 4, 64]), ALU.mult)
            S.tt("dve", Mv, Mv, pvm, ALU.add)
            S.cp("dve", M_bf[d][:, 4 * g:4 * g + 4, :], Mv)
        for pt_, ch in zip(parents, kids):
            S.alias_release(pt_, ch)
        for src, dstT, eng in ((qt_bf, qtT, "act"), (kt_bf, ktT, "dve")):
            pt = PT()
            ptv = v4(pt, 64)
            for hh in range(4):
                S.tr(ptv[:, hh, :], src[:, hh * 64:(hh + 1) * 64], ident)
            S.cp(eng, dstT, ptv)
        p = P()
        pv = v4(p)
        for hh in range(4):
            S.mm(pv[:, hh, :], ktT[:, hh, :], qtT[:, hh, :])
        S.tt("dve", AqkT, pv, m_ji_i.us(1).bc([128, 4, 128]), ALU.mult)
        if want_out:
            p = P()
            pv = v4(p)
            for hh in range(4):
                S.mm(pv[:, hh, :], AqkT[:, hh, :], vg_bf[:, hh * 128:(hh + 1) * 128], start=True, stop=False)
                S.mm(pv[:, hh, :], qtT[:, hh, :], Mg_bf[d][:, hh, :], start=False, stop=True)
            S.cp("act", o_sb, p)
        p = P()
        pvg = v4(p, 64)
        for hh in range(4):
            S.mm(pvg[:, hh, :], khg_bf[:, hh * 64:(hh + 1) * 64], vg_bf[:, hh * 128:(hh + 1) * 128])
        S.tt("dve", Mg_f[d], Mg_f[d], pcg_sb.us(2).bc([64, 4, 128]), ALU.mult)
        S.tt("dve", Mg_f[d], Mg_f[d], pvg, ALU.add)
        S.cp("dve", Mg_bf[d], Mg_f[d])
        if not want_out:
            return
        if d == 0:
            S.dma("sp", yf_d[idx, :, 0:512], y_sb)
            S.dma("sp", yf_d[idx, :, 512:1024], bo_sb)
            S.dma("sp", yf_d[idx, :, 1024:1536], o_sb)
            return
        pl2 = P()
        for kc in range(8):
            S.mm(pl2[0:96, 0:128], Wgd[:, kc, :], hT[:, kc, :], start=(kc == 0), stop=False)
        for kc in range(8):
            S.mm(pl2[0:96, 0:128], Wgdb[:, kc, :], hnb[:, kc, :], start=False, stop=(kc == 7))
        S.act(lo_gd, pl2[0:96, 0:128], AF.Sigmoid)
        pgo = P()
        S.mm(pgo, lo_gd, g2w)
        S.cp("act", gout_sb, pgo)
        p = P(); proj_block(p, hT, hnb, 2560, 512, False)
        S.act(gsil, p, AF.Silu)
        S.dma("sp", yfl, yf_d[idx])
        S.tt("dve", y_sb, y_sb, yfl[:, 0:512], ALU.add)
        S.red("dve", s8a, h8(y_sb))
        S.tt("pool", t0, y_sb, y_sb, ALU.mult)
        S.red("dve", s8b, h8(t0))
        S.ts("dve", s8a, s8a, 1.0 / 64.0, ALU.mult)
        S.tt("dve", s8c, s8a, s8a, ALU.mult)
        S.stt("dve", s8b, s8b, 1.0 / 64.0, s8c, ALU.mult, ALU.subtract)
        S.act(s8b, s8b, AF.Sqrt, bias=GN_EPS)
        S.op("dve", lambda e: e.reciprocal(s8b.ap, s8b.ap), reads=[s8b.buf], writes=[s8b.buf])
        S.tt("dve", h8(y_sb), h8(y_sb), s8a.us(2).bc([128, 8, 64]), ALU.subtract)
        S.tt("dve", h8(y_sb), h8(y_sb), s8b.us(2).bc([128, 8, 64]), ALU.mult)
        S.tt("dve", y_sb, y_sb, gnwrow, ALU.mult)
        S.tt("dve", y_sb, y_sb, gnbrow, ALU.add)
        S.tt("dve", y_sb, y_sb, bo_sb, ALU.add)
        S.tt("dve", y_sb, y_sb, yfl[:, 512:1024], ALU.add)
        S.tt("dve", mix_bf[:, 0:512], y_sb, gout_sb, ALU.mult)
        S.tt("dve", o_sb, o_sb, yfl[:, 1024:1536], ALU.add)
        S.tt("pool", t0, o_sb, o_sb, ALU.mult)
        S.red("dve", s8d[:, 0:4], t0.re("p (h c) -> p h c", h=4))
        S.act(s8d[:, 0:4], s8d[:, 0:4], AF.Sqrt, bias=1e-5, scale=1.0 / 128.0)
        S.op("dve", lambda e: e.reciprocal(s8d.ap[:, 0:4], s8d.ap[:, 0:4]), reads=[s8d.buf], writes=[s8d.buf])
        o4 = o_sb.re("p (h c) -> p h c", h=4)
        S.tt("dve", o4, o4, s8d[:, 0:4].us(2).bc([128, 4, 128]), ALU.mult)
        S.tt("dve", o_sb, o_sb, gnormrow, ALU.mult)
        S.tt("dve", mix_bf[:, 512:1024], o_sb, gsil, ALU.mult)
        S.dma("sp", mix_d[b, idx * 128:(idx + 1) * 128, :], mix_bf)

    if DEBUG["stop_after"] == "Bs":
        S.pop()
        S.finish()
        return nc
    for b in range(NB):
        for d in range(2):
            S.memset("dve", M_f[d], 0.0)
            S.memset("dve", M_bf[d], 0.0)
            S.memset("pool", Mg_f[d], 0.0)
            S.memset("pool", Mg_bf[d], 0.0)
        for d in range(2):
            for kind, n_t in (("c", 2), ("l", 16)):
                order = list(range(n_t)) if d == 0 else list(range(n_t - 1, -1, -1))
                step = 1 if d == 0 else -1
                produce(b, kind, order[0])
                produce(b, kind, order[1])
                nb_done = False
                for i, idx in enumerate(order):
                    if not nb_done:
                        neighbor(kind, idx, n_t)
                    nb_next = (i + 1 < n_t) and not (d == 1 and kind == "l")

                    def hoist(i=i, nb_next=nb_next):
                        if i + 2 < n_t:
                            produce(b, kind, order[i + 2])
                        if nb_next:
                            neighbor(kind, order[i + 1], n_t)
                    mix_tile(b, kind, idx, d, kind == "l", hoist)
                    nb_done = nb_next
                    if DEBUG["stop_after"] == "B1":
                        break
                if DEBUG["stop_after"] == "B1":
                    break
    S.pop()
    if DEBUG["stop_after"] in ("B", "B1"):
        S.finish()
        return nc
    return moe_phase(nc, S, locals())


def moe_phase(nc, S, L):
    out_d, modrow_d = L["out_d"], L["modrow_d"]
    rtr_d, exg_d, exu_d, exd_d = L["rtr_d"], L["exg_d"], L["exu_d"], L["exd_d"]
    ln2g_d, ln2b_d, eoh_d = L["ln2g_d"], L["ln2b_d"], L["eoh_d"]
    ident, mUi, ones_bf = L["ident"], L["mUi"], L["ones_bf"]
    iota_f = L["iota_f"]
    pbanks, ptbanks = L["pbanks"], L["ptbanks"]
    layer_norm_stats = L["layer_norm_stats"]
    rot = {"p": 0, "t": 0}

    def P():
        rot["p"] = (rot["p"] + 1) % 2
        return pbanks[4 + rot["p"]]

    def PT():
        rot["t"] = (rot["t"] + 1) % 2
        return ptbanks[rot["t"]]

    Y = pbanks[0:4]
    exg_v = [exg_d[e].re("(kc p) f -> p kc f", p=128) for e in range(16)]
    exu_v = [exu_d[e].re("(kc p) f -> p kc f", p=128) for e in range(16)]
    exd_v = [exd_d[e].re("(fc p) d -> p fc d", p=128) for e in range(16)]

    x_d, mix_d, wout_d, ln1g_d, ln1b_d = L["x_d"], L["mix_d"], L["wout_d"], L["ln1g_d"], L["ln1b_d"]
    for b in range(NB):
        S.epoch()
        S.push()
        h2 = S.sb("h2", [128, 16, 1024], BF16)
        acc = S.sb("acc", [128, 16, 1024], F32)
        mrows = S.sb("mrows", [128, 3, 1024], F32)
        S.dma("sp", mrows[:, 0, :], modrow_d[b, 1:2, :].pb(128))
        S.dma("sp", mrows[:, 1, :], modrow_d[b, 2:3, :].pb(128))
        S.dma("sp", mrows[:, 2, :], modrow_d[b, 3:4, :].pb(128))
        identf = S.sb("identf", [128, 128], F32)
        S.cp("dve", identf, ident)
        aff_tok = S.sb("aff_tok", [128, 16, 16], F32)
        mask_tok = S.sb("mask_tok", [128, 16, 16], BF16)
        gate_tok = S.sb("gate_tok", [128, 16, 16], BF16)
        slot_tok = S.sb("slot_tok", [128, 16, 16], F32)
        m8 = S.sb("m8", [16, 8], F32)
        st6 = S.sb("mst6", [128, 2, 6], F32)
        mv2 = S.sb("mmv2", [128, 2], F32)
        rstd1 = S.sb("mrstd1", [128, 1], F32)
        xt = [S.sb("mx%d" % i, [128, 1024], F32) for i in range(2)]

        S.push()
        affT = S.sb("affT", [16, 2048], F32)
        wk = S.sb("wk", [16, 2048], F32)
        S.push()
        woutg = S.sb("woutg", [128, 8, 1024], BF16)
        g1row = S.sb("g1row", [128, 1024], F32)
        wstg = [S.sb("wstg%d" % i, [128, 1024], F32) for i in range(2)]
        lnrows = S.sb("lnrows", [128, 2, 1024], F32)
        S.dma("sp", lnrows[:, 0, :], ln1g_d.pb(128))
        S.dma("sp", lnrows[:, 1, :], ln1b_d.pb(128))
        S.dma("sp", g1row, modrow_d[b, 0:1, :].pb(128))
        for kc in range(8):
            ws = wstg[kc % 2]
            S.dma("sp", ws, wout_d[kc * 128:(kc + 1) * 128, :])
            S.tt("pool", woutg[:, kc, :], ws, g1row, ALU.mult)
        rtr = S.sb("rtr", [128, 8, 16], BF16)
        S.dma("pool", rtr, rtr_d.re("(kc p) e -> p kc e", p=128))
        mixt = [S.sb("mixt%d" % i, [128, 1024], BF16) for i in range(2)]
        mixT = S.sb("mixT", [128, 8, 128], BF16)
        x1t = S.sb("x1t", [128, 1024], F32)
        xnf = S.sb("xnf", [128, 1024], F32)
        h2T = S.sb("h2T", [128, 8, 128], BF16)
        sm1 = S.sb("sm1", [128, 1], F32)
        sm2 = S.sb("sm2", [128, 1], F32)
        nmr = S.sb("nmr", [128, 1], F32)
        lgt = S.sb("lgt", [128, 16], F32)
        for t in range(16):
            x = xt[t % 2]
            mt = mixt[t % 2]
            S.dma("sp", x, x_d[b, t * 128:(t + 1) * 128, :])
            S.dma("sp", mt, mix_d[b, t * 128:(t + 1) * 128, :])
            pt = PT()
            ptv = pt.re("p (k t) -> p k t", k=8)
            for kc in range(8):
                S.tr(ptv[:, kc, :], mt[:, kc * 128:(kc + 1) * 128], ident)
            S.cp("act", mixT, ptv)
            for half in range(2):
                pp_ = P()
                for kc in range(8):
                    S.mm(pp_, mixT[:, kc, :], woutg[:, kc, half * 512:(half + 1) * 512], start=(kc == 0), stop=(kc == 7))
                S.stt("dve", x1t[:, half * 512:(half + 1) * 512], x[:, half * 512:(half + 1) * 512], ALPHA, pp_,
                      ALU.mult, ALU.add)
            layer_norm_stats(x1t, st6, mv2, rstd1)
            S.stt("dve", nmr, mv2[:, 0:1], -1.0, rstd1, ALU.mult, ALU.mult)
            S.act(x1t, x1t, AF.Identity, bias=nmr, scale=rstd1)
            S.tt("dve", x1t, x1t, lnrows[:, 0, :], ALU.mult)
            S.tt("dve", x1t, x1t, lnrows[:, 1, :], ALU.add)
            S.act(acc[:, t, :], x1t, AF.Copy, scale=ALPHA)
            layer_norm_stats(x1t, st6, mv2, rstd1)
            S.stt("dve", nmr, mv2[:, 0:1], -1.0, rstd1, ALU.mult, ALU.mult)
            S.act(xnf, x1t, AF.Identity, bias=nmr, scale=rstd1)
            S.tt("dve", xnf, xnf, mrows[:, 1, :], ALU.mult)
            S.tt("dve", h2[:, t, :], xnf, mrows[:, 0, :], ALU.add)
            pt = PT()
            ptv = pt.re("p (k t) -> p k t", k=8)
            for kc in range(8):
                S.tr(ptv[:, kc, :], h2[:, t, kc * 128:(kc + 1) * 128], ident)
            S.cp("act", h2T, ptv)
            p = P()
            for kc in range(8):
                S.mm(p[:, 0:16], h2T[:, kc, :], rtr[:, kc, :], start=(kc == 0), stop=(kc == 7))
            S.op("dve", lambda e, p=p: e.reduce_max(sm1.ap, p.ap[:, 0:16], AX.X), reads=[p.buf], writes=[sm1.buf])
            S.ts("dve", sm1, sm1, -1.0, ALU.mult)
            S.act(lgt, p[:, 0:16], AF.Exp, bias=sm1)
            S.red("dve", sm2, lgt)
            S.op("dve", lambda e: e.reciprocal(sm2.ap, sm2.ap), reads=[sm2.buf], writes=[sm2.buf])
            S.ts("dve", aff_tok[:, t, :], lgt, sm2, ALU.mult)
            p2 = P()
            S.mm(p2[0:16, 0:128], aff_tok[:, t, :], identf)
            S.cp("act", affT[:, t * 128:(t + 1) * 128], p2[0:16, 0:128])
        S.pop()
        if DEBUG["stop_after"] == "C0":
            S.pop()
            S.pop()
            S.finish()
            return nc
        S.cp("dve", wk, affT)
        for r in range(32):
            S.op("dve", lambda e: e.max(m8.ap, wk.ap), reads=[wk.buf], writes=[m8.buf])
            if r < 31:
                S.op("dve", lambda e: e.match_replace(wk.ap, m8.ap, wk.ap, -1.0), reads=[wk.buf, m8.buf], writes=[wk.buf])
        S.ts("dve", wk, affT, m8[:, 7:8], ALU.is_ge)
        for t in range(16):
            p = P()
            S.mm(p[:, 0:16], wk[:, t * 128:(t + 1) * 128], identf[0:16, 0:16])
            S.cp("act", mask_tok[:, t, :], p[:, 0:16])
        for t in range(16):
            S.tt("dve", gate_tok[:, t, :], aff_tok[:, t, :], mask_tok[:, t, :], ALU.mult)
            p = P()
            for t2 in range(t + 1):
                S.mm(p[:, 0:16], mUi if t2 == t else ones_bf, mask_tok[:, t2, :], start=(t2 == 0), stop=(t2 == t))
            S.tt("dve", slot_tok[:, t, :], p[:, 0:16], mask_tok[:, t, :], ALU.mult)
            S.ts("dve", slot_tok[:, t, :], slot_tok[:, t, :], -1.0, ALU.add)
        S.pop()

        S.push()
        sel = S.sb("sel", [128, 16, 256], BF16)
        selT = [S.sb("selT%d" % i, [128, 2, 2048], BF16) for i in range(2)]
        xeT = [S.sb("xeT%d" % i, [128, 8, 256], BF16) for i in range(2)]
        actT = S.sb("actT", [128, 2, 256], BF16)
        sl = S.sb("sl", [128, 256], F32)
        y_bf = S.sb("y_bf", [128, 2, 1024], BF16)
        gslot = [S.sb("gslot%d" % i, [128, 2], F32) for i in range(2)]
        NW = 3
        wg_t = [S.sb("wg%d" % i, [128, 8, 256], BF16) for i in range(NW)]
        wu_t = [S.sb("wu%d" % i, [128, 8, 256], BF16) for i in range(NW)]
        wd_t = [S.sb("wd%d" % i, [128, 2, 1024], BF16) for i in range(NW)]
        paux = V(ptbanks[0].buf, ptbanks[0].ap.bitcast(F32))
        paux2 = V(ptbanks[1].buf, ptbanks[1].ap.bitcast(F32))
        ptr = ptbanks[1]

        def prep_pieces(e):
            k = e % 2
            pieces = []

            def mk_sel(t0_):
                def f():
                    for t in range(t0_, t0_ + 4):
                        S.ts("dve", sel[:, t, :], iota_f, slot_tok[:, t, e:e + 1], ALU.is_equal)
                return f
            for t0_ in range(0, 16, 4):
                pieces.append(mk_sel(t0_))

            def f_gslot():
                for jc in range(2):
                    for t in range(16):
                        S.mm(paux[:, jc:jc + 1], sel[:, t, jc * 128:(jc + 1) * 128], gate_tok[:, t, e:e + 1],
                             start=(t == 0), stop=(t == 15))
                S.cp("act", gslot[k], paux[:, 0:2])
            pieces.append(f_gslot)

            def mk_tr(jc, tq):
                def f():
                    ptv = ptr.re("p (k t) -> p k t", k=8)
                    for tt_ in range(8):
                        t = tq * 8 + tt_
                        S.tr(ptv[:, tt_, :], sel[:, t, jc * 128:(jc + 1) * 128], ident)
                    S.cp("act", selT[k][:, jc, tq * 1024:(tq + 1) * 1024], ptr)
                return f
            tail = []
            for jc in range(2):
                for tq in range(2):
                    tail.append(mk_tr(jc, tq))

            def mk_gather(dp):
                def f():
                    pv = paux.re("p (c j) -> p c j", c=2)
                    for c2 in range(2):
                        dc = dp * 2 + c2
                        for t in range(16):
                            S.mm(pv[:, c2, :], h2[:, t, dc * 128:(dc + 1) * 128], sel[:, t, :], start=(t == 0), stop=(t == 15))
                    S.cp("act", xeT[k][:, dp * 2:dp * 2 + 2, :], pv)
                return f
            for dp in range(4):
                pieces.append(mk_gather(dp))
            return pieces, tail

        def scatter_pieces(e):
            k = e % 2
            pieces = []

            def mk(t, dh):
                def f():
                    pa = paux if dh == 0 else paux2
                    for jc in range(2):
                        S.mm(pa, selT[k][:, jc, t * 128:(t + 1) * 128], y_bf[:, jc, dh * 512:(dh + 1) * 512],
                             start=(jc == 0), stop=(jc == 1))
                    a_ = acc[:, t, dh * 512:(dh + 1) * 512]
                    S.tt("dve", a_, a_, pa, ALU.add)
                return f
            for t in range(16):
                for dh in range(2):
                    pieces.append(mk(t, dh))
            return pieces

        pm_, pt_ = prep_pieces(0)
        for f in pm_ + pt_:
            f()
        gcount = 0
        for e in range(16):
            k = e % 2
            pend_sc = scatter_pieces(e - 1) if e > 0 else []
            pend_pr, pend_tr = prep_pieces(e + 1) if e < 15 else ([], [])
            chunk_i = 0
            wsel = {}

            def issue_w(fg):
                nonlocal gcount
                wi = gcount % NW
                gcount += 1
                f0 = fg * 256
                S.dma("pool", wg_t[wi], exg_v[e][:, :, f0:f0 + 256])
                S.dma("pool", wu_t[wi], exu_v[e][:, :, f0:f0 + 256])
                S.dma("pool", wd_t[wi], exd_v[e][:, fg * 2:fg * 2 + 2, :])
                wsel[fg] = wi

            def GU(n):
                fg, fc = n // 2, n % 2
                if fg not in wsel:
                    issue_w(fg)
                wi = wsel[fg]
                p = P()
                for kc in range(8):
                    S.mm(p[:, 0:256], wg_t[wi][:, kc, fc * 128:(fc + 1) * 128], xeT[k][:, kc, :], start=(kc == 0), stop=(kc == 7))
                for kc in range(8):
                    S.mm(p[:, 256:512], wu_t[wi][:, kc, fc * 128:(fc + 1) * 128], xeT[k][:, kc, :], start=(kc == 0), stop=(kc == 7))
                return p

            pcur = GU(0)
            for n in range(22):
                fg, fc = n // 2, n % 2
                wi = wsel[fg]
                pnext = GU(n + 1) if n < 21 else None
                S.act(sl, pcur[:, 0:256], AF.Silu)
                S.tt("dve", actT[:, fc, :], sl, pcur[:, 256:512], ALU.mult)
                for jc in range(2):
                    for dh in range(2):
                        S.mm(Y[jc * 2 + dh], actT[:, fc, jc * 128:(jc + 1) * 128], wd_t[wi][:, fc, dh * 512:(dh + 1) * 512],
                             start=(n == 0), stop=(n == 21))
                pcur = pnext
                chunk_i += 1
                if chunk_i <= 16:
                    for _ in range(min(2, len(pend_sc))):
                        pend_sc.pop(0)()
                if chunk_i <= 18 and chunk_i % 2 == 0 and pend_pr:
                    pend_pr.pop(0)()
                if chunk_i > 18:
                    while pend_sc:
                        pend_sc.pop(0)()
                    while pend_pr:
                        pend_pr.pop(0)()
                    if pend_tr:
                        pend_tr.pop(0)()
            while pend_sc:
                pend_sc.pop(0)()
            while pend_pr:
                pend_pr.pop(0)()
            while pend_tr:
                pend_tr.pop(0)()
            for jc in range(2):
                for dh in range(2):
                    S.stt("dve", y_bf[:, jc, dh * 512:(dh + 1) * 512], Y[jc * 2 + dh], gslot[k][:, jc:jc + 1],
                          mrows[:, 2, dh * 512:(dh + 1) * 512], ALU.mult, ALU.mult)
        for f in scatter_pieces(15):
            f()
        S.pop()
        S.push()
        ln2rows = S.sb("ln2rows", [128, 2, 1024], F32)
        S.dma("sp", ln2rows[:, 0, :], ln2g_d.pb(128))
        S.dma("sp", ln2rows[:, 1, :], ln2b_d.pb(128))
        for t in range(16):
            a_ = acc[:, t, :]
            layer_norm_stats(a_, st6, mv2, rstd1)
            o_ = xt[t % 2]
            S.ts("dve", o_, a_, mv2[:, 0:1], ALU.subtract, rstd1, ALU.mult)
            S.tt("dve", o_, o_, ln2rows[:, 0, :], ALU.mult)
            S.tt("dve", o_, o_, ln2rows[:, 1, :], ALU.add)
            S.dma("sp", out_d[b, t * 128:(t + 1) * 128, :], o_)
        S.pop()
        S.pop()
    S.finish()
    return nc


def kernel(x, c, ctx, c_ctx, ada_w, ada_b, w_in, rw_mu, rw_w0, rw_w2, rw_a0, rw_a2, rw_g2,
           rw_k_k, rw_k_a, rw_r_k, rw_gn_w, rw_gn_b, gla_a2, gla_a_b, gla_norm_w, w_out,
           ln1_g, ln1_b, router_w, ex_gate, ex_up, ex_down, ln2_g, ln2_b):
    f = lambda a: np.ascontiguousarray(np.asarray(a, dtype=np.float32))
    x, c, ctx, c_ctx = f(x), f(c), f(ctx), f(c_ctx)
    cbf, cfc = host_consts()
    eoh = np.zeros((16, 16 * 128), np.float32)
    for e in range(16):
        eoh[e, e * 128:(e + 1) * 128] = 1.0
    mu_ext = np.zeros((1, 3328), np.float32)
    mu_ext[0, :1760] = f(rw_mu)[0]
    shared = {
        "ada_w": f(ada_w)[0], "ada_b": f(ada_b)[0][None, :],
        "ada_bT": np.ascontiguousarray(f(ada_b)[0].reshape(48, 128).T),
        "w_in": f(w_in)[0], "mu_ext": mu_ext,
        "rw_w0": f(rw_w0)[0], "rw_w2": f(rw_w2)[0], "rw_a0": f(rw_a0)[0], "rw_a2": f(rw_a2)[0],
        "rw_g2": f(rw_g2)[0], "rw_k_k": f(rw_k_k)[0][None, :], "rw_k_a": f(rw_k_a)[0][None, :],
        "rw_r_k": f(rw_r_k)[0].reshape(1, 512), "rw_gn_w": f(rw_gn_w)[0][None, :], "rw_gn_b": f(rw_gn_b)[0][None, :],
        "gla_a2": f(gla_a2)[0], "gla_a_b": f(gla_a_b)[0], "gla_norm_w": f(gla_norm_w)[0][None, :],
        "w_out": f(w_out)[0], "ln1_g": f(ln1_g)[0][None, :], "ln1_b": f(ln1_b)[0][None, :],
        "router_w": f(router_w)[0], "ex_gate": f(ex_gate)[0], "ex_up": f(ex_up)[0], "ex_down": f(ex_down)[0],
        "ln2_g": f(ln2_g)[0][None, :], "ln2_b": f(ln2_b)[0][None, :],
        "cbf": cbf, "cf": cfc, "eoh": eoh,
    }
    in_maps = []
    for core in range(8):
        bs = slice(core * NB, (core + 1) * NB)
        cc = np.stack([c[core * NB], c[core * NB + 1], c_ctx], axis=1)
        cT = np.ascontiguousarray(cc.reshape(8, 128, 3).transpose(1, 0, 2))
        m = dict(shared)
        m["x"] = np.ascontiguousarray(x[bs])
        m["ctx"] = np.ascontiguousarray(ctx[bs])
        m["cT"] = cT
        in_maps.append(m)
    nc = build()
    res = run_bass_kernel_spmd(nc, in_maps, core_ids=list(range(8)))
    if DEBUG["dump"]:
        DEBUG["res"] = res.results
    return np.concatenate([np.asarray(r["out"], dtype=np.float32) for r in res.results], axis=0)
```

```python
from contextlib import ExitStack
import numpy as np
import concourse.bass as bass
import concourse.mybir as mybir
from concourse.bass_utils import run_bass_kernel_spmd

F32 = mybir.dt.float32
BF16 = mybir.dt.bfloat16
AF = mybir.ActivationFunctionType
ALU = mybir.AluOpType
AX = mybir.AxisListType


class Buf:
    __slots__ = ("name", "lw", "rd")

    def __init__(self, name):
        self.name = name
        self.lw = None
        self.rd = {}


class V:
    __slots__ = ("buf", "ap")

    def __init__(self, buf, ap):
        self.buf = buf
        self.ap = ap

    def __getitem__(self, k):
        return V(self.buf, self.ap[k])

    def bc(self, shape):
        return V(self.buf, self.ap.to_broadcast(list(shape)))

    def us(self, axis):
        return V(self.buf, self.ap.unsqueeze(axis))

    def re(self, pat, **kw):
        return V(self.buf, self.ap.rearrange(pat, **kw))

    def pb(self, n=128):
        return V(self.buf, self.ap.partition_broadcast(n))


class Sched:
    ENGS = ("pe", "act", "dve", "pool", "sp")

    def __init__(self, nc, ndma=12):
        self.nc = nc
        self.stack = ExitStack()
        self.scopes = [self.stack]
        self.sem = {}
        self.cnt = {}
        self.prog = {}
        self.seen = {}
        for e in self.ENGS:
            self.sem[e] = self.stack.enter_context(nc.semaphore("s_" + e))
            self.cnt[e] = 0
            self.prog[e] = []
            self.seen[e] = {}
        self.ep = 0
        self.dq = {}
        for q in ("sp", "pool", "act"):
            sems = [self.stack.enter_context(nc.semaphore("d_%s%d" % (q, i))) for i in range(ndma)]
            self.dq[q] = dict(sems=sems, vals=[0] * ndma, nxt=0)
        self.n_ops = 0

    def sb(self, name, shape, dtype):
        self.n_alloc = getattr(self, "n_alloc", 0) + 1
        t = self.scopes[-1].enter_context(self.nc.sbuf_tensor("t%d_%s" % (self.n_alloc, name), list(shape), dtype))
        return V(Buf(name), t[:])

    def ps(self, name, shape, dtype):
        t = self.scopes[-1].enter_context(self.nc.psum_tensor("p_" + name, list(shape), dtype))
        return V(Buf(name), t[:])

    def dram(self, name, shape, dtype, kind="Internal"):
        if DEBUG["dump"]:
            kind = "ExternalOutput"
        t = self.nc.dram_tensor(name, list(shape), dtype, kind=kind)
        return V(Buf(name), t.ap())

    def epoch(self):
        self.barrier()
        self.ep += 1
        for e in self.ENGS:
            self.sem[e] = self.stack.enter_context(self.nc.semaphore("s_%s_%d" % (e, self.ep)))
            self.cnt[e] = 0
            for k in [k for k in self.seen[e] if isinstance(k, str)]:
                del self.seen[e][k]

    def alias_acquire(self, parent, aps, name="al"):
        out = []
        for i, ap in enumerate(aps):
            b = Buf("%s_%s%d" % (parent.buf.name, name, i))
            b.lw = parent.buf.lw
            b.rd = dict(parent.buf.rd)
            out.append(V(b, ap))
        return out

    def alias_release(self, parent, children):
        for c in children:
            evs = list(c.buf.rd.values())
            if c.buf.lw is not None:
                evs.append(c.buf.lw)
            for ev in evs:
                old = parent.buf.rd.get(ev[0])
                if old is None or (old[2], old[1]) < (ev[2], ev[1]):
                    parent.buf.rd[ev[0]] = ev

    def push(self):
        self.scopes.append(ExitStack())

    def pop(self):
        self.barrier()
        self.flush()
        self.scopes.pop().close()

    def _semof(self, key):
        if isinstance(key, str):
            return self.sem[key]
        q, slot = key
        return self.dq[q]["sems"][slot]

    def _collect(self, eng, reads, writes, is_dma):
        waits = {}

        def need(ev, war=False):
            if ev is None:
                return
            key, val, dep_ep = ev
            if dep_ep < self.ep:
                return
            if not is_dma and isinstance(key, str) and key == eng:
                if eng == "pe" or war:
                    return
            if self.seen[eng].get(key, 0) >= val:
                return
            if waits.get(key, 0) < val:
                waits[key] = val

        for b in reads:
            need(b.lw)
        for b in writes:
            need(b.lw)
            for ev in b.rd.values():
                need(ev, war=True)
        return waits

    def _commit(self, eng, waits):
        for k, v in waits.items():
            self.seen[eng][k] = v
        return [(self._semof(k), v) for k, v in waits.items()]

    def _record(self, ev, reads, writes):
        for b in reads:
            b.rd[ev[0]] = ev
        for b in writes:
            b.lw = ev
            b.rd = {}

    def op(self, eng, fn, reads=(), writes=()):
        waits = self._collect(eng, reads, writes, False)
        wl = self._commit(eng, waits)
        self.cnt[eng] += 1
        ev = (eng, self.cnt[eng], self.ep)
        self.prog[eng].append((wl, fn, (self.sem[eng], 1)))
        self._record(ev, reads, writes)
        self.n_ops += 1

    def dma(self, q, out, in_, **kw):
        oap, iap = out.ap, in_.ap
        d = self.dq[q]
        slot = d["nxt"]
        d["nxt"] = (slot + 1) % len(d["sems"])
        waits = self._collect(q, [in_.buf], [out.buf], True)
        key = (q, slot)
        prev = d["vals"][slot]
        if prev and self.seen[q].get(key, 0) < prev:
            waits[key] = max(waits.get(key, 0), prev)
        wl = self._commit(q, waits)
        d["vals"][slot] = prev + 16
        ev = (key, prev + 16, self.ep)
        sem = d["sems"][slot]
        self.prog[q].append((wl, lambda e: e.dma_start(out=oap, in_=iap, **kw), (sem, 16)))
        self._record(ev, [in_.buf], [out.buf])
        self.n_ops += 1

    @staticmethod
    def _bufs(*xs):
        return [x.buf for x in xs if isinstance(x, V)]

    @staticmethod
    def _a(x):
        return x.ap if isinstance(x, V) else x

    def mm(self, out, lhsT, rhs, start=True, stop=True):
        o, l, r = out.ap, lhsT.ap, rhs.ap
        self.op("pe", lambda e: e.matmul(o, l, r, start=start, stop=stop),
                reads=self._bufs(lhsT, rhs), writes=[out.buf])

    def tr(self, out, in_, ident):
        o, i, d = out.ap, in_.ap, ident.ap
        self.op("pe", lambda e: e.transpose(o, i, d), reads=self._bufs(in_, ident), writes=[out.buf])

    def act(self, out, in_, func, bias=None, scale=None):
        kw = {}
        if bias is not None:
            kw["bias"] = self._a(bias)
        if scale is not None:
            kw["scale"] = self._a(scale)
        o, i = out.ap, in_.ap
        self.op("act", lambda e: e.activation(o, i, func, **kw),
                reads=self._bufs(in_, bias, scale), writes=[out.buf])

    def tt(self, eng, out, a, b, op):
        o, x, y = out.ap, a.ap, b.ap
        self.op(eng, lambda e: e.tensor_tensor(o, x, y, op), reads=self._bufs(a, b), writes=[out.buf])

    def ts(self, eng, out, a, s1, op0, s2=None, op1=None):
        o, x, p1, p2 = out.ap, a.ap, self._a(s1), self._a(s2)
        if op1 is None:
            self.op(eng, lambda e: e.tensor_scalar(o, x, p1, None, op0),
                    reads=self._bufs(a, s1), writes=[out.buf])
        else:
            self.op(eng, lambda e: e.tensor_scalar(o, x, p1, p2, op0, op1),
                    reads=self._bufs(a, s1, s2), writes=[out.buf])

    def stt(self, eng, out, a, sc, b, op0, op1):
        o, x, p, y = out.ap, a.ap, self._a(sc), b.ap
        eng = "dve"
        self.op(eng, lambda e: e.scalar_tensor_tensor(o, x, p, y, op0, op1),
                reads=self._bufs(a, sc, b), writes=[out.buf])

    def cp(self, eng, out, a):
        o, x = out.ap, a.ap
        if eng == "act":
            self.op("act", lambda e: e.activation(o, x, AF.Copy), reads=[a.buf], writes=[out.buf])
        else:
            self.op(eng, lambda e: e.tensor_copy(o, x), reads=[a.buf], writes=[out.buf])

    def red(self, eng, out, a, op=None):
        o, x = out.ap, a.ap
        op = ALU.add if op is None else op
        self.op(eng, lambda e: e.tensor_reduce(o, x, AX.X, op), reads=[a.buf], writes=[out.buf])

    def memset(self, eng, out, val):
        o = out.ap
        self.op(eng, lambda e: e.memset(o, val), writes=[out.buf])

    def barrier(self):
        for e in self.ENGS:
            waits = {}
            for e2 in self.ENGS:
                if e2 != e and self.cnt[e2] > self.seen[e].get(e2, 0):
                    waits[e2] = self.cnt[e2]
            if e not in ("pe",) and self.cnt[e] > self.seen[e].get(e, 0):
                waits[e] = self.cnt[e]
            for q, d in self.dq.items():
                for slot, v in enumerate(d["vals"]):
                    if v and self.seen[e].get((q, slot), 0) < v:
                        waits[(q, slot)] = v
            wl = self._commit(e, waits)
            if wl:
                self.prog[e].append((wl, None, None))

    def flush(self):
        nc = self.nc
        prog = self.prog
        self.prog = {e: [] for e in self.ENGS}

        def replay(name, e):
            for wl, fn, inc in prog[name]:
                for sem, val in wl:
                    e.wait_ge(sem, val)
                if fn is not None:
                    ins = fn(e)
                    ins.then_inc(inc[0], inc[1])

        with nc.Block() as block:
            @block.tensor
            def _(e):
                replay("pe", e)

            @block.scalar
            def _(e):
                replay("act", e)

            @block.vector
            def _(e):
                replay("dve", e)

            @block.gpsimd
            def _(e):
                replay("pool", e)

            @block.sync
            def _(e):
                replay("sp", e)

    def finish(self):
        self.barrier()
        self.flush()
        self.stack.close()


NB = 2
ALPHA = float(2.0 ** 0.25)
CRW = -float(np.exp(-0.5))
GN_EPS = 64e-5
DEBUG = {"stop_after": None, "cut": 99, "dump": False, "res": None, "dbg": False}


def host_consts():
    p = np.arange(128)[:, None]
    q = np.arange(128)[None, :]
    Us = (p < q).astype(np.float32)
    Ui = (p <= q).astype(np.float32)
    Ls = (p > q).astype(np.float32)
    Li = (p >= q).astype(np.float32)
    ident = (p == q).astype(np.float32)
    sm = []
    for l in range(7):
        sz = 2 ** l
        same = (p // (2 * sz)) == (q // (2 * sz))
        lo = same & ((p % (2 * sz)) >= sz) & ((q % (2 * sz)) < sz)
        sm.append((lo | lo.T).astype(np.float32))
    cbf = np.concatenate([ident, Us, Ui, Ls, Li, np.ones((128, 128), np.float32)] + sm, axis=1)
    cf = np.concatenate([Ui * CRW, Us * CRW, Ls * CRW, Li * CRW,
                         Ui / 16.0, Us / 16.0, Ls / 16.0, Li / 16.0,
                         np.full((128, 1), CRW, np.float32), np.full((128, 1), 1.0 / 16.0, np.float32),
                         np.arange(128, dtype=np.float32)[:, None], np.arange(128, dtype=np.float32)[:, None] + 128.0,
                         np.tile(np.arange(256, dtype=np.float32)[None, :], (128, 1))], axis=1)
    return np.ascontiguousarray(cbf), np.ascontiguousarray(cf.astype(np.float32))


def build():
    nc = bass.Bass("TRN2", target_bir_lowering=False)
    S = Sched(nc)

    def din(name, shape):
        return V(Buf(name), nc.dram_tensor(name, list(shape), F32, kind="ExternalInput").ap())

    x_d = din("x", [NB, 2048, 1024])
    ctx_d = din("ctx", [NB, 256, 1024])
    cT_d = din("cT", [128, 8, 3])
    adaw_d = din("ada_w", [1024, 6144])
    adab_d = din("ada_b", [1, 6144])
    adabT_d = din("ada_bT", [128, 48])
    win_d = din("w_in", [1024, 3328])
    mu_d = din("mu_ext", [1, 3328])
    w0_d = din("rw_w0", [2, 512])
    w2_d = din("rw_w2", [2, 32, 512])
    a0_d = din("rw_a0", [2, 512])
    a2_d = din("rw_a2", [2, 32, 512])
    g2_d = din("rw_g2", [96, 512])
    kk_d = din("rw_k_k", [1, 512])
    ka_d = din("rw_k_a", [1, 512])
    rk_d = din("rw_r_k", [1, 512])
    gnw_d = din("rw_gn_w", [1, 512])
    gnb_d = din("rw_gn_b", [1, 512])
    ga2_d = din("gla_a2", [2, 16, 256])
    gab_d = din("gla_a_b", [2, 256])
    gnorm_d = din("gla_norm_w", [1, 512])
    wout_d = din("w_out", [1024, 1024])
    ln1g_d = din("ln1_g", [1, 1024])
    ln1b_d = din("ln1_b", [1, 1024])
    rtr_d = din("router_w", [1024, 16])
    exg_d = din("ex_gate", [16, 1024, 2816])
    exu_d = din("ex_up", [16, 1024, 2816])
    exd_d = din("ex_down", [16, 2816, 1024])
    ln2g_d = din("ln2_g", [1, 1024])
    ln2b_d = din("ln2_b", [1, 1024])
    cbf_d = din("cbf", [128, 1664])
    cf_d = din("cf", [128, 8 * 128 + 4 + 256])
    eoh_d = din("eoh", [16, 16 * 128])
    out_d = V(Buf("out"), nc.dram_tensor("out", [NB, 2048, 1024], F32, kind="ExternalOutput").ap())
    mix_d = S.dram("mixs", [NB, 2048, 1024], BF16)
    yf_d = S.dram("yfs", [16, 128, 1536], F32)
    modrow_d = S.dram("modrows", [NB, 4, 1024], F32)
    dbgf_d = S.dram("dbgf", [24, 128, 512], F32)
    dbgb_d = S.dram("dbgb", [24, 128, 512], BF16)
    dbgn = {"f": 0, "b": 0, "names": []}

    def dump(name, v, bf):
        k = "b" if bf else "f"
        i = dbgn[k]
        dbgn[k] += 1
        dbgn["names"].append((name, k, i, tuple(v.ap.shape)))
        dst = (dbgb_d if bf else dbgf_d)
        p = v.ap.shape[0]
        n = 1
        for d_ in v.ap.shape[1:]:
            n *= d_
        dv = dst[i, 0:p, 0:n]
        if len(v.ap.shape) == 3:
            dv = dv.re("p (a c) -> p a c", a=v.ap.shape[1])
        S.dma("sp", dv, v)
    DEBUG["names"] = dbgn["names"]

    cb = S.sb("cb", [128, 1664], BF16)
    S.dma("pool", cb, cbf_d)
    ident, mUs, mUi, mLs, mLi = (cb[:, i * 128:(i + 1) * 128] for i in range(5))
    ones_bf = cb[:, 640:768]
    smask = [cb[:, 768 + l * 128:896 + l * 128] for l in range(7)]
    cf = S.sb("cf", [128, 8 * 128 + 4 + 256], F32)
    S.dma("sp", cf, cf_d)
    fm = [cf[:, i * 128:(i + 1) * 128] for i in range(8)]
    negcol = cf[:, 1024:1025]
    g16col = cf[:, 1025:1026]
    iota_p0 = cf[:, 1026:1027]
    iota_p1 = cf[:, 1027:1028]
    iota_f = cf[:, 1028:1284]
    modT = S.sb("modT", [128, 16, 3], F32)

    pbanks = [S.ps("pb%d" % i, [128, 512], F32) for i in range(6)]
    ptbanks = [S.ps("ptb%d" % i, [128, 1024], BF16) for i in range(2)]
    rot = {"p": 0, "t": 0}

    allbanks = pbanks + [V(t_.buf, t_.ap.bitcast(F32)) for t_ in ptbanks]

    def P():
        rot["p"] = (rot["p"] + 1) % 8
        return allbanks[rot["p"]]

    def PT():
        p_ = P()
        return V(p_.buf, p_.ap.bitcast(BF16))

    def layer_norm_stats(xt, st, mv, rstd, eps=1e-5):
        for c in range(2):
            o, i = st.ap[:, c, :], xt.ap[:, c * 512:(c + 1) * 512]
            S.op("dve", lambda e, o=o, i=i: e.bn_stats(o, i), reads=[xt.buf], writes=[st.buf])
        S.op("dve", lambda e: e.bn_aggr(mv.ap, st.ap), reads=[st.buf], writes=[mv.buf])
        S.act(rstd, mv[:, 1:2], AF.Sqrt, bias=eps)
        S.op("dve", lambda e: e.reciprocal(rstd.ap, rstd.ap), reads=[rstd.buf], writes=[rstd.buf])

    S.push()
    cT = S.sb("cT", [128, 8, 3], F32)
    S.dma("sp", cT, cT_d)
    scb = S.sb("scb", [128, 8, 3], BF16)
    S.act(scb, cT, AF.Silu)
    rep = []
    for b in range(NB):
        r_ = S.sb("rep%d" % b, [128, 8, 128], BF16)
        S.cp("dve", r_, scb[:, :, b:b + 1].bc([128, 8, 128]))
        rep.append(r_)
    adabT = S.sb("adabT", [128, 48], F32)
    S.dma("sp", adabT, adabT_d)
    blk = [S.sb("adablk%d" % i, [128, 8, 512], BF16) for i in range(2)]
    brow = [S.sb("adabrow%d" % i, [128, 512], F32) for i in range(2)]
    mrow = [S.sb("mrow%d" % i, [128, 512], F32) for i in range(2)]
    psT = P()
    psTv = psT[:, 0:48].re("p (j c) -> p j c", c=3)
    adaw_v = adaw_d.re("(kc p) c -> p kc c", p=128)
    for jb in range(12):
        bk = blk[jb % 2]
        S.dma("pool", bk, adaw_v[:, :, jb * 512:(jb + 1) * 512])
        if jb < 4:
            for jj in range(4):
                j = jb * 4 + jj
                for kc in range(8):
                    S.mm(psTv[:, j, :], bk[:, kc, jj * 128:(jj + 1) * 128], scb[:, kc, :],
                         start=(kc == 0), stop=(kc == 7))
            if jb == 3:
                for c in range(3):
                    S.tt("dve", modT[:, 0:8, c], psTv[:, 0:8, c], adabT[:, 0:8], ALU.add)
                    S.stt("dve", modT[:, 8:16, c], psTv[:, 8:16, c], 1.0, adabT[:, 8:16], ALU.add, ALU.add)
        else:
            which = (jb - 4) // 2
            half = (jb - 4) % 2
            br = brow[jb % 2]
            S.dma("sp", br, adab_d[:, jb * 512:(jb + 1) * 512].pb(128))
            for b in range(NB):
                pr = P()
                for kc in range(8):
                    S.mm(pr, rep[b][:, kc, :], bk[:, kc, :], start=(kc == 0), stop=(kc == 7))
                mr = mrow[b]
                if which == 2:
                    S.stt("dve", mr, pr, 1.0, br, ALU.add, ALU.add)
                else:
                    S.tt("dve", mr, pr, br, ALU.add)
                S.dma("sp", modrow_d[b, which:which + 1, half * 512:(half + 1) * 512], mr[0:1, :])
    S.pop()
    if DEBUG["stop_after"] == "A":
        S.finish()
        return nc

    S.push()
    Wa = S.sb("Wa", [128, 8, 3072], BF16)
    Wb = S.sb("Wb", [128, 8, 1536], BF16)
    Wl = [S.sb("Wl%d" % d, [128, 8, 96], BF16) for d in range(2)]
    Wlb = [S.sb("Wlb%d" % d, [128, 8, 96], BF16) for d in range(2)]
    Wgd = S.sb("Wgd", [128, 8, 96], BF16)
    Wgdb = S.sb("Wgdb", [128, 8, 96], BF16)
    sw = [S.sb("sw%d" % d, [96, 512], BF16) for d in range(2)]
    g2w = S.sb("g2w", [96, 512], BF16)
    br3 = S.sb("br3", [96, 2, 512], F32)
    onesf = S.sb("onesf", [96, 128], F32)
    S.memset("dve", onesf, 1.0)
    for d in range(2):
        S.dma("sp", br3[0:1, d, :], w0_d[d:d + 1, :])
        S.dma("sp", br3[32:33, d, :], a0_d[d:d + 1, :])
        S.dma("sp", br3[64:65, d, 0:256], gab_d[d:d + 1, :])
    rows = S.sb("rows", [128, 6, 512], F32)
    kkrow, karow, rkrow, gnwrow, gnbrow, gnormrow = (rows[:, i, :] for i in range(6))
    for i, dsrc in enumerate((kk_d, ka_d, rk_d, gnw_d, gnb_d, gnorm_d)):
        S.dma("sp", rows[:, i, :], dsrc.pb(128))
    for d in range(2):
        S.dma("pool", sw[d][0:32, :], w2_d[d])
        S.dma("pool", sw[d][32:64, :], a2_d[d])
        S.dma("pool", sw[d][64:80, 0:256], ga2_d[d])
    S.dma("pool", g2w, g2_d)

    S.push()
    mub = S.sb("mub", [128, 3328], F32)
    omm = S.sb("omm", [128, 3328], F32)
    S.dma("sp", mub, mu_d.pb(128))
    S.ts("dve", omm, mub, -1.0, ALU.mult, 1.0, ALU.add)
    stg = [S.sb("stg%d" % i, [128, 3328], F32) for i in range(2)]
    for kc in range(8):
        st_ = stg[kc % 2]
        S.dma("sp", st_, win_d[kc * 128:(kc + 1) * 128, :])
        S.tt("dve", Wa[:, kc, 0:1536], st_[:, 0:1536], omm[:, 0:1536], ALU.mult)
        S.tt("dve", Wa[:, kc, 1536:3072], st_[:, 1760:3296], omm[:, 1760:3296], ALU.mult)
        S.tt("pool", Wb[:, kc, :], st_[:, 0:1536], mub[:, 0:1536], ALU.mult)
        for d in range(2):
            for (o0, c0, n) in ((0, 1536 + 32 * d, 32), (32, 1600 + 32 * d, 32), (64, 3296 + 16 * d, 16)):
                S.tt("pool", Wl[d][:, kc, o0:o0 + n], st_[:, c0:c0 + n], omm[:, c0:c0 + n], ALU.mult)
            for (o0, c0, n) in ((0, 1536 + 32 * d, 32), (32, 1600 + 32 * d, 32)):
                S.tt("pool", Wlb[d][:, kc, o0:o0 + n], st_[:, c0:c0 + n], mub[:, c0:c0 + n], ALU.mult)
        S.tt("pool", Wgd[:, kc, :], st_[:, 1664:1760], omm[:, 1664:1760], ALU.mult)
        S.tt("pool", Wgdb[:, kc, :], st_[:, 1664:1760], mub[:, 1664:1760], ALU.mult)
    S.pop()
    for d in range(2):
        S.memset("pool", Wl[d][:, :, 80:96], 0.0)
        S.memset("pool", Wlb[d][:, :, 64:96], 0.0)

    def f32t(name, n=512, p=128):
        return S.sb(name, [p, n], F32)

    def bft(name, n=512, p=128):
        return S.sb(name, [p, n], BF16)

    xring = [f32t("xr%d" % i, 1024) for i in range(1)]
    hring = [S.sb("hT%d" % i, [128, 8, 128], BF16) for i in range(3)]
    hnb = S.sb("hnb", [128, 8, 128], BF16)
    st6 = S.sb("st6", [128, 2, 6], F32)
    mv2 = S.sb("mv2", [128, 2], F32)
    rstd1 = S.sb("rstd1", [128, 1], F32)
    r_sb, k_sb, v_sb, qk_sb = f32t("r_sb"), f32t("k_sb"), f32t("v_sb"), f32t("qk_sb")
    v_bf, vg_bf = bft("v_bf"), bft("vg_bf")
    lo = S.sb("lo", [96, 128], BF16)
    lo_gd = S.sb("lo_gd", [96, 128], BF16)
    sg, a_sb, lg = f32t("sg"), f32t("a_sb"), f32t("lg", 256)
    E1, E2, E3, E4 = f32t("E1"), f32t("E2"), f32t("E3"), f32t("E4")
    t0, t1, kk, kka, kh = f32t("t0"), f32t("t1"), f32t("kk"), f32t("kka"), f32t("kh")
    xn_bf = V(t0.buf, t0.ap.bitcast(BF16))
    nbacc = V(t1.buf, t1.ap.bitcast(BF16)).re("p (k t) -> p k t", k=8)
    ss = S.sb("ss", [128, 8], F32)
    bs = S.sb("bs", [128, 8], F32)
    pc_sb = S.sb("pc_sb", [64, 8], F32)
    pcg_sb = S.sb("pcg_sb", [64, 4], F32)
    At_bf, Bt_bf, Kt_bf, Rt_bf, Bh_bf, Kh_bf = (bft(n) for n in ("At", "Bt", "Kt", "Rt", "Bh", "Kh"))
    qt_bf, kt_bf, khg_bf = bft("qt", 256), bft("kt", 256), bft("khg", 256)

    def ht(name, p=128):
        return S.sb(name, [p, 4, 128], BF16)

    AtT, BtT, KtT, RtT = ht("AtT", 64), ht("BtT", 64), ht("KtT", 64), ht("RtT", 64)
    LakT, ArbT, ArkT, AqkT = ht("LakT"), ht("ArbT"), ht("ArkT"), ht("AqkT")
    Xr = [ht("Xr0"), ht("Xr1")]
    XTr = [ht("XTr0"), ht("XTr1")]
    Zr = [ht("Zr0"), ht("Zr1")]
    def half_aps(t):
        tb = t.ap.bitcast(BF16)
        return [tb[:, i * 512:(i + 1) * 512].rearrange("p (h t) -> p h t", h=4) for i in range(2)]
    QeffT, qtT, ktT = ht("QeffT", 64), ht("qtT", 64), ht("ktT", 64)
    mkB = ht("mkB")
    HT = S.sb("HT", [64, 4, 64], BF16)
    M_f = [S.sb("M_f%d" % d, [64, 8, 64], F32) for d in range(2)]
    M_bf = [S.sb("M_bf%d" % d, [64, 8, 64], BF16) for d in range(2)]
    Mg_f = [S.sb("Mg_f%d" % d, [64, 4, 128], F32) for d in range(2)]
    Mg_bf = [S.sb("Mg_bf%d" % d, [64, 4, 128], BF16) for d in range(2)]
    y_sb, bo_sb, o_sb = f32t("y_sb"), f32t("bo_sb"), f32t("o_sb")
    yfl = f32t("yfl", 1536)
    gout_sb, gsil = sg, a_sb
    mix_bf = bft("mix_bf", 1024)
    s8a, s8b, s8c, s8d = (S.sb("s8%s" % n, [128, 8], F32) for n in "abcd")

    if DEBUG.get("verbose"):
        print("mixer SBUF bytes remaining per partition:", nc.sbuf_bytes_remaining)

    def v4(pbank, p=128, w=128):
        return pbank[0:p, 0:4 * w].re("p (h t) -> p h t", h=4)

    def proj_block(dst, hT, hn, c0, ncols, rw):
        n = 16 if rw else 8
        i = 0
        for kc in range(8):
            S.mm(dst[:, 0:ncols], hT[:, kc, :], Wa[:, kc, c0:c0 + ncols], start=(i == 0), stop=(i == n - 1))
            i += 1
        if rw:
            for kc in range(8):
                S.mm(dst[:, 0:ncols], hn[:, kc, :], Wb[:, kc, c0:c0 + ncols], start=False, stop=(i == n - 1))
                i += 1

    def produce(b, kind, idx):
        xt = xring[0]
        src = ctx_d[b, idx * 128:(idx + 1) * 128, :] if kind == "c" else x_d[b, idx * 128:(idx + 1) * 128, :]
        S.dma("sp", xt, src)
        layer_norm_stats(xt, st6, mv2, rstd1)
        S.ts("dve", xn_bf, xt, mv2[:, 0:1], ALU.subtract, rstd1, ALU.mult)
        pt = PT()
        ptv = pt.re("p (k t) -> p k t", k=8)
        col = 2 if kind == "c" else b
        hs = hring[idx % 3]
        for kc in range(8):
            S.tr(ptv[:, kc, :], xn_bf[:, kc * 128:(kc + 1) * 128], ident)
        for kc in range(8):
            S.act(hs[:, kc, :], ptv[:, kc, :], AF.Identity, bias=modT[:, kc, col:col + 1],
                  scale=modT[:, 8 + kc, col:col + 1])

    def neighbor(kind, idx, n_tiles):
        C = hring[idx % 3]
        Pv = hring[(idx - 1) % 3] if idx > 0 else None
        Nx = hring[(idx + 1) % 3] if idx < n_tiles - 1 else None
        if kind == "l":
            if Pv is not None:
                S.tt("dve", nbacc[:, :, 0:64], C[:, :, 64:128], Pv[:, :, 64:128], ALU.add)
            else:
                S.cp("dve", nbacc[:, :, 0:64], C[:, :, 64:128])
            if Nx is not None:
                S.tt("dve", nbacc[:, :, 64:128], C[:, :, 0:64], Nx[:, :, 0:64], ALU.add)
            else:
                S.cp("dve", nbacc[:, :, 64:128], C[:, :, 0:64])
            a4 = nbacc.re("p k (r c) -> p k r c", r=2)
            c4 = C.re("p k (r c) -> p k r c", r=2)
            S.tt("dve", a4[:, :, :, 1:64], a4[:, :, :, 1:64], c4[:, :, :, 0:63], ALU.add)
            S.tt("dve", a4[:, :, :, 0:63], a4[:, :, :, 0:63], c4[:, :, :, 1:64], ALU.add)
            S.ts("dve", hnb, nbacc, 0.25, ALU.mult)
        else:
            S.cp("dve", nbacc[:, :, 1:128], C[:, :, 0:127])
            if Pv is not None:
                S.cp("dve", nbacc[:, :, 0:1], Pv[:, :, 127:128])
            else:
                S.memset("dve", nbacc[:, :, 0:1], 0.0)
            S.tt("dve", nbacc[:, :, 0:127], nbacc[:, :, 0:127], C[:, :, 1:128], ALU.add)
            if Nx is not None:
                S.tt("dve", nbacc[:, :, 127:128], nbacc[:, :, 127:128], Nx[:, :, 0:1], ALU.add)
            S.ts("dve", hnb, nbacc, 0.5, ALU.mult)

    def mix_tile(b, kind, idx, d, want_out, hoist=None, hoist2=None):
        hT = hring[idx % 3]
        final = want_out and d == 1
        if final:
            S.dma("sp", yfl, yf_d[idx])
        if d == 0:
            mi, me, mr = fm[0], fm[1], fm[2]
            gmi, gmr = fm[4], fm[6]
            m_ij_s, m_ji_s, m_ji_i = mLs, mUs, mUi
        else:
            mi, me, mr = fm[3], fm[2], fm[1]
            gmi, gmr = fm[7], fm[5]
            m_ij_s, m_ji_s, m_ji_i = mUs, mLs, mLi
        pl = P()
        for kc in range(8):
            S.mm(pl[0:96, 0:128], Wl[d][:, kc, :], hT[:, kc, :], start=(kc == 0), stop=False)
        for kc in range(8):
            S.mm(pl[0:96, 0:128], Wlb[d][:, kc, :], hnb[:, kc, :], start=False, stop=(kc == 7))
        S.act(lo[0:32, :], pl[0:32, 0:128], AF.Tanh)
        S.act(lo[32:64, :], pl[32:64, 0:128], AF.Copy)
        S.act(lo[64:80, :], pl[64:80, 0:128], AF.Copy)
        pr_ = P(); proj_block(pr_, hT, hnb, 0, 512, True)
        pw = P()
        S.mm(pw, lo[0:32, :], sw[d][0:32, :], start=True, stop=False)
        S.mm(pw, onesf[0:1, :], br3[0:1, d, :], start=False, stop=True)
        pa = P()
        S.mm(pa, lo[32:64, :], sw[d][32:64, :], start=True, stop=False)
        S.mm(pa, onesf[32:33, :], br3[32:33, d, :], start=False, stop=True)
        pg = P()
        S.mm(pg[:, 0:256], lo[64:80, :], sw[d][64:80, 0:256], start=True, stop=False)
        S.mm(pg[:, 0:256], onesf[64:65, :], br3[64:65, d, 0:256], start=False, stop=True)
        S.act(sg, pw, AF.Sigmoid)
        S.act(a_sb, pa, AF.Sigmoid)
        S.act(lg, pg[:, 0:256], AF.Sigmoid)
        S.cp("act", r_sb, pr_)
        pk_ = P(); proj_block(pk_, hT, hnb, 512, 512, True)
        pc1 = P(); S.mm(pc1, mi, sg)
        pc2 = P(); S.mm(pc2, me, sg)
        S.act(lg, lg, AF.Ln)
        S.act(E1, pc1, AF.Exp); S.act(E2, pc1, AF.Exp, scale=-1.0)
        S.act(E3, pc2, AF.Exp)
        S.cp("act", k_sb, pk_)
        S.tt("dve", t0, k_sb, kkrow, ALU.mult)
        S.tt("pool", t1, t0, t0, ALU.mult)
        S.red("dve", ss, t1.re("p (h c) -> p h c", h=8))
        S.act(ss, ss, AF.Ln, bias=1e-12)
        S.act(ss, ss, AF.Exp, scale=-0.5)
        S.tt("dve", kk.re("p (h c) -> p h c", h=8), t0.re("p (h c) -> p h c", h=8), ss.us(2).bc([128, 8, 64]), ALU.mult)
        pc3 = P(); S.mm(pc3, mr, sg)
        pp = P()
        for h in range(8):
            S.mm(pp[0:64, h:h + 1], sg[:, h * 64:(h + 1) * 64], negcol)
        for h in range(4):
            S.mm(pp[0:64, 8 + h:9 + h], lg[:, h * 64:(h + 1) * 64], g16col)
        pv_ = P(); proj_block(pv_, hT, hnb, 1024, 512, True)
        S.act(E4, pc3, AF.Exp)
        S.act(pc_sb, pp[0:64, 0:8], AF.Exp)
        S.act(pcg_sb, pp[0:64, 8:12], AF.Exp)
        S.cp("act", v_sb, pv_); S.cp("pool", v_bf, v_sb)
        p = P(); proj_block(p, hT, hnb, 1536, 512, False); S.cp("act", qk_sb, p)
        p = P(); proj_block(p, hT, hnb, 2048, 512, False); S.cp("act", vg_bf, p)
        if DEBUG["cut"] <= 2:
            return
        h8 = lambda t: t.re("p (h c) -> p h c", h=8)
        S.tt("pool", kka, kk, a_sb, ALU.mult)
        S.stt("pool", t1, a_sb, -1.0, karow, ALU.add, ALU.mult)
        S.stt("pool", kh, t1, 1.0, k_sb, ALU.add, ALU.mult)
        S.stt("dve", At_bf, kk, -1.0, E3, ALU.mult, ALU.mult)
        S.tt("dve", Bt_bf, kka, E2, ALU.mult)
        S.tt("dve", Kt_bf, kh, E2, ALU.mult)
        S.tt("dve", Rt_bf, r_sb, E1, ALU.mult)
        S.tt("pool", Bh_bf, kka, E4, ALU.mult)
        S.tt("pool", Kh_bf, kh, E4, ALU.mult)
        if want_out:
            S.tt("pool", t0, r_sb, kh, ALU.mult)
            S.tt("pool", t0, t0, rkrow, ALU.mult)
            S.red("dve", bs, h8(t0))
            S.tt("pool", h8(bo_sb), h8(v_sb), bs.us(2).bc([128, 8, 64]), ALU.mult)
        if DEBUG["cut"] <= 3:
            return
        dbg = DEBUG["dbg"] and b == 0 and kind == "c" and idx == 0 and d == 0
        if dbg:
            for nm, t_ in (("E1", E1), ("E2", E2), ("E3", E3), ("E4", E4), ("kk", kk), ("a", a_sb), ("sg", sg), ("kh", kh),
                           ("r", r_sb), ("v", v_sb)):
                dump(nm, t_, False)
            for nm, t_ in (("At", At_bf), ("Bt", Bt_bf), ("Kt", Kt_bf), ("Rt", Rt_bf), ("Bh", Bh_bf), ("Kh", Kh_bf), ("vb", v_bf)):
                dump(nm, t_, True)
            dump("pc", pc_sb, False)
        G1, G2, G4 = E1, E2, E3
        pg1 = P(); S.mm(pg1[:, 0:256], gmi, lg)
        pg3 = P(); S.mm(pg3[:, 0:256], gmr, lg)
        S.act(G1[:, 0:256], pg1[:, 0:256], AF.Exp)
        S.act(G2[:, 0:256], pg1[:, 0:256], AF.Exp, scale=-1.0)
        S.act(G4[:, 0:256], pg3[:, 0:256], AF.Exp)
        S.stt("dve", qt_bf, qk_sb[:, 0:256], 0.125, G1[:, 0:256], ALU.mult, ALU.mult)
        S.tt("dve", kt_bf, qk_sb[:, 256:512], G2[:, 0:256], ALU.mult)
        S.tt("pool", khg_bf, qk_sb[:, 256:512], G4[:, 0:256], ALU.mult)
        if DEBUG["cut"] <= 4:
            return
        if hoist is not None:
            hoist()
        parents = [E1, E2, E3, E4, sg, a_sb, t0, t1, kk, kka, kh, k_sb, qk_sb, r_sb]
        kids = [S.alias_acquire(pt_, half_aps(pt_)) for pt_ in parents]
        flat = [c for pair in kids[3:] for c in pair]
        G = [dict(AtT=AtT, BtT=BtT, KtT=KtT, RtT=RtT, X0=Xr[0], XT0=XTr[0], Lm=Xr[1], LTm=XTr[1],
                  LakT=LakT, ArbT=ArbT, ArkT=ArkT, Z0=Zr[0], Zf=Zr[1], D=kids[0], DT=kids[1],
                  Y1=kids[2][0], Y1t=kids[2][1], QeffT=QeffT, HT=HT),
             dict(AtT=flat[0][0:64], BtT=flat[1][0:64], KtT=flat[2][0:64], RtT=flat[3][0:64],
                  X0=flat[4], XT0=flat[5], Lm=flat[6], LTm=flat[7], LakT=flat[8], ArbT=flat[9], ArkT=flat[10],
                  Z0=flat[11], Zf=flat[12], D=[flat[13], flat[14]], DT=[flat[15], flat[16]],
                  Y1=flat[17], Y1t=flat[18], QeffT=flat[19][0:64], HT=flat[20][0:64, :, 0:64])]
        identb = ident.us(1).bc([128, 4, 128])

        def score(dst, L, R, mask):
            p_ = P()
            pv = v4(p_)
            for hh in range(4):
                S.mm(pv[:, hh, :], L[0:64, hh, :], R[0:64, hh, :])
            S.tt("dve", dst, pv, mask.us(1).bc([128, 4, 128]), ALU.mult)

        for g in range(2):
            T = G[g]
            for src, key, eng in ((At_bf, "AtT", "act"), (Bt_bf, "BtT", "dve"), (Kt_bf, "KtT", "act"), (Rt_bf, "RtT", "dve")):
                pt = PT()
                ptv = v4(pt, 64)
                for hh in range(4):
                    h = 4 * g + hh
                    S.tr(ptv[:, hh, :], src[:, h * 64:(h + 1) * 64], ident)
                S.cp(eng, T[key], ptv)
        for g in range(2):
            T = G[g]
            score(T["X0"], T["AtT"], T["BtT"], m_ij_s)
            score(T["XT0"], T["BtT"], T["AtT"], m_ji_s)
            score(T["LakT"], T["KtT"], T["AtT"], m_ji_s)
            score(T["ArbT"], T["BtT"], T["RtT"], m_ji_i)
            score(T["ArkT"], T["KtT"], T["RtT"], m_ji_i)
        for g in range(2):
            T = G[g]
            p = P()
            pv = p[:, 0:256].re("p (h v) -> p h v", h=4)
            for hh in range(4):
                h = 4 * g + hh
                S.mm(pv[:, hh, :], T["LakT"][:, hh, :], v_bf[:, h * 64:(h + 1) * 64])
            S.cp("act", T["Z0"][:, :, 0:64], pv)
            S.cp("pool", T["Z0"][:, :, 64:128], At_bf[:, g * 256:(g + 1) * 256].re("p (h c) -> p h c", h=4))
            m0 = smask[0].us(1).bc([128, 4, 128])
            S.tt("pool", T["Lm"], T["X0"], m0, ALU.mult)
            S.tt("pool", T["LTm"], T["XT0"], m0, ALU.mult)
            S.tt("dve", T["D"][0], T["Lm"], identb, ALU.add)
            S.tt("dve", T["DT"][0], T["LTm"], identb, ALU.add)
        U16 = mybir.dt.uint16
        mk_tiles = [flat[21], mkB]
        for l in range(1, 7):
            mk_bf = mk_tiles[l % 2]
            mk = V(mk_bf.buf, mk_bf.ap.bitcast(U16).rearrange("p h t -> p (h t)"))
            S.cp("pool", mk_bf, smask[l].us(1).bc([128, 4, 128]))
            for g in range(2):
                T = G[g]
                Dc, DTc = T["D"][0], T["DT"][0]
                p = P(); pv = v4(p)
                for hh in range(4):
                    S.mm(pv[:, hh, :], T["XT0"][:, hh, :], Dc[:, hh, :])
                S.cp("act", T["Y1"], pv)
                p = P(); pv = v4(p)
                for hh in range(4):
                    S.mm(pv[:, hh, :], T["X0"][:, hh, :], DTc[:, hh, :])
                S.cp("act", T["Y1t"], pv)
            for g in range(2):
                T = G[g]
                Dc, DTc = T["D"][0], T["DT"][0]
                p1 = P(); pv1 = v4(p1)
                for hh in range(4):
                    S.mm(pv1[:, hh, :], DTc[:, hh, :], T["Y1"][:, hh, :])
                p2 = P(); pv2 = v4(p2)
                for hh in range(4):
                    S.mm(pv2[:, hh, :], Dc[:, hh, :], T["Y1t"][:, hh, :])
                for dst, pvx in ((Dc, p1), (DTc, p2)):
                    o_, m_, d_ = dst.ap.rearrange("p h t -> p (h t)"), mk.ap, pvx.ap[:, 0:512]
                    S.op("dve", lambda e, o_=o_, m_=m_, d_=d_: e.copy_predicated(o_, m_, d_),
                         reads=[pvx.buf, mk.buf, dst.buf], writes=[dst.buf])
        for g in range(2):
            T = G[g]
            DTc = T["DT"][0]
            p = P(); pv = v4(p)
            for hh in range(4):
                S.mm(pv[:, hh, :], DTc[:, hh, :], T["Z0"][:, hh, :])
            S.cp("act" if g == 0 else "dve", T["Zf"], pv)
        for g in range(2):
            T = G[g]
            Zf = T["Zf"]
            p = P()
            pv = v4(p, 64)
            for hh in range(4):
                h = 4 * g + hh
                S.mm(pv[:, hh, :], Rt_bf[:, h * 64:(h + 1) * 64], ident, start=True, stop=False)
                S.mm(pv[:, hh, :], Zf[:, hh, 64:128], T["ArbT"][:, hh, :], start=False, stop=True)
            S.cp("act", T["QeffT"], pv)
            p = P()
            pvh = p[0:64, 0:256].re("p (h k) -> p h k", h=4)
            for hh in range(4):
                h = 4 * g + hh
                S.mm(pvh[:, hh, :], Zf[:, hh, 64:128], Bh_bf[:, h * 64:(h + 1) * 64])
            S.cp("dve", T["HT"], pvh)
        for g in range(2):
            T = G[g]
            Zf = T["Zf"]
            if want_out:
                p = P()
                pv = p[:, 0:256].re("p (h v) -> p h v", h=4)
                for hh in range(4):
                    h = 4 * g + hh
                    S.mm(pv[:, hh, :], T["QeffT"][:, hh, :], M_bf[d][:, h, :], start=True, stop=False)
                    S.mm(pv[:, hh, :], T["ArbT"][:, hh, :], Zf[:, hh, 0:64], start=False, stop=False)
                    S.mm(pv[:, hh, :], T["ArkT"][:, hh, :], v_bf[:, h * 64:(h + 1) * 64], start=False, stop=True)
                S.cp("act", y_sb[:, g * 256:(g + 1) * 256], p[:, 0:256])
            p = P()
            pvm = p[0:64, 0:256].re("p (h v) -> p h v", h=4)
            for hh in range(4):
                h = 4 * g + hh
                S.mm(pvm[:, hh, :], T["HT"][:, hh, :], M_bf[d][:, h, :], start=True, stop=False)
                S.mm(pvm[:, hh, :], Bh_bf[:, h * 64:(h + 1) * 64], Zf[:, hh, 0:64], start=False, stop=False)
                S.mm(pvm[:, hh, :], Kh_bf[:, h * 64:(h + 1) * 64], v_bf[:, h * 64:(h + 1) * 64], start=False, stop=True)
            Mv = M_f[d][:, 4 * g:4 * g + 4, :]
            S.tt("dve", Mv, Mv, pc_sb[:, 4 * g:4 * g + 4].us(2).bc([64, 4, 64]), ALU.mult)
            S.tt("dve", Mv, Mv, pvm, ALU.add)
            S.cp("dve", M_bf[d][:, 4 * g:4 * g + 4, :], Mv)
        for pt_, ch in zip(parents, kids):
            S.alias_release(pt_, ch)
        for src, dstT, eng in ((qt_bf, qtT, "act"), (kt_bf, ktT, "dve")):
            pt = PT()
            ptv = v4(pt, 64)
            for hh in range(4):
                S.tr(ptv[:, hh, :], src[:, hh * 64:(hh + 1) * 64], ident)
            S.cp(eng, dstT, ptv)
        p = P()
        pv = v4(p)
        for hh in range(4):
            S.mm(pv[:, hh, :], ktT[:, hh, :], qtT[:, hh, :])
        S.tt("dve", AqkT, pv, m_ji_i.us(1).bc([128, 4, 128]), ALU.mult)
        if want_out:
            p = P()
            pv = v4(p)
            for hh in range(4):
                S.mm(pv[:, hh, :], AqkT[:, hh, :], vg_bf[:, hh * 128:(hh + 1) * 128], start=True, stop=False)
                S.mm(pv[:, hh, :], qtT[:, hh, :], Mg_bf[d][:, hh, :], start=False, stop=True)
            S.cp("act", o_sb, p)
        p = P()
        pvg = v4(p, 64)
        for hh in range(4):
            S.mm(pvg[:, hh, :], khg_bf[:, hh * 64:(hh + 1) * 64], vg_bf[:, hh * 128:(hh + 1) * 128])
        S.tt("dve", Mg_f[d], Mg_f[d], pcg_sb.us(2).bc([64, 4, 128]), ALU.mult)
        S.tt("dve", Mg_f[d], Mg_f[d], pvg, ALU.add)
        S.cp("dve", Mg_bf[d], Mg_f[d])
        if not want_out:
            return
        if d == 0:
            S.dma("sp", yf_d[idx, :, 0:512], y_sb)
            S.dma("sp", yf_d[idx, :, 512:1024], bo_sb)
            S.dma("sp", yf_d[idx, :, 1024:1536], o_sb)
            return
        pl2 = P()
        for kc in range(8):
            S.mm(pl2[0:96, 0:128], Wgd[:, kc, :], hT[:, kc, :], start=(kc == 0), stop=False)
        for kc in range(8):
            S.mm(pl2[0:96, 0:128], Wgdb[:, kc, :], hnb[:, kc, :], start=False, stop=(kc == 7))
        S.act(lo_gd, pl2[0:96, 0:128], AF.Sigmoid)
        pgo = P()
        S.mm(pgo, lo_gd, g2w)
        S.cp("act", gout_sb, pgo)
        p = P(); proj_block(p, hT, hnb, 2560, 512, False)
        S.act(gsil, p, AF.Silu)
        if hoist2 is not None:
            hoist2()
        S.tt("dve", y_sb, y_sb, yfl[:, 0:512], ALU.add)
        S.red("dve", s8a, h8(y_sb))
        S.tt("pool", t0, y_sb, y_sb, ALU.mult)
        S.red("dve", s8b, h8(t0))
        S.ts("dve", s8a, s8a, 1.0 / 64.0, ALU.mult)
        S.tt("dve", s8c, s8a, s8a, ALU.mult)
        S.stt("dve", s8b, s8b, 1.0 / 64.0, s8c, ALU.mult, ALU.subtract)
        S.act(s8b, s8b, AF.Sqrt, bias=GN_EPS)
        S.op("dve", lambda e: e.reciprocal(s8b.ap, s8b.ap), reads=[s8b.buf], writes=[s8b.buf])
        S.tt("dve", h8(y_sb), h8(y_sb), s8a.us(2).bc([128, 8, 64]), ALU.subtract)
        S.tt("dve", h8(y_sb), h8(y_sb), s8b.us(2).bc([128, 8, 64]), ALU.mult)
        S.tt("dve", y_sb, y_sb, gnwrow, ALU.mult)
        S.tt("dve", y_sb, y_sb, gnbrow, ALU.add)
        S.tt("dve", y_sb, y_sb, bo_sb, ALU.add)
        S.tt("dve", y_sb, y_sb, yfl[:, 512:1024], ALU.add)
        S.tt("dve", mix_bf[:, 0:512], y_sb, gout_sb, ALU.mult)
        S.tt("dve", o_sb, o_sb, yfl[:, 1024:1536], ALU.add)
        S.tt("pool", t0, o_sb, o_sb, ALU.mult)
        S.red("dve", s8d[:, 0:4], t0.re("p (h c) -> p h c", h=4))
        S.act(s8d[:, 0:4], s8d[:, 0:4], AF.Sqrt, bias=1e-5, scale=1.0 / 128.0)
        S.op("dve", lambda e: e.reciprocal(s8d.ap[:, 0:4], s8d.ap[:, 0:4]), reads=[s8d.buf], writes=[s8d.buf])
        o4 = o_sb.re("p (h c) -> p h c", h=4)
        S.tt("dve", o4, o4, s8d[:, 0:4].us(2).bc([128, 4, 128]), ALU.mult)
        S.tt("dve", o_sb, o_sb, gnormrow, ALU.mult)
        S.tt("dve", mix_bf[:, 512:1024], o_sb, gsil, ALU.mult)
        S.dma("sp", mix_d[b, idx * 128:(idx + 1) * 128, :], mix_bf)

    if DEBUG["stop_after"] == "Bs":
        S.pop()
        S.finish()
        return nc
    for b in range(NB):
        for d in range(2):
            S.memset("dve", M_f[d], 0.0)
            S.memset("dve", M_bf[d], 0.0)
            S.memset("pool", Mg_f[d], 0.0)
            S.memset("pool", Mg_bf[d], 0.0)
        for d in range(2):
            for kind, n_t in (("c", 2), ("l", 16)):
                order = list(range(n_t)) if d == 0 else list(range(n_t - 1, -1, -1))
                step = 1 if d == 0 else -1
                produce(b, kind, order[0])
                produce(b, kind, order[1])
                nb_done = False
                for i, idx in enumerate(order):
                    if not nb_done:
                        neighbor(kind, idx, n_t)
                    late = (d == 1 and kind == "l")
                    nb_next = (i + 1 < n_t)

                    def hoist(i=i, nb_now=(nb_next and not late)):
                        if i + 2 < n_t:
                            produce(b, kind, order[i + 2])
                        if nb_now:
                            neighbor(kind, order[i + 1], n_t)

                    def hoist2(i=i, nb_late=(nb_next and late)):
                        if nb_late:
                            neighbor(kind, order[i + 1], n_t)
                    mix_tile(b, kind, idx, d, kind == "l", hoist, hoist2)
                    nb_done = nb_next
                    if DEBUG["stop_after"] == "B1":
                        break
                if DEBUG["stop_after"] == "B1":
                    break
    S.pop()
    if DEBUG["stop_after"] in ("B", "B1"):
        S.finish()
        return nc
    return moe_phase(nc, S, locals())


def moe_phase(nc, S, L):
    out_d, modrow_d = L["out_d"], L["modrow_d"]
    rtr_d, exg_d, exu_d, exd_d = L["rtr_d"], L["exg_d"], L["exu_d"], L["exd_d"]
    ln2g_d, ln2b_d, eoh_d = L["ln2g_d"], L["ln2b_d"], L["eoh_d"]
    ident, mUi, ones_bf = L["ident"], L["mUi"], L["ones_bf"]
    iota_f = L["iota_f"]
    pbanks, ptbanks = L["pbanks"], L["ptbanks"]
    layer_norm_stats = L["layer_norm_stats"]
    rot = {"p": 0, "t": 0}

    def P():
        rot["p"] = (rot["p"] + 1) % 2
        return pbanks[4 + rot["p"]]

    def PT():
        rot["t"] = (rot["t"] + 1) % 2
        return ptbanks[rot["t"]]

    Y = pbanks[0:4]
    exg_v = [exg_d[e].re("(kc p) f -> p kc f", p=128) for e in range(16)]
    exu_v = [exu_d[e].re("(kc p) f -> p kc f", p=128) for e in range(16)]
    exd_v = [exd_d[e].re("(fc p) d -> p fc d", p=128) for e in range(16)]

    x_d, mix_d, wout_d, ln1g_d, ln1b_d = L["x_d"], L["mix_d"], L["wout_d"], L["ln1g_d"], L["ln1b_d"]
    for b in range(NB):
        S.epoch()
        S.push()
        h2 = S.sb("h2", [128, 16, 1024], BF16)
        acc = S.sb("acc", [128, 16, 1024], F32)
        mrows = S.sb("mrows", [128, 3, 1024], F32)
        S.dma("sp", mrows[:, 0, :], modrow_d[b, 1:2, :].pb(128))
        S.dma("sp", mrows[:, 1, :], modrow_d[b, 2:3, :].pb(128))
        S.dma("sp", mrows[:, 2, :], modrow_d[b, 3:4, :].pb(128))
        identf = S.sb("identf", [128, 128], F32)
        S.cp("dve", identf, ident)
        aff_tok = S.sb("aff_tok", [128, 16, 16], F32)
        mask_tok = S.sb("mask_tok", [128, 16, 16], BF16)
        gate_tok = S.sb("gate_tok", [128, 16, 16], BF16)
        slot_tok = S.sb("slot_tok", [128, 16, 16], F32)
        m8 = S.sb("m8", [16, 8], F32)
        st6 = S.sb("mst6", [128, 2, 6], F32)
        mv2 = S.sb("mmv2", [128, 2], F32)
        rstd1 = S.sb("mrstd1", [128, 1], F32)
        xt = [S.sb("mx%d" % i, [128, 1024], F32) for i in range(2)]

        S.push()
        affT = S.sb("affT", [16, 2048], F32)
        wk = S.sb("wk", [16, 2048], F32)
        S.push()
        woutg = S.sb("woutg", [128, 8, 1024], BF16)
        g1row = S.sb("g1row", [128, 1024], F32)
        wstg = [S.sb("wstg%d" % i, [128, 1024], F32) for i in range(2)]
        lnrows = S.sb("lnrows", [128, 2, 1024], F32)
        S.dma("sp", lnrows[:, 0, :], ln1g_d.pb(128))
        S.dma("sp", lnrows[:, 1, :], ln1b_d.pb(128))
        S.dma("sp", g1row, modrow_d[b, 0:1, :].pb(128))
        for kc in range(8):
            ws = wstg[kc % 2]
            S.dma("sp", ws, wout_d[kc * 128:(kc + 1) * 128, :])
            S.tt("pool", woutg[:, kc, :], ws, g1row, ALU.mult)
        rtr = S.sb("rtr", [128, 8, 16], BF16)
        S.dma("pool", rtr, rtr_d.re("(kc p) e -> p kc e", p=128))
        mixt = [S.sb("mixt%d" % i, [128, 1024], BF16) for i in range(2)]
        mixT = S.sb("mixT", [128, 8, 128], BF16)
        x1t = S.sb("x1t", [128, 1024], F32)
        xnf = S.sb("xnf", [128, 1024], F32)
        h2T = S.sb("h2T", [128, 8, 128], BF16)
        sm1 = S.sb("sm1", [128, 1], F32)
        sm2 = S.sb("sm2", [128, 1], F32)
        nmr = S.sb("nmr", [128, 1], F32)
        lgt = S.sb("lgt", [128, 16], F32)
        for t in range(16):
            x = xt[t % 2]
            mt = mixt[t % 2]
            S.dma("sp", x, x_d[b, t * 128:(t + 1) * 128, :])
            S.dma("sp", mt, mix_d[b, t * 128:(t + 1) * 128, :])
            pt = PT()
            ptv = pt.re("p (k t) -> p k t", k=8)
            for kc in range(8):
                S.tr(ptv[:, kc, :], mt[:, kc * 128:(kc + 1) * 128], ident)
            S.cp("act", mixT, ptv)
            for half in range(2):
                pp_ = P()
                for kc in range(8):
                    S.mm(pp_, mixT[:, kc, :], woutg[:, kc, half * 512:(half + 1) * 512], start=(kc == 0), stop=(kc == 7))
                S.stt("dve", x1t[:, half * 512:(half + 1) * 512], x[:, half * 512:(half + 1) * 512], ALPHA, pp_,
                      ALU.mult, ALU.add)
            layer_norm_stats(x1t, st6, mv2, rstd1)
            S.stt("dve", nmr, mv2[:, 0:1], -1.0, rstd1, ALU.mult, ALU.mult)
            S.act(x1t, x1t, AF.Identity, bias=nmr, scale=rstd1)
            S.tt("dve", x1t, x1t, lnrows[:, 0, :], ALU.mult)
            S.tt("dve", x1t, x1t, lnrows[:, 1, :], ALU.add)
            S.act(acc[:, t, :], x1t, AF.Copy, scale=ALPHA)
            layer_norm_stats(x1t, st6, mv2, rstd1)
            S.stt("dve", nmr, mv2[:, 0:1], -1.0, rstd1, ALU.mult, ALU.mult)
            S.act(xnf, x1t, AF.Identity, bias=nmr, scale=rstd1)
            S.tt("dve", xnf, xnf, mrows[:, 1, :], ALU.mult)
            S.tt("dve", h2[:, t, :], xnf, mrows[:, 0, :], ALU.add)
            pt = PT()
            ptv = pt.re("p (k t) -> p k t", k=8)
            for kc in range(8):
                S.tr(ptv[:, kc, :], h2[:, t, kc * 128:(kc + 1) * 128], ident)
            S.cp("act", h2T, ptv)
            p = P()
            for kc in range(8):
                S.mm(p[:, 0:16], h2T[:, kc, :], rtr[:, kc, :], start=(kc == 0), stop=(kc == 7))
            S.op("dve", lambda e, p=p: e.reduce_max(sm1.ap, p.ap[:, 0:16], AX.X), reads=[p.buf], writes=[sm1.buf])
            S.ts("dve", sm1, sm1, -1.0, ALU.mult)
            S.act(lgt, p[:, 0:16], AF.Exp, bias=sm1)
            S.red("dve", sm2, lgt)
            S.op("dve", lambda e: e.reciprocal(sm2.ap, sm2.ap), reads=[sm2.buf], writes=[sm2.buf])
            S.ts("dve", aff_tok[:, t, :], lgt, sm2, ALU.mult)
            p2 = P()
            S.mm(p2[0:16, 0:128], aff_tok[:, t, :], identf)
            S.cp("act", affT[:, t * 128:(t + 1) * 128], p2[0:16, 0:128])
        S.pop()
        if DEBUG["stop_after"] == "C0":
            S.pop()
            S.pop()
            S.finish()
            return nc
        S.cp("dve", wk, affT)
        for r in range(32):
            S.op("dve", lambda e: e.max(m8.ap, wk.ap), reads=[wk.buf], writes=[m8.buf])
            if r < 31:
                S.op("dve", lambda e: e.match_replace(wk.ap, m8.ap, wk.ap, -1.0), reads=[wk.buf, m8.buf], writes=[wk.buf])
        S.ts("dve", wk, affT, m8[:, 7:8], ALU.is_ge)
        for t in range(16):
            p = P()
            S.mm(p[:, 0:16], wk[:, t * 128:(t + 1) * 128], identf[0:16, 0:16])
            S.cp("act", mask_tok[:, t, :], p[:, 0:16])
        for t in range(16):
            S.tt("dve", gate_tok[:, t, :], aff_tok[:, t, :], mask_tok[:, t, :], ALU.mult)
            p = P()
            for t2 in range(t + 1):
                S.mm(p[:, 0:16], mUi if t2 == t else ones_bf, mask_tok[:, t2, :], start=(t2 == 0), stop=(t2 == t))
            S.tt("dve", slot_tok[:, t, :], p[:, 0:16], mask_tok[:, t, :], ALU.mult)
            S.ts("dve", slot_tok[:, t, :], slot_tok[:, t, :], -1.0, ALU.add)
        S.pop()

        S.push()
        sel = S.sb("sel", [128, 16, 256], BF16)
        selT = [S.sb("selT%d" % i, [128, 2, 2048], BF16) for i in range(2)]
        xeT = [S.sb("xeT%d" % i, [128, 8, 256], BF16) for i in range(2)]
        actT = S.sb("actT", [128, 2, 256], BF16)
        sl = S.sb("sl", [128, 256], F32)
        y_bf = S.sb("y_bf", [128, 2, 1024], BF16)
        gslot = [S.sb("gslot%d" % i, [128, 2], F32) for i in range(2)]
        NW = 3
        wg_t = [S.sb("wg%d" % i, [128, 8, 256], BF16) for i in range(NW)]
        wu_t = [S.sb("wu%d" % i, [128, 8, 256], BF16) for i in range(NW)]
        wd_t = [S.sb("wd%d" % i, [128, 2, 1024], BF16) for i in range(NW)]
        paux = V(ptbanks[0].buf, ptbanks[0].ap.bitcast(F32))
        paux2 = V(ptbanks[1].buf, ptbanks[1].ap.bitcast(F32))
        ptr = ptbanks[1]

        def prep_pieces(e):
            k = e % 2
            pieces = []

            def mk_sel(t0_):
                def f():
                    for t in range(t0_, t0_ + 4):
                        S.ts("dve", sel[:, t, :], iota_f, slot_tok[:, t, e:e + 1], ALU.is_equal)
                return f
            for t0_ in range(0, 16, 4):
                pieces.append(mk_sel(t0_))

            def f_gslot():
                for jc in range(2):
                    for t in range(16):
                        S.mm(paux[:, jc:jc + 1], sel[:, t, jc * 128:(jc + 1) * 128], gate_tok[:, t, e:e + 1],
                             start=(t == 0), stop=(t == 15))
                S.cp("act", gslot[k], paux[:, 0:2])
            pieces.append(f_gslot)

            def mk_tr(jc, tq):
                def f():
                    ptv = ptr.re("p (k t) -> p k t", k=8)
                    for tt_ in range(8):
                        t = tq * 8 + tt_
                        S.tr(ptv[:, tt_, :], sel[:, t, jc * 128:(jc + 1) * 128], ident)
                    S.cp("act", selT[k][:, jc, tq * 1024:(tq + 1) * 1024], ptr)
                return f
            tail = []
            for jc in range(2):
                for tq in range(2):
                    tail.append(mk_tr(jc, tq))

            def mk_gather(dp):
                def f():
                    pv = paux.re("p (c j) -> p c j", c=2)
                    for c2 in range(2):
                        dc = dp * 2 + c2
                        for t in range(16):
                            S.mm(pv[:, c2, :], h2[:, t, dc * 128:(dc + 1) * 128], sel[:, t, :], start=(t == 0), stop=(t == 15))
                    S.cp("act", xeT[k][:, dp * 2:dp * 2 + 2, :], pv)
                return f
            for dp in range(4):
                pieces.append(mk_gather(dp))
            return pieces, tail

        def scatter_pieces(e):
            k = e % 2
            pieces = []

            def mk(t, dh):
                def f():
                    pa = paux if dh == 0 else paux2
                    for jc in range(2):
                        S.mm(pa, selT[k][:, jc, t * 128:(t + 1) * 128], y_bf[:, jc, dh * 512:(dh + 1) * 512],
                             start=(jc == 0), stop=(jc == 1))
                    a_ = acc[:, t, dh * 512:(dh + 1) * 512]
                    S.tt("dve", a_, a_, pa, ALU.add)
                return f
            for t in range(16):
                for dh in range(2):
                    pieces.append(mk(t, dh))
            return pieces

        pm_, pt_ = prep_pieces(0)
        for f in pm_ + pt_:
            f()
        gcount = 0
        for e in range(16):
            k = e % 2
            pend_sc = scatter_pieces(e - 1) if e > 0 else []
            pend_pr, pend_tr = prep_pieces(e + 1) if e < 15 else ([], [])
            chunk_i = 0
            wsel = {}

            def issue_w(fg):
                nonlocal gcount
                wi = gcount % NW
                gcount += 1
                f0 = fg * 256
                S.dma("pool", wg_t[wi], exg_v[e][:, :, f0:f0 + 256])
                S.dma("pool", wu_t[wi], exu_v[e][:, :, f0:f0 + 256])
                S.dma("pool", wd_t[wi], exd_v[e][:, fg * 2:fg * 2 + 2, :])
                wsel[fg] = wi

            def GU(n):
                fg, fc = n // 2, n % 2
                if fg not in wsel:
                    issue_w(fg)
                wi = wsel[fg]
                p = P()
                for kc in range(8):
                    S.mm(p[:, 0:256], wg_t[wi][:, kc, fc * 128:(fc + 1) * 128], xeT[k][:, kc, :], start=(kc == 0), stop=(kc == 7))
                for kc in range(8):
                    S.mm(p[:, 256:512], wu_t[wi][:, kc, fc * 128:(fc + 1) * 128], xeT[k][:, kc, :], start=(kc == 0), stop=(kc == 7))
                return p

            pcur = GU(0)
            for n in range(22):
                fg, fc = n // 2, n % 2
                wi = wsel[fg]
                pnext = GU(n + 1) if n < 21 else None
                S.act(sl, pcur[:, 0:256], AF.Silu)
                S.tt("dve", actT[:, fc, :], sl, pcur[:, 256:512], ALU.mult)
                for jc in range(2):
                    for dh in range(2):
                        S.mm(Y[jc * 2 + dh], actT[:, fc, jc * 128:(jc + 1) * 128], wd_t[wi][:, fc, dh * 512:(dh + 1) * 512],
                             start=(n == 0), stop=(n == 21))
                pcur = pnext
                chunk_i += 1
                if chunk_i <= 16:
                    for _ in range(min(2, len(pend_sc))):
                        pend_sc.pop(0)()
                if chunk_i <= 18 and chunk_i % 2 == 0 and pend_pr:
                    pend_pr.pop(0)()
                if chunk_i > 18:
                    while pend_sc:
                        pend_sc.pop(0)()
                    while pend_pr:
                        pend_pr.pop(0)()
                    if pend_tr:
                        pend_tr.pop(0)()
            while pend_sc:
                pend_sc.pop(0)()
            while pend_pr:
                pend_pr.pop(0)()
            while pend_tr:
                pend_tr.pop(0)()
            for jc in range(2):
                for dh in range(2):
                    S.stt("dve", y_bf[:, jc, dh * 512:(dh + 1) * 512], Y[jc * 2 + dh], gslot[k][:, jc:jc + 1],
                          mrows[:, 2, dh * 512:(dh + 1) * 512], ALU.mult, ALU.mult)
        for f in scatter_pieces(15):
            f()
        S.pop()
        S.push()
        ln2rows = S.sb("ln2rows", [128, 2, 1024], F32)
        S.dma("sp", ln2rows[:, 0, :], ln2g_d.pb(128))
        S.dma("sp", ln2rows[:, 1, :], ln2b_d.pb(128))
        for t in range(16):
            a_ = acc[:, t, :]
            layer_norm_stats(a_, st6, mv2, rstd1)
            o_ = xt[t % 2]
            S.ts("dve", o_, a_, mv2[:, 0:1], ALU.subtract, rstd1, ALU.mult)
            S.tt("dve", o_, o_, ln2rows[:, 0, :], ALU.mult)
            S.tt("dve", o_, o_, ln2rows[:, 1, :], ALU.add)
            S.dma("sp", out_d[b, t * 128:(t + 1) * 128, :], o_)
        S.pop()
        S.pop()
    S.finish()
    return nc


def kernel(x, c, ctx, c_ctx, ada_w, ada_b, w_in, rw_mu, rw_w0, rw_w2, rw_a0, rw_a2, rw_g2,
           rw_k_k, rw_k_a, rw_r_k, rw_gn_w, rw_gn_b, gla_a2, gla_a_b, gla_norm_w, w_out,
           ln1_g, ln1_b, router_w, ex_gate, ex_up, ex_down, ln2_g, ln2_b):
    f = lambda a: np.ascontiguousarray(np.asarray(a, dtype=np.float32))
    x, c, ctx, c_ctx = f(x), f(c), f(ctx), f(c_ctx)
    cbf, cfc = host_consts()
    eoh = np.zeros((16, 16 * 128), np.float32)
    for e in range(16):
        eoh[e, e * 128:(e + 1) * 128] = 1.0
    mu_ext = np.zeros((1, 3328), np.float32)
    mu_ext[0, :1760] = f(rw_mu)[0]
    shared = {
        "ada_w": f(ada_w)[0], "ada_b": f(ada_b)[0][None, :],
        "ada_bT": np.ascontiguousarray(f(ada_b)[0].reshape(48, 128).T),
        "w_in": f(w_in)[0], "mu_ext": mu_ext,
        "rw_w0": f(rw_w0)[0], "rw_w2": f(rw_w2)[0], "rw_a0": f(rw_a0)[0], "rw_a2": f(rw_a2)[0],
        "rw_g2": f(rw_g2)[0], "rw_k_k": f(rw_k_k)[0][None, :], "rw_k_a": f(rw_k_a)[0][None, :],
        "rw_r_k": f(rw_r_k)[0].reshape(1, 512), "rw_gn_w": f(rw_gn_w)[0][None, :], "rw_gn_b": f(rw_gn_b)[0][None, :],
        "gla_a2": f(gla_a2)[0], "gla_a_b": f(gla_a_b)[0], "gla_norm_w": f(gla_norm_w)[0][None, :],
        "w_out": f(w_out)[0], "ln1_g": f(ln1_g)[0][None, :], "ln1_b": f(ln1_b)[0][None, :],
        "router_w": f(router_w)[0], "ex_gate": f(ex_gate)[0], "ex_up": f(ex_up)[0], "ex_down": f(ex_down)[0],
        "ln2_g": f(ln2_g)[0][None, :], "ln2_b": f(ln2_b)[0][None, :],
        "cbf": cbf, "cf": cfc, "eoh": eoh,
    }
    in_maps = []
    for core in range(8):
        bs = slice(core * NB, (core + 1) * NB)
        cc = np.stack([c[core * NB], c[core * NB + 1], c_ctx], axis=1)
        cT = np.ascontiguousarray(cc.reshape(8, 128, 3).transpose(1, 0, 2))
        m = dict(shared)
        m["x"] = np.ascontiguousarray(x[bs])
        m["ctx"] = np.ascontiguousarray(ctx[bs])
        m["cT"] = cT
        in_maps.append(m)
    nc = build()
    res = run_bass_kernel_spmd(nc, in_maps, core_ids=list(range(8)))
    if DEBUG["dump"]:
        DEBUG["res"] = res.results
    return np.concatenate([np.asarray(r["out"], dtype=np.float32) for r in res.results], axis=0)
```
